# Optimizing a Trainium2 kernel written in Bass

```python
import math
import jax, jax.numpy as jnp
from jax import lax
import numpy as np

D_MODEL = 1024
BATCH = 16
SEQ = 2048
DEPTH = 1

HEAD_DIM = 64
N_HEADS_DIL = D_MODEL // (2 * HEAD_DIM)
N_HEADS_NA = D_MODEL // (2 * HEAD_DIM)
WIDTH_DIL = N_HEADS_DIL * HEAD_DIM
WIDTH_NA = N_HEADS_NA * HEAD_DIM
MIX_WIDTH = WIDTH_DIL + WIDTH_NA
DIL_PATTERNS = ((128, 1), (512, 4), (2048, 16))
DIL_BLOCK = 64
GRID_W = 64
NA_WIN_ROWS = 8
NA_WIN_COLS = 16
NA_Q_ROWS = 4
NA_Q_COLS = 16
NA_K_COLS = 32
N_GROUPS = 4
EXPERTS_PER_GROUP = 4
N_EXPERTS = N_GROUPS * EXPERTS_PER_GROUP
TOP_K_EXPERT = 2
D_EXPERT = 256
RMS_EPS = 1e-6
NEG_INF = -1e30

kernel_name = 'hybrid_dilated_neighbourhood_hmoe_encoder'


def rms_norm(x, gain):
    xf = x.astype(jnp.float32)
    xf = xf * lax.rsqrt(jnp.mean(xf * xf, axis=-1, keepdims=True) + RMS_EPS)
    return (xf * gain.astype(jnp.float32)).astype(x.dtype)


def alibi_slopes(n_heads):
    return jnp.exp2(-8.0 * jnp.arange(1, n_heads + 1, dtype=jnp.float32) / n_heads)


def dilated_window_attention(q, k, v, slopes, window, dilation):
    B, H, S, Dh = q.shape
    L = S // dilation
    radius = (window // 2) // dilation
    blk = DIL_BLOCK
    n_blk = -(-L // blk)
    Lp = n_blk * blk

    def residue_major(t, lo, hi):
        t = t.reshape(B, H, L, dilation, Dh).transpose(0, 1, 3, 2, 4)
        return jnp.pad(t, ((0, 0), (0, 0), (0, 0), (lo, hi), (0, 0)))

    def key_blocks(t):
        tp = residue_major(t, blk, Lp - L + blk).reshape(B, H, dilation, n_blk + 2, blk, Dh)
        return jnp.concatenate([tp[:, :, :, i:i + n_blk] for i in range(3)], axis=4)

    qb = residue_major(q, 0, Lp - L).reshape(B, H, dilation, n_blk, blk, Dh)
    kb = key_blocks(k)
    vb = key_blocks(v)

    q_pos = jnp.arange(Lp).reshape(n_blk, blk)
    k_pos = (jnp.arange(n_blk)[:, None] - 1) * blk + jnp.arange(3 * blk)[None, :]
    off = jnp.abs(k_pos[:, None, :] - q_pos[:, :, None])
    valid = ((off <= radius) & (k_pos[:, None, :] >= 0) & (k_pos[:, None, :] < L)
             & (q_pos[:, :, None] < L))
    dist = (off * dilation).astype(jnp.float32)

    s = jnp.einsum('bhrnqd,bhrnkd->bhrnqk', qb, kb).astype(jnp.float32) * (HEAD_DIM ** -0.5)
    s = s - slopes[None, :, None, None, None, None] * dist
    s = jnp.where(valid, s, NEG_INF)
    mx = jnp.max(s, axis=-1, keepdims=True)
    p = jnp.exp(s - mx)
    den = jnp.sum(p, axis=-1)
    o = jnp.einsum('bhrnqk,bhrnkd->bhrnqd', p, vb.astype(jnp.float32)) / den[..., None]
    lse = mx[..., 0] + jnp.log(den)

    o = o.reshape(B, H, dilation, Lp, Dh)[:, :, :, :L].transpose(0, 1, 3, 2, 4).reshape(B, H, S, Dh)
    lse = lse.reshape(B, H, dilation, Lp)[..., :L].transpose(0, 1, 3, 2).reshape(B, H, S)
    return o, lse


def dilated_mixture_attention(q, k, v, slopes):
    results = [dilated_window_attention(q, k, v, slopes, w, d) for (w, d) in DIL_PATTERNS]
    outs = jnp.stack([r[0] for r in results])
    lses = jnp.stack([r[1] for r in results])
    weights = jax.nn.softmax(lses, axis=0)
    return jnp.sum(weights[..., None] * outs, axis=0).astype(q.dtype)


def neighbourhood_attention(q, k, v, rpb):
    B, H, S, Dh = q.shape
    rows = S // GRID_W
    wh = min(NA_WIN_ROWS, rows)
    q_rows = math.gcd(rows, NA_Q_ROWS)
    k_rows = min(wh + q_rows - 1, rows)
    n_rb = rows // q_rows
    n_cb = GRID_W // NA_Q_COLS

    r = jnp.arange(rows)
    c = jnp.arange(GRID_W)
    row_start = jnp.clip(r - wh // 2, 0, rows - wh)
    col_start = jnp.clip(c - NA_WIN_COLS // 2, 0, GRID_W - NA_WIN_COLS)
    key_row = jnp.clip(row_start[::q_rows], 0, rows - k_rows)[:, None] + jnp.arange(k_rows)
    key_col = (jnp.clip(jnp.arange(n_cb) * NA_Q_COLS - NA_WIN_COLS // 2, 0, GRID_W - NA_K_COLS)[:, None]
               + jnp.arange(NA_K_COLS))

    q_row = r.reshape(n_rb, q_rows)
    q_col = c.reshape(n_cb, NA_Q_COLS)
    rs = row_start.reshape(n_rb, q_rows)[:, :, None]
    cs = col_start.reshape(n_cb, NA_Q_COLS)[:, :, None]
    row_ok = (key_row[:, None, :] >= rs) & (key_row[:, None, :] < rs + wh)
    col_ok = (key_col[:, None, :] >= cs) & (key_col[:, None, :] < cs + NA_WIN_COLS)
    mask = row_ok[:, None, :, None, :, None] & col_ok[None, :, None, :, None, :]
    dr = jnp.clip(key_row[:, None, :] - q_row[:, :, None] + NA_WIN_ROWS - 1, 0, 2 * NA_WIN_ROWS - 2)
    dc = jnp.clip(key_col[:, None, :] - q_col[:, :, None] + NA_WIN_COLS - 1, 0, 2 * NA_WIN_COLS - 2)
    bias = rpb[:, dr[:, None, :, None, :, None], dc[None, :, None, :, None, :]].astype(jnp.float32)

    def grid(t):
        return t.reshape(B, H, rows, GRID_W, Dh)

    qb = grid(q).reshape(B, H, n_rb, q_rows, n_cb, NA_Q_COLS, Dh).transpose(0, 1, 2, 4, 3, 5, 6)
    ridx = key_row[:, None, :, None]
    cidx = key_col[None, :, None, :]
    kg = grid(k)[:, :, ridx, cidx]
    vg = grid(v)[:, :, ridx, cidx]

    s = jnp.einsum('bhijqcd,bhijkld->bhijqckl', qb, kg).astype(jnp.float32) * (HEAD_DIM ** -0.5)
    s = jnp.where(mask, s + bias[None], NEG_INF)
    p = jax.nn.softmax(s, axis=(-2, -1))
    o = jnp.einsum('bhijqckl,bhijkld->bhijqcd', p, vg.astype(jnp.float32))
    o = o.transpose(0, 1, 2, 4, 3, 5, 6).reshape(B, H, S, Dh)
    return o.astype(q.dtype)


def hierarchical_moe(x, w_group, b_group, w_router, b_router, w_gate, w_up, w_down):
    B, S, D = x.shape
    n_tok = B * S
    xf = x.reshape(n_tok, D)
    g_logits = (xf @ w_group).astype(jnp.float32) + b_group.astype(jnp.float32)
    g_weight, g_idx = lax.top_k(jax.nn.softmax(g_logits, axis=-1), 1)
    e_logits = ((xf @ w_router).astype(jnp.float32) + b_router.astype(jnp.float32)
                ).reshape(n_tok, N_GROUPS, EXPERTS_PER_GROUP)
    e_in_group = jnp.take_along_axis(e_logits, g_idx[:, :, None], axis=1)[:, 0]
    e_val, e_idx = lax.top_k(e_in_group, TOP_K_EXPERT)
    e_weight = jax.nn.softmax(e_val, axis=-1) * g_weight
    expert_id = g_idx * EXPERTS_PER_GROUP + e_idx
    combine = jnp.sum(jax.nn.one_hot(expert_id, N_EXPERTS, dtype=jnp.float32) * e_weight[..., None], axis=1)
    gate = jnp.einsum('nd,edf->nef', xf, w_gate)
    up = jnp.einsum('nd,edf->nef', xf, w_up)
    act = jax.nn.silu(gate) * up * combine[:, :, None].astype(x.dtype)
    out = jnp.einsum('nef,efd->nd', act, w_down)
    return out.reshape(B, S, D).astype(x.dtype)


def setup_inputs(seed: int = 0) -> dict:
    key = jax.random.key(seed)
    ks = jax.random.split(key, 17)
    f32 = jnp.float32

    def nrm(k, shape, scale):
        return jax.random.normal(k, shape, f32) * scale

    return {
        'x': nrm(ks[0], (BATCH, SEQ, D_MODEL), 1.0),
        'norm_mix_g': 1.0 + nrm(ks[1], (DEPTH, D_MODEL), 0.02),
        'w_in': nrm(ks[2], (DEPTH, D_MODEL, 3 * MIX_WIDTH), D_MODEL ** -0.5),
        'rpb': nrm(ks[3], (DEPTH, N_HEADS_NA, 2 * NA_WIN_ROWS - 1, 2 * NA_WIN_COLS - 1), 0.1),
        'g_out_dil': 1.0 + nrm(ks[4], (DEPTH, WIDTH_DIL), 0.02),
        'g_out_na': 1.0 + nrm(ks[5], (DEPTH, WIDTH_NA), 0.02),
        'w_out': nrm(ks[6], (DEPTH, MIX_WIDTH, D_MODEL), MIX_WIDTH ** -0.5),
        'norm_ffn_g': 1.0 + nrm(ks[7], (DEPTH, D_MODEL), 0.02),
        'w_group': nrm(ks[8], (DEPTH, D_MODEL, N_GROUPS), D_MODEL ** -0.5),
        'b_group': nrm(ks[9], (DEPTH, N_GROUPS), 0.01),
        'w_router': nrm(ks[10], (DEPTH, D_MODEL, N_EXPERTS), D_MODEL ** -0.5),
        'b_router': nrm(ks[11], (DEPTH, N_EXPERTS), 0.01),
        'w_gate': nrm(ks[12], (DEPTH, N_EXPERTS, D_MODEL, D_EXPERT), D_MODEL ** -0.5),
        'w_up': nrm(ks[13], (DEPTH, N_EXPERTS, D_MODEL, D_EXPERT), D_MODEL ** -0.5),
        'w_down': nrm(ks[14], (DEPTH, N_EXPERTS, D_EXPERT, D_MODEL), D_EXPERT ** -0.5),
        'norm_final_g': 1.0 + nrm(ks[15], (D_MODEL,), 0.02),
    }


def reference(x, norm_mix_g, w_in, rpb, g_out_dil, g_out_na, w_out, norm_ffn_g,
              w_group, b_group, w_router, b_router, w_gate, w_up, w_down, norm_final_g):
    B, S, _ = x.shape
    slopes = alibi_slopes(N_HEADS_DIL)
    h = x
    for layer in range(DEPTH):
        hn = rms_norm(h, norm_mix_g[layer])
        proj = hn @ w_in[layer]
        p_dil = proj[..., :3 * WIDTH_DIL].reshape(B, S, 3, N_HEADS_DIL, HEAD_DIM).transpose(2, 0, 3, 1, 4)
        p_na = proj[..., 3 * WIDTH_DIL:].reshape(B, S, 3, N_HEADS_NA, HEAD_DIM).transpose(2, 0, 3, 1, 4)
        y_dil = dilated_mixture_attention(p_dil[0], p_dil[1], p_dil[2], slopes)
        y_na = neighbourhood_attention(p_na[0], p_na[1], p_na[2], rpb[layer])
        y_dil = rms_norm(y_dil.transpose(0, 2, 1, 3).reshape(B, S, WIDTH_DIL), g_out_dil[layer])
        y_na = rms_norm(y_na.transpose(0, 2, 1, 3).reshape(B, S, WIDTH_NA), g_out_na[layer])
        h = h + jnp.concatenate([y_dil, y_na], axis=-1) @ w_out[layer]
        h = h + hierarchical_moe(rms_norm(h, norm_ffn_g[layer]), w_group[layer], b_group[layer],
                                 w_router[layer], b_router[layer], w_gate[layer], w_up[layer], w_down[layer])
    return rms_norm(h, norm_final_g)
```

```python
import numpy as np
from contextlib import ExitStack
import concourse.bass as bass
import concourse.mybir as mybir
from concourse.bass_utils import run_bass_kernel_spmd

F32 = mybir.dt.float32
BF16 = mybir.dt.bfloat16
AF = mybir.ActivationFunctionType
ALU = mybir.AluOpType
AX = mybir.AxisListType

NCORES = 8
S = 2048
D = 1024
TOK = 4096
NEG = -30000.0
EPS = 1e-6
import os
DEBUG = bool(os.environ.get('KSTOP', ''))
STOP = os.environ.get('KSTOP', '')


class StopBuild(Exception):
    pass


DUMPS = {}


def stage(name):
    if STOP and name == STOP:
        if name in DUMPS:
            DUMPS[name]()
        raise StopBuild()


class Op:
    __slots__ = ("eng", "fn", "deps", "is_dma", "sem", "semval", "needs_signal", "sigcount", "eidx", "idx", "wo")


class Prog:
    ENGS = ("pe", "act", "dve", "pool", "sp")

    def __init__(self):
        self.ops = []
        self.last_writer = {}
        self.readers = {}
        self.eng_count = {e: 0 for e in self.ENGS}
        self.dma_slots = set()

    def add(self, eng, fn, reads=(), writes=(), dma=None, waitonly=False):
        op = Op()
        op.eng = eng; op.fn = fn; op.idx = len(self.ops)
        op.is_dma = dma is not None
        op.wo = waitonly
        op.sem = dma; op.semval = 0; op.needs_signal = False; op.sigcount = 0
        op.eidx = self.eng_count[eng]; self.eng_count[eng] += 1
        if dma is not None:
            self.dma_slots.add(dma)
        deps = {}
        for t in reads:
            w = self.last_writer.get(t)
            if w is not None:
                deps[w] = "raw"
        for t in writes:
            w = self.last_writer.get(t)
            if w is not None and w not in deps:
                deps[w] = "waw"
            for r in self.readers.get(t, ()):
                if r not in deps and r != op.idx:
                    deps[r] = "war"
        for t in (() if waitonly else reads):
            lst = self.readers.setdefault(t, [])
            if not op.is_dma:
                lst[:] = [r for r in lst if self.ops[r].is_dma or self.ops[r].eng != eng]
            lst.append(op.idx)
        for t in writes:
            self.last_writer[t] = op.idx
            self.readers[t] = []
        op.deps = deps
        self.ops.append(op)
        return op

    def _need_wait(self, op, p, kind):
        if p.is_dma:
            return True
        if p.eng != op.eng:
            return True
        if op.is_dma:
            return True
        if p.eng == "pe":
            return False
        if kind == "raw" and ((op.eidx - p.eidx) <= 3 or p.eng == "pool"):
            return True
        return False

    def emit(self, sem_ctx):
        ops = self.ops
        for op in ops:
            for d, kind in op.deps.items():
                p = ops[d]
                if (not p.is_dma) and self._need_wait(op, p, kind):
                    p.needs_signal = True
        if os.environ.get('KALLSIG'):
            for op in ops:
                if not op.is_dma and op.fn(None) if False else (not op.is_dma and not getattr(op, "wo", False)):
                    op.needs_signal = True
        cnt = {e: 0 for e in self.ENGS}
        dcnt = {}
        for op in ops:
            if op.is_dma:
                dcnt[op.sem] = dcnt.get(op.sem, 0) + 16
                op.semval = dcnt[op.sem]
            elif op.needs_signal:
                cnt[op.eng] += 1
                op.sigcount = cnt[op.eng]
        by_eng = {e: [o for o in ops if o.eng == e] for e in self.ENGS}
        self.stats = (dict(cnt), {e: len(v) for e, v in by_eng.items()})

        def body(ename):
            def run(eng):
                waited = {}
                for op in by_eng[ename]:
                    for d, kind in op.deps.items():
                        p = ops[d]
                        if not self._need_wait(op, p, kind):
                            continue
                        if p.is_dma:
                            key = "dma:" + p.sem; val = p.semval
                        else:
                            key = "eng:" + p.eng; val = p.sigcount
                        if waited.get(key, 0) >= val:
                            continue
                        waited[key] = val
                        eng.wait_ge(sem_ctx[key], val)
                    ins = op.fn(eng)
                    if op.is_dma:
                        ins.then_inc(sem_ctx["dma:" + op.sem], 16)
                    elif op.needs_signal:
                        ins.then_inc(sem_ctx["eng:" + ename], 1)
                for op in by_eng[ename]:
                    if op.is_dma:
                        key = "dma:" + op.sem
                        if waited.get(key, 0) < dcnt[op.sem]:
                            waited[key] = dcnt[op.sem]
                            eng.wait_ge(sem_ctx[key], dcnt[op.sem])
            return run
        return body


def tsl(r, d, p0, n):
    return slice(r + d * p0, r + d * (p0 + n - 1) + 1, d)


def build_program():
    nc = bass.Bass("TRN2", target_bir_lowering=False)
    dt_in = lambda n, s, dt=F32: nc.dram_tensor(n, s, dt, kind="ExternalInput").ap()
    x_d = dt_in("x", [TOK, D])
    win_d = dt_in("w_in", [D, 3072])
    wout_d = dt_in("w_out", [D, D])
    wg_d = dt_in("w_gate", [16, D, 256])
    wu_d = dt_in("w_up", [16, D, 256])
    wd_d = dt_in("w_down", [16, 256, D])
    wr_d = dt_in("wr", [D, 20])
    vec_d = dt_in("vecs", [128, 24 + 20])
    gfin_d = dt_in("gfin", [128, D])
    dilb_d = dt_in("dilb", [128, 24 * 256])
    nas_d = dt_in("nas", [4, 128, 4 * 896])
    y_d = nc.dram_tensor("y", [TOK, D], F32, kind="ExternalOutput").ap()
    hs_d = nc.dram_tensor("hscr", [TOK, D], F32, kind="Internal").ap()
    wgs_d = nc.dram_tensor("wg_bf", [16, D, 256], BF16, kind="Internal").ap()
    wus_d = nc.dram_tensor("wu_bf", [16, D, 256], BF16, kind="Internal").ap()
    wds_d = nc.dram_tensor("wd_bf", [16, 256, D], BF16, kind="Internal").ap()
    dbg_d = None
    if DEBUG:
        dbg_d = nc.dram_tensor("dbg", [128, 8 * 2048], F32, kind="ExternalOutput").ap()

    P = Prog()
    es = ExitStack()
    ARENA = 53200
    arena = es.enter_context(nc.sbuf_tensor("arena", [128, ARENA], F32))
    arena_bf = arena.bitcast(BF16)
    ptr = [0]

    def alloc(ncols, dt=F32, shape=None):
        n32 = ncols if dt == F32 else (ncols + 1) // 2
        a = ptr[0]
        ptr[0] += n32
        assert ptr[0] <= ARENA, ("SBUF overflow", ptr[0])
        if dt == F32:
            ap = arena[:, a:a + ncols]
        else:
            ap = arena_bf[:, 2 * a:2 * a + ncols]
        if shape is not None:
            names = " ".join("d%d" % i for i in range(len(shape)))
            kw = {"d%d" % i: shape[i] for i in range(len(shape))}
            ap = ap.rearrange("p (%s) -> p %s" % (names, names), **kw)
        return ap

    pb_t = es.enter_context(nc.psum_tensor("pb_t", [128, 1024], BF16))
    banks = [es.enter_context(nc.psum_tensor("bk%d" % i, [128, 512], F32)) for i in range(7)]
    tbank = [(pb_t, "bk7"), (banks[6].bitcast(BF16), "bk6")]

    ident = alloc(128, BF16)
    sel = alloc(16 * 128, BF16, (16, 128))
    vecs = alloc(44)
    gfin = alloc(D)
    wr_bf = alloc(8 * 20, BF16, (8, 20))
    mhalf = alloc(4)
    ones1 = alloc(2, BF16)
    bar = alloc(4)
    persist_end = ptr[0]

    try:
        P.add("pool", lambda e: e.memset(ident, 0.0), writes=["ident"])
        P.add("pool", lambda e: e.affine_select(out=ident, in_=ident, pattern=[[-1, 128]], compare_op=ALU.not_equal,
                                                fill=1.0, base=0, channel_multiplier=1), reads=["ident"], writes=["ident"])
        P.add("pool", lambda e: e.memset(sel, 0.0), writes=["sel"])
        P.add("pool", lambda e: e.affine_select(out=sel[0:16], in_=sel[0:16], pattern=[[-1, 16], [0, 128]],
                                                compare_op=ALU.not_equal, fill=1.0, base=0, channel_multiplier=1),
              reads=["sel"], writes=["sel"])
        P.add("pool", lambda e: e.memset(mhalf, -0.5), writes=["mhalf"])
        P.add("pool", lambda e: e.memset(ones1, 1.0), writes=["ones1"])
        P.add("sp", lambda e: e.dma_start(out=vecs, in_=vec_d), writes=["vecs"], dma="c0")
        P.add("sp", lambda e: e.dma_start(out=gfin, in_=gfin_d), writes=["gfin"], dma="c1")
        P.add("pool", lambda e: e.dma_start(out=wr_bf, in_=wr_d.rearrange("(c p) n -> p c n", p=128)),
              writes=["wr_bf"], dma="c2")
        gmix = vecs[:, 0:8]; gffn = vecs[:, 8:16]; gout = vecs[:, 16:24]; brep = vecs[:, 24:44]

        def rms_rstd(src, ss, ms, rstd, junk, tag, ncols=D):
            P.add("act", lambda e: e.activation(out=junk, in_=src, func=AF.Square, accum_out=ss),
                  reads=[tag + "_src"], writes=[tag + "_junk", tag + "_ss"])
            P.add("dve", lambda e: e.tensor_scalar(out=ms, in0=ss, scalar1=1.0 / ncols, scalar2=EPS, op0=ALU.mult, op1=ALU.add),
                  reads=[tag + "_ss"], writes=[tag + "_ms"])
            P.add("pool", lambda e: e.tensor_tensor(out=rstd, in0=ms, in1=mhalf[:, 0:1], op=ALU.pow),
                  reads=[tag + "_ms", "mhalf"], writes=[tag + "_rstd"])

        hnT = alloc(8 * S, BF16, (8, S))
        yT = alloc(8 * S, BF16, (8, S))
        dilb = alloc(24 * 256, BF16)
        wblk = [[alloc(8 * 128, BF16, (8, 128)) for _ in range(3)] for _ in range(2)]
        QT = [alloc(S, BF16) for _ in range(2)]
        KT = [alloc(S, BF16) for _ in range(2)]
        Vt_raw = alloc(3 * 16 * 256, BF16)
        Vt = Vt_raw.rearrange("p (l t h c) -> p l t h c", l=3, t=16, h=2, c=128)
        wout_bf = Vt_raw[:, 0:8 * D].rearrange("p (c n) -> p c n", c=8)
        acc = [alloc(S) for _ in range(2)]
        rden = alloc(S)
        PT = alloc(3 * 512, BF16)
        nastrip = alloc(4 * 896, BF16, (2, 2, 896))
        xs = [alloc(D, BF16) for _ in range(2)]
        xt = [alloc(D) for _ in range(2)]
        sqt = alloc(8 * 128, BF16, (8, 128))
        small = alloc(64)
        p1_end = ptr[0]


        def dump(items):
            col = [0]
            for ap, toks in items:
                n = ap.shape[-1] if len(ap.shape) == 2 else None
                assert n is not None
                a = col[0]; col[0] += n
                P.add("pool", lambda e, ap=ap, a=a, n=n: e.dma_start(out=dbg_d[0:ap.shape[0], a:a + n], in_=ap, max_dma_last_dim=2048),
                      reads=toks, dma="dbg")
        alltok = lambda: list(P.last_writer.keys())
        DUMPS['H0'] = lambda: dump([(hnT[:, c, :], alltok()) for c in range(8)])
        DUMPS['I0_1'] = lambda: dump([(QT[0], alltok()), (KT[0], alltok()), (acc[0], alltok()), (acc[1], alltok()), (yT[:, 0, :], alltok()),
                                      (Vt_raw[:, 0:4096], alltok())])
        DUMPS['I0_5'] = lambda: dump([(QT[0], alltok()), (KT[0], alltok()), (acc[0], alltok()), (acc[1], alltok()), (yT[:, 4, :], alltok()),
                                      (Vt_raw[:, 0:4096], alltok())])
        for k_ in (2, 3, 4, 6, 7):
            DUMPS['I0_%d' % k_] = lambda: dump([(yT[:, c, :], alltok()) for c in range(8)])
        if os.environ.get('KD2'):
            DUMPS['I0_2'] = lambda: dump([(QT[0], alltok()), (KT[0], alltok()), (QT[1], alltok()), (KT[1], alltok()), (wblk[0][0][:, 0, :], alltok()), (wblk[0][2][:, 0, :], alltok()), (wblk[1][0][:, 0, :], alltok())])
        DUMPS['P0'] = lambda: dump([(yT[:, c, :], alltok()) for c in range(8)])
        bP = [banks[0], banks[1]]
        bS = [banks[2], banks[3], banks[4]]
        bO = [banks[5], banks[6]]

        P.add("pool", lambda e: e.dma_start(out=dilb, in_=dilb_d, max_dma_last_dim=4096), writes=["dilb"], dma="c3")
        if DEBUG:
            P.add("pool", lambda e: e.memset(yT, 0.0), writes=["yT%d_%d" % (c, hp) for c in range(8) for hp in range(2)])

        ctr = {"S": 0, "O": 0, "P": 0, "x": 0}

        for s in range(2):
            tb = s * S
            for tt in range(16):
                xi = ctr["x"] % 2; ctr["x"] += 1
                xtile = xt[xi]; xsb = xs[xi]
                row0 = tb + tt * 128
                P.add("sp", lambda e, xtile=xtile, row0=row0: e.dma_start(out=xtile, in_=x_d[row0:row0 + 128, :]),
                      writes=["xt%d_src" % xi], dma="x%d" % xi)
                ss = small[:, xi * 4:xi * 4 + 1]; ms = small[:, xi * 4 + 1:xi * 4 + 2]; rstd = small[:, xi * 4 + 2:xi * 4 + 3]
                P.add("act", lambda e, xsb=xsb, xtile=xtile, ss=ss: e.activation(out=xsb, in_=xtile, func=AF.Square, accum_out=ss),
                      reads=["xt%d_src" % xi], writes=["xs%d" % xi, "xt%d_ss" % xi])
                P.add("dve", lambda e, ms=ms, ss=ss: e.tensor_scalar(out=ms, in0=ss, scalar1=1.0 / D, scalar2=EPS, op0=ALU.mult, op1=ALU.add),
                      reads=["xt%d_ss" % xi], writes=["xt%d_ms" % xi])
                P.add("pool", lambda e, ms=ms, rstd=rstd: e.tensor_tensor(out=rstd, in0=ms, in1=mhalf[:, 0:1], op=ALU.pow),
                      reads=["xt%d_ms" % xi, "mhalf"], writes=["xt%d_rstd" % xi])
                P.add("dve", lambda e, xsb=xsb, xtile=xtile, rstd=rstd: e.tensor_scalar(out=xsb, in0=xtile, scalar1=rstd, scalar2=None, op0=ALU.mult),
                      reads=["xt%d_src" % xi, "xt%d_rstd" % xi], writes=["xs%d" % xi])
                tbk, ttok = tbank[tt % 2]
                for c in range(8):
                    P.add("pe", lambda e, xsb=xsb, c=c, tbk=tbk: e.transpose(tbk[:, c * 128:(c + 1) * 128], xsb[:, c * 128:(c + 1) * 128], ident),
                          reads=["xs%d" % xi, "ident"], writes=[ttok])
                for c in range(8):
                    dst = hnT[:, c, tt * 128:(tt + 1) * 128]
                    src = tbk[:, c * 128:(c + 1) * 128]
                    if tt % 2 == 0:
                        P.add("dve", lambda e, dst=dst, src=src, c=c: e.tensor_scalar(out=dst, in0=src, scalar1=gmix[:, c:c + 1], scalar2=None, op0=ALU.mult),
                              reads=[ttok, "vecs"], writes=["hn%d_a" % tt])
                    else:
                        P.add("act", lambda e, dst=dst, src=src, c=c: e.activation(out=dst, in_=src, func=AF.Copy, scale=gmix[:, c:c + 1]),
                              reads=[ttok, "vecs"], writes=["hn%d_b" % tt])

            def hn_tokens(tiles):
                out = []
                for t in tiles:
                    out += ["hn%d_a" % t, "hn%d_b" % t]
                return out

            stage('H%d' % s)
            for lay_ in range(3):
                P.add("pool", lambda e, lay_=lay_: e.memset(Vt[:, lay_, :, :, 64:128], 1.0), writes=["V%d" % lay_])
            for item in range(8):
                stage('I%d_%d' % (s, item))
                if os.environ.get('KBAR'):
                    P.add("act", lambda e: e.activation(out=bar[:, 0:1], in_=mhalf[:, 0:1], func=AF.Copy), reads=["mhalf"], writes=["bar_act"])
                    P.add("dve", lambda e: e.tensor_copy(out=bar[:, 1:2], in_=mhalf[:, 0:1]), reads=["mhalf"], writes=["bar_dve"])
                    P.add("pool", lambda e: e.tensor_copy(out=bar[:, 2:3], in_=mhalf[:, 0:1]), reads=["mhalf"], writes=["bar_pool"])
                    for en in ("pe", "act", "dve", "pool", "sp"):
                        P.add(en, lambda e: None, reads=["bar_act", "bar_dve", "bar_pool"], waitonly=True)
                if os.environ.get('KSNAP') and item == 1:
                    P.add("pool", lambda e: e.tensor_copy(out=yT[:, 7, :], in_=yT[:, 0, :]), reads=["yT0_0", "yT0_1"], writes=["yT7_0", "yT7_1"])
                    P.add("pool", lambda e: e.tensor_copy(out=yT[:, 6, :], in_=acc[0][:, :]), reads=["acc0"], writes=["yT6_0", "yT6_1"])
                if s == 0:
                    for ex_ in (2 * item, 2 * item + 1):
                        for nm_, src_, dst_ in (("g", wg_d, wgs_d), ("u", wu_d, wus_d), ("d", wd_d, wds_d)):
                            P.add("pool", lambda e, src_=src_, dst_=dst_, ex_=ex_: e.dma_start(out=dst_[ex_], in_=src_[ex_], max_dma_last_dim=4096),
                                  writes=["cv%s%d" % (nm_, ex_)], dma="cv%s%d" % (nm_, ex_))
                is_dil = item < 4
                j = item % 4
                st = item % 2
                colbase = 0 if is_dil else 1536
                wq, wk, wv = wblk[st]
                for wi, (wb, off) in enumerate(((wq, 0), (wk, 512), (wv, 1024))):
                    c0 = colbase + off + j * 128
                    P.add("pool", lambda e, wb=wb, c0=c0: e.dma_start(out=wb, in_=win_d.rearrange("(c p) n -> p c n", p=128)[:, :, c0:c0 + 128]),
                          writes=["wb%d_%d" % (st, wi)], dma="wb%d_%d" % (st, wi))
                if not is_dil:
                    P.add("pool", lambda e, j=j: e.dma_start(out=nastrip, in_=nas_d[j].rearrange("p (h v n) -> p h v n", h=2, v=2), max_dma_last_dim=3584),
                          writes=["nastrip"], dma="nas")
                for which, (wb, dstT, scl) in enumerate(((wq, QT[st], 0.125), (wk, KT[st], 1.0))):
                    for tc in range(4):
                        bi = ctr["P"] % 2; ctr["P"] += 1
                        bk = bP[bi]
                        for c in range(8):
                            P.add("pe", lambda e, bk=bk, wb=wb, c=c, tc=tc: e.matmul(bk[:, 0:512], lhsT=wb[:, c, :], rhs=hnT[:, c, tc * 512:(tc + 1) * 512],
                                                                                     start=(c == 0), stop=(c == 7)),
                                  reads=["wb%d_%d" % (st, which)] + hn_tokens(range(4 * tc, 4 * tc + 4)), writes=["bk%d" % bi])
                        dst = dstT[:, tc * 512:(tc + 1) * 512]
                        tokn = ("QT%d" if which == 0 else "KT%d") % st
                        if which == 0:
                            P.add("dve", lambda e, dst=dst, bk=bk: e.tensor_scalar(out=dst, in0=bk[:, 0:512], scalar1=0.125, scalar2=None, op0=ALU.mult),
                                  reads=["bk%d" % bi], writes=[tokn])
                        else:
                            P.add("dve", lambda e, dst=dst, bk=bk: e.tensor_copy(out=dst, in_=bk[:, 0:512]),
                                  reads=["bk%d" % bi], writes=[tokn])
                layouts = ((0, 1), (1, 4), (2, 16)) if is_dil else ((0, 1),)
                for (lay, d) in layouts:
                    L = S // d; nt = L // 128
                    tiles = [(r, m) for r in range(d) for m in range(nt)]
                    for g4 in range(4):
                        bi = ctr["P"] % 2; ctr["P"] += 1
                        bk = bP[bi]
                        for q in range(4):
                            r, m = tiles[g4 * 4 + q]
                            tsl_ = tsl(r, d, 128 * m, 128)
                            lo_t = (r + d * 128 * m) // 128; hi_t = (r + d * (128 * m + 127)) // 128
                            for c in range(8):
                                P.add("pe", lambda e, bk=bk, c=c, q=q, tsl_=tsl_, wv=wv: e.matmul(bk[:, q * 128:(q + 1) * 128], lhsT=hnT[:, c, tsl_], rhs=wv[:, c, :],
                                                                                          start=(c == 0), stop=(c == 7)),
                                      reads=["wb%d_2" % st] + hn_tokens(range(lo_t, hi_t + 1)), writes=["bk%d" % bi])
                        for q in range(4):
                            dst = Vt[:, lay, g4 * 4 + q, :, 0:64]
                            src = bk[:, q * 128:(q + 1) * 128].rearrange("p (h c) -> p h c", h=2)
                            P.add("dve", lambda e, dst=dst, src=src: e.tensor_copy(out=dst, in_=src),
                                  reads=["bk%d" % bi], writes=["V%d" % lay])

                tasks = []
                for hp in range(2):
                    h = 2 * j + hp
                    rows = slice(hp * 64, hp * 64 + 64)
                    qT = QT[st][rows, :]; kT = KT[st][rows, :]
                    acc_h = acc[hp]
                    atok = "acc%d" % hp
                    if is_dil:
                        for pi, d in enumerate((1, 4, 16)):
                            L = S // d; nt = L // 128
                            bias = dilb[:, (h * 3 + pi) * 256:(h * 3 + pi + 1) * 256]
                            for r in range(d):
                                for sb0 in range(0, nt + 1, 2):
                                    sblk = list(range(nt + 1))[sb0:sb0 + 2]

                                    def s_part(sblk=sblk, r=r, d=d, nt=nt, bias=bias, kT=kT, qT=qT, st=st):
                                        si = ctr["S"] % 3; ctr["S"] += 1
                                        bkS = bS[si]
                                        pt = PT[:, si * 512:(si + 1) * 512]
                                        vlo = None; vhi = None
                                        for bi_, b in enumerate(sblk):
                                            base = bi_ * 256
                                            if b == 0:
                                                clo, chi = base + 192, base + 256
                                            elif b == nt:
                                                clo, chi = base, base + 64
                                            else:
                                                clo, chi = base, base + 256
                                            if vlo is None:
                                                vlo = clo
                                            vhi = chi
                                            P.add("pe", lambda e, bkS=bkS, clo=clo, chi=chi, base=base: e.matmul(
                                                bkS[:, clo:chi], lhsT=ident, rhs=bias[:, clo - base:chi - base], start=True, stop=False),
                                                reads=["ident", "dilb"], writes=["bk%d" % (2 + si)])
                                            if b > 0:
                                                nq = 64 if b == nt else 128
                                                P.add("pe", lambda e, bkS=bkS, base=base, nq=nq, b=b: e.matmul(
                                                    bkS[:, base:base + nq], lhsT=kT[:, tsl(r, d, 128 * (b - 1), 128)],
                                                    rhs=qT[:, tsl(r, d, 128 * b - 64, nq)], start=False, stop=(b == nt)),
                                                    reads=["QT%d" % st, "KT%d" % st], writes=["bk%d" % (2 + si)])
                                            if b < nt:
                                                q0 = 64 if b == 0 else 0
                                                P.add("pe", lambda e, bkS=bkS, base=base, q0=q0, b=b: e.matmul(
                                                    bkS[:, base + 128 + q0:base + 256], lhsT=kT[:, tsl(r, d, 128 * b, 128)],
                                                    rhs=qT[:, tsl(r, d, 128 * b - 64 + q0, 128 - q0)], start=False, stop=True),
                                                    reads=["QT%d" % st, "KT%d" % st], writes=["bk%d" % (2 + si)])
                                        P.add("act", lambda e, pt=pt, bkS=bkS, vlo=vlo, vhi=vhi: e.activation(out=pt[:, vlo:vhi], in_=bkS[:, vlo:vhi], func=AF.Exp),
                                              reads=["bk%d" % (2 + si)], writes=["PT%d" % si])
                                        return si

                                    def pv_part(si, sblk=sblk, r=r, d=d, nt=nt, pi=pi, hp=hp, acc_h=acc_h, atok=atok):
                                        oi = ctr["O"] % 2; ctr["O"] += 1
                                        bkO = bO[oi]
                                        pt = PT[:, si * 512:(si + 1) * 512]
                                        for bi_, b in enumerate(sblk):
                                            base = bi_ * 256
                                            ob = bi_ * 128
                                            if b == 0:
                                                P.add("pe", lambda e, bkO=bkO, ob=ob, base=base: e.matmul(
                                                    bkO[:, ob + 64:ob + 128], lhsT=Vt[:, pi, r * nt + 0, hp, :], rhs=pt[:, base + 192:base + 256], start=True, stop=True),
                                                    reads=["PT%d" % si, "V%d" % pi], writes=["bk%d" % (5 + oi)])
                                            elif b == nt:
                                                P.add("pe", lambda e, bkO=bkO, ob=ob, base=base: e.matmul(
                                                    bkO[:, ob:ob + 64], lhsT=Vt[:, pi, r * nt + nt - 1, hp, :], rhs=pt[:, base:base + 64], start=True, stop=True),
                                                    reads=["PT%d" % si, "V%d" % pi], writes=["bk%d" % (5 + oi)])
                                            else:
                                                P.add("pe", lambda e, bkO=bkO, ob=ob, base=base, b=b: e.matmul(
                                                    bkO[:, ob:ob + 128], lhsT=Vt[:, pi, r * nt + b - 1, hp, :], rhs=pt[:, base:base + 128], start=True, stop=False),
                                                    reads=["PT%d" % si, "V%d" % pi], writes=["bk%d" % (5 + oi)])
                                                P.add("pe", lambda e, bkO=bkO, ob=ob, base=base, b=b: e.matmul(
                                                    bkO[:, ob:ob + 128], lhsT=Vt[:, pi, r * nt + b, hp, :], rhs=pt[:, base + 128:base + 256], start=False, stop=True),
                                                    reads=["PT%d" % si, "V%d" % pi], writes=["bk%d" % (5 + oi)])
                                        b0 = sblk[0]
                                        c_lo = 64 if b0 == 0 else 0
                                        c_hi = len(sblk) * 128 - (64 if sblk[-1] == nt else 0)
                                        p_lo = 128 * b0 - 64 + c_lo
                                        dsta = acc_h[:, tsl(r, d, p_lo, c_hi - c_lo)]
                                        srca = bkO[:, c_lo:c_hi]
                                        if pi == 0:
                                            P.add("dve", lambda e: e.tensor_copy(out=dsta, in_=srca),
                                                  reads=["bk%d" % (5 + oi)], writes=[atok])
                                        else:
                                            P.add("dve", lambda e: e.tensor_tensor(out=dsta, in0=srca, in1=dsta, op=ALU.add),
                                                  reads=["bk%d" % (5 + oi), atok], writes=[atok])
                                    tasks.append((s_part, pv_part))
                    else:
                        for g in range(8):
                            if g == 0:
                                ms_, var = [0, 1, 2, 3], 1
                            elif g == 7:
                                ms_, var = [12, 13, 14, 15], 1
                            else:
                                ms_, var = list(range(2 * g - 2, 2 * g + 4)), 0
                            npairs = (len(ms_) + 1) // 2
                            ostate = {}
                            for k0 in range(0, len(ms_), 2):
                                pair = ms_[k0:k0 + 2]

                                def s_part(pair=pair, g=g, var=var, hp=hp, kT=kT, qT=qT, st=st):
                                    si = ctr["S"] % 3; ctr["S"] += 1
                                    bkS = bS[si]
                                    pt = PT[:, si * 512:(si + 1) * 512]
                                    for ki, m in enumerate(pair):
                                        base = ki * 256
                                        sft = 6 - (2 * m - 4 * g)
                                        assert 0 <= sft and sft * 64 + 256 <= 896
                                        P.add("pe", lambda e, bkS=bkS, base=base, sft=sft: e.matmul(
                                            bkS[:, base:base + 256], lhsT=ident, rhs=nastrip[:, hp, var, sft * 64:sft * 64 + 256], start=True, stop=False),
                                            reads=["ident", "nastrip"], writes=["bk%d" % (2 + si)])
                                        P.add("pe", lambda e, bkS=bkS, base=base, m=m: e.matmul(
                                            bkS[:, base:base + 256], lhsT=kT[:, m * 128:(m + 1) * 128], rhs=qT[:, g * 256:(g + 1) * 256], start=False, stop=True),
                                            reads=["QT%d" % st, "KT%d" % st], writes=["bk%d" % (2 + si)])
                                    ncol = 256 * len(pair)
                                    P.add("act", lambda e, pt=pt, bkS=bkS, ncol=ncol: e.activation(out=pt[:, 0:ncol], in_=bkS[:, 0:ncol], func=AF.Exp),
                                          reads=["bk%d" % (2 + si)], writes=["PT%d" % si])
                                    return si

                                def pv_part(si, pair=pair, k0=k0, g=g, hp=hp, acc_h=acc_h, atok=atok, ostate=ostate, nms=len(ms_)):
                                    if k0 == 0:
                                        ostate["oi"] = ctr["O"] % 2; ctr["O"] += 1
                                    oi = ostate["oi"]
                                    bkO = bO[oi]
                                    pt = PT[:, si * 512:(si + 1) * 512]
                                    for ki, m in enumerate(pair):
                                        ti = k0 + ki
                                        P.add("pe", lambda e, bkO=bkO, ki=ki, m=m, ti=ti: e.matmul(
                                            bkO[:, 0:256], lhsT=Vt[:, 0, m, hp, :], rhs=pt[:, ki * 256:(ki + 1) * 256], start=(ti == 0), stop=(ti == nms - 1)),
                                            reads=["PT%d" % si, "V0"], writes=["bk%d" % (5 + oi)])
                                    if k0 + len(pair) == nms:
                                        dsta = acc_h[:, g * 256:(g + 1) * 256]
                                        P.add("dve", lambda e: e.tensor_copy(out=dsta, in_=bkO[:, 0:256]),
                                              reads=["bk%d" % (5 + oi)], writes=[atok])
                                tasks.append((s_part, pv_part))

                    def fin_part(_si, acc_h=acc_h, atok=atok, rows=rows, hp=hp, chunk=(j if is_dil else 4 + j)):
                        P.add("act", lambda e: e.activation(out=rden[0:64, :], in_=acc_h[64:128, :], func=AF.Ln),
                              reads=[atok], writes=["rden"])
                        P.add("act", lambda e: e.activation(out=rden[0:64, :], in_=rden[0:64, :], func=AF.Exp, scale=-1.0),
                              reads=["rden"], writes=["rden"])
                        P.add("dve", lambda e: e.tensor_tensor(out=yT[rows, chunk, :], in0=acc_h[0:64, :], in1=rden[0:64, :], op=ALU.mult),
                              reads=[atok, "rden"], writes=["yT%d_%d" % (chunk, hp)])
                    tasks.append((None, fin_part))

                LOOK = 2
                sis = []
                for ti_, (sp_, pv_) in enumerate(tasks):
                    sis.append(sp_() if sp_ is not None else None)
                    if ti_ >= LOOK:
                        tasks[ti_ - LOOK][1](sis[ti_ - LOOK])
                for ti_ in range(max(0, len(tasks) - LOOK), len(tasks)):
                    tasks[ti_][1](sis[ti_])


            stage('P%d' % s)
            ytoks = ["yT%d_%d" % (c, hp) for c in range(8) for hp in range(2)]
            P.add("pool", lambda e: e.dma_start(out=wout_bf, in_=wout_d.rearrange("(c p) n -> p c n", p=128)),
                  writes=["V0", "V1", "V2", "wout"], dma="wout")
            for c in range(8):
                P.add("dve", lambda e, c=c: e.tensor_scalar(out=wout_bf[:, c, :], in0=wout_bf[:, c, :], scalar1=gout[:, c:c + 1], scalar2=None, op0=ALU.mult),
                      reads=["wout", "vecs"], writes=["wout"])
            for tt in range(16):
                row0 = tb + tt * 128
                tsl_ = slice(tt * 128, (tt + 1) * 128)
                xi = ctr["x"] % 2; ctr["x"] += 1
                xtile = xt[xi]
                P.add("sp", lambda e, xtile=xtile, row0=row0: e.dma_start(out=xtile, in_=x_d[row0:row0 + 128, :]),
                      writes=["xt%d_src" % xi], dma="x%d" % xi)
                P.add("pool", lambda e, tsl_=tsl_: e.tensor_tensor(out=sqt, in0=yT[:, :, tsl_], in1=yT[:, :, tsl_], op=ALU.mult),
                      reads=ytoks, writes=["sqt"])
                bq = bS[2]
                for c in range(8):
                    col = c // 4
                    P.add("pe", lambda e, c=c, col=col, bq=bq: e.matmul(bq[:, col:col + 1], lhsT=sqt[:, c, :], rhs=ones1[:, 0:1], start=(c % 4 == 0), stop=(c % 4 == 3)),
                          reads=["sqt", "ones1"], writes=["bk4"])
                ms2 = small[:, 16 + xi * 4:16 + xi * 4 + 2]; rs2 = small[:, 16 + xi * 4 + 2:16 + xi * 4 + 4]
                P.add("dve", lambda e, ms2=ms2, bq=bq: e.tensor_scalar(out=ms2, in0=bq[:, 0:2], scalar1=1.0 / 512, scalar2=EPS, op0=ALU.mult, op1=ALU.add),
                      reads=["bk4"], writes=["ms2_%d" % xi])
                P.add("pool", lambda e, ms2=ms2, rs2=rs2: e.tensor_tensor(out=rs2, in0=ms2, in1=mhalf[:, 0:2], op=ALU.pow),
                      reads=["ms2_%d" % xi, "mhalf"], writes=["rs2_%d" % xi])
                ht = acc[xi][:, 0:D]
                for half in range(2):
                    hs_ = slice(half * 512, (half + 1) * 512)
                    bA = [bP[0], bP[1]][half]; bB = [bS[0], bS[1]][half]
                    for c in range(4):
                        P.add("pe", lambda e, c=c, bA=bA, tsl_=tsl_, hs_=hs_: e.matmul(bA[:, 0:512], lhsT=yT[:, c, tsl_], rhs=wout_bf[:, c, hs_], start=(c == 0), stop=(c == 3)),
                              reads=ytoks + ["wout", "V0", "V1", "V2"], writes=["bk%d" % half])
                    for c in range(4, 8):
                        P.add("pe", lambda e, c=c, bB=bB, tsl_=tsl_, hs_=hs_: e.matmul(bB[:, 0:512], lhsT=yT[:, c, tsl_], rhs=wout_bf[:, c, hs_], start=(c == 4), stop=(c == 7)),
                              reads=ytoks + ["wout", "V0", "V1", "V2"], writes=["bk%d" % (2 + half)])
                    P.add("dve", lambda e, ht=ht, hs_=hs_, bA=bA, rs2=rs2, xtile=xtile: e.scalar_tensor_tensor(out=ht[:, hs_], in0=bA[:, 0:512], scalar=rs2[:, 0:1], in1=xtile[:, hs_], op0=ALU.mult, op1=ALU.add),
                          reads=["bk%d" % half, "rs2_%d" % xi, "xt%d_src" % xi], writes=["acc%d" % xi])
                    P.add("dve", lambda e, ht=ht, hs_=hs_, bB=bB, rs2=rs2: e.scalar_tensor_tensor(out=ht[:, hs_], in0=bB[:, 0:512], scalar=rs2[:, 1:2], in1=ht[:, hs_], op0=ALU.mult, op1=ALU.add),
                          reads=["bk%d" % (2 + half), "rs2_%d" % xi, "acc%d" % xi], writes=["acc%d" % xi])
                P.add("sp", lambda e, ht=ht, row0=row0: e.dma_start(out=hs_d[row0:row0 + 128, :], in_=ht),
                      reads=["acc%d" % xi], writes=["hs%d" % (row0 // 128)], dma="hst%d" % xi)

        stage('O')
        P.add("act", lambda e: e.activation(out=bar[:, 0:1], in_=mhalf[:, 0:1], func=AF.Copy), reads=["mhalf"], writes=["bar_act"])
        P.add("dve", lambda e: e.tensor_copy(out=bar[:, 1:2], in_=mhalf[:, 0:1]), reads=["mhalf"], writes=["bar_dve"])
        P.add("pool", lambda e: e.tensor_copy(out=bar[:, 2:3], in_=mhalf[:, 0:1]), reads=["mhalf"], writes=["bar_pool"])
        for en in ("pe", "act", "dve", "pool", "sp"):
            P.add(en, lambda e: None, reads=["bar_act", "bar_dve", "bar_pool"], waitonly=True)

        ptr[0] = persist_end
        wd_bf = alloc(32 * D, BF16, (32, D))
        hn2T = alloc(8 * 1024, BF16, (8, 1024))
        actT = alloc(32 * 1024, BF16, (32, 1024))
        wgu = [[alloc(8 * 256, BF16, (8, 256)) for _ in range(2)] for _ in range(3)]
        hb = [alloc(D) for _ in range(2)]
        xs2 = [alloc(D, BF16) for _ in range(2)]
        combT = alloc(1024, BF16)
        Cs = [alloc(512, BF16) for _ in range(2)]
        ssb = [alloc(512) for _ in range(2)]
        s2b = [alloc(512) for _ in range(2)]
        rt = alloc(960)
        comb_bf = alloc(8 * 16, BF16, (8, 16))
        small2 = alloc(32)

        bG = [banks[0], banks[1]]; bU = [banks[2], banks[3]]; bC = banks[4]; bD = [banks[5], banks[6]]

        for k in range(4):
            P.add("sp", lambda e, k=k: e.dma_start(out=wd_bf[:, 8 * k:8 * k + 8, :],
                                                   in_=wds_d[4 * k:4 * k + 4].rearrange("e (f p) n -> p (e f) n", p=128)),
                  reads=["cvd%d" % ee for ee in range(4 * k, 4 * k + 4)], writes=["wd%d" % k], dma="wd%d" % k)
        wd_toks = ["wd%d" % k for k in range(4)]

        def load_gu(n):
            if n >= 64:
                return
            ex_ = n % 16; wi_ = n % 3
            wgb_, wub_ = wgu[wi_]
            P.add("sp", lambda e: e.dma_start(out=wgb_, in_=wgs_d[ex_].rearrange("(c p) n -> p c n", p=128)),
                  reads=["cvg%d" % ex_], writes=["wg%d" % wi_], dma="wg%d" % wi_)
            P.add("sp", lambda e: e.dma_start(out=wub_, in_=wus_d[ex_].rearrange("(c p) n -> p c n", p=128)),
                  reads=["cvu%d" % ex_], writes=["wu%d" % wi_], dma="wu%d" % wi_)
        load_gu(0)
        load_gu(1)

        c2 = {"h": 0, "G": 0, "C": 0, "D": 0, "w": 0, "y": 0}
        h2_all = [("h2_%d_a" % st) for st in range(8)] + [("h2_%d_b" % st) for st in range(8)]

        def emit_hn2(T, st):
            t0 = T * 1024
            if True:
                row0 = t0 + st * 128
                hi = c2["h"] % 2; c2["h"] += 1
                htile = hb[hi]; xsb = xs2[hi]
                P.add("sp", lambda e, htile=htile, row0=row0: e.dma_start(out=htile, in_=hs_d[row0:row0 + 128, :]),
                      reads=["hs%d" % (row0 // 128)], writes=["hb%d" % hi], dma="hb%d" % hi)
                ss = small2[:, hi * 4:hi * 4 + 1]; ms = small2[:, hi * 4 + 1:hi * 4 + 2]; rstd = small2[:, hi * 4 + 2:hi * 4 + 3]
                P.add("act", lambda e, xsb=xsb, htile=htile, ss=ss: e.activation(out=xsb, in_=htile, func=AF.Square, accum_out=ss),
                      reads=["hb%d" % hi], writes=["xs2_%d" % hi, "h_ss%d" % hi])
                P.add("dve", lambda e, ms=ms, ss=ss: e.tensor_scalar(out=ms, in0=ss, scalar1=1.0 / D, scalar2=EPS, op0=ALU.mult, op1=ALU.add),
                      reads=["h_ss%d" % hi], writes=["h_ms%d" % hi])
                P.add("pool", lambda e, ms=ms, rstd=rstd: e.tensor_tensor(out=rstd, in0=ms, in1=mhalf[:, 0:1], op=ALU.pow),
                      reads=["h_ms%d" % hi, "mhalf"], writes=["h_rstd%d" % hi])
                P.add("dve", lambda e, xsb=xsb, htile=htile, rstd=rstd: e.tensor_scalar(out=xsb, in0=htile, scalar1=rstd, scalar2=None, op0=ALU.mult),
                      reads=["hb%d" % hi, "h_rstd%d" % hi], writes=["xs2_%d" % hi])
                tbk, ttok = tbank[0]
                for c in range(8):
                    P.add("pe", lambda e, xsb=xsb, c=c, tbk=tbk: e.transpose(tbk[:, c * 128:(c + 1) * 128], xsb[:, c * 128:(c + 1) * 128], ident),
                          reads=["xs2_%d" % hi, "ident"], writes=[ttok])
                for c in range(8):
                    dst = hn2T[:, c, st * 128:(st + 1) * 128]
                    src = tbk[:, c * 128:(c + 1) * 128]
                    if st % 2 == 0:
                        P.add("dve", lambda e, dst=dst, src=src, c=c: e.tensor_scalar(out=dst, in0=src, scalar1=gffn[:, c:c + 1], scalar2=None, op0=ALU.mult),
                              reads=[ttok, "vecs"], writes=["h2_%d_a" % st])
                    else:
                        P.add("act", lambda e, dst=dst, src=src, c=c: e.activation(out=dst, in_=src, func=AF.Copy, scale=gffn[:, c:c + 1]),
                              reads=[ttok, "vecs"], writes=["h2_%d_b" % st])

        def emit_router(T):
            if True:
                for st in range(8):
                    for c in range(8):
                        P.add("pe", lambda e, st=st, c=c: e.matmul(bC[:, st * 32:st * 32 + 20], lhsT=hn2T[:, c, st * 128:(st + 1) * 128], rhs=wr_bf[:, c, :],
                                                                   start=(c == 0), stop=(c == 7)),
                              reads=["h2_%d_a" % st, "h2_%d_b" % st, "wr_bf"], writes=["bk4"])
                lg = rt[:, 0:160].rearrange("p (s n) -> p s n", s=8)
                bc3 = bC[:, 0:256].rearrange("p (s n) -> p s n", s=8)[:, :, 0:20]
                RT = "rt"
                r3 = lambda lo, n: rt[:, lo:lo + 8 * n].rearrange("p (s n) -> p s n", s=8)
                gl = lg[:, :, 0:4]; el = lg[:, :, 4:20]
                gmax = rt[:, 160:168]; g1 = r3(168, 4); gex = r3(200, 4); gsum = rt[:, 232:240]; gw = rt[:, 240:248]
                pen = r3(248, 16); ml = r3(376, 16); m1 = rt[:, 504:512]; m2 = rt[:, 512:520]; k1 = r3(520, 16)
                dm = rt[:, 648:656]; w1 = rt[:, 656:664]; w2 = rt[:, 664:672]; k2 = r3(672, 16); ml2 = r3(800, 16)
                bcast = lambda ap, n: ap.unsqueeze(2).to_broadcast([128, 8, n])
                P.add("dve", lambda e: e.tensor_tensor(out=lg, in0=bc3, in1=brep.unsqueeze(1).to_broadcast([128, 8, 20]), op=ALU.add),
                      reads=["bk4", "vecs"], writes=[RT])
                P.add("dve", lambda e: e.tensor_reduce(out=gmax, in_=gl, axis=AX.X, op=ALU.max), reads=[RT], writes=[RT + "a"])
                P.add("dve", lambda e: e.tensor_tensor(out=g1, in0=gl, in1=bcast(gmax, 4), op=ALU.is_equal), reads=[RT, RT + "a"], writes=[RT + "b"])
                P.add("dve", lambda e: e.tensor_tensor(out=gex, in0=gl, in1=bcast(gmax, 4), op=ALU.subtract), reads=[RT, RT + "a"], writes=[RT + "c"])
                P.add("act", lambda e: e.activation(out=gex, in_=gex, func=AF.Exp), reads=[RT + "c"], writes=[RT + "d"])
                P.add("dve", lambda e: e.tensor_reduce(out=gsum, in_=gex, axis=AX.X, op=ALU.add), reads=[RT + "d"], writes=[RT + "e"])
                P.add("dve", lambda e: e.reciprocal(out=gw, in_=gsum), reads=[RT + "e"], writes=[RT + "f"])
                P.add("dve", lambda e: e.tensor_scalar(out=pen.rearrange("p s (g n) -> p s g n", g=4), in0=g1.unsqueeze(3).to_broadcast([128, 8, 4, 4]),
                                                       scalar1=-1.0, scalar2=30000.0, op0=ALU.add, op1=ALU.mult), reads=[RT + "b"], writes=[RT + "g"])
                P.add("dve", lambda e: e.tensor_tensor(out=ml, in0=el, in1=pen, op=ALU.add), reads=[RT, RT + "g"], writes=[RT + "h"])
                P.add("dve", lambda e: e.tensor_reduce(out=m1, in_=ml, axis=AX.X, op=ALU.max), reads=[RT + "h"], writes=[RT + "i"])
                P.add("dve", lambda e: e.tensor_tensor(out=k1, in0=ml, in1=bcast(m1, 16), op=ALU.is_equal), reads=[RT + "h", RT + "i"], writes=[RT + "j"])
                P.add("dve", lambda e: e.scalar_tensor_tensor(out=ml2, in0=k1, scalar=-60000.0, in1=ml, op0=ALU.mult, op1=ALU.add), reads=[RT + "h", RT + "j"], writes=[RT + "k"])
                P.add("dve", lambda e: e.tensor_reduce(out=m2, in_=ml2, axis=AX.X, op=ALU.max), reads=[RT + "k"], writes=[RT + "l"])
                P.add("dve", lambda e: e.tensor_tensor(out=k2, in0=ml2, in1=bcast(m2, 16), op=ALU.is_equal), reads=[RT + "k", RT + "l"], writes=[RT + "m"])
                P.add("dve", lambda e: e.tensor_tensor(out=dm, in0=m2, in1=m1, op=ALU.subtract), reads=[RT + "l", RT + "i"], writes=[RT + "n"])
                P.add("act", lambda e: e.activation(out=dm, in_=dm, func=AF.Exp), reads=[RT + "n"], writes=[RT + "o"])
                P.add("dve", lambda e: e.tensor_scalar(out=dm, in0=dm, scalar1=1.0, scalar2=None, op0=ALU.add), reads=[RT + "o"], writes=[RT + "p"])
                P.add("dve", lambda e: e.reciprocal(out=w1, in_=dm), reads=[RT + "p"], writes=[RT + "q"])
                P.add("dve", lambda e: e.tensor_scalar(out=w2, in0=w1, scalar1=-1.0, scalar2=1.0, op0=ALU.mult, op1=ALU.add), reads=[RT + "q"], writes=[RT + "r"])
                P.add("dve", lambda e: e.tensor_tensor(out=w1, in0=w1, in1=gw, op=ALU.mult), reads=[RT + "q", RT + "r", RT + "f"], writes=[RT + "s"])
                P.add("dve", lambda e: e.tensor_tensor(out=w2, in0=w2, in1=gw, op=ALU.mult), reads=[RT + "r", RT + "f"], writes=[RT + "t"])
                P.add("dve", lambda e: e.tensor_tensor(out=k1, in0=k1, in1=bcast(w1, 16), op=ALU.mult), reads=[RT + "j", RT + "s", RT + "k"], writes=[RT + "u"])
                P.add("dve", lambda e: e.tensor_tensor(out=k2, in0=k2, in1=bcast(w2, 16), op=ALU.mult), reads=[RT + "m", RT + "t"], writes=[RT + "v"])
                P.add("dve", lambda e: e.tensor_tensor(out=comb_bf, in0=k1, in1=k2, op=ALU.add), reads=[RT + "u", RT + "v"], writes=["comb_bf"])
                for st in range(8):
                    P.add("pe", lambda e, st=st: e.transpose(pb_t[0:16, st * 128:(st + 1) * 128], comb_bf[:, st, :], ident),
                          reads=["comb_bf", "ident"] + h2_all, writes=["bk7"])
                P.add("dve", lambda e: e.tensor_copy(out=combT[0:16, :], in_=pb_t[0:16, 0:1024]), reads=["bk7"], writes=["combT"])


        def emit_experts(T):
            if True:
                for ex in range(16):
                    nflat = T * 16 + ex
                    wi = nflat % 3
                    wgb, wub = wgu[wi]
                    load_gu(nflat + 2)
                    for half in range(2):
                        hs_ = slice(half * 512, (half + 1) * 512)
                        ci = c2["C"] % 2; c2["C"] += 1
                        P.add("pe", lambda e, ex=ex, hs_=hs_: e.matmul(bC[:, 0:512], lhsT=sel[0:16, ex, :], rhs=combT[0:16, hs_], start=True, stop=True),
                              reads=["sel", "combT"], writes=["bk4"])
                        P.add("dve", lambda e, ci=ci: e.tensor_copy(out=Cs[ci], in_=bC[:, 0:512]), reads=["bk4"], writes=["Cs%d" % ci])
                        for fc in range(2):
                            gi = c2["G"] % 2; c2["G"] += 1
                            for c in range(8):
                                P.add("pe", lambda e, gi=gi, c=c, fc=fc, hs_=hs_, wgb=wgb: e.matmul(bG[gi][:, 0:512], lhsT=wgb[:, c, fc * 128:(fc + 1) * 128], rhs=hn2T[:, c, hs_],
                                                                                                  start=(c == 0), stop=(c == 7)),
                                      reads=["wg%d" % wi] + h2_all, writes=["bk%d" % gi])
                            for c in range(8):
                                P.add("pe", lambda e, gi=gi, c=c, fc=fc, hs_=hs_, wub=wub: e.matmul(bU[gi][:, 0:512], lhsT=wub[:, c, fc * 128:(fc + 1) * 128], rhs=hn2T[:, c, hs_],
                                                                                                  start=(c == 0), stop=(c == 7)),
                                      reads=["wu%d" % wi] + h2_all, writes=["bk%d" % (2 + gi)])
                            P.add("act", lambda e, gi=gi: e.activation(out=ssb[gi], in_=bG[gi][:, 0:512], func=AF.Silu), reads=["bk%d" % gi], writes=["ssb%d" % gi])
                            P.add("pool", lambda e, gi=gi, ci=ci: e.tensor_tensor(out=s2b[gi], in0=ssb[gi], in1=Cs[ci], op=ALU.mult),
                                  reads=["ssb%d" % gi, "Cs%d" % ci], writes=["s2b%d" % gi])
                            P.add("dve", lambda e, gi=gi, ex=ex, fc=fc, hs_=hs_: e.tensor_tensor(out=actT[:, ex * 2 + fc, hs_], in0=bU[gi][:, 0:512], in1=s2b[gi], op=ALU.mult),
                                  reads=["bk%d" % (2 + gi), "s2b%d" % gi], writes=["actT"])


        def emit_down(T, st):
            t0 = T * 1024
            if True:
                row0 = t0 + st * 128
                hi = c2["h"] % 2; c2["h"] += 1
                htile = hb[hi]
                P.add("sp", lambda e, htile=htile, row0=row0: e.dma_start(out=htile, in_=hs_d[row0:row0 + 128, :]),
                      reads=["hs%d" % (row0 // 128)], writes=["hb%d" % hi], dma="hb%d" % hi)
                for dh in range(2):
                    di = c2["D"] % 2; c2["D"] += 1
                    ds_ = slice(dh * 512, (dh + 1) * 512)
                    for kk in range(32):
                        P.add("pe", lambda e, di=di, kk=kk, st=st, ds_=ds_: e.matmul(bD[di][:, 0:512], lhsT=actT[:, kk, st * 128:(st + 1) * 128], rhs=wd_bf[:, kk, ds_],
                                                                                   start=(kk == 0), stop=(kk == 31)),
                              reads=["actT"] + wd_toks, writes=["bk%d" % (5 + di)])
                    P.add("dve", lambda e, di=di, htile=htile, ds_=ds_: e.tensor_tensor(out=htile[:, ds_], in0=bD[di][:, 0:512], in1=htile[:, ds_], op=ALU.add),
                          reads=["bk%d" % (5 + di), "hb%d" % hi], writes=["hb%d" % hi])
                yi = c2["y"] % 2; c2["y"] += 1
                junk = xs2[yi]
                ss = small2[:, 16 + yi * 4:16 + yi * 4 + 1]; ms = small2[:, 16 + yi * 4 + 1:16 + yi * 4 + 2]; rstd = small2[:, 16 + yi * 4 + 2:16 + yi * 4 + 3]
                P.add("act", lambda e, junk=junk, htile=htile, ss=ss: e.activation(out=junk, in_=htile, func=AF.Square, accum_out=ss),
                      reads=["hb%d" % hi], writes=["xs2_%d" % yi, "f_ss%d" % yi])
                P.add("dve", lambda e, ms=ms, ss=ss: e.tensor_scalar(out=ms, in0=ss, scalar1=1.0 / D, scalar2=EPS, op0=ALU.mult, op1=ALU.add),
                      reads=["f_ss%d" % yi], writes=["f_ms%d" % yi])
                P.add("pool", lambda e, ms=ms, rstd=rstd: e.tensor_tensor(out=rstd, in0=ms, in1=mhalf[:, 0:1], op=ALU.pow),
                      reads=["f_ms%d" % yi, "mhalf"], writes=["f_rstd%d" % yi])
                P.add("dve", lambda e, htile=htile, rstd=rstd: e.scalar_tensor_tensor(out=htile, in0=htile, scalar=rstd, in1=gfin, op0=ALU.mult, op1=ALU.mult),
                      reads=["hb%d" % hi, "f_rstd%d" % yi, "gfin"], writes=["hb%d" % hi])
                P.add("sp", lambda e, htile=htile, row0=row0: e.dma_start(out=y_d[row0:row0 + 128, :], in_=htile),
                      reads=["hb%d" % hi], dma="yst%d" % hi)


        for st in range(8):
            emit_hn2(0, st)
        emit_router(0)
        for T in range(4):
            emit_experts(T)
            for st in range(8):
                emit_down(T, st)
                if T < 3:
                    emit_hn2(T + 1, st)
            if T < 3:
                emit_router(T + 1)

    except StopBuild:
        pass

    sems = {}
    for en in Prog.ENGS:
        sems["eng:" + en] = es.enter_context(nc.semaphore("s_" + en))
    for sl in sorted(P.dma_slots):
        sems["dma:" + sl] = es.enter_context(nc.semaphore("d_" + sl))
    if os.environ.get('KMAXOPS'):
        P.ops = P.ops[:int(os.environ['KMAXOPS'])]
        for i_, o_ in enumerate(P.ops[-3:]):
            print('LASTOPS', o_.eng, o_.idx)
    body = P.emit(sems)
    if os.environ.get('KSTATS'):
        print('PROG stats', P.stats, 'ndma_slots', len(P.dma_slots))
    with nc.Block() as block:
        block.sync(body("sp"))
        block.tensor(body("pe"))
        block.scalar(body("act"))
        block.vector(body("dve"))
        block.gpsimd(body("pool"))
    es.close()
    return nc


def _dil_bias():
    out = np.full((128, 24 * 256), NEG, np.float32)
    k = np.arange(128)[:, None]; q = np.arange(128)[None, :]
    for h in range(8):
        slope = 2.0 ** (-(h + 1))
        for pi, d in enumerate((1, 4, 16)):
            for half, off in enumerate((-64, 64)):
                delta = k + off - q
                val = np.where(np.abs(delta) <= 64, -slope * d * np.abs(delta), NEG).astype(np.float32)
                c0 = (h * 3 + pi) * 256 + half * 128
                out[:, c0:c0 + 128] = val
    return out


def _na_strips(rpb):
    kc = np.arange(64)[:, None]; qc = np.arange(64)[None, :]
    cs = np.clip(qc - 8, 0, 48)
    col_ok = (kc >= cs) & (kc < cs + 16)
    dcidx = np.clip(kc - qc + 15, 0, 30)
    out = np.full((4, 128, 2, 2, 896), NEG, np.float32)
    for h in range(8):
        j, hp = divmod(h, 2)
        for var in range(2):
            for i in range(2):
                for u in range(14):
                    dlt = 6 + i - u
                    ok = (-4 <= dlt <= 3) if var == 0 else (-7 <= dlt <= 7)
                    if not ok:
                        continue
                    blk = np.where(col_ok, rpb[h, dlt + 7][dcidx], NEG).astype(np.float32)
                    out[j, i * 64:(i + 1) * 64, hp, var, u * 64:(u + 1) * 64] = blk
    return out.reshape(4, 128, 4 * 896)


_NC_CACHE = {}


def kernel(x, norm_mix_g, w_in, rpb, g_out_dil, g_out_na, w_out, norm_ffn_g, w_group, b_group, w_router, b_router,
           w_gate, w_up, w_down, norm_final_g):
    f = lambda a: np.ascontiguousarray(np.asarray(a, dtype=np.float32))
    x = f(x).reshape(16 * S, D)
    col = lambda g: f(g).reshape(8, 128).T
    vecs = np.concatenate([col(norm_mix_g[0]), col(norm_ffn_g[0]),
                           col(np.concatenate([f(g_out_dil[0]), f(g_out_na[0])])),
                           np.broadcast_to(np.concatenate([f(b_group[0]), f(b_router[0])])[None, :], (128, 20))], axis=1)
    vecs = np.ascontiguousarray(vecs, dtype=np.float32)
    gfin = np.ascontiguousarray(np.broadcast_to(f(norm_final_g)[None, :], (128, D)))
    wr = np.ascontiguousarray(np.concatenate([f(w_group[0]), f(w_router[0])], axis=1))
    shared = {
        "w_in": f(w_in[0]), "w_out": f(w_out[0]), "w_gate": f(w_gate[0]), "w_up": f(w_up[0]), "w_down": f(w_down[0]),
        "wr": wr, "vecs": vecs, "gfin": gfin, "dilb": _dil_bias(), "nas": _na_strips(f(rpb[0])),
    }
    if "nc" not in _NC_CACHE:
        _NC_CACHE["nc"] = build_program()
    nc = _NC_CACHE["nc"]
    in_maps = []
    for i in range(NCORES):
        m = dict(shared)
        m["x"] = np.ascontiguousarray(x[i * TOK:(i + 1) * TOK])
        in_maps.append(m)
    res = run_bass_kernel_spmd(nc, in_maps, core_ids=list(range(NCORES)))
    y = np.concatenate([r["y"] for r in res.results], axis=0)
    if DEBUG:
        kernel.dbg = [r.get("dbg") for r in res.results]
    return y.reshape(16, S, D).astype(np.float32)
```

```python
import numpy as np
from contextlib import ExitStack
import concourse.bass as bass
import concourse.mybir as mybir
from concourse.bass_utils import run_bass_kernel_spmd

F32 = mybir.dt.float32
BF16 = mybir.dt.bfloat16
AF = mybir.ActivationFunctionType
ALU = mybir.AluOpType
AX = mybir.AxisListType

NCORES = 8
S = 2048
D = 1024
TOK = 4096
NEG = -30000.0
EPS = 1e-6
import os
DEBUG = bool(os.environ.get('KSTOP', ''))
STOP = os.environ.get('KSTOP', '')


class StopBuild(Exception):
    pass


DUMPS = {}


def stage(name):
    if STOP and name == STOP:
        if name in DUMPS:
            DUMPS[name]()
        raise StopBuild()


class Op:
    __slots__ = ("eng", "fn", "deps", "is_dma", "sem", "semval", "needs_signal", "sigcount", "eidx", "idx", "wo")


class Prog:
    ENGS = ("pe", "act", "dve", "pool", "sp")

    def __init__(self):
        self.ops = []
        self.last_writer = {}
        self.readers = {}
        self.eng_count = {e: 0 for e in self.ENGS}
        self.dma_slots = set()

    def add(self, eng, fn, reads=(), writes=(), dma=None, waitonly=False):
        op = Op()
        op.eng = eng; op.fn = fn; op.idx = len(self.ops)
        op.is_dma = dma is not None
        op.wo = waitonly
        op.sem = dma; op.semval = 0; op.needs_signal = False; op.sigcount = 0
        op.eidx = self.eng_count[eng]; self.eng_count[eng] += 1
        if dma is not None:
            self.dma_slots.add(dma)
        deps = {}
        for t in reads:
            w = self.last_writer.get(t)
            if w is not None:
                deps[w] = "raw"
        for t in writes:
            w = self.last_writer.get(t)
            if w is not None and w not in deps:
                deps[w] = "waw"
            for r in self.readers.get(t, ()):
                if r not in deps and r != op.idx:
                    deps[r] = "war"
        for t in (() if waitonly else reads):
            lst = self.readers.setdefault(t, [])
            if not op.is_dma:
                lst[:] = [r for r in lst if self.ops[r].is_dma or self.ops[r].eng != eng]
            lst.append(op.idx)
        for t in writes:
            self.last_writer[t] = op.idx
            self.readers[t] = []
        op.deps = deps
        self.ops.append(op)
        return op

    def _need_wait(self, op, p, kind):
        if p.is_dma:
            return True
        if p.eng != op.eng:
            return True
        if op.is_dma:
            return True
        if p.eng == "pe":
            return False
        if kind == "raw" and ((op.eidx - p.eidx) <= 3 or p.eng == "pool"):
            return True
        return False

    def emit(self, sem_ctx):
        ops = self.ops
        for op in ops:
            for d, kind in op.deps.items():
                p = ops[d]
                if (not p.is_dma) and self._need_wait(op, p, kind):
                    p.needs_signal = True
        if os.environ.get('KALLSIG'):
            for op in ops:
                if not op.is_dma and op.fn(None) if False else (not op.is_dma and not getattr(op, "wo", False)):
                    op.needs_signal = True
        cnt = {e: 0 for e in self.ENGS}
        dcnt = {}
        for op in ops:
            if op.is_dma:
                dcnt[op.sem] = dcnt.get(op.sem, 0) + 16
                op.semval = dcnt[op.sem]
            elif op.needs_signal:
                cnt[op.eng] += 1
                op.sigcount = cnt[op.eng]
        by_eng = {e: [o for o in ops if o.eng == e] for e in self.ENGS}
        self.stats = (dict(cnt), {e: len(v) for e, v in by_eng.items()})

        def body(ename):
            def run(eng):
                waited = {}
                for op in by_eng[ename]:
                    for d, kind in op.deps.items():
                        p = ops[d]
                        if not self._need_wait(op, p, kind):
                            continue
                        if p.is_dma:
                            key = "dma:" + p.sem; val = p.semval
                        else:
                            key = "eng:" + p.eng; val = p.sigcount
                        if waited.get(key, 0) >= val:
                            continue
                        waited[key] = val
                        eng.wait_ge(sem_ctx[key], val)
                    ins = op.fn(eng)
                    if op.is_dma:
                        ins.then_inc(sem_ctx["dma:" + op.sem], 16)
                    elif op.needs_signal:
                        ins.then_inc(sem_ctx["eng:" + ename], 1)
                for op in by_eng[ename]:
                    if op.is_dma:
                        key = "dma:" + op.sem
                        if waited.get(key, 0) < dcnt[op.sem]:
                            waited[key] = dcnt[op.sem]
                            eng.wait_ge(sem_ctx[key], dcnt[op.sem])
            return run
        return body


def tsl(r, d, p0, n):
    return slice(r + d * p0, r + d * (p0 + n - 1) + 1, d)


def build_program():
    nc = bass.Bass("TRN2", target_bir_lowering=False)
    dt_in = lambda n, s, dt=F32: nc.dram_tensor(n, s, dt, kind="ExternalInput").ap()
    x_d = dt_in("x", [TOK, D])
    win_d = dt_in("w_in", [D, 3072])
    wout_d = dt_in("w_out", [D, D])
    wg_d = dt_in("w_gate", [16, D, 256])
    wu_d = dt_in("w_up", [16, D, 256])
    wd_d = dt_in("w_down", [16, 256, D])
    wr_d = dt_in("wr", [D, 20])
    vec_d = dt_in("vecs", [128, 24 + 20])
    gfin_d = dt_in("gfin", [128, D])
    dilb_d = dt_in("dilb", [128, 24 * 256])
    nas_d = dt_in("nas", [4, 128, 4 * 896])
    y_d = nc.dram_tensor("y", [TOK, D], F32, kind="ExternalOutput").ap()
    hs_d = nc.dram_tensor("hscr", [TOK, D], F32, kind="Internal").ap()
    wgs_d = nc.dram_tensor("wg_bf", [16, D, 256], BF16, kind="Internal").ap()
    wus_d = nc.dram_tensor("wu_bf", [16, D, 256], BF16, kind="Internal").ap()
    wds_d = nc.dram_tensor("wd_bf", [16, 256, D], BF16, kind="Internal").ap()
    dbg_d = None
    if DEBUG:
        dbg_d = nc.dram_tensor("dbg", [128, 8 * 2048], F32, kind="ExternalOutput").ap()

    P = Prog()
    es = ExitStack()
    ARENA = 53200
    arena = es.enter_context(nc.sbuf_tensor("arena", [128, ARENA], F32))
    arena_bf = arena.bitcast(BF16)
    ptr = [0]

    def alloc(ncols, dt=F32, shape=None):
        n32 = ncols if dt == F32 else (ncols + 1) // 2
        a = ptr[0]
        ptr[0] += n32
        assert ptr[0] <= ARENA, ("SBUF overflow", ptr[0])
        if dt == F32:
            ap = arena[:, a:a + ncols]
        else:
            ap = arena_bf[:, 2 * a:2 * a + ncols]
        if shape is not None:
            names = " ".join("d%d" % i for i in range(len(shape)))
            kw = {"d%d" % i: shape[i] for i in range(len(shape))}
            ap = ap.rearrange("p (%s) -> p %s" % (names, names), **kw)
        return ap

    pb_t = es.enter_context(nc.psum_tensor("pb_t", [128, 1024], BF16))
    banks = [es.enter_context(nc.psum_tensor("bk%d" % i, [128, 512], F32)) for i in range(7)]
    tbank = [(pb_t, "bk7"), (banks[6].bitcast(BF16), "bk6")]

    ident = alloc(128, BF16)
    sel = alloc(16 * 128, BF16, (16, 128))
    vecs = alloc(44)
    gfin = alloc(D)
    wr_bf = alloc(8 * 20, BF16, (8, 20))
    mhalf = alloc(4)
    ones1 = alloc(2, BF16)
    bar = alloc(4)
    persist_end = ptr[0]

    try:
        P.add("pool", lambda e: e.memset(ident, 0.0), writes=["ident"])
        P.add("pool", lambda e: e.affine_select(out=ident, in_=ident, pattern=[[-1, 128]], compare_op=ALU.not_equal,
                                                fill=1.0, base=0, channel_multiplier=1), reads=["ident"], writes=["ident"])
        P.add("pool", lambda e: e.memset(sel, 0.0), writes=["sel"])
        P.add("pool", lambda e: e.affine_select(out=sel[0:16], in_=sel[0:16], pattern=[[-1, 16], [0, 128]],
                                                compare_op=ALU.not_equal, fill=1.0, base=0, channel_multiplier=1),
              reads=["sel"], writes=["sel"])
        P.add("pool", lambda e: e.memset(mhalf, -0.5), writes=["mhalf"])
        P.add("pool", lambda e: e.memset(ones1, 1.0), writes=["ones1"])
        P.add("sp", lambda e: e.dma_start(out=vecs, in_=vec_d), writes=["vecs"], dma="c0")
        P.add("sp", lambda e: e.dma_start(out=gfin, in_=gfin_d), writes=["gfin"], dma="c1")
        P.add("pool", lambda e: e.dma_start(out=wr_bf, in_=wr_d.rearrange("(c p) n -> p c n", p=128)),
              writes=["wr_bf"], dma="c2")
        gmix = vecs[:, 0:8]; gffn = vecs[:, 8:16]; gout = vecs[:, 16:24]; brep = vecs[:, 24:44]

        def rms_rstd(src, ss, ms, rstd, junk, tag, ncols=D):
            P.add("act", lambda e: e.activation(out=junk, in_=src, func=AF.Square, accum_out=ss),
                  reads=[tag + "_src"], writes=[tag + "_junk", tag + "_ss"])
            P.add("dve", lambda e: e.tensor_scalar(out=ms, in0=ss, scalar1=1.0 / ncols, scalar2=EPS, op0=ALU.mult, op1=ALU.add),
                  reads=[tag + "_ss"], writes=[tag + "_ms"])
            P.add("pool", lambda e: e.tensor_tensor(out=rstd, in0=ms, in1=mhalf[:, 0:1], op=ALU.pow),
                  reads=[tag + "_ms", "mhalf"], writes=[tag + "_rstd"])

        hnT = alloc(8 * S, BF16, (8, S))
        yT = alloc(8 * S, BF16, (8, S))
        dilb = alloc(24 * 256, BF16)
        wblk = [[alloc(8 * 128, BF16, (8, 128)) for _ in range(3)] for _ in range(2)]
        QT = [alloc(S, BF16) for _ in range(2)]
        KT = [alloc(S, BF16) for _ in range(2)]
        Vt_raw = alloc(3 * 16 * 256, BF16)
        Vt = Vt_raw.rearrange("p (l t h c) -> p l t h c", l=3, t=16, h=2, c=128)
        wout_bf = Vt_raw[:, 0:8 * D].rearrange("p (c n) -> p c n", c=8)
        acc = [alloc(S) for _ in range(2)]
        VTb = alloc(S, BF16)
        rden = alloc(S)
        PT = alloc(3 * 512, BF16)
        nastrip = alloc(4 * 896, BF16, (2, 2, 896))
        xs = [alloc(D, BF16) for _ in range(2)]
        xt = [alloc(D) for _ in range(2)]
        sqt = alloc(8 * 128, BF16, (8, 128))
        small = alloc(64)
        p1_end = ptr[0]


        def dump(items):
            col = [0]
            for ap, toks in items:
                n = ap.shape[-1] if len(ap.shape) == 2 else None
                assert n is not None
                a = col[0]; col[0] += n
                P.add("pool", lambda e, ap=ap, a=a, n=n: e.dma_start(out=dbg_d[0:ap.shape[0], a:a + n], in_=ap, max_dma_last_dim=2048),
                      reads=toks, dma="dbg")
        alltok = lambda: list(P.last_writer.keys())
        DUMPS['H0'] = lambda: dump([(hnT[:, c, :], alltok()) for c in range(8)])
        DUMPS['I0_1'] = lambda: dump([(QT[0], alltok()), (KT[0], alltok()), (acc[0], alltok()), (acc[1], alltok()), (yT[:, 0, :], alltok()),
                                      (Vt_raw[:, 0:4096], alltok())])
        DUMPS['I0_5'] = lambda: dump([(QT[0], alltok()), (KT[0], alltok()), (acc[0], alltok()), (acc[1], alltok()), (yT[:, 4, :], alltok()),
                                      (Vt_raw[:, 0:4096], alltok())])
        for k_ in (2, 3, 4, 6, 7):
            DUMPS['I0_%d' % k_] = lambda: dump([(yT[:, c, :], alltok()) for c in range(8)])
        if os.environ.get('KD2'):
            DUMPS['I0_2'] = lambda: dump([(QT[0], alltok()), (KT[0], alltok()), (QT[1], alltok()), (KT[1], alltok()), (wblk[0][0][:, 0, :], alltok()), (wblk[0][2][:, 0, :], alltok()), (wblk[1][0][:, 0, :], alltok())])
        DUMPS['P0'] = lambda: dump([(yT[:, c, :], alltok()) for c in range(8)])
        bP = [banks[0], banks[1]]
        bPbf = [banks[0].bitcast(BF16), banks[1].bitcast(BF16)]
        bS = [banks[2], banks[3], banks[4]]
        bO = [banks[5], banks[6]]

        P.add("pool", lambda e: e.dma_start(out=dilb, in_=dilb_d, max_dma_last_dim=4096), writes=["dilb"], dma="c3")
        if DEBUG:
            P.add("pool", lambda e: e.memset(yT, 0.0), writes=["yT%d_%d" % (c, hp) for c in range(8) for hp in range(2)])

        ctr = {"S": 0, "O": 0, "P": 0, "x": 0}

        for s in range(2):
            tb = s * S
            for tt in range(16):
                xi = ctr["x"] % 2; ctr["x"] += 1
                xtile = xt[xi]; xsb = xs[xi]
                row0 = tb + tt * 128
                P.add("sp", lambda e, xtile=xtile, row0=row0: e.dma_start(out=xtile, in_=x_d[row0:row0 + 128, :]),
                      writes=["xt%d_src" % xi], dma="x%d" % xi)
                ss = small[:, xi * 4:xi * 4 + 1]; ms = small[:, xi * 4 + 1:xi * 4 + 2]; rstd = small[:, xi * 4 + 2:xi * 4 + 3]
                P.add("act", lambda e, xsb=xsb, xtile=xtile, ss=ss: e.activation(out=xsb, in_=xtile, func=AF.Square, accum_out=ss),
                      reads=["xt%d_src" % xi], writes=["xs%d" % xi, "xt%d_ss" % xi])
                P.add("dve", lambda e, ms=ms, ss=ss: e.tensor_scalar(out=ms, in0=ss, scalar1=1.0 / D, scalar2=EPS, op0=ALU.mult, op1=ALU.add),
                      reads=["xt%d_ss" % xi], writes=["xt%d_ms" % xi])
                P.add("pool", lambda e, ms=ms, rstd=rstd: e.tensor_tensor(out=rstd, in0=ms, in1=mhalf[:, 0:1], op=ALU.pow),
                      reads=["xt%d_ms" % xi, "mhalf"], writes=["xt%d_rstd" % xi])
                P.add("dve", lambda e, xsb=xsb, xtile=xtile, rstd=rstd: e.tensor_scalar(out=xsb, in0=xtile, scalar1=rstd, scalar2=None, op0=ALU.mult),
                      reads=["xt%d_src" % xi, "xt%d_rstd" % xi], writes=["xs%d" % xi])
                tbk, ttok = tbank[tt % 2]
                for c in range(8):
                    P.add("pe", lambda e, xsb=xsb, c=c, tbk=tbk: e.transpose(tbk[:, c * 128:(c + 1) * 128], xsb[:, c * 128:(c + 1) * 128], ident),
                          reads=["xs%d" % xi, "ident"], writes=[ttok])
                for c in range(8):
                    dst = hnT[:, c, tt * 128:(tt + 1) * 128]
                    src = tbk[:, c * 128:(c + 1) * 128]
                    if tt % 2 == 0:
                        P.add("dve", lambda e, dst=dst, src=src, c=c: e.tensor_scalar(out=dst, in0=src, scalar1=gmix[:, c:c + 1], scalar2=None, op0=ALU.mult),
                              reads=[ttok, "vecs"], writes=["hn%d_a" % tt])
                    else:
                        P.add("act", lambda e, dst=dst, src=src, c=c: e.activation(out=dst, in_=src, func=AF.Copy, scale=gmix[:, c:c + 1]),
                              reads=[ttok, "vecs"], writes=["hn%d_b" % tt])

            def hn_tokens(tiles):
                out = []
                for t in tiles:
                    out += ["hn%d_a" % t, "hn%d_b" % t]
                return out

            stage('H%d' % s)
            for lay_ in range(3):
                P.add("pool", lambda e, lay_=lay_: e.memset(Vt[:, lay_, :, :, 64:128], 1.0), writes=["V%d" % lay_])
            for item in range(8):
                stage('I%d_%d' % (s, item))
                if os.environ.get('KBAR'):
                    P.add("act", lambda e: e.activation(out=bar[:, 0:1], in_=mhalf[:, 0:1], func=AF.Copy), reads=["mhalf"], writes=["bar_act"])
                    P.add("dve", lambda e: e.tensor_copy(out=bar[:, 1:2], in_=mhalf[:, 0:1]), reads=["mhalf"], writes=["bar_dve"])
                    P.add("pool", lambda e: e.tensor_copy(out=bar[:, 2:3], in_=mhalf[:, 0:1]), reads=["mhalf"], writes=["bar_pool"])
                    for en in ("pe", "act", "dve", "pool", "sp"):
                        P.add(en, lambda e: None, reads=["bar_act", "bar_dve", "bar_pool"], waitonly=True)
                if os.environ.get('KSNAP') and item == 1:
                    P.add("pool", lambda e: e.tensor_copy(out=yT[:, 7, :], in_=yT[:, 0, :]), reads=["yT0_0", "yT0_1"], writes=["yT7_0", "yT7_1"])
                    P.add("pool", lambda e: e.tensor_copy(out=yT[:, 6, :], in_=acc[0][:, :]), reads=["acc0"], writes=["yT6_0", "yT6_1"])
                if s == 0:
                    for ex_ in (2 * item, 2 * item + 1):
                        for nm_, src_, dst_ in (("g", wg_d, wgs_d), ("u", wu_d, wus_d), ("d", wd_d, wds_d)):
                            P.add("pool", lambda e, src_=src_, dst_=dst_, ex_=ex_: e.dma_start(out=dst_[ex_], in_=src_[ex_], max_dma_last_dim=4096),
                                  writes=["cv%s%d" % (nm_, ex_)], dma="cv%s%d" % (nm_, ex_))
                is_dil = item < 4
                j = item % 4
                st = item % 2
                colbase = 0 if is_dil else 1536
                wq, wk, wv = wblk[st]
                for wi, (wb, off) in enumerate(((wq, 0), (wk, 512), (wv, 1024))):
                    c0 = colbase + off + j * 128
                    P.add("pool", lambda e, wb=wb, c0=c0: e.dma_start(out=wb, in_=win_d.rearrange("(c p) n -> p c n", p=128)[:, :, c0:c0 + 128]),
                          writes=["wb%d_%d" % (st, wi)], dma="wb%d_%d" % (st, wi))
                if not is_dil:
                    P.add("pool", lambda e, j=j: e.dma_start(out=nastrip, in_=nas_d[j].rearrange("p (h v n) -> p h v n", h=2, v=2), max_dma_last_dim=3584),
                          writes=["nastrip"], dma="nas")
                for which, (wb, dstT, scl) in enumerate(((wq, QT[st], 0.125), (wk, KT[st], 1.0))):
                    for tc in range(4):
                        bi = ctr["P"] % 2; ctr["P"] += 1
                        bk = bP[bi]
                        for c in range(8):
                            P.add("pe", lambda e, bk=bk, wb=wb, c=c, tc=tc: e.matmul(bk[:, 0:512], lhsT=wb[:, c, :], rhs=hnT[:, c, tc * 512:(tc + 1) * 512],
                                                                                     start=(c == 0), stop=(c == 7)),
                                  reads=["wb%d_%d" % (st, which)] + hn_tokens(range(4 * tc, 4 * tc + 4)), writes=["bk%d" % bi])
                        dst = dstT[:, tc * 512:(tc + 1) * 512]
                        tokn = ("QT%d" if which == 0 else "KT%d") % st
                        if which == 0:
                            P.add("dve", lambda e, dst=dst, bk=bk: e.tensor_scalar(out=dst, in0=bk[:, 0:512], scalar1=0.125, scalar2=None, op0=ALU.mult),
                                  reads=["bk%d" % bi], writes=[tokn])
                        else:
                            P.add("dve", lambda e, dst=dst, bk=bk: e.tensor_copy(out=dst, in_=bk[:, 0:512]),
                                  reads=["bk%d" % bi], writes=[tokn])
                for tc in range(4):
                    bi = ctr["P"] % 2; ctr["P"] += 1
                    bk = bP[bi]
                    for c in range(8):
                        P.add("pe", lambda e, bk=bk, wv=wv, c=c, tc=tc: e.matmul(bk[:, 0:512], lhsT=wv[:, c, :], rhs=hnT[:, c, tc * 512:(tc + 1) * 512],
                                                                                 start=(c == 0), stop=(c == 7)),
                              reads=["wb%d_2" % st] + hn_tokens(range(4 * tc, 4 * tc + 4)), writes=["bk%d" % bi])
                    dst = VTb[:, tc * 512:(tc + 1) * 512]
                    P.add("dve", lambda e, dst=dst, bk=bk: e.tensor_copy(out=dst, in_=bk[:, 0:512]),
                          reads=["bk%d" % bi], writes=["VT"])
                layouts = ((0, 1), (1, 4), (2, 16)) if is_dil else ((0, 1),)
                for (lay, d) in layouts:
                    L = S // d; nt = L // 128
                    tiles = [(r, m) for r in range(d) for m in range(nt)]
                    for g8 in range(2):
                        bi = ctr["P"] % 2; ctr["P"] += 1
                        bkb = bPbf[bi]
                        for q in range(8):
                            r, m = tiles[g8 * 8 + q]
                            tsl_ = tsl(r, d, 128 * m, 128)
                            P.add("pe", lambda e, bkb=bkb, q=q, tsl_=tsl_: e.transpose(bkb[:, q * 128:(q + 1) * 128], VTb[:, tsl_], ident),
                                  reads=["VT", "ident"], writes=["bk%d" % bi])
                        dst = Vt[:, lay, g8 * 8:(g8 + 1) * 8, :, 0:64]
                        src = bkb[:, 0:1024].rearrange("p (t h c) -> p t h c", t=8, h=2)
                        P.add("dve", lambda e, dst=dst, src=src: e.tensor_copy(out=dst, in_=src),
                              reads=["bk%d" % bi], writes=["V%d" % lay])

                tasks = []
                for hp in range(2):
                    h = 2 * j + hp
                    rows = slice(hp * 64, hp * 64 + 64)
                    qT = QT[st][rows, :]; kT = KT[st][rows, :]
                    acc_h = acc[hp]
                    atok = "acc%d" % hp
                    if is_dil:
                        for pi, d in enumerate((1, 4, 16)):
                            L = S // d; nt = L // 128
                            bias = dilb[:, (h * 3 + pi) * 256:(h * 3 + pi + 1) * 256]
                            for r in range(d):
                                for sb0 in range(0, nt + 1, 2):
                                    sblk = list(range(nt + 1))[sb0:sb0 + 2]

                                    def s_part(sblk=sblk, r=r, d=d, nt=nt, bias=bias, kT=kT, qT=qT, st=st):
                                        si = ctr["S"] % 3; ctr["S"] += 1
                                        bkS = bS[si]
                                        pt = PT[:, si * 512:(si + 1) * 512]
                                        vlo = None; vhi = None
                                        for bi_, b in enumerate(sblk):
                                            base = bi_ * 256
                                            if b == 0:
                                                clo, chi = base + 192, base + 256
                                            elif b == nt:
                                                clo, chi = base, base + 64
                                            else:
                                                clo, chi = base, base + 256
                                            if vlo is None:
                                                vlo = clo
                                            vhi = chi
                                            P.add("pe", lambda e, bkS=bkS, clo=clo, chi=chi, base=base: e.matmul(
                                                bkS[:, clo:chi], lhsT=ident, rhs=bias[:, clo - base:chi - base], start=True, stop=False),
                                                reads=["ident", "dilb"], writes=["bk%d" % (2 + si)])
                                            if b > 0:
                                                nq = 64 if b == nt else 128
                                                P.add("pe", lambda e, bkS=bkS, base=base, nq=nq, b=b: e.matmul(
                                                    bkS[:, base:base + nq], lhsT=kT[:, tsl(r, d, 128 * (b - 1), 128)],
                                                    rhs=qT[:, tsl(r, d, 128 * b - 64, nq)], start=False, stop=(b == nt)),
                                                    reads=["QT%d" % st, "KT%d" % st], writes=["bk%d" % (2 + si)])
                                            if b < nt:
                                                q0 = 64 if b == 0 else 0
                                                P.add("pe", lambda e, bkS=bkS, base=base, q0=q0, b=b: e.matmul(
                                                    bkS[:, base + 128 + q0:base + 256], lhsT=kT[:, tsl(r, d, 128 * b, 128)],
                                                    rhs=qT[:, tsl(r, d, 128 * b - 64 + q0, 128 - q0)], start=False, stop=True),
                                                    reads=["QT%d" % st, "KT%d" % st], writes=["bk%d" % (2 + si)])
                                        P.add("act", lambda e, pt=pt, bkS=bkS, vlo=vlo, vhi=vhi: e.activation(out=pt[:, vlo:vhi], in_=bkS[:, vlo:vhi], func=AF.Exp),
                                              reads=["bk%d" % (2 + si)], writes=["PT%d" % si])
                                        return si

                                    def pv_part(si, sblk=sblk, r=r, d=d, nt=nt, pi=pi, hp=hp, acc_h=acc_h, atok=atok):
                                        oi = ctr["O"] % 2; ctr["O"] += 1
                                        bkO = bO[oi]
                                        pt = PT[:, si * 512:(si + 1) * 512]
                                        for bi_, b in enumerate(sblk):
                                            base = bi_ * 256
                                            ob = bi_ * 128
                                            if b == 0:
                                                P.add("pe", lambda e, bkO=bkO, ob=ob, base=base: e.matmul(
                                                    bkO[:, ob + 64:ob + 128], lhsT=Vt[:, pi, r * nt + 0, hp, :], rhs=pt[:, base + 192:base + 256], start=True, stop=True),
                                                    reads=["PT%d" % si, "V%d" % pi], writes=["bk%d" % (5 + oi)])
                                            elif b == nt:
                                                P.add("pe", lambda e, bkO=bkO, ob=ob, base=base: e.matmul(
                                                    bkO[:, ob:ob + 64], lhsT=Vt[:, pi, r * nt + nt - 1, hp, :], rhs=pt[:, base:base + 64], start=True, stop=True),
                                                    reads=["PT%d" % si, "V%d" % pi], writes=["bk%d" % (5 + oi)])
                                            else:
                                                P.add("pe", lambda e, bkO=bkO, ob=ob, base=base, b=b: e.matmul(
                                                    bkO[:, ob:ob + 128], lhsT=Vt[:, pi, r * nt + b - 1, hp, :], rhs=pt[:, base:base + 128], start=True, stop=False),
                                                    reads=["PT%d" % si, "V%d" % pi], writes=["bk%d" % (5 + oi)])
                                                P.add("pe", lambda e, bkO=bkO, ob=ob, base=base, b=b: e.matmul(
                                                    bkO[:, ob:ob + 128], lhsT=Vt[:, pi, r * nt + b, hp, :], rhs=pt[:, base + 128:base + 256], start=False, stop=True),
                                                    reads=["PT%d" % si, "V%d" % pi], writes=["bk%d" % (5 + oi)])
                                        b0 = sblk[0]
                                        c_lo = 64 if b0 == 0 else 0
                                        c_hi = len(sblk) * 128 - (64 if sblk[-1] == nt else 0)
                                        p_lo = 128 * b0 - 64 + c_lo
                                        dsta = acc_h[:, tsl(r, d, p_lo, c_hi - c_lo)]
                                        srca = bkO[:, c_lo:c_hi]
                                        if pi == 0:
                                            P.add("dve", lambda e: e.tensor_copy(out=dsta, in_=srca),
                                                  reads=["bk%d" % (5 + oi)], writes=[atok])
                                        else:
                                            P.add("dve", lambda e: e.tensor_tensor(out=dsta, in0=srca, in1=dsta, op=ALU.add),
                                                  reads=["bk%d" % (5 + oi), atok], writes=[atok])
                                    tasks.append((s_part, pv_part))
                    else:
                        for g in range(8):
                            if g == 0:
                                ms_, var = [0, 1, 2, 3], 1
                            elif g == 7:
                                ms_, var = [12, 13, 14, 15], 1
                            else:
                                ms_, var = list(range(2 * g - 2, 2 * g + 4)), 0
                            npairs = (len(ms_) + 1) // 2
                            ostate = {}
                            for k0 in range(0, len(ms_), 2):
                                pair = ms_[k0:k0 + 2]

                                def s_part(pair=pair, g=g, var=var, hp=hp, kT=kT, qT=qT, st=st):
                                    si = ctr["S"] % 3; ctr["S"] += 1
                                    bkS = bS[si]
                                    pt = PT[:, si * 512:(si + 1) * 512]
                                    for ki, m in enumerate(pair):
                                        base = ki * 256
                                        sft = 6 - (2 * m - 4 * g)
                                        assert 0 <= sft and sft * 64 + 256 <= 896
                                        P.add("pe", lambda e, bkS=bkS, base=base, sft=sft: e.matmul(
                                            bkS[:, base:base + 256], lhsT=ident, rhs=nastrip[:, hp, var, sft * 64:sft * 64 + 256], start=True, stop=False),
                                            reads=["ident", "nastrip"], writes=["bk%d" % (2 + si)])
                                        P.add("pe", lambda e, bkS=bkS, base=base, m=m: e.matmul(
                                            bkS[:, base:base + 256], lhsT=kT[:, m * 128:(m + 1) * 128], rhs=qT[:, g * 256:(g + 1) * 256], start=False, stop=True),
                                            reads=["QT%d" % st, "KT%d" % st], writes=["bk%d" % (2 + si)])
                                    ncol = 256 * len(pair)
                                    P.add("act", lambda e, pt=pt, bkS=bkS, ncol=ncol: e.activation(out=pt[:, 0:ncol], in_=bkS[:, 0:ncol], func=AF.Exp),
                                          reads=["bk%d" % (2 + si)], writes=["PT%d" % si])
                                    return si

                                def pv_part(si, pair=pair, k0=k0, g=g, hp=hp, acc_h=acc_h, atok=atok, ostate=ostate, nms=len(ms_)):
                                    if k0 == 0:
                                        ostate["oi"] = ctr["O"] % 2; ctr["O"] += 1
                                    oi = ostate["oi"]
                                    bkO = bO[oi]
                                    pt = PT[:, si * 512:(si + 1) * 512]
                                    for ki, m in enumerate(pair):
                                        ti = k0 + ki
                                        P.add("pe", lambda e, bkO=bkO, ki=ki, m=m, ti=ti: e.matmul(
                                            bkO[:, 0:256], lhsT=Vt[:, 0, m, hp, :], rhs=pt[:, ki * 256:(ki + 1) * 256], start=(ti == 0), stop=(ti == nms - 1)),
                                            reads=["PT%d" % si, "V0"], writes=["bk%d" % (5 + oi)])
                                    if k0 + len(pair) == nms:
                                        dsta = acc_h[:, g * 256:(g + 1) * 256]
                                        P.add("dve", lambda e: e.tensor_copy(out=dsta, in_=bkO[:, 0:256]),
                                              reads=["bk%d" % (5 + oi)], writes=[atok])
                                tasks.append((s_part, pv_part))

                    def fin_part(_si, acc_h=acc_h, atok=atok, rows=rows, hp=hp, chunk=(j if is_dil else 4 + j)):
                        P.add("act", lambda e: e.activation(out=rden[0:64, :], in_=acc_h[64:128, :], func=AF.Ln),
                              reads=[atok], writes=["rden"])
                        P.add("act", lambda e: e.activation(out=rden[0:64, :], in_=rden[0:64, :], func=AF.Exp, scale=-1.0),
                              reads=["rden"], writes=["rden"])
                        P.add("dve", lambda e: e.tensor_tensor(out=yT[rows, chunk, :], in0=acc_h[0:64, :], in1=rden[0:64, :], op=ALU.mult),
                              reads=[atok, "rden"], writes=["yT%d_%d" % (chunk, hp)])
                    tasks.append((None, fin_part))

                if os.environ.get('KILV', '1') == '1':
                    cut = next(i_ for i_, t_ in enumerate(tasks) if t_[0] is None) + 1
                    ta, tb_ = tasks[:cut], tasks[cut:]
                    tasks = []
                    for i_ in range(max(len(ta), len(tb_))):
                        if i_ < len(ta):
                            tasks.append(ta[i_])
                        if i_ < len(tb_):
                            tasks.append(tb_[i_])
                LOOK = int(os.environ.get('KLOOK', '2'))
                sis = []
                for ti_, (sp_, pv_) in enumerate(tasks):
                    sis.append(sp_() if sp_ is not None else None)
                    if ti_ >= LOOK:
                        tasks[ti_ - LOOK][1](sis[ti_ - LOOK])
                for ti_ in range(max(0, len(tasks) - LOOK), len(tasks)):
                    tasks[ti_][1](sis[ti_])


            stage('P%d' % s)
            ytoks = ["yT%d_%d" % (c, hp) for c in range(8) for hp in range(2)]
            P.add("pool", lambda e: e.dma_start(out=wout_bf, in_=wout_d.rearrange("(c p) n -> p c n", p=128)),
                  writes=["V0", "V1", "V2", "wout"], dma="wout")
            for c in range(8):
                P.add("dve", lambda e, c=c: e.tensor_scalar(out=wout_bf[:, c, :], in0=wout_bf[:, c, :], scalar1=gout[:, c:c + 1], scalar2=None, op0=ALU.mult),
                      reads=["wout", "vecs"], writes=["wout"])
            for tt in range(16):
                row0 = tb + tt * 128
                tsl_ = slice(tt * 128, (tt + 1) * 128)
                xi = ctr["x"] % 2; ctr["x"] += 1
                xtile = xt[xi]
                P.add("sp", lambda e, xtile=xtile, row0=row0: e.dma_start(out=xtile, in_=x_d[row0:row0 + 128, :]),
                      writes=["xt%d_src" % xi], dma="x%d" % xi)
                P.add("pool", lambda e, tsl_=tsl_: e.tensor_tensor(out=sqt, in0=yT[:, :, tsl_], in1=yT[:, :, tsl_], op=ALU.mult),
                      reads=ytoks, writes=["sqt"])
                bq = bS[2]
                for c in range(8):
                    col = c // 4
                    P.add("pe", lambda e, c=c, col=col, bq=bq: e.matmul(bq[:, col:col + 1], lhsT=sqt[:, c, :], rhs=ones1[:, 0:1], start=(c % 4 == 0), stop=(c % 4 == 3)),
                          reads=["sqt", "ones1"], writes=["bk4"])
                ms2 = small[:, 16 + xi * 4:16 + xi * 4 + 2]; rs2 = small[:, 16 + xi * 4 + 2:16 + xi * 4 + 4]
                P.add("dve", lambda e, ms2=ms2, bq=bq: e.tensor_scalar(out=ms2, in0=bq[:, 0:2], scalar1=1.0 / 512, scalar2=EPS, op0=ALU.mult, op1=ALU.add),
                      reads=["bk4"], writes=["ms2_%d" % xi])
                P.add("pool", lambda e, ms2=ms2, rs2=rs2: e.tensor_tensor(out=rs2, in0=ms2, in1=mhalf[:, 0:2], op=ALU.pow),
                      reads=["ms2_%d" % xi, "mhalf"], writes=["rs2_%d" % xi])
                ht = acc[xi][:, 0:D]
                for half in range(2):
                    hs_ = slice(half * 512, (half + 1) * 512)
                    bA = [bP[0], bP[1]][half]; bB = [bS[0], bS[1]][half]
                    for c in range(4):
                        P.add("pe", lambda e, c=c, bA=bA, tsl_=tsl_, hs_=hs_: e.matmul(bA[:, 0:512], lhsT=yT[:, c, tsl_], rhs=wout_bf[:, c, hs_], start=(c == 0), stop=(c == 3)),
                              reads=ytoks + ["wout", "V0", "V1", "V2"], writes=["bk%d" % half])
                    for c in range(4, 8):
                        P.add("pe", lambda e, c=c, bB=bB, tsl_=tsl_, hs_=hs_: e.matmul(bB[:, 0:512], lhsT=yT[:, c, tsl_], rhs=wout_bf[:, c, hs_], start=(c == 4), stop=(c == 7)),
                              reads=ytoks + ["wout", "V0", "V1", "V2"], writes=["bk%d" % (2 + half)])
                    P.add("dve", lambda e, ht=ht, hs_=hs_, bA=bA, rs2=rs2, xtile=xtile: e.scalar_tensor_tensor(out=ht[:, hs_], in0=bA[:, 0:512], scalar=rs2[:, 0:1], in1=xtile[:, hs_], op0=ALU.mult, op1=ALU.add),
                          reads=["bk%d" % half, "rs2_%d" % xi, "xt%d_src" % xi], writes=["acc%d" % xi])
                    P.add("dve", lambda e, ht=ht, hs_=hs_, bB=bB, rs2=rs2: e.scalar_tensor_tensor(out=ht[:, hs_], in0=bB[:, 0:512], scalar=rs2[:, 1:2], in1=ht[:, hs_], op0=ALU.mult, op1=ALU.add),
                          reads=["bk%d" % (2 + half), "rs2_%d" % xi, "acc%d" % xi], writes=["acc%d" % xi])
                P.add("sp", lambda e, ht=ht, row0=row0: e.dma_start(out=hs_d[row0:row0 + 128, :], in_=ht),
                      reads=["acc%d" % xi], writes=["hs%d" % (row0 // 128)], dma="hst%d" % xi)

        stage('O')
        P.add("act", lambda e: e.activation(out=bar[:, 0:1], in_=mhalf[:, 0:1], func=AF.Copy), reads=["mhalf"], writes=["bar_act"])
        P.add("dve", lambda e: e.tensor_copy(out=bar[:, 1:2], in_=mhalf[:, 0:1]), reads=["mhalf"], writes=["bar_dve"])
        P.add("pool", lambda e: e.tensor_copy(out=bar[:, 2:3], in_=mhalf[:, 0:1]), reads=["mhalf"], writes=["bar_pool"])
        for en in ("pe", "act", "dve", "pool", "sp"):
            P.add(en, lambda e: None, reads=["bar_act", "bar_dve", "bar_pool"], waitonly=True)

        ptr[0] = persist_end
        wd_bf = alloc(32 * D, BF16, (32, D))
        hn2T = alloc(8 * 1024, BF16, (8, 1024))
        actT = alloc(32 * 1024, BF16, (32, 1024))
        wgu = [[alloc(8 * 256, BF16, (8, 256)) for _ in range(2)] for _ in range(3)]
        hb = [alloc(D) for _ in range(2)]
        xs2 = [alloc(D, BF16) for _ in range(2)]
        combT = alloc(1024, BF16)
        Cs = [alloc(512, BF16) for _ in range(2)]
        ssb = [alloc(512) for _ in range(2)]
        s2b = [alloc(512) for _ in range(2)]
        rt = alloc(960)
        comb_bf = alloc(8 * 16, BF16, (8, 16))
        small2 = alloc(32)

        bG = [banks[0], banks[1]]; bU = [banks[2], banks[3]]; bC = banks[4]; bD = [banks[5], banks[6]]

        for k in range(4):
            P.add("sp", lambda e, k=k: e.dma_start(out=wd_bf[:, 8 * k:8 * k + 8, :],
                                                   in_=wds_d[4 * k:4 * k + 4].rearrange("e (f p) n -> p (e f) n", p=128)),
                  reads=["cvd%d" % ee for ee in range(4 * k, 4 * k + 4)], writes=["wd%d" % k], dma="wd%d" % k)
        wd_toks = ["wd%d" % k for k in range(4)]

        def load_gu(n):
            if n >= 64:
                return
            ex_ = n % 16; wi_ = n % 3
            wgb_, wub_ = wgu[wi_]
            P.add("sp", lambda e: e.dma_start(out=wgb_, in_=wgs_d[ex_].rearrange("(c p) n -> p c n", p=128)),
                  reads=["cvg%d" % ex_], writes=["wg%d" % wi_], dma="wg%d" % wi_)
            P.add("sp", lambda e: e.dma_start(out=wub_, in_=wus_d[ex_].rearrange("(c p) n -> p c n", p=128)),
                  reads=["cvu%d" % ex_], writes=["wu%d" % wi_], dma="wu%d" % wi_)
        load_gu(0)
        load_gu(1)

        c2 = {"h": 0, "G": 0, "C": 0, "D": 0, "w": 0, "y": 0}
        h2_all = [("h2_%d_a" % st) for st in range(8)] + [("h2_%d_b" % st) for st in range(8)]

        def emit_hn2(T, st):
            t0 = T * 1024
            if True:
                row0 = t0 + st * 128
                hi = c2["h"] % 2; c2["h"] += 1
                htile = hb[hi]; xsb = xs2[hi]
                P.add("sp", lambda e, htile=htile, row0=row0: e.dma_start(out=htile, in_=hs_d[row0:row0 + 128, :]),
                      reads=["hs%d" % (row0 // 128)], writes=["hb%d" % hi], dma="hb%d" % hi)
                ss = small2[:, hi * 4:hi * 4 + 1]; ms = small2[:, hi * 4 + 1:hi * 4 + 2]; rstd = small2[:, hi * 4 + 2:hi * 4 + 3]
                P.add("act", lambda e, xsb=xsb, htile=htile, ss=ss: e.activation(out=xsb, in_=htile, func=AF.Square, accum_out=ss),
                      reads=["hb%d" % hi], writes=["xs2_%d" % hi, "h_ss%d" % hi])
                P.add("dve", lambda e, ms=ms, ss=ss: e.tensor_scalar(out=ms, in0=ss, scalar1=1.0 / D, scalar2=EPS, op0=ALU.mult, op1=ALU.add),
                      reads=["h_ss%d" % hi], writes=["h_ms%d" % hi])
                P.add("pool", lambda e, ms=ms, rstd=rstd: e.tensor_tensor(out=rstd, in0=ms, in1=mhalf[:, 0:1], op=ALU.pow),
                      reads=["h_ms%d" % hi, "mhalf"], writes=["h_rstd%d" % hi])
                P.add("dve", lambda e, xsb=xsb, htile=htile, rstd=rstd: e.tensor_scalar(out=xsb, in0=htile, scalar1=rstd, scalar2=None, op0=ALU.mult),
                      reads=["hb%d" % hi, "h_rstd%d" % hi], writes=["xs2_%d" % hi])
                tbk, ttok = tbank[st % 2]
                for c in range(8):
                    P.add("pe", lambda e, xsb=xsb, c=c, tbk=tbk: e.transpose(tbk[:, c * 128:(c + 1) * 128], xsb[:, c * 128:(c + 1) * 128], ident),
                          reads=["xs2_%d" % hi, "ident"], writes=[ttok])
                for c in range(8):
                    dst = hn2T[:, c, st * 128:(st + 1) * 128]
                    src = tbk[:, c * 128:(c + 1) * 128]
                    if st % 2 == 0:
                        P.add("dve", lambda e, dst=dst, src=src, c=c: e.tensor_scalar(out=dst, in0=src, scalar1=gffn[:, c:c + 1], scalar2=None, op0=ALU.mult),
                              reads=[ttok, "vecs"], writes=["h2_%d_a" % st])
                    else:
                        P.add("act", lambda e, dst=dst, src=src, c=c: e.activation(out=dst, in_=src, func=AF.Copy, scale=gffn[:, c:c + 1]),
                              reads=[ttok, "vecs"], writes=["h2_%d_b" % st])

        def emit_router(T):
            if True:
                for st in range(8):
                    for c in range(8):
                        P.add("pe", lambda e, st=st, c=c: e.matmul(bC[:, st * 32:st * 32 + 20], lhsT=hn2T[:, c, st * 128:(st + 1) * 128], rhs=wr_bf[:, c, :],
                                                                   start=(c == 0), stop=(c == 7)),
                              reads=["h2_%d_a" % st, "h2_%d_b" % st, "wr_bf"], writes=["bk4"])
                lg = rt[:, 0:160].rearrange("p (s n) -> p s n", s=8)
                bc3 = bC[:, 0:256].rearrange("p (s n) -> p s n", s=8)[:, :, 0:20]
                RT = "rt"
                r3 = lambda lo, n: rt[:, lo:lo + 8 * n].rearrange("p (s n) -> p s n", s=8)
                gl = lg[:, :, 0:4]; el = lg[:, :, 4:20]
                gmax = rt[:, 160:168]; g1 = r3(168, 4); gex = r3(200, 4); gsum = rt[:, 232:240]; gw = rt[:, 240:248]
                pen = r3(248, 16); ml = r3(376, 16); m1 = rt[:, 504:512]; m2 = rt[:, 512:520]; k1 = r3(520, 16)
                dm = rt[:, 648:656]; w1 = rt[:, 656:664]; w2 = rt[:, 664:672]; k2 = r3(672, 16); ml2 = r3(800, 16)
                bcast = lambda ap, n: ap.unsqueeze(2).to_broadcast([128, 8, n])
                P.add("dve", lambda e: e.tensor_tensor(out=lg, in0=bc3, in1=brep.unsqueeze(1).to_broadcast([128, 8, 20]), op=ALU.add),
                      reads=["bk4", "vecs"], writes=[RT])
                P.add("dve", lambda e: e.tensor_reduce(out=gmax, in_=gl, axis=AX.X, op=ALU.max), reads=[RT], writes=[RT + "a"])
                P.add("dve", lambda e: e.tensor_tensor(out=g1, in0=gl, in1=bcast(gmax, 4), op=ALU.is_equal), reads=[RT, RT + "a"], writes=[RT + "b"])
                P.add("dve", lambda e: e.tensor_tensor(out=gex, in0=gl, in1=bcast(gmax, 4), op=ALU.subtract), reads=[RT, RT + "a"], writes=[RT + "c"])
                P.add("act", lambda e: e.activation(out=gex, in_=gex, func=AF.Exp), reads=[RT + "c"], writes=[RT + "d"])
                P.add("dve", lambda e: e.tensor_reduce(out=gsum, in_=gex, axis=AX.X, op=ALU.add), reads=[RT + "d"], writes=[RT + "e"])
                P.add("dve", lambda e: e.reciprocal(out=gw, in_=gsum), reads=[RT + "e"], writes=[RT + "f"])
                P.add("dve", lambda e: e.tensor_scalar(out=pen.rearrange("p s (g n) -> p s g n", g=4), in0=g1.unsqueeze(3).to_broadcast([128, 8, 4, 4]),
                                                       scalar1=-1.0, scalar2=30000.0, op0=ALU.add, op1=ALU.mult), reads=[RT + "b"], writes=[RT + "g"])
                P.add("dve", lambda e: e.tensor_tensor(out=ml, in0=el, in1=pen, op=ALU.add), reads=[RT, RT + "g"], writes=[RT + "h"])
                P.add("dve", lambda e: e.tensor_reduce(out=m1, in_=ml, axis=AX.X, op=ALU.max), reads=[RT + "h"], writes=[RT + "i"])
                P.add("dve", lambda e: e.tensor_tensor(out=k1, in0=ml, in1=bcast(m1, 16), op=ALU.is_equal), reads=[RT + "h", RT + "i"], writes=[RT + "j"])
                P.add("dve", lambda e: e.scalar_tensor_tensor(out=ml2, in0=k1, scalar=-60000.0, in1=ml, op0=ALU.mult, op1=ALU.add), reads=[RT + "h", RT + "j"], writes=[RT + "k"])
                P.add("dve", lambda e: e.tensor_reduce(out=m2, in_=ml2, axis=AX.X, op=ALU.max), reads=[RT + "k"], writes=[RT + "l"])
                P.add("dve", lambda e: e.tensor_tensor(out=k2, in0=ml2, in1=bcast(m2, 16), op=ALU.is_equal), reads=[RT + "k", RT + "l"], writes=[RT + "m"])
                P.add("dve", lambda e: e.tensor_tensor(out=dm, in0=m2, in1=m1, op=ALU.subtract), reads=[RT + "l", RT + "i"], writes=[RT + "n"])
                P.add("act", lambda e: e.activation(out=dm, in_=dm, func=AF.Exp), reads=[RT + "n"], writes=[RT + "o"])
                P.add("dve", lambda e: e.tensor_scalar(out=dm, in0=dm, scalar1=1.0, scalar2=None, op0=ALU.add), reads=[RT + "o"], writes=[RT + "p"])
                P.add("dve", lambda e: e.reciprocal(out=w1, in_=dm), reads=[RT + "p"], writes=[RT + "q"])
                P.add("dve", lambda e: e.tensor_scalar(out=w2, in0=w1, scalar1=-1.0, scalar2=1.0, op0=ALU.mult, op1=ALU.add), reads=[RT + "q"], writes=[RT + "r"])
                P.add("dve", lambda e: e.tensor_tensor(out=w1, in0=w1, in1=gw, op=ALU.mult), reads=[RT + "q", RT + "r", RT + "f"], writes=[RT + "s"])
                P.add("dve", lambda e: e.tensor_tensor(out=w2, in0=w2, in1=gw, op=ALU.mult), reads=[RT + "r", RT + "f"], writes=[RT + "t"])
                P.add("dve", lambda e: e.tensor_tensor(out=k1, in0=k1, in1=bcast(w1, 16), op=ALU.mult), reads=[RT + "j", RT + "s", RT + "k"], writes=[RT + "u"])
                P.add("dve", lambda e: e.tensor_tensor(out=k2, in0=k2, in1=bcast(w2, 16), op=ALU.mult), reads=[RT + "m", RT + "t"], writes=[RT + "v"])
                P.add("dve", lambda e: e.tensor_tensor(out=comb_bf, in0=k1, in1=k2, op=ALU.add), reads=[RT + "u", RT + "v"], writes=["comb_bf"])
                for st in range(8):
                    P.add("pe", lambda e, st=st: e.transpose(pb_t[0:16, st * 128:(st + 1) * 128], comb_bf[:, st, :], ident),
                          reads=["comb_bf", "ident"] + h2_all, writes=["bk7"])
                P.add("dve", lambda e: e.tensor_copy(out=combT[0:16, :], in_=pb_t[0:16, 0:1024]), reads=["bk7"], writes=["combT"])


        def emit_experts(T):
            if True:
                for ex in range(16):
                    nflat = T * 16 + ex
                    wi = nflat % 3
                    wgb, wub = wgu[wi]
                    load_gu(nflat + 2)
                    for half in range(2):
                        hs_ = slice(half * 512, (half + 1) * 512)
                        ci = c2["C"] % 2; c2["C"] += 1
                        P.add("pe", lambda e, ex=ex, hs_=hs_: e.matmul(bC[:, 0:512], lhsT=sel[0:16, ex, :], rhs=combT[0:16, hs_], start=True, stop=True),
                              reads=["sel", "combT"], writes=["bk4"])
                        P.add("dve", lambda e, ci=ci: e.tensor_copy(out=Cs[ci], in_=bC[:, 0:512]), reads=["bk4"], writes=["Cs%d" % ci])
                        for fc in range(2):
                            gi = c2["G"] % 2; c2["G"] += 1
                            for c in range(8):
                                P.add("pe", lambda e, gi=gi, c=c, fc=fc, hs_=hs_, wgb=wgb: e.matmul(bG[gi][:, 0:512], lhsT=wgb[:, c, fc * 128:(fc + 1) * 128], rhs=hn2T[:, c, hs_],
                                                                                                  start=(c == 0), stop=(c == 7)),
                                      reads=["wg%d" % wi] + h2_all, writes=["bk%d" % gi])
                            for c in range(8):
                                P.add("pe", lambda e, gi=gi, c=c, fc=fc, hs_=hs_, wub=wub: e.matmul(bU[gi][:, 0:512], lhsT=wub[:, c, fc * 128:(fc + 1) * 128], rhs=hn2T[:, c, hs_],
                                                                                                  start=(c == 0), stop=(c == 7)),
                                      reads=["wu%d" % wi] + h2_all, writes=["bk%d" % (2 + gi)])
                            P.add("act", lambda e, gi=gi: e.activation(out=ssb[gi], in_=bG[gi][:, 0:512], func=AF.Silu), reads=["bk%d" % gi], writes=["ssb%d" % gi])
                            P.add("pool", lambda e, gi=gi, ci=ci: e.tensor_tensor(out=s2b[gi], in0=ssb[gi], in1=Cs[ci], op=ALU.mult),
                                  reads=["ssb%d" % gi, "Cs%d" % ci], writes=["s2b%d" % gi])
                            P.add("dve", lambda e, gi=gi, ex=ex, fc=fc, hs_=hs_: e.tensor_tensor(out=actT[:, ex * 2 + fc, hs_], in0=bU[gi][:, 0:512], in1=s2b[gi], op=ALU.mult),
                                  reads=["bk%d" % (2 + gi), "s2b%d" % gi], writes=["actT"])


        def emit_down(T, st):
            t0 = T * 1024
            if True:
                row0 = t0 + st * 128
                hi = c2["h"] % 2; c2["h"] += 1
                htile = hb[hi]
                P.add("sp", lambda e, htile=htile, row0=row0: e.dma_start(out=htile, in_=hs_d[row0:row0 + 128, :]),
                      reads=["hs%d" % (row0 // 128)], writes=["hb%d" % hi], dma="hb%d" % hi)
                for dh in range(2):
                    di = c2["D"] % 2; c2["D"] += 1
                    ds_ = slice(dh * 512, (dh + 1) * 512)
                    for kk in range(32):
                        P.add("pe", lambda e, di=di, kk=kk, st=st, ds_=ds_: e.matmul(bD[di][:, 0:512], lhsT=actT[:, kk, st * 128:(st + 1) * 128], rhs=wd_bf[:, kk, ds_],
                                                                                   start=(kk == 0), stop=(kk == 31)),
                              reads=["actT"] + wd_toks, writes=["bk%d" % (5 + di)])
                    P.add("dve", lambda e, di=di, htile=htile, ds_=ds_: e.tensor_tensor(out=htile[:, ds_], in0=bD[di][:, 0:512], in1=htile[:, ds_], op=ALU.add),
                          reads=["bk%d" % (5 + di), "hb%d" % hi], writes=["hb%d" % hi])
                yi = c2["y"] % 2; c2["y"] += 1
                junk = xs2[yi]
                ss = small2[:, 16 + yi * 4:16 + yi * 4 + 1]; ms = small2[:, 16 + yi * 4 + 1:16 + yi * 4 + 2]; rstd = small2[:, 16 + yi * 4 + 2:16 + yi * 4 + 3]
                P.add("act", lambda e, junk=junk, htile=htile, ss=ss: e.activation(out=junk, in_=htile, func=AF.Square, accum_out=ss),
                      reads=["hb%d" % hi], writes=["xs2_%d" % yi, "f_ss%d" % yi])
                P.add("dve", lambda e, ms=ms, ss=ss: e.tensor_scalar(out=ms, in0=ss, scalar1=1.0 / D, scalar2=EPS, op0=ALU.mult, op1=ALU.add),
                      reads=["f_ss%d" % yi], writes=["f_ms%d" % yi])
                P.add("pool", lambda e, ms=ms, rstd=rstd: e.tensor_tensor(out=rstd, in0=ms, in1=mhalf[:, 0:1], op=ALU.pow),
                      reads=["f_ms%d" % yi, "mhalf"], writes=["f_rstd%d" % yi])
                P.add("dve", lambda e, htile=htile, rstd=rstd: e.scalar_tensor_tensor(out=htile, in0=htile, scalar=rstd, in1=gfin, op0=ALU.mult, op1=ALU.mult),
                      reads=["hb%d" % hi, "f_rstd%d" % yi, "gfin"], writes=["hb%d" % hi])
                P.add("sp", lambda e, htile=htile, row0=row0: e.dma_start(out=y_d[row0:row0 + 128, :], in_=htile),
                      reads=["hb%d" % hi], dma="yst%d" % hi)


        for T in range(4):
            for st in range(8):
                emit_hn2(T, st)
            emit_router(T)
            emit_experts(T)
            for st in range(8):
                emit_down(T, st)

    except StopBuild:
        pass

    sems = {}
    for en in Prog.ENGS:
        sems["eng:" + en] = es.enter_context(nc.semaphore("s_" + en))
    for sl in sorted(P.dma_slots):
        sems["dma:" + sl] = es.enter_context(nc.semaphore("d_" + sl))
    if os.environ.get('KMAXOPS'):
        P.ops = P.ops[:int(os.environ['KMAXOPS'])]
        for i_, o_ in enumerate(P.ops[-3:]):
            print('LASTOPS', o_.eng, o_.idx)
    body = P.emit(sems)
    if os.environ.get('KSTATS'):
        print('PROG stats', P.stats, 'ndma_slots', len(P.dma_slots))
    with nc.Block() as block:
        block.sync(body("sp"))
        block.tensor(body("pe"))
        block.scalar(body("act"))
        block.vector(body("dve"))
        block.gpsimd(body("pool"))
    es.close()
    return nc


def _dil_bias():
    out = np.full((128, 24 * 256), NEG, np.float32)
    k = np.arange(128)[:, None]; q = np.arange(128)[None, :]
    for h in range(8):
        slope = 2.0 ** (-(h + 1))
        for pi, d in enumerate((1, 4, 16)):
            for half, off in enumerate((-64, 64)):
                delta = k + off - q
                val = np.where(np.abs(delta) <= 64, -slope * d * np.abs(delta), NEG).astype(np.float32)
                c0 = (h * 3 + pi) * 256 + half * 128
                out[:, c0:c0 + 128] = val
    return out


def _na_strips(rpb):
    kc = np.arange(64)[:, None]; qc = np.arange(64)[None, :]
    cs = np.clip(qc - 8, 0, 48)
    col_ok = (kc >= cs) & (kc < cs + 16)
    dcidx = np.clip(kc - qc + 15, 0, 30)
    out = np.full((4, 128, 2, 2, 896), NEG, np.float32)
    for h in range(8):
        j, hp = divmod(h, 2)
        for var in range(2):
            for i in range(2):
                for u in range(14):
                    dlt = 6 + i - u
                    ok = (-4 <= dlt <= 3) if var == 0 else (-7 <= dlt <= 7)
                    if not ok:
                        continue
                    blk = np.where(col_ok, rpb[h, dlt + 7][dcidx], NEG).astype(np.float32)
                    out[j, i * 64:(i + 1) * 64, hp, var, u * 64:(u + 1) * 64] = blk
    return out.reshape(4, 128, 4 * 896)


_NC_CACHE = {}


def kernel(x, norm_mix_g, w_in, rpb, g_out_dil, g_out_na, w_out, norm_ffn_g, w_group, b_group, w_router, b_router,
           w_gate, w_up, w_down, norm_final_g):
    f = lambda a: np.ascontiguousarray(np.asarray(a, dtype=np.float32))
    x = f(x).reshape(16 * S, D)
    col = lambda g: f(g).reshape(8, 128).T
    vecs = np.concatenate([col(norm_mix_g[0]), col(norm_ffn_g[0]),
                           col(np.concatenate([f(g_out_dil[0]), f(g_out_na[0])])),
                           np.broadcast_to(np.concatenate([f(b_group[0]), f(b_router[0])])[None, :], (128, 20))], axis=1)
    vecs = np.ascontiguousarray(vecs, dtype=np.float32)
    gfin = np.ascontiguousarray(np.broadcast_to(f(norm_final_g)[None, :], (128, D)))
    wr = np.ascontiguousarray(np.concatenate([f(w_group[0]), f(w_router[0])], axis=1))
    shared = {
        "w_in": f(w_in[0]), "w_out": f(w_out[0]), "w_gate": f(w_gate[0]), "w_up": f(w_up[0]), "w_down": f(w_down[0]),
        "wr": wr, "vecs": vecs, "gfin": gfin, "dilb": _dil_bias(), "nas": _na_strips(f(rpb[0])),
    }
    if "nc" not in _NC_CACHE:
        _NC_CACHE["nc"] = build_program()
    nc = _NC_CACHE["nc"]
    in_maps = []
    for i in range(NCORES):
        m = dict(shared)
        m["x"] = np.ascontiguousarray(x[i * TOK:(i + 1) * TOK])
        in_maps.append(m)
    res = run_bass_kernel_spmd(nc, in_maps, core_ids=list(range(NCORES)))
    y = np.concatenate([r["y"] for r in res.results], axis=0)
    if DEBUG:
        kernel.dbg = [r.get("dbg") for r in res.results]
    return y.reshape(16, S, D).astype(np.float32)
```

```python
import numpy as np
from contextlib import ExitStack
import concourse.bass as bass
import concourse.mybir as mybir
from concourse.bass_utils import run_bass_kernel_spmd

F32 = mybir.dt.float32
BF16 = mybir.dt.bfloat16
AF = mybir.ActivationFunctionType
ALU = mybir.AluOpType
AX = mybir.AxisListType

NCORES = 8
S = 2048
D = 1024
TOK = 4096
NEG = -30000.0
EPS = 1e-6
import os
DEBUG = bool(os.environ.get('KSTOP', ''))
STOP = os.environ.get('KSTOP', '')


class StopBuild(Exception):
    pass


DUMPS = {}


def stage(name):
    if STOP and name == STOP:
        if name in DUMPS:
            DUMPS[name]()
        raise StopBuild()


class Op:
    __slots__ = ("eng", "fn", "deps", "is_dma", "sem", "semval", "needs_signal", "sigcount", "eidx", "idx", "wo")


class Prog:
    ENGS = ("pe", "act", "dve", "pool", "sp")

    def __init__(self):
        self.ops = []
        self.last_writer = {}
        self.readers = {}
        self.eng_count = {e: 0 for e in self.ENGS}
        self.dma_slots = set()

    def add(self, eng, fn, reads=(), writes=(), dma=None, waitonly=False):
        op = Op()
        op.eng = eng; op.fn = fn; op.idx = len(self.ops)
        op.is_dma = dma is not None
        op.wo = waitonly
        op.sem = dma; op.semval = 0; op.needs_signal = False; op.sigcount = 0
        op.eidx = self.eng_count[eng]; self.eng_count[eng] += 1
        if dma is not None:
            self.dma_slots.add(dma)
        deps = {}
        for t in reads:
            w = self.last_writer.get(t)
            if w is not None:
                deps[w] = "raw"
        for t in writes:
            w = self.last_writer.get(t)
            if w is not None and w not in deps:
                deps[w] = "waw"
            for r in self.readers.get(t, ()):
                if r not in deps and r != op.idx:
                    deps[r] = "war"
        for t in (() if waitonly else reads):
            lst = self.readers.setdefault(t, [])
            if not op.is_dma:
                lst[:] = [r for r in lst if self.ops[r].is_dma or self.ops[r].eng != eng]
            lst.append(op.idx)
        for t in writes:
            self.last_writer[t] = op.idx
            self.readers[t] = []
        op.deps = deps
        self.ops.append(op)
        return op

    def _need_wait(self, op, p, kind):
        if p.is_dma:
            return True
        if p.eng != op.eng:
            return True
        if op.is_dma:
            return True
        if p.eng == "pe":
            return False
        if kind == "raw" and ((op.eidx - p.eidx) <= 3 or p.eng == "pool"):
            return True
        return False

    def emit(self, sem_ctx):
        ops = self.ops
        for op in ops:
            for d, kind in op.deps.items():
                p = ops[d]
                if (not p.is_dma) and self._need_wait(op, p, kind):
                    p.needs_signal = True
        if os.environ.get('KALLSIG'):
            for op in ops:
                if not op.is_dma and op.fn(None) if False else (not op.is_dma and not getattr(op, "wo", False)):
                    op.needs_signal = True
        cnt = {e: 0 for e in self.ENGS}
        dcnt = {}
        for op in ops:
            if op.is_dma:
                dcnt[op.sem] = dcnt.get(op.sem, 0) + 16
                op.semval = dcnt[op.sem]
            elif op.needs_signal:
                cnt[op.eng] += 1
                op.sigcount = cnt[op.eng]
        by_eng = {e: [o for o in ops if o.eng == e] for e in self.ENGS}
        self.stats = (dict(cnt), {e: len(v) for e, v in by_eng.items()})

        def body(ename):
            def run(eng):
                waited = {}
                for op in by_eng[ename]:
                    for d, kind in op.deps.items():
                        p = ops[d]
                        if not self._need_wait(op, p, kind):
                            continue
                        if p.is_dma:
                            key = "dma:" + p.sem; val = p.semval
                        else:
                            key = "eng:" + p.eng; val = p.sigcount
                        if waited.get(key, 0) >= val:
                            continue
                        waited[key] = val
                        eng.wait_ge(sem_ctx[key], val)
                    ins = op.fn(eng)
                    if op.is_dma:
                        ins.then_inc(sem_ctx["dma:" + op.sem], 16)
                    elif op.needs_signal:
                        ins.then_inc(sem_ctx["eng:" + ename], 1)
                for op in by_eng[ename]:
                    if op.is_dma:
                        key = "dma:" + op.sem
                        if waited.get(key, 0) < dcnt[op.sem]:
                            waited[key] = dcnt[op.sem]
                            eng.wait_ge(sem_ctx[key], dcnt[op.sem])
            return run
        return body


def tsl(r, d, p0, n):
    return slice(r + d * p0, r + d * (p0 + n - 1) + 1, d)


def build_program():
    nc = bass.Bass("TRN2", target_bir_lowering=False)
    dt_in = lambda n, s, dt=F32: nc.dram_tensor(n, s, dt, kind="ExternalInput").ap()
    x_d = dt_in("x", [TOK, D])
    win_d = dt_in("w_in", [D, 3072])
    wout_d = dt_in("w_out", [D, D])
    wg_d = dt_in("w_gate", [16, D, 256])
    wu_d = dt_in("w_up", [16, D, 256])
    wd_d = dt_in("w_down", [16, 256, D])
    wr_d = dt_in("wr", [D, 20])
    vec_d = dt_in("vecs", [128, 24 + 20])
    gfin_d = dt_in("gfin", [128, D])
    dilb_d = dt_in("dilb", [128, 24 * 256 + 8 * 128])
    nas_d = dt_in("nas", [4, 128, 4 * 896])
    y_d = nc.dram_tensor("y", [TOK, D], F32, kind="ExternalOutput").ap()
    hs_d = nc.dram_tensor("hscr", [TOK, D], F32, kind="Internal").ap()
    wgs_d = nc.dram_tensor("wg_bf", [16, D, 256], BF16, kind="Internal").ap()
    wus_d = nc.dram_tensor("wu_bf", [16, D, 256], BF16, kind="Internal").ap()
    wds_d = nc.dram_tensor("wd_bf", [16, 256, D], BF16, kind="Internal").ap()
    dbg_d = None
    if DEBUG:
        dbg_d = nc.dram_tensor("dbg", [128, 8 * 2048], F32, kind="ExternalOutput").ap()

    P = Prog()
    es = ExitStack()
    ARENA = 53200
    arena = es.enter_context(nc.sbuf_tensor("arena", [128, ARENA], F32))
    arena_bf = arena.bitcast(BF16)
    ptr = [0]

    def alloc(ncols, dt=F32, shape=None):
        n32 = ncols if dt == F32 else (ncols + 1) // 2
        a = ptr[0]
        ptr[0] += n32
        assert ptr[0] <= ARENA, ("SBUF overflow", ptr[0])
        if dt == F32:
            ap = arena[:, a:a + ncols]
        else:
            ap = arena_bf[:, 2 * a:2 * a + ncols]
        if shape is not None:
            names = " ".join("d%d" % i for i in range(len(shape)))
            kw = {"d%d" % i: shape[i] for i in range(len(shape))}
            ap = ap.rearrange("p (%s) -> p %s" % (names, names), **kw)
        return ap

    pb_t = es.enter_context(nc.psum_tensor("pb_t", [128, 1024], BF16))
    banks = [es.enter_context(nc.psum_tensor("bk%d" % i, [128, 512], F32)) for i in range(7)]
    tbank = [(pb_t, "bk7"), (banks[6].bitcast(BF16), "bk6")]

    ident = alloc(128, BF16)
    sel = alloc(16 * 128, BF16, (16, 128))
    vecs = alloc(44)
    gfin = alloc(D)
    wr_bf = alloc(8 * 20, BF16, (8, 20))
    mhalf = alloc(4)
    ones1 = alloc(2, BF16)
    bar = alloc(4)
    persist_end = ptr[0]

    try:
        P.add("pool", lambda e: e.memset(ident, 0.0), writes=["ident"])
        P.add("pool", lambda e: e.affine_select(out=ident, in_=ident, pattern=[[-1, 128]], compare_op=ALU.not_equal,
                                                fill=1.0, base=0, channel_multiplier=1), reads=["ident"], writes=["ident"])
        P.add("pool", lambda e: e.memset(sel, 0.0), writes=["sel"])
        P.add("pool", lambda e: e.affine_select(out=sel[0:16], in_=sel[0:16], pattern=[[-1, 16], [0, 128]],
                                                compare_op=ALU.not_equal, fill=1.0, base=0, channel_multiplier=1),
              reads=["sel"], writes=["sel"])
        P.add("pool", lambda e: e.memset(mhalf, -0.5), writes=["mhalf"])
        P.add("pool", lambda e: e.memset(ones1, 1.0), writes=["ones1"])
        P.add("sp", lambda e: e.dma_start(out=vecs, in_=vec_d), writes=["vecs"], dma="c0")
        P.add("sp", lambda e: e.dma_start(out=gfin, in_=gfin_d), writes=["gfin"], dma="c1")
        P.add("pool", lambda e: e.dma_start(out=wr_bf, in_=wr_d.rearrange("(c p) n -> p c n", p=128)),
              writes=["wr_bf"], dma="c2")
        gmix = vecs[:, 0:8]; gffn = vecs[:, 8:16]; gout = vecs[:, 16:24]; brep = vecs[:, 24:44]

        def rms_rstd(src, ss, ms, rstd, junk, tag, ncols=D):
            P.add("act", lambda e: e.activation(out=junk, in_=src, func=AF.Square, accum_out=ss),
                  reads=[tag + "_src"], writes=[tag + "_junk", tag + "_ss"])
            P.add("dve", lambda e: e.tensor_scalar(out=ms, in0=ss, scalar1=1.0 / ncols, scalar2=EPS, op0=ALU.mult, op1=ALU.add),
                  reads=[tag + "_ss"], writes=[tag + "_ms"])
            P.add("pool", lambda e: e.tensor_tensor(out=rstd, in0=ms, in1=mhalf[:, 0:1], op=ALU.pow),
                  reads=[tag + "_ms", "mhalf"], writes=[tag + "_rstd"])

        hnT = alloc(8 * S, BF16, (8, S))
        yT = alloc(8 * S, BF16, (8, S))
        dilb = alloc(24 * 256 + 8 * 128, BF16)
        wblk = [[alloc(8 * 128, BF16, (8, 128)) for _ in range(3)] for _ in range(2)]
        QT = [alloc(S, BF16) for _ in range(2)]
        KT = [alloc(S, BF16) for _ in range(2)]
        Vt_raw = alloc(3 * 16 * 256, BF16)
        Vt = Vt_raw.rearrange("p (l t h c) -> p l t h c", l=3, t=16, h=2, c=128)
        wout_bf = Vt_raw[:, 0:8 * D].rearrange("p (c n) -> p c n", c=8)
        acc = [alloc(S) for _ in range(2)]
        VTb = alloc(S, BF16)
        rden = alloc(S)
        PT = alloc(3 * 512, BF16)
        nastrip = alloc(4 * 896, BF16, (2, 2, 896))
        xs = [alloc(D, BF16) for _ in range(2)]
        xt = [alloc(D) for _ in range(2)]
        sqt = alloc(8 * 128, BF16, (8, 128))
        small = alloc(64)
        p1_end = ptr[0]


        def dump(items):
            col = [0]
            for ap, toks in items:
                n = ap.shape[-1] if len(ap.shape) == 2 else None
                assert n is not None
                a = col[0]; col[0] += n
                P.add("pool", lambda e, ap=ap, a=a, n=n: e.dma_start(out=dbg_d[0:ap.shape[0], a:a + n], in_=ap, max_dma_last_dim=2048),
                      reads=toks, dma="dbg")
        alltok = lambda: list(P.last_writer.keys())
        DUMPS['H0'] = lambda: dump([(hnT[:, c, :], alltok()) for c in range(8)])
        DUMPS['I0_1'] = lambda: dump([(QT[0], alltok()), (KT[0], alltok()), (acc[0], alltok()), (acc[1], alltok()), (yT[:, 0, :], alltok()),
                                      (Vt_raw[:, 0:4096], alltok())])
        DUMPS['I0_5'] = lambda: dump([(QT[0], alltok()), (KT[0], alltok()), (acc[0], alltok()), (acc[1], alltok()), (yT[:, 4, :], alltok()),
                                      (Vt_raw[:, 0:4096], alltok())])
        for k_ in (2, 3, 4, 6, 7):
            DUMPS['I0_%d' % k_] = lambda: dump([(yT[:, c, :], alltok()) for c in range(8)])
        if os.environ.get('KD2'):
            DUMPS['I0_2'] = lambda: dump([(QT[0], alltok()), (KT[0], alltok()), (QT[1], alltok()), (KT[1], alltok()), (wblk[0][0][:, 0, :], alltok()), (wblk[0][2][:, 0, :], alltok()), (wblk[1][0][:, 0, :], alltok())])
        DUMPS['P0'] = lambda: dump([(yT[:, c, :], alltok()) for c in range(8)])
        bP = [banks[0], banks[1]]
        bPbf = [banks[0].bitcast(BF16), banks[1].bitcast(BF16)]
        bS = [banks[2], banks[3], banks[4]]
        bO = [banks[5], banks[6]]

        P.add("pool", lambda e: e.dma_start(out=dilb, in_=dilb_d, max_dma_last_dim=4096), writes=["dilb"], dma="c3")
        if DEBUG:
            P.add("pool", lambda e: e.memset(yT, 0.0), writes=["yT%d_%d" % (c, hp) for c in range(8) for hp in range(2)])

        ctr = {"S": 0, "O": 0, "P": 0, "x": 0}

        for s in range(2):
            tb = s * S
            for tt in range(16):
                xi = ctr["x"] % 2; ctr["x"] += 1
                xtile = xt[xi]; xsb = xs[xi]
                row0 = tb + tt * 128
                P.add("sp", lambda e, xtile=xtile, row0=row0: e.dma_start(out=xtile, in_=x_d[row0:row0 + 128, :]),
                      writes=["xt%d_src" % xi], dma="x%d" % xi)
                ss = small[:, xi * 4:xi * 4 + 1]; ms = small[:, xi * 4 + 1:xi * 4 + 2]; rstd = small[:, xi * 4 + 2:xi * 4 + 3]
                P.add("act", lambda e, xsb=xsb, xtile=xtile, ss=ss: e.activation(out=xsb, in_=xtile, func=AF.Square, accum_out=ss),
                      reads=["xt%d_src" % xi], writes=["xs%d" % xi, "xt%d_ss" % xi])
                P.add("dve", lambda e, ms=ms, ss=ss: e.tensor_scalar(out=ms, in0=ss, scalar1=1.0 / D, scalar2=EPS, op0=ALU.mult, op1=ALU.add),
                      reads=["xt%d_ss" % xi], writes=["xt%d_ms" % xi])
                P.add("pool", lambda e, ms=ms, rstd=rstd: e.tensor_tensor(out=rstd, in0=ms, in1=mhalf[:, 0:1], op=ALU.pow),
                      reads=["xt%d_ms" % xi, "mhalf"], writes=["xt%d_rstd" % xi])
                P.add("dve", lambda e, xsb=xsb, xtile=xtile, rstd=rstd: e.tensor_scalar(out=xsb, in0=xtile, scalar1=rstd, scalar2=None, op0=ALU.mult),
                      reads=["xt%d_src" % xi, "xt%d_rstd" % xi], writes=["xs%d" % xi])
                tbk, ttok = tbank[tt % 2]
                for c in range(8):
                    P.add("pe", lambda e, xsb=xsb, c=c, tbk=tbk: e.transpose(tbk[:, c * 128:(c + 1) * 128], xsb[:, c * 128:(c + 1) * 128], ident),
                          reads=["xs%d" % xi, "ident"], writes=[ttok])
                for c in range(8):
                    dst = hnT[:, c, tt * 128:(tt + 1) * 128]
                    src = tbk[:, c * 128:(c + 1) * 128]
                    if tt % 2 == 0:
                        P.add("dve", lambda e, dst=dst, src=src, c=c: e.tensor_scalar(out=dst, in0=src, scalar1=gmix[:, c:c + 1], scalar2=None, op0=ALU.mult),
                              reads=[ttok, "vecs"], writes=["hn%d_a" % tt])
                    else:
                        P.add("act", lambda e, dst=dst, src=src, c=c: e.activation(out=dst, in_=src, func=AF.Copy, scale=gmix[:, c:c + 1]),
                              reads=[ttok, "vecs"], writes=["hn%d_b" % tt])

            def hn_tokens(tiles):
                out = []
                for t in tiles:
                    out += ["hn%d_a" % t, "hn%d_b" % t]
                return out

            stage('H%d' % s)
            for lay_ in range(3):
                P.add("pool", lambda e, lay_=lay_: e.memset(Vt[:, lay_, :, :, 64:128], 1.0), writes=["V%d" % lay_])
            for item in range(8):
                stage('I%d_%d' % (s, item))
                if os.environ.get('KBAR'):
                    P.add("act", lambda e: e.activation(out=bar[:, 0:1], in_=mhalf[:, 0:1], func=AF.Copy), reads=["mhalf"], writes=["bar_act"])
                    P.add("dve", lambda e: e.tensor_copy(out=bar[:, 1:2], in_=mhalf[:, 0:1]), reads=["mhalf"], writes=["bar_dve"])
                    P.add("pool", lambda e: e.tensor_copy(out=bar[:, 2:3], in_=mhalf[:, 0:1]), reads=["mhalf"], writes=["bar_pool"])
                    for en in ("pe", "act", "dve", "pool", "sp"):
                        P.add(en, lambda e: None, reads=["bar_act", "bar_dve", "bar_pool"], waitonly=True)
                if os.environ.get('KSNAP') and item == 1:
                    P.add("pool", lambda e: e.tensor_copy(out=yT[:, 7, :], in_=yT[:, 0, :]), reads=["yT0_0", "yT0_1"], writes=["yT7_0", "yT7_1"])
                    P.add("pool", lambda e: e.tensor_copy(out=yT[:, 6, :], in_=acc[0][:, :]), reads=["acc0"], writes=["yT6_0", "yT6_1"])
                if s == 0:
                    for ex_ in (2 * item, 2 * item + 1):
                        for nm_, src_, dst_ in (("g", wg_d, wgs_d), ("u", wu_d, wus_d), ("d", wd_d, wds_d)):
                            P.add("pool", lambda e, src_=src_, dst_=dst_, ex_=ex_: e.dma_start(out=dst_[ex_], in_=src_[ex_], max_dma_last_dim=4096),
                                  writes=["cv%s%d" % (nm_, ex_)], dma="cv%s%d" % (nm_, ex_))
                is_dil = item < 4
                j = item % 4
                st = item % 2
                colbase = 0 if is_dil else 1536
                wq, wk, wv = wblk[st]
                for wi, (wb, off) in enumerate(((wq, 0), (wk, 512), (wv, 1024))):
                    c0 = colbase + off + j * 128
                    P.add("pool", lambda e, wb=wb, c0=c0: e.dma_start(out=wb, in_=win_d.rearrange("(c p) n -> p c n", p=128)[:, :, c0:c0 + 128]),
                          writes=["wb%d_%d" % (st, wi)], dma="wb%d_%d" % (st, wi))
                if not is_dil:
                    P.add("pool", lambda e, j=j: e.dma_start(out=nastrip, in_=nas_d[j].rearrange("p (h v n) -> p h v n", h=2, v=2), max_dma_last_dim=3584),
                          writes=["nastrip"], dma="nas")
                for which, (wb, dstT, scl) in enumerate(((wq, QT[st], 0.125), (wk, KT[st], 1.0))):
                    for tc in range(4):
                        bi = ctr["P"] % 2; ctr["P"] += 1
                        bk = bP[bi]
                        for c in range(8):
                            P.add("pe", lambda e, bk=bk, wb=wb, c=c, tc=tc: e.matmul(bk[:, 0:512], lhsT=wb[:, c, :], rhs=hnT[:, c, tc * 512:(tc + 1) * 512],
                                                                                     start=(c == 0), stop=(c == 7)),
                                  reads=["wb%d_%d" % (st, which)] + hn_tokens(range(4 * tc, 4 * tc + 4)), writes=["bk%d" % bi])
                        dst = dstT[:, tc * 512:(tc + 1) * 512]
                        tokn = ("QT%d" if which == 0 else "KT%d") % st
                        if which == 0:
                            P.add("dve", lambda e, dst=dst, bk=bk: e.tensor_scalar(out=dst, in0=bk[:, 0:512], scalar1=0.125, scalar2=None, op0=ALU.mult),
                                  reads=["bk%d" % bi], writes=[tokn])
                        else:
                            P.add("dve", lambda e, dst=dst, bk=bk: e.tensor_copy(out=dst, in_=bk[:, 0:512]),
                                  reads=["bk%d" % bi], writes=[tokn])
                for tc in range(4):
                    bi = ctr["P"] % 2; ctr["P"] += 1
                    bk = bP[bi]
                    for c in range(8):
                        P.add("pe", lambda e, bk=bk, wv=wv, c=c, tc=tc: e.matmul(bk[:, 0:512], lhsT=wv[:, c, :], rhs=hnT[:, c, tc * 512:(tc + 1) * 512],
                                                                                 start=(c == 0), stop=(c == 7)),
                              reads=["wb%d_2" % st] + hn_tokens(range(4 * tc, 4 * tc + 4)), writes=["bk%d" % bi])
                    dst = VTb[:, tc * 512:(tc + 1) * 512]
                    P.add("dve", lambda e, dst=dst, bk=bk: e.tensor_copy(out=dst, in_=bk[:, 0:512]),
                          reads=["bk%d" % bi], writes=["VT"])
                layouts = ((0, 1), (1, 4), (2, 16)) if is_dil else ((0, 1),)
                for (lay, d) in layouts:
                    L = S // d; nt = L // 128
                    tiles = [(r, m) for r in range(d) for m in range(nt)]
                    for g8 in range(2):
                        bi = ctr["P"] % 2; ctr["P"] += 1
                        bkb = bPbf[bi]
                        for q in range(8):
                            r, m = tiles[g8 * 8 + q]
                            tsl_ = tsl(r, d, 128 * m, 128)
                            P.add("pe", lambda e, bkb=bkb, q=q, tsl_=tsl_: e.transpose(bkb[:, q * 128:(q + 1) * 128], VTb[:, tsl_], ident),
                                  reads=["VT", "ident"], writes=["bk%d" % bi])
                        dst = Vt[:, lay, g8 * 8:(g8 + 1) * 8, :, 0:64]
                        src = bkb[:, 0:1024].rearrange("p (t h c) -> p t h c", t=8, h=2)
                        P.add("dve", lambda e, dst=dst, src=src: e.tensor_copy(out=dst, in_=src),
                              reads=["bk%d" % bi], writes=["V%d" % lay])

                tasks = []
                for hp in range(2):
                    h = 2 * j + hp
                    rows = slice(hp * 64, hp * 64 + 64)
                    qT = QT[st][rows, :]; kT = KT[st][rows, :]
                    acc_h = acc[hp]
                    atok = "acc%d" % hp
                    if is_dil:
                        for pi, d in enumerate((1, 4, 16)):
                            L = S // d; nt = L // 128
                            bias = dilb[:, (h * 3 + pi) * 256:(h * 3 + pi + 1) * 256]
                            if d == 16:
                                bias3 = dilb[:, 24 * 256 + h * 128:24 * 256 + (h + 1) * 128]
                                for r0 in range(0, 16, 4):
                                    def s_part(r0=r0, bias3=bias3, kT=kT, qT=qT, st=st):
                                        si = ctr["S"] % 3; ctr["S"] += 1
                                        bkS = bS[si]
                                        pt = PT[:, si * 512:(si + 1) * 512]
                                        for rr in range(4):
                                            r_ = r0 + rr
                                            P.add("pe", lambda e, bkS=bkS, rr=rr: e.matmul(bkS[:, rr * 128:(rr + 1) * 128], lhsT=ident, rhs=bias3, start=True, stop=False),
                                                  reads=["ident", "dilb"], writes=["bk%d" % (2 + si)])
                                            P.add("pe", lambda e, bkS=bkS, rr=rr, r_=r_: e.matmul(bkS[:, rr * 128:(rr + 1) * 128], lhsT=kT[:, tsl(r_, 16, 0, 128)],
                                                                                               rhs=qT[:, tsl(r_, 16, 0, 128)], start=False, stop=True),
                                                  reads=["QT%d" % st, "KT%d" % st], writes=["bk%d" % (2 + si)])
                                        P.add("act", lambda e, pt=pt, bkS=bkS: e.activation(out=pt[:, 0:512], in_=bkS[:, 0:512], func=AF.Exp),
                                              reads=["bk%d" % (2 + si)], writes=["PT%d" % si])
                                        return si

                                    def pv_part(si, r0=r0, hp=hp, acc_h=acc_h, atok=atok):
                                        oi = ctr["O"] % 2; ctr["O"] += 1
                                        bkO = bO[oi]
                                        pt = PT[:, si * 512:(si + 1) * 512]
                                        for rr in range(4):
                                            P.add("pe", lambda e, bkO=bkO, rr=rr: e.matmul(bkO[:, rr * 128:(rr + 1) * 128], lhsT=Vt[:, 2, r0 + rr, hp, :],
                                                                                        rhs=pt[:, rr * 128:(rr + 1) * 128], start=True, stop=True),
                                                  reads=["PT%d" % si, "V2"], writes=["bk%d" % (5 + oi)])
                                        dsta = acc_h.rearrange("p (q r) -> p q r", r=16)[:, :, r0:r0 + 4]
                                        srca = bkO[:, 0:512].rearrange("p (r q) -> p q r", r=4)
                                        P.add("dve", lambda e: e.tensor_tensor(out=dsta, in0=srca, in1=dsta, op=ALU.add),
                                              reads=["bk%d" % (5 + oi), atok], writes=[atok])
                                    tasks.append((s_part, pv_part))
                                continue
                            for r in range(d):
                                for sb0 in range(0, nt + 1, 2):
                                    sblk = list(range(nt + 1))[sb0:sb0 + 2]

                                    def s_part(sblk=sblk, r=r, d=d, nt=nt, bias=bias, kT=kT, qT=qT, st=st):
                                        si = ctr["S"] % 3; ctr["S"] += 1
                                        bkS = bS[si]
                                        pt = PT[:, si * 512:(si + 1) * 512]
                                        vlo = None; vhi = None
                                        for bi_, b in enumerate(sblk):
                                            base = bi_ * 256
                                            if b == 0:
                                                clo, chi = base + 192, base + 256
                                            elif b == nt:
                                                clo, chi = base, base + 64
                                            else:
                                                clo, chi = base, base + 256
                                            if vlo is None:
                                                vlo = clo
                                            vhi = chi
                                            P.add("pe", lambda e, bkS=bkS, clo=clo, chi=chi, base=base: e.matmul(
                                                bkS[:, clo:chi], lhsT=ident, rhs=bias[:, clo - base:chi - base], start=True, stop=False),
                                                reads=["ident", "dilb"], writes=["bk%d" % (2 + si)])
                                            if b > 0:
                                                nq = 64 if b == nt else 128
                                                P.add("pe", lambda e, bkS=bkS, base=base, nq=nq, b=b: e.matmul(
                                                    bkS[:, base:base + nq], lhsT=kT[:, tsl(r, d, 128 * (b - 1), 128)],
                                                    rhs=qT[:, tsl(r, d, 128 * b - 64, nq)], start=False, stop=(b == nt)),
                                                    reads=["QT%d" % st, "KT%d" % st], writes=["bk%d" % (2 + si)])
                                            if b < nt:
                                                q0 = 64 if b == 0 else 0
                                                P.add("pe", lambda e, bkS=bkS, base=base, q0=q0, b=b: e.matmul(
                                                    bkS[:, base + 128 + q0:base + 256], lhsT=kT[:, tsl(r, d, 128 * b, 128)],
                                                    rhs=qT[:, tsl(r, d, 128 * b - 64 + q0, 128 - q0)], start=False, stop=True),
                                                    reads=["QT%d" % st, "KT%d" % st], writes=["bk%d" % (2 + si)])
                                        P.add("act", lambda e, pt=pt, bkS=bkS, vlo=vlo, vhi=vhi: e.activation(out=pt[:, vlo:vhi], in_=bkS[:, vlo:vhi], func=AF.Exp),
                                              reads=["bk%d" % (2 + si)], writes=["PT%d" % si])
                                        return si

                                    def pv_part(si, sblk=sblk, r=r, d=d, nt=nt, pi=pi, hp=hp, acc_h=acc_h, atok=atok):
                                        oi = ctr["O"] % 2; ctr["O"] += 1
                                        bkO = bO[oi]
                                        pt = PT[:, si * 512:(si + 1) * 512]
                                        for bi_, b in enumerate(sblk):
                                            base = bi_ * 256
                                            ob = bi_ * 128
                                            if b == 0:
                                                P.add("pe", lambda e, bkO=bkO, ob=ob, base=base: e.matmul(
                                                    bkO[:, ob + 64:ob + 128], lhsT=Vt[:, pi, r * nt + 0, hp, :], rhs=pt[:, base + 192:base + 256], start=True, stop=True),
                                                    reads=["PT%d" % si, "V%d" % pi], writes=["bk%d" % (5 + oi)])
                                            elif b == nt:
                                                P.add("pe", lambda e, bkO=bkO, ob=ob, base=base: e.matmul(
                                                    bkO[:, ob:ob + 64], lhsT=Vt[:, pi, r * nt + nt - 1, hp, :], rhs=pt[:, base:base + 64], start=True, stop=True),
                                                    reads=["PT%d" % si, "V%d" % pi], writes=["bk%d" % (5 + oi)])
                                            else:
                                                P.add("pe", lambda e, bkO=bkO, ob=ob, base=base, b=b: e.matmul(
                                                    bkO[:, ob:ob + 128], lhsT=Vt[:, pi, r * nt + b - 1, hp, :], rhs=pt[:, base:base + 128], start=True, stop=False),
                                                    reads=["PT%d" % si, "V%d" % pi], writes=["bk%d" % (5 + oi)])
                                                P.add("pe", lambda e, bkO=bkO, ob=ob, base=base, b=b: e.matmul(
                                                    bkO[:, ob:ob + 128], lhsT=Vt[:, pi, r * nt + b, hp, :], rhs=pt[:, base + 128:base + 256], start=False, stop=True),
                                                    reads=["PT%d" % si, "V%d" % pi], writes=["bk%d" % (5 + oi)])
                                        b0 = sblk[0]
                                        c_lo = 64 if b0 == 0 else 0
                                        c_hi = len(sblk) * 128 - (64 if sblk[-1] == nt else 0)
                                        p_lo = 128 * b0 - 64 + c_lo
                                        dsta = acc_h[:, tsl(r, d, p_lo, c_hi - c_lo)]
                                        srca = bkO[:, c_lo:c_hi]
                                        if pi == 0:
                                            P.add("dve", lambda e: e.tensor_copy(out=dsta, in_=srca),
                                                  reads=["bk%d" % (5 + oi)], writes=[atok])
                                        else:
                                            P.add("dve", lambda e: e.tensor_tensor(out=dsta, in0=srca, in1=dsta, op=ALU.add),
                                                  reads=["bk%d" % (5 + oi), atok], writes=[atok])
                                    tasks.append((s_part, pv_part))
                    else:
                        for g in range(8):
                            if g == 0:
                                ms_, var = [0, 1, 2, 3], 1
                            elif g == 7:
                                ms_, var = [12, 13, 14, 15], 1
                            else:
                                ms_, var = list(range(2 * g - 2, 2 * g + 4)), 0
                            npairs = (len(ms_) + 1) // 2
                            ostate = {}
                            for k0 in range(0, len(ms_), 2):
                                pair = ms_[k0:k0 + 2]

                                def s_part(pair=pair, g=g, var=var, hp=hp, kT=kT, qT=qT, st=st):
                                    si = ctr["S"] % 3; ctr["S"] += 1
                                    bkS = bS[si]
                                    pt = PT[:, si * 512:(si + 1) * 512]
                                    for ki, m in enumerate(pair):
                                        base = ki * 256
                                        sft = 6 - (2 * m - 4 * g)
                                        assert 0 <= sft and sft * 64 + 256 <= 896
                                        P.add("pe", lambda e, bkS=bkS, base=base, sft=sft: e.matmul(
                                            bkS[:, base:base + 256], lhsT=ident, rhs=nastrip[:, hp, var, sft * 64:sft * 64 + 256], start=True, stop=False),
                                            reads=["ident", "nastrip"], writes=["bk%d" % (2 + si)])
                                        P.add("pe", lambda e, bkS=bkS, base=base, m=m: e.matmul(
                                            bkS[:, base:base + 256], lhsT=kT[:, m * 128:(m + 1) * 128], rhs=qT[:, g * 256:(g + 1) * 256], start=False, stop=True),
                                            reads=["QT%d" % st, "KT%d" % st], writes=["bk%d" % (2 + si)])
                                    ncol = 256 * len(pair)
                                    P.add("act", lambda e, pt=pt, bkS=bkS, ncol=ncol: e.activation(out=pt[:, 0:ncol], in_=bkS[:, 0:ncol], func=AF.Exp),
                                          reads=["bk%d" % (2 + si)], writes=["PT%d" % si])
                                    return si

                                def pv_part(si, pair=pair, k0=k0, g=g, hp=hp, acc_h=acc_h, atok=atok, ostate=ostate, nms=len(ms_)):
                                    if k0 == 0:
                                        ostate["oi"] = ctr["O"] % 2; ctr["O"] += 1
                                    oi = ostate["oi"]
                                    bkO = bO[oi]
                                    pt = PT[:, si * 512:(si + 1) * 512]
                                    for ki, m in enumerate(pair):
                                        ti = k0 + ki
                                        P.add("pe", lambda e, bkO=bkO, ki=ki, m=m, ti=ti: e.matmul(
                                            bkO[:, 0:256], lhsT=Vt[:, 0, m, hp, :], rhs=pt[:, ki * 256:(ki + 1) * 256], start=(ti == 0), stop=(ti == nms - 1)),
                                            reads=["PT%d" % si, "V0"], writes=["bk%d" % (5 + oi)])
                                    if k0 + len(pair) == nms:
                                        dsta = acc_h[:, g * 256:(g + 1) * 256]
                                        P.add("dve", lambda e: e.tensor_copy(out=dsta, in_=bkO[:, 0:256]),
                                              reads=["bk%d" % (5 + oi)], writes=[atok])
                                tasks.append((s_part, pv_part))

                    def fin_part(_si, acc_h=acc_h, atok=atok, rows=rows, hp=hp, chunk=(j if is_dil else 4 + j)):
                        P.add("act", lambda e: e.activation(out=rden[0:64, :], in_=acc_h[64:128, :], func=AF.Ln),
                              reads=[atok], writes=["rden"])
                        P.add("act", lambda e: e.activation(out=rden[0:64, :], in_=rden[0:64, :], func=AF.Exp, scale=-1.0),
                              reads=["rden"], writes=["rden"])
                        P.add("dve", lambda e: e.tensor_tensor(out=yT[rows, chunk, :], in0=acc_h[0:64, :], in1=rden[0:64, :], op=ALU.mult),
                              reads=[atok, "rden"], writes=["yT%d_%d" % (chunk, hp)])
                    tasks.append((None, fin_part))

                if os.environ.get('KILV', '1') == '1':
                    cut = next(i_ for i_, t_ in enumerate(tasks) if t_[0] is None) + 1
                    ta, tb_ = tasks[:cut], tasks[cut:]
                    tasks = []
                    for i_ in range(max(len(ta), len(tb_))):
                        if i_ < len(ta):
                            tasks.append(ta[i_])
                        if i_ < len(tb_):
                            tasks.append(tb_[i_])
                LOOK = int(os.environ.get('KLOOK', '2'))
                sis = []
                for ti_, (sp_, pv_) in enumerate(tasks):
                    sis.append(sp_() if sp_ is not None else None)
                    if ti_ >= LOOK:
                        tasks[ti_ - LOOK][1](sis[ti_ - LOOK])
                for ti_ in range(max(0, len(tasks) - LOOK), len(tasks)):
                    tasks[ti_][1](sis[ti_])


            stage('P%d' % s)
            ytoks = ["yT%d_%d" % (c, hp) for c in range(8) for hp in range(2)]
            P.add("pool", lambda e: e.dma_start(out=wout_bf, in_=wout_d.rearrange("(c p) n -> p c n", p=128)),
                  writes=["V0", "V1", "V2", "wout"], dma="wout")
            for c in range(8):
                P.add("dve", lambda e, c=c: e.tensor_scalar(out=wout_bf[:, c, :], in0=wout_bf[:, c, :], scalar1=gout[:, c:c + 1], scalar2=None, op0=ALU.mult),
                      reads=["wout", "vecs"], writes=["wout"])
            for tt in range(16):
                row0 = tb + tt * 128
                tsl_ = slice(tt * 128, (tt + 1) * 128)
                xi = ctr["x"] % 2; ctr["x"] += 1
                xtile = xt[xi]
                P.add("sp", lambda e, xtile=xtile, row0=row0: e.dma_start(out=xtile, in_=x_d[row0:row0 + 128, :]),
                      writes=["xt%d_src" % xi], dma="x%d" % xi)
                P.add("pool", lambda e, tsl_=tsl_: e.tensor_tensor(out=sqt, in0=yT[:, :, tsl_], in1=yT[:, :, tsl_], op=ALU.mult),
                      reads=ytoks, writes=["sqt"])
                bq = bS[2]
                for c in range(8):
                    col = c // 4
                    P.add("pe", lambda e, c=c, col=col, bq=bq: e.matmul(bq[:, col:col + 1], lhsT=sqt[:, c, :], rhs=ones1[:, 0:1], start=(c % 4 == 0), stop=(c % 4 == 3)),
                          reads=["sqt", "ones1"], writes=["bk4"])
                ms2 = small[:, 16 + xi * 4:16 + xi * 4 + 2]; rs2 = small[:, 16 + xi * 4 + 2:16 + xi * 4 + 4]
                P.add("dve", lambda e, ms2=ms2, bq=bq: e.tensor_scalar(out=ms2, in0=bq[:, 0:2], scalar1=1.0 / 512, scalar2=EPS, op0=ALU.mult, op1=ALU.add),
                      reads=["bk4"], writes=["ms2_%d" % xi])
                P.add("pool", lambda e, ms2=ms2, rs2=rs2: e.tensor_tensor(out=rs2, in0=ms2, in1=mhalf[:, 0:2], op=ALU.pow),
                      reads=["ms2_%d" % xi, "mhalf"], writes=["rs2_%d" % xi])
                ht = acc[xi][:, 0:D]
                for half in range(2):
                    hs_ = slice(half * 512, (half + 1) * 512)
                    bA = [bP[0], bP[1]][half]; bB = [bS[0], bS[1]][half]
                    for c in range(4):
                        P.add("pe", lambda e, c=c, bA=bA, tsl_=tsl_, hs_=hs_: e.matmul(bA[:, 0:512], lhsT=yT[:, c, tsl_], rhs=wout_bf[:, c, hs_], start=(c == 0), stop=(c == 3)),
                              reads=ytoks + ["wout", "V0", "V1", "V2"], writes=["bk%d" % half])
                    for c in range(4, 8):
                        P.add("pe", lambda e, c=c, bB=bB, tsl_=tsl_, hs_=hs_: e.matmul(bB[:, 0:512], lhsT=yT[:, c, tsl_], rhs=wout_bf[:, c, hs_], start=(c == 4), stop=(c == 7)),
                              reads=ytoks + ["wout", "V0", "V1", "V2"], writes=["bk%d" % (2 + half)])
                    P.add("dve", lambda e, ht=ht, hs_=hs_, bA=bA, rs2=rs2, xtile=xtile: e.scalar_tensor_tensor(out=ht[:, hs_], in0=bA[:, 0:512], scalar=rs2[:, 0:1], in1=xtile[:, hs_], op0=ALU.mult, op1=ALU.add),
                          reads=["bk%d" % half, "rs2_%d" % xi, "xt%d_src" % xi], writes=["acc%d" % xi])
                    P.add("dve", lambda e, ht=ht, hs_=hs_, bB=bB, rs2=rs2: e.scalar_tensor_tensor(out=ht[:, hs_], in0=bB[:, 0:512], scalar=rs2[:, 1:2], in1=ht[:, hs_], op0=ALU.mult, op1=ALU.add),
                          reads=["bk%d" % (2 + half), "rs2_%d" % xi, "acc%d" % xi], writes=["acc%d" % xi])
                P.add("sp", lambda e, ht=ht, row0=row0: e.dma_start(out=hs_d[row0:row0 + 128, :], in_=ht),
                      reads=["acc%d" % xi], writes=["hs%d" % (row0 // 128)], dma="hst%d" % xi)

        stage('O')
        P.add("act", lambda e: e.activation(out=bar[:, 0:1], in_=mhalf[:, 0:1], func=AF.Copy), reads=["mhalf"], writes=["bar_act"])
        P.add("dve", lambda e: e.tensor_copy(out=bar[:, 1:2], in_=mhalf[:, 0:1]), reads=["mhalf"], writes=["bar_dve"])
        P.add("pool", lambda e: e.tensor_copy(out=bar[:, 2:3], in_=mhalf[:, 0:1]), reads=["mhalf"], writes=["bar_pool"])
        for en in ("pe", "act", "dve", "pool", "sp"):
            P.add(en, lambda e: None, reads=["bar_act", "bar_dve", "bar_pool"] + ["hs%d" % i_ for i_ in range(32)], waitonly=True)

        ptr[0] = persist_end
        wd_bf = alloc(32 * D, BF16, (32, D))
        hn2T = alloc(8 * 1024, BF16, (8, 1024))
        actT = alloc(32 * 1024, BF16, (32, 1024))
        wgu = [[alloc(8 * 256, BF16, (8, 256)) for _ in range(2)] for _ in range(3)]
        hbA = alloc(D); hbD = [alloc(D) for _ in range(2)]
        xsA = alloc(D, BF16); xsD = alloc(D, BF16)
        combT = alloc(1024, BF16)
        Cs = [alloc(512, BF16) for _ in range(2)]
        ssb = [alloc(512, BF16) for _ in range(2)]
        s2b = [alloc(512, BF16) for _ in range(2)]
        rt = alloc(960)
        comb_bf = alloc(8 * 16, BF16, (8, 16))
        small2 = alloc(32)

        bG = [banks[0], banks[1]]; bU = [banks[2], banks[3]]; bC = banks[4]; bD = [banks[5], banks[6]]

        for k in range(4):
            P.add("sp", lambda e, k=k: e.dma_start(out=wd_bf[:, 8 * k:8 * k + 8, :],
                                                   in_=wds_d[4 * k:4 * k + 4].rearrange("e (f p) n -> p (e f) n", p=128)),
                  reads=["cvd%d" % ee for ee in range(4 * k, 4 * k + 4)], writes=["wd%d" % k], dma="wd%d" % k)
        wd_toks = ["wd%d" % k for k in range(4)]

        def load_gu(n):
            if n >= 64:
                return
            ex_ = n % 16; wi_ = n % 3
            wgb_, wub_ = wgu[wi_]
            P.add("sp", lambda e: e.dma_start(out=wgb_, in_=wgs_d[ex_].rearrange("(c p) n -> p c n", p=128)),
                  reads=["cvg%d" % ex_], writes=["wg%d" % wi_], dma="wg%d" % wi_)
            P.add("sp", lambda e: e.dma_start(out=wub_, in_=wus_d[ex_].rearrange("(c p) n -> p c n", p=128)),
                  reads=["cvu%d" % ex_], writes=["wu%d" % wi_], dma="wu%d" % wi_)
        load_gu(0)
        load_gu(1)

        c2 = {"h": 0, "G": 0, "C": 0, "D": 0, "w": 0, "y": 0}
        h2_all = [("h2_%d_a" % st) for st in range(8)] + [("h2_%d_b" % st) for st in range(8)]

        hpool = [(hbA, "hbA"), (hbD[0], "hbD0"), (hbD[1], "hbD1")]
        xpool = [(xsA, "xsA"), (xsD, "xsD")]
        tbank2 = [(pb_t, "bk7"), (banks[2].bitcast(BF16), "bk2")]
        bD4 = [(banks[5], "bk5"), (banks[6], "bk6"), (banks[0], "bk0"), (banks[1], "bk1")]

        def prep_hn2(T, st):
            row0 = T * 1024 + st * 128
            if T == 0:
                htile, htok = hpool[st % 3]; xsb, xtok = xpool[st % 2]
            else:
                htile, htok = hpool[0]; xsb, xtok = xpool[0]
            si_ = c2["h"] % 4; c2["h"] += 1
            P.add("sp", lambda e: e.dma_start(out=htile, in_=hs_d[row0:row0 + 128, :]),
                  reads=["hs%d" % (row0 // 128)], writes=[htok], dma=htok)
            ss = small2[:, si_ * 4:si_ * 4 + 1]; ms = small2[:, si_ * 4 + 1:si_ * 4 + 2]; rstd = small2[:, si_ * 4 + 2:si_ * 4 + 3]
            P.add("act", lambda e: e.activation(out=xsb, in_=htile, func=AF.Square, accum_out=ss),
                  reads=[htok], writes=[xtok, "h_ss%d" % si_])
            P.add("dve", lambda e: e.tensor_scalar(out=ms, in0=ss, scalar1=1.0 / D, scalar2=EPS, op0=ALU.mult, op1=ALU.add),
                  reads=["h_ss%d" % si_], writes=["h_ms%d" % si_])
            P.add("pool", lambda e: e.tensor_tensor(out=rstd, in0=ms, in1=mhalf[:, 0:1], op=ALU.pow),
                  reads=["h_ms%d" % si_, "mhalf"], writes=["h_rstd%d" % si_])
            P.add("dve", lambda e: e.tensor_scalar(out=xsb, in0=htile, scalar1=rstd, scalar2=None, op0=ALU.mult),
                  reads=[htok, "h_rstd%d" % si_], writes=[xtok])
            return (xsb, xtok)

        def trans_hn2(ctx, T, st):
            xsb, xtok = ctx
            tbk, ttok = tbank2[st % 2]
            for c in range(8):
                P.add("pe", lambda e, c=c: e.transpose(tbk[:, c * 128:(c + 1) * 128], xsb[:, c * 128:(c + 1) * 128], ident),
                      reads=[xtok, "ident"], writes=[ttok])
            for c in range(8):
                dst = hn2T[:, c, st * 128:(st + 1) * 128]
                src = tbk[:, c * 128:(c + 1) * 128]
                if st % 2 == 0:
                    P.add("dve", lambda e, dst=dst, src=src, c=c: e.tensor_scalar(out=dst, in0=src, scalar1=gffn[:, c:c + 1], scalar2=None, op0=ALU.mult),
                          reads=[ttok, "vecs"], writes=["h2_%d_a" % st])
                else:
                    P.add("act", lambda e, dst=dst, src=src, c=c: e.activation(out=dst, in_=src, func=AF.Copy, scale=gffn[:, c:c + 1]),
                          reads=[ttok, "vecs"], writes=["h2_%d_b" % st])

        def emit_router(T):
            if True:
                for st in range(8):
                    for c in range(8):
                        P.add("pe", lambda e, st=st, c=c: e.matmul(bC[:, st * 32:st * 32 + 20], lhsT=hn2T[:, c, st * 128:(st + 1) * 128], rhs=wr_bf[:, c, :],
                                                                   start=(c == 0), stop=(c == 7)),
                              reads=["h2_%d_a" % st, "h2_%d_b" % st, "wr_bf"], writes=["bk4"])
                lg = rt[:, 0:160].rearrange("p (s n) -> p s n", s=8)
                bc3 = bC[:, 0:256].rearrange("p (s n) -> p s n", s=8)[:, :, 0:20]
                RT = "rt"
                r3 = lambda lo, n: rt[:, lo:lo + 8 * n].rearrange("p (s n) -> p s n", s=8)
                gl = lg[:, :, 0:4]; el = lg[:, :, 4:20]
                gmax = rt[:, 160:168]; g1 = r3(168, 4); gex = r3(200, 4); gsum = rt[:, 232:240]; gw = rt[:, 240:248]
                pen = r3(248, 16); ml = r3(376, 16); m1 = rt[:, 504:512]; m2 = rt[:, 512:520]; k1 = r3(520, 16)
                dm = rt[:, 648:656]; w1 = rt[:, 656:664]; w2 = rt[:, 664:672]; k2 = r3(672, 16); ml2 = r3(800, 16)
                bcast = lambda ap, n: ap.unsqueeze(2).to_broadcast([128, 8, n])
                P.add("dve", lambda e: e.tensor_tensor(out=lg, in0=bc3, in1=brep.unsqueeze(1).to_broadcast([128, 8, 20]), op=ALU.add),
                      reads=["bk4", "vecs"], writes=[RT])
                P.add("dve", lambda e: e.tensor_reduce(out=gmax, in_=gl, axis=AX.X, op=ALU.max), reads=[RT], writes=[RT + "a"])
                P.add("dve", lambda e: e.tensor_tensor(out=g1, in0=gl, in1=bcast(gmax, 4), op=ALU.is_equal), reads=[RT, RT + "a"], writes=[RT + "b"])
                P.add("dve", lambda e: e.tensor_tensor(out=gex, in0=gl, in1=bcast(gmax, 4), op=ALU.subtract), reads=[RT, RT + "a"], writes=[RT + "c"])
                P.add("act", lambda e: e.activation(out=gex, in_=gex, func=AF.Exp), reads=[RT + "c"], writes=[RT + "d"])
                P.add("dve", lambda e: e.tensor_reduce(out=gsum, in_=gex, axis=AX.X, op=ALU.add), reads=[RT + "d"], writes=[RT + "e"])
                P.add("dve", lambda e: e.reciprocal(out=gw, in_=gsum), reads=[RT + "e"], writes=[RT + "f"])
                P.add("dve", lambda e: e.tensor_scalar(out=pen.rearrange("p s (g n) -> p s g n", g=4), in0=g1.unsqueeze(3).to_broadcast([128, 8, 4, 4]),
                                                       scalar1=-1.0, scalar2=30000.0, op0=ALU.add, op1=ALU.mult), reads=[RT + "b"], writes=[RT + "g"])
                P.add("dve", lambda e: e.tensor_tensor(out=ml, in0=el, in1=pen, op=ALU.add), reads=[RT, RT + "g"], writes=[RT + "h"])
                P.add("dve", lambda e: e.tensor_reduce(out=m1, in_=ml, axis=AX.X, op=ALU.max), reads=[RT + "h"], writes=[RT + "i"])
                P.add("dve", lambda e: e.tensor_tensor(out=k1, in0=ml, in1=bcast(m1, 16), op=ALU.is_equal), reads=[RT + "h", RT + "i"], writes=[RT + "j"])
                P.add("dve", lambda e: e.scalar_tensor_tensor(out=ml2, in0=k1, scalar=-60000.0, in1=ml, op0=ALU.mult, op1=ALU.add), reads=[RT + "h", RT + "j"], writes=[RT + "k"])
                P.add("dve", lambda e: e.tensor_reduce(out=m2, in_=ml2, axis=AX.X, op=ALU.max), reads=[RT + "k"], writes=[RT + "l"])
                P.add("dve", lambda e: e.tensor_tensor(out=k2, in0=ml2, in1=bcast(m2, 16), op=ALU.is_equal), reads=[RT + "k", RT + "l"], writes=[RT + "m"])
                P.add("dve", lambda e: e.tensor_tensor(out=dm, in0=m2, in1=m1, op=ALU.subtract), reads=[RT + "l", RT + "i"], writes=[RT + "n"])
                P.add("act", lambda e: e.activation(out=dm, in_=dm, func=AF.Exp), reads=[RT + "n"], writes=[RT + "o"])
                P.add("dve", lambda e: e.tensor_scalar(out=dm, in0=dm, scalar1=1.0, scalar2=None, op0=ALU.add), reads=[RT + "o"], writes=[RT + "p"])
                P.add("dve", lambda e: e.reciprocal(out=w1, in_=dm), reads=[RT + "p"], writes=[RT + "q"])
                P.add("dve", lambda e: e.tensor_scalar(out=w2, in0=w1, scalar1=-1.0, scalar2=1.0, op0=ALU.mult, op1=ALU.add), reads=[RT + "q"], writes=[RT + "r"])
                P.add("dve", lambda e: e.tensor_tensor(out=w1, in0=w1, in1=gw, op=ALU.mult), reads=[RT + "q", RT + "r", RT + "f"], writes=[RT + "s"])
                P.add("dve", lambda e: e.tensor_tensor(out=w2, in0=w2, in1=gw, op=ALU.mult), reads=[RT + "r", RT + "f"], writes=[RT + "t"])
                P.add("dve", lambda e: e.tensor_tensor(out=k1, in0=k1, in1=bcast(w1, 16), op=ALU.mult), reads=[RT + "j", RT + "s", RT + "k"], writes=[RT + "u"])
                P.add("dve", lambda e: e.tensor_tensor(out=k2, in0=k2, in1=bcast(w2, 16), op=ALU.mult), reads=[RT + "m", RT + "t"], writes=[RT + "v"])
                P.add("dve", lambda e: e.tensor_tensor(out=comb_bf, in0=k1, in1=k2, op=ALU.add), reads=[RT + "u", RT + "v"], writes=["comb_bf"])
                for st in range(8):
                    P.add("pe", lambda e, st=st: e.transpose(pb_t[0:16, st * 128:(st + 1) * 128], comb_bf[:, st, :], ident),
                          reads=["comb_bf", "ident"] + h2_all, writes=["bk7"])
                P.add("dve", lambda e: e.tensor_copy(out=combT[0:16, :], in_=pb_t[0:16, 0:1024]), reads=["bk7"], writes=["combT"])


        def emit_experts(T):
            if True:
                for ex in range(16):
                    nflat = T * 16 + ex
                    wi = nflat % 3
                    wgb, wub = wgu[wi]
                    load_gu(nflat + 2)
                    for half in range(2):
                        hs_ = slice(half * 512, (half + 1) * 512)
                        ci = c2["C"] % 2; c2["C"] += 1
                        P.add("pe", lambda e, ex=ex, hs_=hs_: e.matmul(bC[:, 0:512], lhsT=sel[0:16, ex, :], rhs=combT[0:16, hs_], start=True, stop=True),
                              reads=["sel", "combT"], writes=["bk4"])
                        P.add("dve", lambda e, ci=ci: e.tensor_copy(out=Cs[ci], in_=bC[:, 0:512]), reads=["bk4"], writes=["Cs%d" % ci])
                        for fc in range(2):
                            gi = c2["G"] % 2; c2["G"] += 1
                            for c in range(8):
                                P.add("pe", lambda e, gi=gi, c=c, fc=fc, hs_=hs_, wgb=wgb: e.matmul(bG[gi][:, 0:512], lhsT=wgb[:, c, fc * 128:(fc + 1) * 128], rhs=hn2T[:, c, hs_],
                                                                                                  start=(c == 0), stop=(c == 7)),
                                      reads=["wg%d" % wi] + h2_all, writes=["bk%d" % gi])
                            for c in range(8):
                                P.add("pe", lambda e, gi=gi, c=c, fc=fc, hs_=hs_, wub=wub: e.matmul(bU[gi][:, 0:512], lhsT=wub[:, c, fc * 128:(fc + 1) * 128], rhs=hn2T[:, c, hs_],
                                                                                                  start=(c == 0), stop=(c == 7)),
                                      reads=["wu%d" % wi] + h2_all, writes=["bk%d" % (2 + gi)])
                            P.add("act", lambda e, gi=gi: e.activation(out=ssb[gi], in_=bG[gi][:, 0:512], func=AF.Silu), reads=["bk%d" % gi], writes=["ssb%d" % gi])
                            P.add("pool", lambda e, gi=gi, ci=ci: e.tensor_tensor(out=s2b[gi], in0=ssb[gi], in1=Cs[ci], op=ALU.mult),
                                  reads=["ssb%d" % gi, "Cs%d" % ci], writes=["s2b%d" % gi])
                            P.add("dve", lambda e, gi=gi, ex=ex, fc=fc, hs_=hs_: e.tensor_tensor(out=actT[:, ex * 2 + fc, hs_], in0=bU[gi][:, 0:512], in1=s2b[gi], op=ALU.mult),
                                  reads=["bk%d" % (2 + gi), "s2b%d" % gi], writes=["actT"])


        def down_mm(T, st):
            row0 = T * 1024 + st * 128
            hi = c2["y"] % 2; c2["y"] += 1
            htile, htok = hpool[1 + hi]
            P.add("sp", lambda e: e.dma_start(out=htile, in_=hs_d[row0:row0 + 128, :]),
                  reads=["hs%d" % (row0 // 128)], writes=[htok], dma=htok)
            for dh in range(2):
                bk_, btok = bD4[c2["D"] % 4]; c2["D"] += 1
                ds_ = slice(dh * 512, (dh + 1) * 512)
                for kk in range(32):
                    P.add("pe", lambda e, bk_=bk_, kk=kk, ds_=ds_: e.matmul(bk_[:, 0:512], lhsT=actT[:, kk, st * 128:(st + 1) * 128], rhs=wd_bf[:, kk, ds_],
                                                                          start=(kk == 0), stop=(kk == 31)),
                          reads=["actT"] + wd_toks, writes=[btok])
                P.add("dve", lambda e, bk_=bk_, ds_=ds_: e.tensor_tensor(out=htile[:, ds_], in0=bk_[:, 0:512], in1=htile[:, ds_], op=ALU.add),
                      reads=[btok, htok], writes=[htok])
            return (htile, htok, row0, hi)

        def down_fin(ctx):
            htile, htok, row0, yi = ctx
            junk, jtok = xpool[1]
            ss = small2[:, 16 + yi * 4:16 + yi * 4 + 1]; ms = small2[:, 16 + yi * 4 + 1:16 + yi * 4 + 2]; rstd = small2[:, 16 + yi * 4 + 2:16 + yi * 4 + 3]
            P.add("act", lambda e: e.activation(out=junk, in_=htile, func=AF.Square, accum_out=ss),
                  reads=[htok], writes=[jtok, "f_ss%d" % yi])
            P.add("dve", lambda e: e.tensor_scalar(out=ms, in0=ss, scalar1=1.0 / D, scalar2=EPS, op0=ALU.mult, op1=ALU.add),
                  reads=["f_ss%d" % yi], writes=["f_ms%d" % yi])
            P.add("pool", lambda e: e.tensor_tensor(out=rstd, in0=ms, in1=mhalf[:, 0:1], op=ALU.pow),
                  reads=["f_ms%d" % yi, "mhalf"], writes=["f_rstd%d" % yi])
            P.add("dve", lambda e: e.scalar_tensor_tensor(out=htile, in0=htile, scalar=rstd, in1=gfin, op0=ALU.mult, op1=ALU.mult),
                  reads=[htok, "f_rstd%d" % yi, "gfin"], writes=[htok])
            P.add("sp", lambda e: e.dma_start(out=y_d[row0:row0 + 128, :], in_=htile),
                  reads=[htok], dma="yst%d" % yi)

        for st in range(8):
            trans_hn2(prep_hn2(0, st), 0, st)
        emit_router(0)
        for T in range(4):
            emit_experts(T)
            for st in range(8):
                cx = prep_hn2(T + 1, st) if T < 3 else None
                dx = down_mm(T, st)
                if cx is not None:
                    trans_hn2(cx, T + 1, st)
                down_fin(dx)
            if T < 3:
                emit_router(T + 1)

    except StopBuild:
        pass

    sems = {}
    for en in Prog.ENGS:
        sems["eng:" + en] = es.enter_context(nc.semaphore("s_" + en))
    for sl in sorted(P.dma_slots):
        sems["dma:" + sl] = es.enter_context(nc.semaphore("d_" + sl))
    if os.environ.get('KMAXOPS'):
        P.ops = P.ops[:int(os.environ['KMAXOPS'])]
        for i_, o_ in enumerate(P.ops[-3:]):
            print('LASTOPS', o_.eng, o_.idx)
    body = P.emit(sems)
    if os.environ.get('KSTATS'):
        print('PROG stats', P.stats, 'ndma_slots', len(P.dma_slots))
    with nc.Block() as block:
        block.sync(body("sp"))
        block.tensor(body("pe"))
        block.scalar(body("act"))
        block.vector(body("dve"))
        block.gpsimd(body("pool"))
    es.close()
    return nc


def _dil_bias():
    out = np.full((128, 24 * 256 + 8 * 128), NEG, np.float32)
    k = np.arange(128)[:, None]; q = np.arange(128)[None, :]
    for h in range(8):
        slope = 2.0 ** (-(h + 1))
        for pi, d in enumerate((1, 4, 16)):
            for half, off in enumerate((-64, 64)):
                delta = k + off - q
                val = np.where(np.abs(delta) <= 64, -slope * d * np.abs(delta), NEG).astype(np.float32)
                c0 = (h * 3 + pi) * 256 + half * 128
                out[:, c0:c0 + 128] = val
        delta = k - q
        out[:, 24 * 256 + h * 128:24 * 256 + (h + 1) * 128] = np.where(np.abs(delta) <= 64, -slope * 16 * np.abs(delta), NEG)
    return out


def _na_strips(rpb):
    kc = np.arange(64)[:, None]; qc = np.arange(64)[None, :]
    cs = np.clip(qc - 8, 0, 48)
    col_ok = (kc >= cs) & (kc < cs + 16)
    dcidx = np.clip(kc - qc + 15, 0, 30)
    out = np.full((4, 128, 2, 2, 896), NEG, np.float32)
    for h in range(8):
        j, hp = divmod(h, 2)
        for var in range(2):
            for i in range(2):
                for u in range(14):
                    dlt = 6 + i - u
                    ok = (-4 <= dlt <= 3) if var == 0 else (-7 <= dlt <= 7)
                    if not ok:
                        continue
                    blk = np.where(col_ok, rpb[h, dlt + 7][dcidx], NEG).astype(np.float32)
                    out[j, i * 64:(i + 1) * 64, hp, var, u * 64:(u + 1) * 64] = blk
    return out.reshape(4, 128, 4 * 896)


_NC_CACHE = {}


def kernel(x, norm_mix_g, w_in, rpb, g_out_dil, g_out_na, w_out, norm_ffn_g, w_group, b_group, w_router, b_router,
           w_gate, w_up, w_down, norm_final_g):
    f = lambda a: np.ascontiguousarray(np.asarray(a, dtype=np.float32))
    x = f(x).reshape(16 * S, D)
    col = lambda g: f(g).reshape(8, 128).T
    vecs = np.concatenate([col(norm_mix_g[0]), col(norm_ffn_g[0]),
                           col(np.concatenate([f(g_out_dil[0]), f(g_out_na[0])])),
                           np.broadcast_to(np.concatenate([f(b_group[0]), f(b_router[0])])[None, :], (128, 20))], axis=1)
    vecs = np.ascontiguousarray(vecs, dtype=np.float32)
    gfin = np.ascontiguousarray(np.broadcast_to(f(norm_final_g)[None, :], (128, D)))
    wr = np.ascontiguousarray(np.concatenate([f(w_group[0]), f(w_router[0])], axis=1))
    shared = {
        "w_in": f(w_in[0]), "w_out": f(w_out[0]), "w_gate": f(w_gate[0]), "w_up": f(w_up[0]), "w_down": f(w_down[0]),
        "wr": wr, "vecs": vecs, "gfin": gfin, "dilb": _dil_bias(), "nas": _na_strips(f(rpb[0])),
    }
    if "nc" not in _NC_CACHE:
        _NC_CACHE["nc"] = build_program()
    nc = _NC_CACHE["nc"]
    in_maps = []
    for i in range(NCORES):
        m = dict(shared)
        m["x"] = np.ascontiguousarray(x[i * TOK:(i + 1) * TOK])
        in_maps.append(m)
    res = run_bass_kernel_spmd(nc, in_maps, core_ids=list(range(NCORES)))
    y = np.concatenate([r["y"] for r in res.results], axis=0)
    if DEBUG:
        kernel.dbg = [r.get("dbg") for r in res.results]
    return y.reshape(16, S, D).astype(np.float32)
```

```python
import numpy as np
from contextlib import ExitStack
import concourse.bass as bass
import concourse.mybir as mybir
from concourse.bass_utils import run_bass_kernel_spmd

F32 = mybir.dt.float32
BF16 = mybir.dt.bfloat16
AF = mybir.ActivationFunctionType
ALU = mybir.AluOpType
AX = mybir.AxisListType

NCORES = 8
S = 2048
D = 1024
TOK = 4096
NEG = -30000.0
EPS = 1e-6
import os
DEBUG = bool(os.environ.get('KSTOP', ''))
STOP = os.environ.get('KSTOP', '')


class StopBuild(Exception):
    pass


DUMPS = {}


def stage(name):
    if STOP and name == STOP:
        if name in DUMPS:
            DUMPS[name]()
        raise StopBuild()


class Op:
    __slots__ = ("eng", "fn", "deps", "is_dma", "sem", "semval", "needs_signal", "sigcount", "eidx", "idx", "wo")


class Prog:
    ENGS = ("pe", "act", "dve", "pool", "sp")

    def __init__(self):
        self.ops = []
        self.last_writer = {}
        self.readers = {}
        self.eng_count = {e: 0 for e in self.ENGS}
        self.dma_slots = set()

    def add(self, eng, fn, reads=(), writes=(), dma=None, waitonly=False):
        op = Op()
        op.eng = eng; op.fn = fn; op.idx = len(self.ops)
        op.is_dma = dma is not None
        op.wo = waitonly
        op.sem = dma; op.semval = 0; op.needs_signal = False; op.sigcount = 0
        op.eidx = self.eng_count[eng]; self.eng_count[eng] += 1
        if dma is not None:
            self.dma_slots.add(dma)
        deps = {}
        for t in reads:
            w = self.last_writer.get(t)
            if w is not None:
                deps[w] = "raw"
        for t in writes:
            w = self.last_writer.get(t)
            if w is not None and w not in deps:
                deps[w] = "waw"
            for r in self.readers.get(t, ()):
                if r not in deps and r != op.idx:
                    deps[r] = "war"
        for t in (() if waitonly else reads):
            lst = self.readers.setdefault(t, [])
            if not op.is_dma:
                lst[:] = [r for r in lst if self.ops[r].is_dma or self.ops[r].eng != eng]
            lst.append(op.idx)
        for t in writes:
            self.last_writer[t] = op.idx
            self.readers[t] = []
        op.deps = deps
        self.ops.append(op)
        return op

    def _need_wait(self, op, p, kind):
        if p.is_dma:
            return True
        if p.eng != op.eng:
            return True
        if op.is_dma:
            return True
        if p.eng == "pe":
            return False
        if kind == "raw" and ((op.eidx - p.eidx) <= 3 or p.eng == "pool"):
            return True
        return False

    def emit(self, sem_ctx):
        ops = self.ops
        for op in ops:
            for d, kind in op.deps.items():
                p = ops[d]
                if (not p.is_dma) and self._need_wait(op, p, kind):
                    p.needs_signal = True
        if os.environ.get('KALLSIG'):
            for op in ops:
                if not op.is_dma and op.fn(None) if False else (not op.is_dma and not getattr(op, "wo", False)):
                    op.needs_signal = True
        cnt = {e: 0 for e in self.ENGS}
        dcnt = {}
        for op in ops:
            if op.is_dma:
                dcnt[op.sem] = dcnt.get(op.sem, 0) + 16
                op.semval = dcnt[op.sem]
            elif op.needs_signal:
                cnt[op.eng] += 1
                op.sigcount = cnt[op.eng]
        by_eng = {e: [o for o in ops if o.eng == e] for e in self.ENGS}
        self.stats = (dict(cnt), {e: len(v) for e, v in by_eng.items()})

        def body(ename):
            def run(eng):
                waited = {}
                for op in by_eng[ename]:
                    for d, kind in op.deps.items():
                        p = ops[d]
                        if not self._need_wait(op, p, kind):
                            continue
                        if p.is_dma:
                            key = "dma:" + p.sem; val = p.semval
                        else:
                            key = "eng:" + p.eng; val = p.sigcount
                        if waited.get(key, 0) >= val:
                            continue
                        waited[key] = val
                        eng.wait_ge(sem_ctx[key], val)
                    ins = op.fn(eng)
                    if op.is_dma:
                        ins.then_inc(sem_ctx["dma:" + op.sem], 16)
                    elif op.needs_signal:
                        ins.then_inc(sem_ctx["eng:" + ename], 1)
                for op in by_eng[ename]:
                    if op.is_dma:
                        key = "dma:" + op.sem
                        if waited.get(key, 0) < dcnt[op.sem]:
                            waited[key] = dcnt[op.sem]
                            eng.wait_ge(sem_ctx[key], dcnt[op.sem])
            return run
        return body


def tsl(r, d, p0, n):
    return slice(r + d * p0, r + d * (p0 + n - 1) + 1, d)


def build_program():
    nc = bass.Bass("TRN2", target_bir_lowering=False)
    dt_in = lambda n, s, dt=F32: nc.dram_tensor(n, s, dt, kind="ExternalInput").ap()
    x_d = dt_in("x", [TOK, D])
    win_d = dt_in("w_in", [D, 3072])
    wout_d = dt_in("w_out", [D, D])
    wg_d = dt_in("w_gate", [16, D, 256])
    wu_d = dt_in("w_up", [16, D, 256])
    wd_d = dt_in("w_down", [16, 256, D])
    wr_d = dt_in("wr", [D, 20])
    vec_d = dt_in("vecs", [128, 24 + 20])
    gfin_d = dt_in("gfin", [128, D])
    dilb_d = dt_in("dilb", [128, 5120])
    nas_d = dt_in("nas", [4, 128, 4 * 896])
    y_d = nc.dram_tensor("y", [TOK, D], F32, kind="ExternalOutput").ap()
    hs_d = nc.dram_tensor("hscr", [TOK, D], F32, kind="Internal").ap()
    wgs_d = nc.dram_tensor("wg_bf", [16, D, 256], BF16, kind="Internal").ap()
    wus_d = nc.dram_tensor("wu_bf", [16, D, 256], BF16, kind="Internal").ap()
    wds_d = nc.dram_tensor("wd_bf", [16, 256, D], BF16, kind="Internal").ap()
    dbg_d = None
    if DEBUG:
        dbg_d = nc.dram_tensor("dbg", [128, 8 * 2048], F32, kind="ExternalOutput").ap()

    P = Prog()
    es = ExitStack()
    ARENA = 53200
    arena = es.enter_context(nc.sbuf_tensor("arena", [128, ARENA], F32))
    arena_bf = arena.bitcast(BF16)
    ptr = [0]

    def alloc(ncols, dt=F32, shape=None):
        n32 = ncols if dt == F32 else (ncols + 1) // 2
        a = ptr[0]
        ptr[0] += n32
        assert ptr[0] <= ARENA, ("SBUF overflow", ptr[0])
        if dt == F32:
            ap = arena[:, a:a + ncols]
        else:
            ap = arena_bf[:, 2 * a:2 * a + ncols]
        if shape is not None:
            names = " ".join("d%d" % i for i in range(len(shape)))
            kw = {"d%d" % i: shape[i] for i in range(len(shape))}
            ap = ap.rearrange("p (%s) -> p %s" % (names, names), **kw)
        return ap

    pb_t = es.enter_context(nc.psum_tensor("pb_t", [128, 1024], BF16))
    banks = [es.enter_context(nc.psum_tensor("bk%d" % i, [128, 512], F32)) for i in range(7)]
    tbank = [(pb_t, "bk7"), (banks[6].bitcast(BF16), "bk6")]

    ident = alloc(128, BF16)
    sel = alloc(16 * 128, BF16, (16, 128))
    vecs = alloc(44)
    gfin = alloc(D)
    wr_bf = alloc(8 * 20, BF16, (8, 20))
    mhalf = alloc(4)
    ones1 = alloc(2, BF16)
    bar = alloc(4)
    persist_end = ptr[0]

    try:
        P.add("pool", lambda e: e.memset(ident, 0.0), writes=["ident"])
        P.add("pool", lambda e: e.affine_select(out=ident, in_=ident, pattern=[[-1, 128]], compare_op=ALU.not_equal,
                                                fill=1.0, base=0, channel_multiplier=1), reads=["ident"], writes=["ident"])
        P.add("pool", lambda e: e.memset(sel, 0.0), writes=["sel"])
        P.add("pool", lambda e: e.affine_select(out=sel[0:16], in_=sel[0:16], pattern=[[-1, 16], [0, 128]],
                                                compare_op=ALU.not_equal, fill=1.0, base=0, channel_multiplier=1),
              reads=["sel"], writes=["sel"])
        P.add("pool", lambda e: e.memset(mhalf, -0.5), writes=["mhalf"])
        P.add("pool", lambda e: e.memset(ones1, 1.0), writes=["ones1"])
        P.add("sp", lambda e: e.dma_start(out=vecs, in_=vec_d), writes=["vecs"], dma="c0")
        P.add("sp", lambda e: e.dma_start(out=gfin, in_=gfin_d), writes=["gfin"], dma="c1")
        P.add("pool", lambda e: e.dma_start(out=wr_bf, in_=wr_d.rearrange("(c p) n -> p c n", p=128)),
              writes=["wr_bf"], dma="c2")
        gmix = vecs[:, 0:8]; gffn = vecs[:, 8:16]; gout = vecs[:, 16:24]; brep = vecs[:, 24:44]

        def rms_rstd(src, ss, ms, rstd, junk, tag, ncols=D):
            P.add("act", lambda e: e.activation(out=junk, in_=src, func=AF.Square, accum_out=ss),
                  reads=[tag + "_src"], writes=[tag + "_junk", tag + "_ss"])
            P.add("dve", lambda e: e.tensor_scalar(out=ms, in0=ss, scalar1=1.0 / ncols, scalar2=EPS, op0=ALU.mult, op1=ALU.add),
                  reads=[tag + "_ss"], writes=[tag + "_ms"])
            P.add("pool", lambda e: e.tensor_tensor(out=rstd, in0=ms, in1=mhalf[:, 0:1], op=ALU.pow),
                  reads=[tag + "_ms", "mhalf"], writes=[tag + "_rstd"])

        hnT = alloc(8 * S, BF16, (8, S))
        yT = alloc(8 * S, BF16, (8, S))
        dilb = alloc(5120, BF16)
        wblk = [[alloc(8 * 128, BF16, (8, 128)) for _ in range(3)] for _ in range(2)]
        QT = [alloc(2 * S, BF16, (2, S)) for _ in range(2)]
        KT = [alloc(S, BF16) for _ in range(2)]
        Vt_raw = alloc(3 * 16 * 256, BF16)
        Vt = Vt_raw.rearrange("p (l t h c) -> p l t h c", l=3, t=16, h=2, c=128)
        wout_bf = Vt_raw[:, 0:8 * D].rearrange("p (c n) -> p c n", c=8)
        acc2 = alloc(2 * S, F32, (2, S))
        acc = [acc2[:, 0, :], acc2[:, 1, :]]
        VTb = alloc(S, BF16)
        rden = alloc(S)
        NS = int(os.environ.get('KNS', '3')); NO = int(os.environ.get('KNO', '2'))
        PT = alloc(NS * 512, BF16)
        nastrip = alloc(4 * 896, BF16, (2, 2, 896))
        xs = [alloc(D, BF16) for _ in range(2)]
        xt = [alloc(D) for _ in range(2)]
        sqt = alloc(8 * 128, BF16, (8, 128))
        small = alloc(64)
        p1_end = ptr[0]


        def dump(items):
            col = [0]
            for ap, toks in items:
                n = ap.shape[-1] if len(ap.shape) == 2 else None
                assert n is not None
                a = col[0]; col[0] += n
                P.add("pool", lambda e, ap=ap, a=a, n=n: e.dma_start(out=dbg_d[0:ap.shape[0], a:a + n], in_=ap, max_dma_last_dim=2048),
                      reads=toks, dma="dbg")
        alltok = lambda: list(P.last_writer.keys())
        DUMPS['H0'] = lambda: dump([(hnT[:, c, :], alltok()) for c in range(8)])
        DUMPS['I0_1'] = lambda: dump([(QT[0], alltok()), (KT[0], alltok()), (acc[0], alltok()), (acc[1], alltok()), (yT[:, 0, :], alltok()),
                                      (Vt_raw[:, 0:4096], alltok())])
        DUMPS['I0_5'] = lambda: dump([(QT[0], alltok()), (KT[0], alltok()), (acc[0], alltok()), (acc[1], alltok()), (yT[:, 4, :], alltok()),
                                      (Vt_raw[:, 0:4096], alltok())])
        for k_ in (2, 3, 4, 6, 7):
            DUMPS['I0_%d' % k_] = lambda: dump([(yT[:, c, :], alltok()) for c in range(8)])
        if os.environ.get('KD2'):
            DUMPS['I0_2'] = lambda: dump([(QT[0], alltok()), (KT[0], alltok()), (QT[1], alltok()), (KT[1], alltok()), (wblk[0][0][:, 0, :], alltok()), (wblk[0][2][:, 0, :], alltok()), (wblk[1][0][:, 0, :], alltok())])
        DUMPS['P0'] = lambda: dump([(yT[:, c, :], alltok()) for c in range(8)])
        bP = [banks[0], banks[1]]
        bPbf = [banks[0].bitcast(BF16), banks[1].bitcast(BF16)]
        bS = [banks[2], banks[3], banks[4], banks[0], banks[1]][:NS]
        SBK = [2, 3, 4, 0, 1]
        bO = [banks[5], banks[6], pb_t.bitcast(F32)][:NO]

        P.add("pool", lambda e: e.dma_start(out=dilb, in_=dilb_d, max_dma_last_dim=4096), writes=["dilb"], dma="c3")
        if DEBUG:
            P.add("pool", lambda e: e.memset(yT, 0.0), writes=["yT%d_%d" % (c, hp) for c in range(8) for hp in range(2)])

        for st_ in range(2):
            P.add("pool", lambda e, st_=st_: e.memset(QT[st_], 0.0), writes=["QT%d" % st_])
        ctr = {"S": 0, "O": 0, "P": 0, "x": 0}

        for s in range(2):
            tb = s * S
            for tt in range(16):
                xi = ctr["x"] % 2; ctr["x"] += 1
                xtile = xt[xi]; xsb = xs[xi]
                row0 = tb + tt * 128
                P.add("sp", lambda e, xtile=xtile, row0=row0: e.dma_start(out=xtile, in_=x_d[row0:row0 + 128, :]),
                      writes=["xt%d_src" % xi], dma="x%d" % xi)
                ss = small[:, xi * 4:xi * 4 + 1]; ms = small[:, xi * 4 + 1:xi * 4 + 2]; rstd = small[:, xi * 4 + 2:xi * 4 + 3]
                P.add("act", lambda e, xsb=xsb, xtile=xtile, ss=ss: e.activation(out=xsb, in_=xtile, func=AF.Square, accum_out=ss),
                      reads=["xt%d_src" % xi], writes=["xs%d" % xi, "xt%d_ss" % xi])
                P.add("dve", lambda e, ms=ms, ss=ss: e.tensor_scalar(out=ms, in0=ss, scalar1=1.0 / D, scalar2=EPS, op0=ALU.mult, op1=ALU.add),
                      reads=["xt%d_ss" % xi], writes=["xt%d_ms" % xi])
                P.add("pool", lambda e, ms=ms, rstd=rstd: e.tensor_tensor(out=rstd, in0=ms, in1=mhalf[:, 0:1], op=ALU.pow),
                      reads=["xt%d_ms" % xi, "mhalf"], writes=["xt%d_rstd" % xi])
                P.add("dve", lambda e, xsb=xsb, xtile=xtile, rstd=rstd: e.tensor_scalar(out=xsb, in0=xtile, scalar1=rstd, scalar2=None, op0=ALU.mult),
                      reads=["xt%d_src" % xi, "xt%d_rstd" % xi], writes=["xs%d" % xi])
                tbk, ttok = tbank[tt % 2]
                for c in range(8):
                    P.add("pe", lambda e, xsb=xsb, c=c, tbk=tbk: e.transpose(tbk[:, c * 128:(c + 1) * 128], xsb[:, c * 128:(c + 1) * 128], ident),
                          reads=["xs%d" % xi, "ident"], writes=[ttok])
                for c in range(8):
                    dst = hnT[:, c, tt * 128:(tt + 1) * 128]
                    src = tbk[:, c * 128:(c + 1) * 128]
                    if tt % 2 == 0:
                        P.add("dve", lambda e, dst=dst, src=src, c=c: e.tensor_scalar(out=dst, in0=src, scalar1=gmix[:, c:c + 1], scalar2=None, op0=ALU.mult),
                              reads=[ttok, "vecs"], writes=["hn%d_a" % tt])
                    else:
                        P.add("act", lambda e, dst=dst, src=src, c=c: e.activation(out=dst, in_=src, func=AF.Copy, scale=gmix[:, c:c + 1]),
                              reads=[ttok, "vecs"], writes=["hn%d_b" % tt])

            def hn_tokens(tiles):
                out = []
                for t in tiles:
                    out += ["hn%d_a" % t, "hn%d_b" % t]
                return out

            stage('H%d' % s)
            for lay_ in range(3):
                P.add("pool", lambda e, lay_=lay_: e.memset(Vt[:, lay_, :, :, 64:128], 1.0), writes=["V%d" % lay_])
            for item in range(8):
                stage('I%d_%d' % (s, item))
                if os.environ.get('KBAR'):
                    P.add("act", lambda e: e.activation(out=bar[:, 0:1], in_=mhalf[:, 0:1], func=AF.Copy), reads=["mhalf"], writes=["bar_act"])
                    P.add("dve", lambda e: e.tensor_copy(out=bar[:, 1:2], in_=mhalf[:, 0:1]), reads=["mhalf"], writes=["bar_dve"])
                    P.add("pool", lambda e: e.tensor_copy(out=bar[:, 2:3], in_=mhalf[:, 0:1]), reads=["mhalf"], writes=["bar_pool"])
                    for en in ("pe", "act", "dve", "pool", "sp"):
                        P.add(en, lambda e: None, reads=["bar_act", "bar_dve", "bar_pool"], waitonly=True)
                if os.environ.get('KSNAP') and item == 1:
                    P.add("pool", lambda e: e.tensor_copy(out=yT[:, 7, :], in_=yT[:, 0, :]), reads=["yT0_0", "yT0_1"], writes=["yT7_0", "yT7_1"])
                    P.add("pool", lambda e: e.tensor_copy(out=yT[:, 6, :], in_=acc[0][:, :]), reads=["acc0"], writes=["yT6_0", "yT6_1"])
                if s == 0:
                    for ex_ in (2 * item, 2 * item + 1):
                        for nm_, src_, dst_ in (("g", wg_d, wgs_d), ("u", wu_d, wus_d), ("d", wd_d, wds_d)):
                            P.add("pool", lambda e, src_=src_, dst_=dst_, ex_=ex_: e.dma_start(out=dst_[ex_], in_=src_[ex_], max_dma_last_dim=4096),
                                  writes=["cv%s%d" % (nm_, ex_)], dma="cv%s%d" % (nm_, ex_))
                is_dil = item < 4
                j = item % 4
                st = item % 2
                colbase = 0 if is_dil else 1536
                wq, wk, wv = wblk[st]
                for wi, (wb, off) in enumerate(((wq, 0), (wk, 512), (wv, 1024))):
                    c0 = colbase + off + j * 128
                    P.add("pool", lambda e, wb=wb, c0=c0: e.dma_start(out=wb, in_=win_d.rearrange("(c p) n -> p c n", p=128)[:, :, c0:c0 + 128]),
                          writes=["wb%d_%d" % (st, wi)], dma="wb%d_%d" % (st, wi))
                if not is_dil:
                    P.add("pool", lambda e, j=j: e.dma_start(out=nastrip, in_=nas_d[j].rearrange("p (h v n) -> p h v n", h=2, v=2), max_dma_last_dim=3584),
                          writes=["nastrip"], dma="nas")
                for which, (wb, dstT, scl) in enumerate(((wq, QT[st], 0.125), (wk, KT[st], 1.0))):
                    for tc in range(4):
                        bi = ctr["P"] % 2; ctr["P"] += 1
                        bk = bP[bi]
                        for c in range(8):
                            P.add("pe", lambda e, bk=bk, wb=wb, c=c, tc=tc: e.matmul(bk[:, 0:512], lhsT=wb[:, c, :], rhs=hnT[:, c, tc * 512:(tc + 1) * 512],
                                                                                     start=(c == 0), stop=(c == 7)),
                                  reads=["wb%d_%d" % (st, which)] + hn_tokens(range(4 * tc, 4 * tc + 4)), writes=["bk%d" % bi])
                        dst = dstT[:, tc * 512:(tc + 1) * 512] if which == 1 else None
                        tokn = ("QT%d" if which == 0 else "KT%d") % st
                        if which == 0:
                            for hp_ in range(2):
                                rw = slice(hp_ * 64, hp_ * 64 + 64)
                                P.add("dve", lambda e, bk=bk, rw=rw, hp_=hp_, tc=tc, dstT=dstT: e.tensor_scalar(out=dstT[rw, hp_, tc * 512:(tc + 1) * 512], in0=bk[rw, 0:512], scalar1=0.125, scalar2=None, op0=ALU.mult),
                                      reads=["bk%d" % bi], writes=[tokn])
                        else:
                            P.add("dve", lambda e, dst=dst, bk=bk: e.tensor_copy(out=dst, in_=bk[:, 0:512]),
                                  reads=["bk%d" % bi], writes=[tokn])
                for tc in range(4):
                    bi = ctr["P"] % 2; ctr["P"] += 1
                    bk = bP[bi]
                    for c in range(8):
                        P.add("pe", lambda e, bk=bk, wv=wv, c=c, tc=tc: e.matmul(bk[:, 0:512], lhsT=wv[:, c, :], rhs=hnT[:, c, tc * 512:(tc + 1) * 512],
                                                                                 start=(c == 0), stop=(c == 7)),
                              reads=["wb%d_2" % st] + hn_tokens(range(4 * tc, 4 * tc + 4)), writes=["bk%d" % bi])
                    dst = VTb[:, tc * 512:(tc + 1) * 512]
                    P.add("dve", lambda e, dst=dst, bk=bk: e.tensor_copy(out=dst, in_=bk[:, 0:512]),
                          reads=["bk%d" % bi], writes=["VT"])
                layouts = ((0, 1), (1, 4), (2, 16)) if is_dil else ((0, 1),)
                for (lay, d) in layouts:
                    L = S // d; nt = L // 128
                    tiles = [(r, m) for r in range(d) for m in range(nt)]
                    for g8 in range(2):
                        bi = ctr["P"] % 2; ctr["P"] += 1
                        bkb = bPbf[bi]
                        for q in range(8):
                            r, m = tiles[g8 * 8 + q]
                            tsl_ = tsl(r, d, 128 * m, 128)
                            P.add("pe", lambda e, bkb=bkb, q=q, tsl_=tsl_: e.transpose(bkb[:, q * 128:(q + 1) * 128], VTb[:, tsl_], ident),
                                  reads=["VT", "ident"], writes=["bk%d" % bi])
                        dst = Vt[:, lay, g8 * 8:(g8 + 1) * 8, :, 0:64]
                        src = bkb[:, 0:1024].rearrange("p (t h c) -> p t h c", t=8, h=2)
                        P.add("dve", lambda e, dst=dst, src=src: e.tensor_copy(out=dst, in_=src),
                              reads=["bk%d" % bi], writes=["V%d" % lay])

                tasks = []
                qz = QT[st]; kTp = KT[st]
                ATOK = ["acc0", "acc1"]
                v2 = lambda ap, lo, n: ap[:, lo:lo + 2 * n].rearrange("p (h q) -> p h q", h=2)
                if is_dil:
                    for pi, d in enumerate((1, 4)):
                        L = S // d; nt = L // 128
                        biasP = dilb[:, (j * 2 + pi) * 512:(j * 2 + pi + 1) * 512]
                        for r in range(d):
                            for b in range(nt + 1):
                                def s_part(b=b, r=r, d=d, nt=nt, biasP=biasP, qz=qz, kTp=kTp, st=st):
                                    si = ctr["S"] % NS; ctr["S"] += 1
                                    bkS = bS[si]; pt = PT[:, si * 512:(si + 1) * 512]
                                    tk = "bk%d" % SBK[si]
                                    if b == 0:
                                        ov = v2(bkS, 256, 128)[:, :, 64:128]; bv = v2(biasP, 256, 128)[:, :, 64:128]; pv_ = v2(pt, 256, 128)[:, :, 64:128]
                                        P.add("pe", lambda e: e.matmul(ov, lhsT=ident, rhs=bv, start=True, stop=False), reads=["ident", "dilb"], writes=[tk])
                                        P.add("pe", lambda e: e.matmul(ov, lhsT=kTp[:, tsl(r, d, 0, 128)], rhs=qz[:, :, tsl(r, d, 0, 64)], start=False, stop=True),
                                              reads=["QT%d" % st, "KT%d" % st], writes=[tk])
                                        P.add("act", lambda e: e.activation(out=pv_, in_=ov, func=AF.Exp), reads=[tk], writes=["PT%d" % si])
                                    elif b == nt:
                                        ov = v2(bkS, 0, 128)[:, :, 0:64]; bv = v2(biasP, 0, 128)[:, :, 0:64]; pv_ = v2(pt, 0, 128)[:, :, 0:64]
                                        P.add("pe", lambda e: e.matmul(ov, lhsT=ident, rhs=bv, start=True, stop=False), reads=["ident", "dilb"], writes=[tk])
                                        P.add("pe", lambda e: e.matmul(ov, lhsT=kTp[:, tsl(r, d, 128 * (nt - 1), 128)], rhs=qz[:, :, tsl(r, d, 128 * nt - 64, 64)], start=False, stop=True),
                                              reads=["QT%d" % st, "KT%d" % st], writes=[tk])
                                        P.add("act", lambda e: e.activation(out=pv_, in_=ov, func=AF.Exp), reads=[tk], writes=["PT%d" % si])
                                    else:
                                        qv = qz[:, :, tsl(r, d, 128 * b - 64, 128)]
                                        P.add("pe", lambda e: e.matmul(bkS[:, 0:512], lhsT=ident, rhs=biasP, start=True, stop=False), reads=["ident", "dilb"], writes=[tk])
                                        P.add("pe", lambda e: e.matmul(v2(bkS, 0, 128), lhsT=kTp[:, tsl(r, d, 128 * (b - 1), 128)], rhs=qv, start=False, stop=False),
                                              reads=["QT%d" % st, "KT%d" % st], writes=[tk])
                                        P.add("pe", lambda e: e.matmul(v2(bkS, 256, 128), lhsT=kTp[:, tsl(r, d, 128 * b, 128)], rhs=qv, start=False, stop=True),
                                              reads=["QT%d" % st, "KT%d" % st], writes=[tk])
                                        P.add("act", lambda e: e.activation(out=pt[:, 0:512], in_=bkS[:, 0:512], func=AF.Exp), reads=[tk], writes=["PT%d" % si])
                                    return si

                                def pv_part(si, b=b, r=r, d=d, nt=nt, pi=pi):
                                    oi = ctr["O"] % NO; ctr["O"] += 1
                                    bkO = bO[oi]; pt = PT[:, si * 512:(si + 1) * 512]
                                    tk = "bk%d" % (5 + oi)
                                    first = [True]
                                    for hp in range(2):
                                        def mm(o_, l_, r_):
                                            stt = first[0]; first[0] = False
                                            P.add("pe", lambda e: e.matmul(o_, lhsT=l_, rhs=r_, start=stt, stop=True), reads=["PT%d" % si, "V%d" % pi], writes=[tk])
                                        ob = hp * 128
                                        if b == 0:
                                            mm(bkO[:, ob + 64:ob + 128], Vt[:, pi, r * nt + 0, hp, :], pt[:, 256 + hp * 128 + 64:256 + hp * 128 + 128])
                                        elif b == nt:
                                            mm(bkO[:, ob:ob + 64], Vt[:, pi, r * nt + nt - 1, hp, :], pt[:, hp * 128:hp * 128 + 64])
                                        else:
                                            mm(bkO[:, ob:ob + 128], Vt[:, pi, r * nt + b - 1, hp, :], pt[:, hp * 128:hp * 128 + 128])
                                            mm(bkO[:, ob:ob + 128], Vt[:, pi, r * nt + b, hp, :], pt[:, 256 + hp * 128:256 + hp * 128 + 128])
                                    c_lo = 64 if b == 0 else 0
                                    c_hi = 64 if b == nt else 128
                                    p_lo = 128 * b - 64 + c_lo
                                    dsta = acc2[:, :, tsl(r, d, p_lo, c_hi - c_lo)]
                                    srca = v2(bkO, 0, 128)[:, :, c_lo:c_hi]
                                    if pi == 0:
                                        P.add("dve", lambda e: e.tensor_copy(out=dsta, in_=srca), reads=[tk], writes=ATOK)
                                    else:
                                        P.add("dve", lambda e: e.tensor_tensor(out=dsta, in0=srca, in1=dsta, op=ALU.add), reads=[tk] + ATOK, writes=ATOK)
                                tasks.append((s_part, pv_part))
                    bias3P = dilb[:, 4096 + j * 256:4096 + (j + 1) * 256]
                    for r0 in range(0, 16, 2):
                        def s_part(r0=r0, bias3P=bias3P, qz=qz, kTp=kTp, st=st):
                            si = ctr["S"] % NS; ctr["S"] += 1
                            bkS = bS[si]; pt = PT[:, si * 512:(si + 1) * 512]
                            tk = "bk%d" % SBK[si]
                            for rr in range(2):
                                r_ = r0 + rr
                                P.add("pe", lambda e, rr=rr: e.matmul(bkS[:, rr * 256:(rr + 1) * 256], lhsT=ident, rhs=bias3P, start=True, stop=False),
                                      reads=["ident", "dilb"], writes=[tk])
                                P.add("pe", lambda e, rr=rr, r_=r_: e.matmul(v2(bkS, rr * 256, 128), lhsT=kTp[:, tsl(r_, 16, 0, 128)], rhs=qz[:, :, tsl(r_, 16, 0, 128)], start=False, stop=True),
                                      reads=["QT%d" % st, "KT%d" % st], writes=[tk])
                            P.add("act", lambda e: e.activation(out=pt[:, 0:512], in_=bkS[:, 0:512], func=AF.Exp), reads=[tk], writes=["PT%d" % si])
                            return si

                        def pv_part(si, r0=r0):
                            oi = ctr["O"] % NO; ctr["O"] += 1
                            bkO = bO[oi]; pt = PT[:, si * 512:(si + 1) * 512]
                            tk = "bk%d" % (5 + oi)
                            for rr in range(2):
                                for hp in range(2):
                                    cs_ = slice(rr * 256 + hp * 128, rr * 256 + hp * 128 + 128)
                                    P.add("pe", lambda e, rr=rr, hp=hp, cs_=cs_: e.matmul(bkO[:, cs_], lhsT=Vt[:, 2, r0 + rr, hp, :], rhs=pt[:, cs_], start=(rr == 0 and hp == 0), stop=True),
                                          reads=["PT%d" % si, "V2"], writes=[tk])
                            dsta = acc2.rearrange("p h (q r) -> p h q r", r=16)[:, :, :, r0:r0 + 2]
                            srca = bkO[:, 0:512].rearrange("p (r h q) -> p h q r", r=2, h=2)
                            P.add("dve", lambda e: e.tensor_tensor(out=dsta, in0=srca, in1=dsta, op=ALU.add), reads=[tk] + ATOK, writes=ATOK)
                        tasks.append((s_part, pv_part))
                else:
                    for g in range(8):
                        if g == 0:
                            ms_, var = [0, 1, 2, 3], 1
                        elif g == 7:
                            ms_, var = [12, 13, 14, 15], 1
                        else:
                            ms_, var = list(range(2 * g - 2, 2 * g + 4)), 0
                        ostate = {}
                        for ki, m in enumerate(ms_):
                            def s_part(m=m, g=g, var=var, qz=qz, kTp=kTp, st=st):
                                si = ctr["S"] % NS; ctr["S"] += 1
                                bkS = bS[si]; pt = PT[:, si * 512:(si + 1) * 512]
                                tk = "bk%d" % SBK[si]
                                sft = 6 - (2 * m - 4 * g)
                                assert 0 <= sft and sft * 64 + 256 <= 896
                                P.add("pe", lambda e: e.matmul(v2(bkS, 0, 256), lhsT=ident, rhs=nastrip[:, :, var, sft * 64:sft * 64 + 256], start=True, stop=False),
                                      reads=["ident", "nastrip"], writes=[tk])
                                P.add("pe", lambda e: e.matmul(v2(bkS, 0, 256), lhsT=kTp[:, m * 128:(m + 1) * 128], rhs=qz[:, :, g * 256:(g + 1) * 256], start=False, stop=True),
                                      reads=["QT%d" % st, "KT%d" % st], writes=[tk])
                                P.add("act", lambda e: e.activation(out=pt[:, 0:512], in_=bkS[:, 0:512], func=AF.Exp), reads=[tk], writes=["PT%d" % si])
                                return si

                            def pv_part(si, m=m, ki=ki, g=g, ostate=ostate, nms=len(ms_)):
                                if ki == 0:
                                    ostate["oi"] = ctr["O"] % NO; ctr["O"] += 1
                                oi = ostate["oi"]
                                bkO = bO[oi]; pt = PT[:, si * 512:(si + 1) * 512]
                                tk = "bk%d" % (5 + oi)
                                for hp in range(2):
                                    P.add("pe", lambda e, hp=hp: e.matmul(bkO[:, hp * 256:(hp + 1) * 256], lhsT=Vt[:, 0, m, hp, :], rhs=pt[:, hp * 256:(hp + 1) * 256],
                                                                         start=(ki == 0 and hp == 0), stop=(ki == nms - 1)),
                                          reads=["PT%d" % si, "V0"], writes=[tk])
                                if ki == nms - 1:
                                    dsta = acc2[:, :, g * 256:(g + 1) * 256]
                                    P.add("dve", lambda e: e.tensor_copy(out=dsta, in_=v2(bkO, 0, 256)), reads=[tk], writes=ATOK)
                            tasks.append((s_part, pv_part))

                for hp in range(2):
                    def fin_part(_si, hp=hp, chunk=(j if is_dil else 4 + j)):
                        rows = slice(hp * 64, hp * 64 + 64)
                        P.add("act", lambda e: e.activation(out=rden[0:64, :], in_=acc2[64:128, hp, :], func=AF.Ln), reads=ATOK, writes=["rden"])
                        P.add("act", lambda e: e.activation(out=rden[0:64, :], in_=rden[0:64, :], func=AF.Exp, scale=-1.0), reads=["rden"], writes=["rden"])
                        P.add("dve", lambda e: e.tensor_tensor(out=yT[rows, chunk, :], in0=acc2[0:64, hp, :], in1=rden[0:64, :], op=ALU.mult),
                              reads=ATOK + ["rden"], writes=["yT%d_%d" % (chunk, hp)])
                    tasks.append((None, fin_part))

                LOOK = int(os.environ.get('KLOOK', '2'))
                sis = []
                for ti_, (sp_, pv_) in enumerate(tasks):
                    sis.append(sp_() if sp_ is not None else None)
                    if ti_ >= LOOK:
                        tasks[ti_ - LOOK][1](sis[ti_ - LOOK])
                for ti_ in range(max(0, len(tasks) - LOOK), len(tasks)):
                    tasks[ti_][1](sis[ti_])


            stage('P%d' % s)
            ytoks = ["yT%d_%d" % (c, hp) for c in range(8) for hp in range(2)]
            P.add("pool", lambda e: e.dma_start(out=wout_bf, in_=wout_d.rearrange("(c p) n -> p c n", p=128)),
                  writes=["V0", "V1", "V2", "wout"], dma="wout")
            for c in range(8):
                P.add("dve", lambda e, c=c: e.tensor_scalar(out=wout_bf[:, c, :], in0=wout_bf[:, c, :], scalar1=gout[:, c:c + 1], scalar2=None, op0=ALU.mult),
                      reads=["wout", "vecs"], writes=["wout"])
            for tt in range(16):
                row0 = tb + tt * 128
                tsl_ = slice(tt * 128, (tt + 1) * 128)
                xi = ctr["x"] % 2; ctr["x"] += 1
                xtile = xt[xi]
                P.add("sp", lambda e, xtile=xtile, row0=row0: e.dma_start(out=xtile, in_=x_d[row0:row0 + 128, :]),
                      writes=["xt%d_src" % xi], dma="x%d" % xi)
                P.add("pool", lambda e, tsl_=tsl_: e.tensor_tensor(out=sqt, in0=yT[:, :, tsl_], in1=yT[:, :, tsl_], op=ALU.mult),
                      reads=ytoks, writes=["sqt"])
                bq = bS[2]
                for c in range(8):
                    col = c // 4
                    P.add("pe", lambda e, c=c, col=col, bq=bq: e.matmul(bq[:, col:col + 1], lhsT=sqt[:, c, :], rhs=ones1[:, 0:1], start=(c % 4 == 0), stop=(c % 4 == 3)),
                          reads=["sqt", "ones1"], writes=["bk4"])
                ms2 = small[:, 16 + xi * 4:16 + xi * 4 + 2]; rs2 = small[:, 16 + xi * 4 + 2:16 + xi * 4 + 4]
                P.add("dve", lambda e, ms2=ms2, bq=bq: e.tensor_scalar(out=ms2, in0=bq[:, 0:2], scalar1=1.0 / 512, scalar2=EPS, op0=ALU.mult, op1=ALU.add),
                      reads=["bk4"], writes=["ms2_%d" % xi])
                P.add("pool", lambda e, ms2=ms2, rs2=rs2: e.tensor_tensor(out=rs2, in0=ms2, in1=mhalf[:, 0:2], op=ALU.pow),
                      reads=["ms2_%d" % xi, "mhalf"], writes=["rs2_%d" % xi])
                ht = acc[xi][:, 0:D]
                for half in range(2):
                    hs_ = slice(half * 512, (half + 1) * 512)
                    bA = [bP[0], bP[1]][half]; bB = [bS[0], bS[1]][half]
                    for c in range(4):
                        P.add("pe", lambda e, c=c, bA=bA, tsl_=tsl_, hs_=hs_: e.matmul(bA[:, 0:512], lhsT=yT[:, c, tsl_], rhs=wout_bf[:, c, hs_], start=(c == 0), stop=(c == 3)),
                              reads=ytoks + ["wout", "V0", "V1", "V2"], writes=["bk%d" % half])
                    for c in range(4, 8):
                        P.add("pe", lambda e, c=c, bB=bB, tsl_=tsl_, hs_=hs_: e.matmul(bB[:, 0:512], lhsT=yT[:, c, tsl_], rhs=wout_bf[:, c, hs_], start=(c == 4), stop=(c == 7)),
                              reads=ytoks + ["wout", "V0", "V1", "V2"], writes=["bk%d" % (2 + half)])
                    P.add("dve", lambda e, ht=ht, hs_=hs_, bA=bA, rs2=rs2, xtile=xtile: e.scalar_tensor_tensor(out=ht[:, hs_], in0=bA[:, 0:512], scalar=rs2[:, 0:1], in1=xtile[:, hs_], op0=ALU.mult, op1=ALU.add),
                          reads=["bk%d" % half, "rs2_%d" % xi, "xt%d_src" % xi], writes=["acc%d" % xi])
                    P.add("dve", lambda e, ht=ht, hs_=hs_, bB=bB, rs2=rs2: e.scalar_tensor_tensor(out=ht[:, hs_], in0=bB[:, 0:512], scalar=rs2[:, 1:2], in1=ht[:, hs_], op0=ALU.mult, op1=ALU.add),
                          reads=["bk%d" % (2 + half), "rs2_%d" % xi, "acc%d" % xi], writes=["acc%d" % xi])
                P.add("sp", lambda e, ht=ht, row0=row0: e.dma_start(out=hs_d[row0:row0 + 128, :], in_=ht),
                      reads=["acc%d" % xi], writes=["hs%d" % (row0 // 128)], dma="hst%d" % xi)

        stage('O')
        P.add("act", lambda e: e.activation(out=bar[:, 0:1], in_=mhalf[:, 0:1], func=AF.Copy), reads=["mhalf"], writes=["bar_act"])
        P.add("dve", lambda e: e.tensor_copy(out=bar[:, 1:2], in_=mhalf[:, 0:1]), reads=["mhalf"], writes=["bar_dve"])
        P.add("pool", lambda e: e.tensor_copy(out=bar[:, 2:3], in_=mhalf[:, 0:1]), reads=["mhalf"], writes=["bar_pool"])
        for en in ("pe", "act", "dve", "pool", "sp"):
            P.add(en, lambda e: None, reads=["bar_act", "bar_dve", "bar_pool"] + ["hs%d" % i_ for i_ in range(32)], waitonly=True)

        ptr[0] = persist_end
        wd_bf = alloc(32 * D, BF16, (32, D))
        hn2T = alloc(8 * 1024, BF16, (8, 1024))
        actT = alloc(32 * 1024, BF16, (32, 1024))
        wgu = [[alloc(8 * 256, BF16, (8, 256)) for _ in range(2)] for _ in range(3)]
        hbA = alloc(D); hbD = [alloc(D) for _ in range(2)]
        xsA = alloc(D, BF16); xsD = alloc(D, BF16)
        combT = alloc(1024, BF16)
        Cs = [alloc(512, BF16) for _ in range(2)]
        ssb = [alloc(512, BF16) for _ in range(2)]
        s2b = [alloc(512, BF16) for _ in range(2)]
        rt = alloc(960)
        comb_bf = alloc(8 * 16, BF16, (8, 16))
        small2 = alloc(32)

        bG = [banks[0], banks[1]]; bU = [banks[2], banks[3]]; bC = banks[4]; bD = [banks[5], banks[6]]

        for k in range(4):
            P.add("sp", lambda e, k=k: e.dma_start(out=wd_bf[:, 8 * k:8 * k + 8, :],
                                                   in_=wds_d[4 * k:4 * k + 4].rearrange("e (f p) n -> p (e f) n", p=128)),
                  reads=["cvd%d" % ee for ee in range(4 * k, 4 * k + 4)], writes=["wd%d" % k], dma="wd%d" % k)
        wd_toks = ["wd%d" % k for k in range(4)]

        def load_gu(n):
            if n >= 64:
                return
            ex_ = n % 16; wi_ = n % 3
            wgb_, wub_ = wgu[wi_]
            P.add("sp", lambda e: e.dma_start(out=wgb_, in_=wgs_d[ex_].rearrange("(c p) n -> p c n", p=128)),
                  reads=["cvg%d" % ex_], writes=["wg%d" % wi_], dma="wg%d" % wi_)
            P.add("sp", lambda e: e.dma_start(out=wub_, in_=wus_d[ex_].rearrange("(c p) n -> p c n", p=128)),
                  reads=["cvu%d" % ex_], writes=["wu%d" % wi_], dma="wu%d" % wi_)
        load_gu(0)
        load_gu(1)

        c2 = {"h": 0, "G": 0, "C": 0, "D": 0, "w": 0, "y": 0}
        h2_all = [("h2_%d_a" % st) for st in range(8)] + [("h2_%d_b" % st) for st in range(8)]

        hpool = [(hbA, "hbA"), (hbD[0], "hbD0"), (hbD[1], "hbD1")]
        xpool = [(xsA, "xsA"), (xsD, "xsD")]
        tbank2 = [(pb_t, "bk7"), (banks[2].bitcast(BF16), "bk2")]
        bD4 = [(banks[5], "bk5"), (banks[6], "bk6"), (banks[0], "bk0"), (banks[1], "bk1")]

        def prep_hn2(T, st):
            row0 = T * 1024 + st * 128
            if T == 0:
                htile, htok = hpool[st % 3]; xsb, xtok = xpool[st % 2]
            else:
                htile, htok = hpool[0]; xsb, xtok = xpool[0]
            si_ = c2["h"] % 4; c2["h"] += 1
            P.add("sp", lambda e: e.dma_start(out=htile, in_=hs_d[row0:row0 + 128, :]),
                  reads=["hs%d" % (row0 // 128)], writes=[htok], dma=htok)
            ss = small2[:, si_ * 4:si_ * 4 + 1]; ms = small2[:, si_ * 4 + 1:si_ * 4 + 2]; rstd = small2[:, si_ * 4 + 2:si_ * 4 + 3]
            P.add("act", lambda e: e.activation(out=xsb, in_=htile, func=AF.Square, accum_out=ss),
                  reads=[htok], writes=[xtok, "h_ss%d" % si_])
            P.add("dve", lambda e: e.tensor_scalar(out=ms, in0=ss, scalar1=1.0 / D, scalar2=EPS, op0=ALU.mult, op1=ALU.add),
                  reads=["h_ss%d" % si_], writes=["h_ms%d" % si_])
            P.add("pool", lambda e: e.tensor_tensor(out=rstd, in0=ms, in1=mhalf[:, 0:1], op=ALU.pow),
                  reads=["h_ms%d" % si_, "mhalf"], writes=["h_rstd%d" % si_])
            P.add("dve", lambda e: e.tensor_scalar(out=xsb, in0=htile, scalar1=rstd, scalar2=None, op0=ALU.mult),
                  reads=[htok, "h_rstd%d" % si_], writes=[xtok])
            return (xsb, xtok)

        def trans_hn2(ctx, T, st):
            xsb, xtok = ctx
            tbk, ttok = tbank2[st % 2]
            for c in range(8):
                P.add("pe", lambda e, c=c: e.transpose(tbk[:, c * 128:(c + 1) * 128], xsb[:, c * 128:(c + 1) * 128], ident),
                      reads=[xtok, "ident"], writes=[ttok])
            for c in range(8):
                dst = hn2T[:, c, st * 128:(st + 1) * 128]
                src = tbk[:, c * 128:(c + 1) * 128]
                if st % 2 == 0:
                    P.add("dve", lambda e, dst=dst, src=src, c=c: e.tensor_scalar(out=dst, in0=src, scalar1=gffn[:, c:c + 1], scalar2=None, op0=ALU.mult),
                          reads=[ttok, "vecs"], writes=["h2_%d_a" % st])
                else:
                    P.add("act", lambda e, dst=dst, src=src, c=c: e.activation(out=dst, in_=src, func=AF.Copy, scale=gffn[:, c:c + 1]),
                          reads=[ttok, "vecs"], writes=["h2_%d_b" % st])

        def emit_router(T):
            if True:
                for st in range(8):
                    for c in range(8):
                        P.add("pe", lambda e, st=st, c=c: e.matmul(bC[:, st * 32:st * 32 + 20], lhsT=hn2T[:, c, st * 128:(st + 1) * 128], rhs=wr_bf[:, c, :],
                                                                   start=(c == 0), stop=(c == 7)),
                              reads=["h2_%d_a" % st, "h2_%d_b" % st, "wr_bf"], writes=["bk4"])
                lg = rt[:, 0:160].rearrange("p (s n) -> p s n", s=8)
                bc3 = bC[:, 0:256].rearrange("p (s n) -> p s n", s=8)[:, :, 0:20]
                RT = "rt"
                r3 = lambda lo, n: rt[:, lo:lo + 8 * n].rearrange("p (s n) -> p s n", s=8)
                gl = lg[:, :, 0:4]; el = lg[:, :, 4:20]
                gmax = rt[:, 160:168]; g1 = r3(168, 4); gex = r3(200, 4); gsum = rt[:, 232:240]; gw = rt[:, 240:248]
                pen = r3(248, 16); ml = r3(376, 16); m1 = rt[:, 504:512]; m2 = rt[:, 512:520]; k1 = r3(520, 16)
                dm = rt[:, 648:656]; w1 = rt[:, 656:664]; w2 = rt[:, 664:672]; k2 = r3(672, 16); ml2 = r3(800, 16)
                bcast = lambda ap, n: ap.unsqueeze(2).to_broadcast([128, 8, n])
                P.add("dve", lambda e: e.tensor_tensor(out=lg, in0=bc3, in1=brep.unsqueeze(1).to_broadcast([128, 8, 20]), op=ALU.add),
                      reads=["bk4", "vecs"], writes=[RT])
                P.add("dve", lambda e: e.tensor_reduce(out=gmax, in_=gl, axis=AX.X, op=ALU.max), reads=[RT], writes=[RT + "a"])
                P.add("dve", lambda e: e.tensor_tensor(out=g1, in0=gl, in1=bcast(gmax, 4), op=ALU.is_equal), reads=[RT, RT + "a"], writes=[RT + "b"])
                P.add("dve", lambda e: e.tensor_tensor(out=gex, in0=gl, in1=bcast(gmax, 4), op=ALU.subtract), reads=[RT, RT + "a"], writes=[RT + "c"])
                P.add("act", lambda e: e.activation(out=gex, in_=gex, func=AF.Exp), reads=[RT + "c"], writes=[RT + "d"])
                P.add("dve", lambda e: e.tensor_reduce(out=gsum, in_=gex, axis=AX.X, op=ALU.add), reads=[RT + "d"], writes=[RT + "e"])
                P.add("dve", lambda e: e.reciprocal(out=gw, in_=gsum), reads=[RT + "e"], writes=[RT + "f"])
                P.add("dve", lambda e: e.tensor_scalar(out=pen.rearrange("p s (g n) -> p s g n", g=4), in0=g1.unsqueeze(3).to_broadcast([128, 8, 4, 4]),
                                                       scalar1=-1.0, scalar2=30000.0, op0=ALU.add, op1=ALU.mult), reads=[RT + "b"], writes=[RT + "g"])
                P.add("dve", lambda e: e.tensor_tensor(out=ml, in0=el, in1=pen, op=ALU.add), reads=[RT, RT + "g"], writes=[RT + "h"])
                P.add("dve", lambda e: e.tensor_reduce(out=m1, in_=ml, axis=AX.X, op=ALU.max), reads=[RT + "h"], writes=[RT + "i"])
                P.add("dve", lambda e: e.tensor_tensor(out=k1, in0=ml, in1=bcast(m1, 16), op=ALU.is_equal), reads=[RT + "h", RT + "i"], writes=[RT + "j"])
                P.add("dve", lambda e: e.scalar_tensor_tensor(out=ml2, in0=k1, scalar=-60000.0, in1=ml, op0=ALU.mult, op1=ALU.add), reads=[RT + "h", RT + "j"], writes=[RT + "k"])
                P.add("dve", lambda e: e.tensor_reduce(out=m2, in_=ml2, axis=AX.X, op=ALU.max), reads=[RT + "k"], writes=[RT + "l"])
                P.add("dve", lambda e: e.tensor_tensor(out=k2, in0=ml2, in1=bcast(m2, 16), op=ALU.is_equal), reads=[RT + "k", RT + "l"], writes=[RT + "m"])
                P.add("dve", lambda e: e.tensor_tensor(out=dm, in0=m2, in1=m1, op=ALU.subtract), reads=[RT + "l", RT + "i"], writes=[RT + "n"])
                P.add("act", lambda e: e.activation(out=dm, in_=dm, func=AF.Exp), reads=[RT + "n"], writes=[RT + "o"])
                P.add("dve", lambda e: e.tensor_scalar(out=dm, in0=dm, scalar1=1.0, scalar2=None, op0=ALU.add), reads=[RT + "o"], writes=[RT + "p"])
                P.add("dve", lambda e: e.reciprocal(out=w1, in_=dm), reads=[RT + "p"], writes=[RT + "q"])
                P.add("dve", lambda e: e.tensor_scalar(out=w2, in0=w1, scalar1=-1.0, scalar2=1.0, op0=ALU.mult, op1=ALU.add), reads=[RT + "q"], writes=[RT + "r"])
                P.add("dve", lambda e: e.tensor_tensor(out=w1, in0=w1, in1=gw, op=ALU.mult), reads=[RT + "q", RT + "r", RT + "f"], writes=[RT + "s"])
                P.add("dve", lambda e: e.tensor_tensor(out=w2, in0=w2, in1=gw, op=ALU.mult), reads=[RT + "r", RT + "f"], writes=[RT + "t"])
                P.add("dve", lambda e: e.tensor_tensor(out=k1, in0=k1, in1=bcast(w1, 16), op=ALU.mult), reads=[RT + "j", RT + "s", RT + "k"], writes=[RT + "u"])
                P.add("dve", lambda e: e.tensor_tensor(out=k2, in0=k2, in1=bcast(w2, 16), op=ALU.mult), reads=[RT + "m", RT + "t"], writes=[RT + "v"])
                P.add("dve", lambda e: e.tensor_tensor(out=comb_bf, in0=k1, in1=k2, op=ALU.add), reads=[RT + "u", RT + "v"], writes=["comb_bf"])
                for st in range(8):
                    P.add("pe", lambda e, st=st: e.transpose(pb_t[0:16, st * 128:(st + 1) * 128], comb_bf[:, st, :], ident),
                          reads=["comb_bf", "ident"] + h2_all, writes=["bk7"])
                P.add("dve", lambda e: e.tensor_copy(out=combT[0:16, :], in_=pb_t[0:16, 0:1024]), reads=["bk7"], writes=["combT"])


        def emit_experts(T):
            if True:
                for ex in range(16):
                    nflat = T * 16 + ex
                    wi = nflat % 3
                    wgb, wub = wgu[wi]
                    load_gu(nflat + 2)
                    for half in range(2):
                        hs_ = slice(half * 512, (half + 1) * 512)
                        ci = c2["C"] % 2; c2["C"] += 1
                        P.add("pe", lambda e, ex=ex, hs_=hs_: e.matmul(bC[:, 0:512], lhsT=sel[0:16, ex, :], rhs=combT[0:16, hs_], start=True, stop=True),
                              reads=["sel", "combT"], writes=["bk4"])
                        P.add("dve", lambda e, ci=ci: e.tensor_copy(out=Cs[ci], in_=bC[:, 0:512]), reads=["bk4"], writes=["Cs%d" % ci])
                        for fc in range(2):
                            gi = c2["G"] % 2; c2["G"] += 1
                            for c in range(8):
                                P.add("pe", lambda e, gi=gi, c=c, fc=fc, hs_=hs_, wgb=wgb: e.matmul(bG[gi][:, 0:512], lhsT=wgb[:, c, fc * 128:(fc + 1) * 128], rhs=hn2T[:, c, hs_],
                                                                                                  start=(c == 0), stop=(c == 7)),
                                      reads=["wg%d" % wi] + h2_all, writes=["bk%d" % gi])
                            for c in range(8):
                                P.add("pe", lambda e, gi=gi, c=c, fc=fc, hs_=hs_, wub=wub: e.matmul(bU[gi][:, 0:512], lhsT=wub[:, c, fc * 128:(fc + 1) * 128], rhs=hn2T[:, c, hs_],
                                                                                                  start=(c == 0), stop=(c == 7)),
                                      reads=["wu%d" % wi] + h2_all, writes=["bk%d" % (2 + gi)])
                            P.add("act", lambda e, gi=gi: e.activation(out=ssb[gi], in_=bG[gi][:, 0:512], func=AF.Silu), reads=["bk%d" % gi], writes=["ssb%d" % gi])
                            P.add("pool", lambda e, gi=gi, ci=ci: e.tensor_tensor(out=s2b[gi], in0=ssb[gi], in1=Cs[ci], op=ALU.mult),
                                  reads=["ssb%d" % gi, "Cs%d" % ci], writes=["s2b%d" % gi])
                            P.add("dve", lambda e, gi=gi, ex=ex, fc=fc, hs_=hs_: e.tensor_tensor(out=actT[:, ex * 2 + fc, hs_], in0=bU[gi][:, 0:512], in1=s2b[gi], op=ALU.mult),
                                  reads=["bk%d" % (2 + gi), "s2b%d" % gi], writes=["actT"])


        def down_mm(T, st):
            row0 = T * 1024 + st * 128
            hi = c2["y"] % 2; c2["y"] += 1
            htile, htok = hpool[1 + hi]
            P.add("sp", lambda e: e.dma_start(out=htile, in_=hs_d[row0:row0 + 128, :]),
                  reads=["hs%d" % (row0 // 128)], writes=[htok], dma=htok)
            for dh in range(2):
                bk_, btok = bD4[c2["D"] % 4]; c2["D"] += 1
                ds_ = slice(dh * 512, (dh + 1) * 512)
                for kk in range(32):
                    P.add("pe", lambda e, bk_=bk_, kk=kk, ds_=ds_: e.matmul(bk_[:, 0:512], lhsT=actT[:, kk, st * 128:(st + 1) * 128], rhs=wd_bf[:, kk, ds_],
                                                                          start=(kk == 0), stop=(kk == 31)),
                          reads=["actT"] + wd_toks, writes=[btok])
                P.add("dve", lambda e, bk_=bk_, ds_=ds_: e.tensor_tensor(out=htile[:, ds_], in0=bk_[:, 0:512], in1=htile[:, ds_], op=ALU.add),
                      reads=[btok, htok], writes=[htok])
            return (htile, htok, row0, hi)

        def down_fin(ctx):
            htile, htok, row0, yi = ctx
            junk, jtok = xpool[1]
            ss = small2[:, 16 + yi * 4:16 + yi * 4 + 1]; ms = small2[:, 16 + yi * 4 + 1:16 + yi * 4 + 2]; rstd = small2[:, 16 + yi * 4 + 2:16 + yi * 4 + 3]
            P.add("act", lambda e: e.activation(out=junk, in_=htile, func=AF.Square, accum_out=ss),
                  reads=[htok], writes=[jtok, "f_ss%d" % yi])
            P.add("dve", lambda e: e.tensor_scalar(out=ms, in0=ss, scalar1=1.0 / D, scalar2=EPS, op0=ALU.mult, op1=ALU.add),
                  reads=["f_ss%d" % yi], writes=["f_ms%d" % yi])
            P.add("pool", lambda e: e.tensor_tensor(out=rstd, in0=ms, in1=mhalf[:, 0:1], op=ALU.pow),
                  reads=["f_ms%d" % yi, "mhalf"], writes=["f_rstd%d" % yi])
            P.add("dve", lambda e: e.scalar_tensor_tensor(out=htile, in0=htile, scalar=rstd, in1=gfin, op0=ALU.mult, op1=ALU.mult),
                  reads=[htok, "f_rstd%d" % yi, "gfin"], writes=[htok])
            P.add("sp", lambda e: e.dma_start(out=y_d[row0:row0 + 128, :], in_=htile),
                  reads=[htok], dma="yst%d" % yi)

        for st in range(8):
            trans_hn2(prep_hn2(0, st), 0, st)
        emit_router(0)
        for T in range(4):
            emit_experts(T)
            for st in range(8):
                cx = prep_hn2(T + 1, st) if T < 3 else None
                dx = down_mm(T, st)
                if cx is not None:
                    trans_hn2(cx, T + 1, st)
                down_fin(dx)
            if T < 3:
                emit_router(T + 1)

    except StopBuild:
        pass

    sems = {}
    for en in Prog.ENGS:
        sems["eng:" + en] = es.enter_context(nc.semaphore("s_" + en))
    for sl in sorted(P.dma_slots):
        sems["dma:" + sl] = es.enter_context(nc.semaphore("d_" + sl))
    if os.environ.get('KMAXOPS'):
        P.ops = P.ops[:int(os.environ['KMAXOPS'])]
        for i_, o_ in enumerate(P.ops[-3:]):
            print('LASTOPS', o_.eng, o_.idx)
    body = P.emit(sems)
    if os.environ.get('KSTATS'):
        print('PROG stats', P.stats, 'ndma_slots', len(P.dma_slots))
    with nc.Block() as block:
        block.sync(body("sp"))
        block.tensor(body("pe"))
        block.scalar(body("act"))
        block.vector(body("dve"))
        block.gpsimd(body("pool"))
    es.close()
    return nc


def _dil_bias():
    out = np.full((128, 4 * 2 * 512 + 4 * 256), NEG, np.float32)
    k = np.arange(128)[:, None]; q = np.arange(128)[None, :]

    def f(delta, slope, d):
        return np.where(np.abs(delta) <= 64, -slope * d * np.abs(delta), NEG).astype(np.float32)
    for j in range(4):
        for hp in range(2):
            slope = 2.0 ** (-(2 * j + hp + 1))
            for pi, d in enumerate((1, 4)):
                c0 = (j * 2 + pi) * 512
                out[:, c0 + hp * 128:c0 + (hp + 1) * 128] = f(k - 64 - q, slope, d)
                out[:, c0 + 256 + hp * 128:c0 + 256 + (hp + 1) * 128] = f(k + 64 - q, slope, d)
            c0 = 4096 + j * 256
            out[:, c0 + hp * 128:c0 + (hp + 1) * 128] = f(k - q, slope, 16)
    return out


def _na_strips(rpb):
    kc = np.arange(64)[:, None]; qc = np.arange(64)[None, :]
    cs = np.clip(qc - 8, 0, 48)
    col_ok = (kc >= cs) & (kc < cs + 16)
    dcidx = np.clip(kc - qc + 15, 0, 30)
    out = np.full((4, 128, 2, 2, 896), NEG, np.float32)
    for h in range(8):
        j, hp = divmod(h, 2)
        for var in range(2):
            for i in range(2):
                for u in range(14):
                    dlt = 6 + i - u
                    ok = (-4 <= dlt <= 3) if var == 0 else (-7 <= dlt <= 7)
                    if not ok:
                        continue
                    blk = np.where(col_ok, rpb[h, dlt + 7][dcidx], NEG).astype(np.float32)
                    out[j, i * 64:(i + 1) * 64, hp, var, u * 64:(u + 1) * 64] = blk
    return out.reshape(4, 128, 4 * 896)


_NC_CACHE = {}


def kernel(x, norm_mix_g, w_in, rpb, g_out_dil, g_out_na, w_out, norm_ffn_g, w_group, b_group, w_router, b_router,
           w_gate, w_up, w_down, norm_final_g):
    f = lambda a: np.ascontiguousarray(np.asarray(a, dtype=np.float32))
    x = f(x).reshape(16 * S, D)
    col = lambda g: f(g).reshape(8, 128).T
    vecs = np.concatenate([col(norm_mix_g[0]), col(norm_ffn_g[0]),
                           col(np.concatenate([f(g_out_dil[0]), f(g_out_na[0])])),
                           np.broadcast_to(np.concatenate([f(b_group[0]), f(b_router[0])])[None, :], (128, 20))], axis=1)
    vecs = np.ascontiguousarray(vecs, dtype=np.float32)
    gfin = np.ascontiguousarray(np.broadcast_to(f(norm_final_g)[None, :], (128, D)))
    wr = np.ascontiguousarray(np.concatenate([f(w_group[0]), f(w_router[0])], axis=1))
    shared = {
        "w_in": f(w_in[0]), "w_out": f(w_out[0]), "w_gate": f(w_gate[0]), "w_up": f(w_up[0]), "w_down": f(w_down[0]),
        "wr": wr, "vecs": vecs, "gfin": gfin, "dilb": _dil_bias(), "nas": _na_strips(f(rpb[0])),
    }
    if "nc" not in _NC_CACHE:
        _NC_CACHE["nc"] = build_program()
    nc = _NC_CACHE["nc"]
    in_maps = []
    for i in range(NCORES):
        m = dict(shared)
        m["x"] = np.ascontiguousarray(x[i * TOK:(i + 1) * TOK])
        in_maps.append(m)
    res = run_bass_kernel_spmd(nc, in_maps, core_ids=list(range(NCORES)))
    y = np.concatenate([r["y"] for r in res.results], axis=0)
    if DEBUG:
        kernel.dbg = [r.get("dbg") for r in res.results]
    return y.reshape(16, S, D).astype(np.float32)
```

```python
import numpy as np
from contextlib import ExitStack
import concourse.bass as bass
import concourse.mybir as mybir
from concourse.bass_utils import run_bass_kernel_spmd

F32 = mybir.dt.float32
BF16 = mybir.dt.bfloat16
AF = mybir.ActivationFunctionType
ALU = mybir.AluOpType
AX = mybir.AxisListType

NCORES = 8
S = 2048
D = 1024
TOK = 4096
NEG = -30000.0
EPS = 1e-6
import os
DEBUG = bool(os.environ.get('KSTOP', ''))
STOP = os.environ.get('KSTOP', '')


class StopBuild(Exception):
    pass


DUMPS = {}


def stage(name):
    if STOP and name == STOP:
        if name in DUMPS:
            DUMPS[name]()
        raise StopBuild()


class Op:
    __slots__ = ("eng", "fn", "deps", "is_dma", "sem", "semval", "needs_signal", "sigcount", "eidx", "idx", "wo")


class Prog:
    ENGS = ("pe", "act", "dve", "pool", "sp")

    def __init__(self):
        self.ops = []
        self.last_writer = {}
        self.readers = {}
        self.eng_count = {e: 0 for e in self.ENGS}
        self.dma_slots = set()

    def add(self, eng, fn, reads=(), writes=(), dma=None, waitonly=False):
        op = Op()
        op.eng = eng; op.fn = fn; op.idx = len(self.ops)
        op.is_dma = dma is not None
        op.wo = waitonly
        op.sem = dma; op.semval = 0; op.needs_signal = False; op.sigcount = 0
        op.eidx = self.eng_count[eng]; self.eng_count[eng] += 1
        if dma is not None:
            self.dma_slots.add(dma)
        deps = {}
        for t in reads:
            w = self.last_writer.get(t)
            if w is not None:
                deps[w] = "raw"
        for t in writes:
            w = self.last_writer.get(t)
            if w is not None and w not in deps:
                deps[w] = "waw"
            for r in self.readers.get(t, ()):
                if r not in deps and r != op.idx:
                    deps[r] = "war"
        for t in (() if waitonly else reads):
            lst = self.readers.setdefault(t, [])
            if not op.is_dma:
                lst[:] = [r for r in lst if self.ops[r].is_dma or self.ops[r].eng != eng]
            lst.append(op.idx)
        for t in writes:
            self.last_writer[t] = op.idx
            self.readers[t] = []
        op.deps = deps
        self.ops.append(op)
        return op

    def _need_wait(self, op, p, kind):
        if p.is_dma:
            return True
        if p.eng != op.eng:
            return True
        if op.is_dma:
            return True
        if p.eng == "pe":
            return False
        if kind == "raw" and ((op.eidx - p.eidx) <= 3 or p.eng == "pool"):
            return True
        return False

    def emit(self, sem_ctx):
        ops = self.ops
        for op in ops:
            for d, kind in op.deps.items():
                p = ops[d]
                if (not p.is_dma) and self._need_wait(op, p, kind):
                    p.needs_signal = True
        if os.environ.get('KALLSIG'):
            for op in ops:
                if not op.is_dma and op.fn(None) if False else (not op.is_dma and not getattr(op, "wo", False)):
                    op.needs_signal = True
        cnt = {e: 0 for e in self.ENGS}
        dcnt = {}
        for op in ops:
            if op.is_dma:
                dcnt[op.sem] = dcnt.get(op.sem, 0) + 16
                op.semval = dcnt[op.sem]
            elif op.needs_signal:
                cnt[op.eng] += 1
                op.sigcount = cnt[op.eng]
        by_eng = {e: [o for o in ops if o.eng == e] for e in self.ENGS}
        self.stats = (dict(cnt), {e: len(v) for e, v in by_eng.items()})

        def body(ename):
            def run(eng):
                waited = {}
                for op in by_eng[ename]:
                    for d, kind in op.deps.items():
                        p = ops[d]
                        if not self._need_wait(op, p, kind):
                            continue
                        if p.is_dma:
                            key = "dma:" + p.sem; val = p.semval
                        else:
                            key = "eng:" + p.eng; val = p.sigcount
                        if waited.get(key, 0) >= val:
                            continue
                        waited[key] = val
                        eng.wait_ge(sem_ctx[key], val)
                    ins = op.fn(eng)
                    if op.is_dma:
                        ins.then_inc(sem_ctx["dma:" + op.sem], 16)
                    elif op.needs_signal:
                        ins.then_inc(sem_ctx["eng:" + ename], 1)
                for op in by_eng[ename]:
                    if op.is_dma:
                        key = "dma:" + op.sem
                        if waited.get(key, 0) < dcnt[op.sem]:
                            waited[key] = dcnt[op.sem]
                            eng.wait_ge(sem_ctx[key], dcnt[op.sem])
            return run
        return body


def tsl(r, d, p0, n):
    return slice(r + d * p0, r + d * (p0 + n - 1) + 1, d)


def build_program():
    nc = bass.Bass("TRN2", target_bir_lowering=False)
    dt_in = lambda n, s, dt=F32: nc.dram_tensor(n, s, dt, kind="ExternalInput").ap()
    x_d = dt_in("x", [TOK, D])
    win_d = dt_in("w_in", [D, 3072])
    wout_d = dt_in("w_out", [D, D])
    wg_d = dt_in("w_gate", [16, D, 256])
    wu_d = dt_in("w_up", [16, D, 256])
    wd_d = dt_in("w_down", [16, 256, D])
    wr_d = dt_in("wr", [D, 20])
    vec_d = dt_in("vecs", [128, 24 + 20])
    gfin_d = dt_in("gfin", [128, D])
    gmixr_d = dt_in("gmixr", [128, D])
    dilb_d = dt_in("dilb", [128, 5120])
    nas_d = dt_in("nas", [4, 128, 4 * 896])
    y_d = nc.dram_tensor("y", [TOK, D], F32, kind="ExternalOutput").ap()
    hs_d = nc.dram_tensor("hscr", [TOK, D], F32, kind="Internal").ap()
    wgs_d = nc.dram_tensor("wg_bf", [16, D, 256], BF16, kind="Internal").ap()
    wus_d = nc.dram_tensor("wu_bf", [16, D, 256], BF16, kind="Internal").ap()
    wds_d = nc.dram_tensor("wd_bf", [16, 256, D], BF16, kind="Internal").ap()
    dbg_d = None
    if DEBUG:
        dbg_d = nc.dram_tensor("dbg", [128, 8 * 2048], F32, kind="ExternalOutput").ap()

    P = Prog()
    es = ExitStack()
    ARENA = 53200
    arena = es.enter_context(nc.sbuf_tensor("arena", [128, ARENA], F32))
    arena_bf = arena.bitcast(BF16)
    ptr = [0]

    def alloc(ncols, dt=F32, shape=None):
        n32 = ncols if dt == F32 else (ncols + 1) // 2
        a = ptr[0]
        ptr[0] += n32
        assert ptr[0] <= ARENA, ("SBUF overflow", ptr[0])
        if dt == F32:
            ap = arena[:, a:a + ncols]
        else:
            ap = arena_bf[:, 2 * a:2 * a + ncols]
        if shape is not None:
            names = " ".join("d%d" % i for i in range(len(shape)))
            kw = {"d%d" % i: shape[i] for i in range(len(shape))}
            ap = ap.rearrange("p (%s) -> p %s" % (names, names), **kw)
        return ap

    pb_t = es.enter_context(nc.psum_tensor("pb_t", [128, 1024], BF16))
    banks = [es.enter_context(nc.psum_tensor("bk%d" % i, [128, 512], F32)) for i in range(7)]
    tbank = [(pb_t, "bk7"), (banks[6].bitcast(BF16), "bk6")]

    ident = alloc(128, BF16)
    sel = alloc(16 * 128, BF16, (16, 128))
    vecs = alloc(44)
    gfin = alloc(D)
    wr_bf = alloc(8 * 20, BF16, (8, 20))
    mhalf = alloc(4)
    ones1 = alloc(2, BF16)
    bar = alloc(4)
    persist_end = ptr[0]

    try:
        P.add("pool", lambda e: e.memset(ident, 0.0), writes=["ident"])
        P.add("pool", lambda e: e.affine_select(out=ident, in_=ident, pattern=[[-1, 128]], compare_op=ALU.not_equal,
                                                fill=1.0, base=0, channel_multiplier=1), reads=["ident"], writes=["ident"])
        P.add("pool", lambda e: e.memset(sel, 0.0), writes=["sel"])
        P.add("pool", lambda e: e.affine_select(out=sel[0:16], in_=sel[0:16], pattern=[[-1, 16], [0, 128]],
                                                compare_op=ALU.not_equal, fill=1.0, base=0, channel_multiplier=1),
              reads=["sel"], writes=["sel"])
        P.add("pool", lambda e: e.memset(mhalf, -0.5), writes=["mhalf"])
        P.add("pool", lambda e: e.memset(ones1, 1.0), writes=["ones1"])
        P.add("sp", lambda e: e.dma_start(out=vecs, in_=vec_d), writes=["vecs"], dma="c0")
        P.add("sp", lambda e: e.dma_start(out=gfin, in_=gfin_d), writes=["gfin"], dma="c1")
        P.add("pool", lambda e: e.dma_start(out=wr_bf, in_=wr_d.rearrange("(c p) n -> p c n", p=128)),
              writes=["wr_bf"], dma="c2")
        gmix = vecs[:, 0:8]; gffn = vecs[:, 8:16]; gout = vecs[:, 16:24]; brep = vecs[:, 24:44]

        def rms_rstd(src, ss, ms, rstd, junk, tag, ncols=D):
            P.add("act", lambda e: e.activation(out=junk, in_=src, func=AF.Square, accum_out=ss),
                  reads=[tag + "_src"], writes=[tag + "_junk", tag + "_ss"])
            P.add("dve", lambda e: e.tensor_scalar(out=ms, in0=ss, scalar1=1.0 / ncols, scalar2=EPS, op0=ALU.mult, op1=ALU.add),
                  reads=[tag + "_ss"], writes=[tag + "_ms"])
            P.add("pool", lambda e: e.tensor_tensor(out=rstd, in0=ms, in1=mhalf[:, 0:1], op=ALU.pow),
                  reads=[tag + "_ms", "mhalf"], writes=[tag + "_rstd"])

        hnT = alloc(8 * S, BF16, (8, S))
        yT = alloc(8 * S, BF16, (8, S))
        dilb = alloc(5120, BF16)
        wblk = [[alloc(8 * 128, BF16, (8, 128)) for _ in range(3)] for _ in range(2)]
        QT = [alloc(2 * S, BF16, (2, S)) for _ in range(2)]
        KT = [alloc(S, BF16) for _ in range(2)]
        Vt_raw = alloc(3 * 16 * 256, BF16)
        Vt = Vt_raw.rearrange("p (l t h c) -> p l t h c", l=3, t=16, h=2, c=128)
        wout_bf = Vt_raw[:, 0:8 * D].rearrange("p (c n) -> p c n", c=8)
        acc2 = alloc(2 * S, F32, (2, S))
        acc = [acc2[:, 0, :], acc2[:, 1, :]]
        VTb = alloc(S, BF16)
        gmixr = alloc(D)
        rden = alloc(S)
        NS = int(os.environ.get('KNS', '3')); NO = int(os.environ.get('KNO', '2'))
        PT = alloc(NS * 512, BF16)
        nastrip = alloc(4 * 896, BF16, (2, 2, 896))
        xs = [alloc(D, BF16) for _ in range(3)]
        xt = [alloc(D) for _ in range(3)]
        sqt = alloc(8 * 128, BF16, (8, 128))
        small = alloc(64)
        p1_end = ptr[0]


        def dump(items):
            col = [0]
            for ap, toks in items:
                n = ap.shape[-1] if len(ap.shape) == 2 else None
                assert n is not None
                a = col[0]; col[0] += n
                P.add("pool", lambda e, ap=ap, a=a, n=n: e.dma_start(out=dbg_d[0:ap.shape[0], a:a + n], in_=ap, max_dma_last_dim=2048),
                      reads=toks, dma="dbg")
        alltok = lambda: list(P.last_writer.keys())
        DUMPS['H0'] = lambda: dump([(hnT[:, c, :], alltok()) for c in range(8)])
        DUMPS['I0_1'] = lambda: dump([(QT[0], alltok()), (KT[0], alltok()), (acc[0], alltok()), (acc[1], alltok()), (yT[:, 0, :], alltok()),
                                      (Vt_raw[:, 0:4096], alltok())])
        DUMPS['I0_5'] = lambda: dump([(QT[0], alltok()), (KT[0], alltok()), (acc[0], alltok()), (acc[1], alltok()), (yT[:, 4, :], alltok()),
                                      (Vt_raw[:, 0:4096], alltok())])
        for k_ in (2, 3, 4, 6, 7):
            DUMPS['I0_%d' % k_] = lambda: dump([(yT[:, c, :], alltok()) for c in range(8)])
        if os.environ.get('KD2'):
            DUMPS['I0_2'] = lambda: dump([(QT[0], alltok()), (KT[0], alltok()), (QT[1], alltok()), (KT[1], alltok()), (wblk[0][0][:, 0, :], alltok()), (wblk[0][2][:, 0, :], alltok()), (wblk[1][0][:, 0, :], alltok())])
        DUMPS['P0'] = lambda: dump([(yT[:, c, :], alltok()) for c in range(8)])
        bP = [banks[0], banks[1]]
        bPbf = [banks[0].bitcast(BF16), banks[1].bitcast(BF16)]
        bS = [banks[2], banks[3], banks[4], banks[0], banks[1]][:NS]
        SBK = [2, 3, 4, 0, 1]
        bO = [banks[5], banks[6], pb_t.bitcast(F32)][:NO]

        P.add("pool", lambda e: e.dma_start(out=dilb, in_=dilb_d, max_dma_last_dim=4096), writes=["dilb"], dma="c3")
        if DEBUG:
            P.add("pool", lambda e: e.memset(yT, 0.0), writes=["yT%d_%d" % (c, hp) for c in range(8) for hp in range(2)])

        for st_ in range(2):
            P.add("pool", lambda e, st_=st_: e.memset(QT[st_], 0.0), writes=["QT%d" % st_])
        P.add("sp", lambda e: e.dma_start(out=gmixr, in_=gmixr_d), writes=["gmixr"], dma="c4")
        ctr = {"S": 0, "O": 0, "P": 0, "x": 0}

        for s in range(2):
            tb = s * S
            def h_prep(tt):
                xi = ctr["x"] % 3; ctr["x"] += 1
                xtile = xt[xi]; xsb = xs[xi]
                row0 = tb + tt * 128
                P.add("sp", lambda e: e.dma_start(out=xtile, in_=x_d[row0:row0 + 128, :]),
                      writes=["xt%d_src" % xi], dma="x%d" % xi)
                ss = small[:, xi * 4:xi * 4 + 1]; ms = small[:, xi * 4 + 1:xi * 4 + 2]; rstd = small[:, xi * 4 + 2:xi * 4 + 3]
                P.add("act", lambda e: e.activation(out=xsb, in_=xtile, func=AF.Square, accum_out=ss),
                      reads=["xt%d_src" % xi], writes=["xs%d" % xi, "xt%d_ss" % xi])
                P.add("dve", lambda e: e.tensor_scalar(out=ms, in0=ss, scalar1=1.0 / D, scalar2=EPS, op0=ALU.mult, op1=ALU.add),
                      reads=["xt%d_ss" % xi], writes=["xt%d_ms" % xi])
                P.add("pool", lambda e: e.tensor_tensor(out=rstd, in0=ms, in1=mhalf[:, 0:1], op=ALU.pow),
                      reads=["xt%d_ms" % xi, "mhalf"], writes=["xt%d_rstd" % xi])
                P.add("dve", lambda e: e.scalar_tensor_tensor(out=xsb, in0=xtile, scalar=rstd, in1=gmixr, op0=ALU.mult, op1=ALU.mult),
                      reads=["xt%d_src" % xi, "xt%d_rstd" % xi, "gmixr"], writes=["xs%d" % xi])
                return (xsb, xi)

            def h_trans(ctx, tt):
                xsb, xi = ctx
                tbk, ttok = tbank[tt % 2]
                for c in range(8):
                    P.add("pe", lambda e, c=c: e.transpose(tbk[:, c * 128:(c + 1) * 128], xsb[:, c * 128:(c + 1) * 128], ident),
                          reads=["xs%d" % xi, "ident"], writes=[ttok])
                dst = hnT[:, :, tt * 128:(tt + 1) * 128]
                src = tbk[:, 0:1024].rearrange("p (c q) -> p c q", c=8)
                if tt % 2 == 0:
                    P.add("dve", lambda e: e.tensor_copy(out=dst, in_=src), reads=[ttok], writes=["hn%d_a" % tt])
                else:
                    P.add("act", lambda e: e.activation(out=dst, in_=src, func=AF.Copy), reads=[ttok], writes=["hn%d_b" % tt])

            hctx = [h_prep(0), h_prep(1)]
            for tt in range(16):
                if tt + 2 < 16:
                    hctx.append(h_prep(tt + 2))
                h_trans(hctx[tt], tt)

            def hn_tokens(tiles):
                out = []
                for t in tiles:
                    out += ["hn%d_a" % t, "hn%d_b" % t]
                return out

            stage('H%d' % s)
            for lay_ in range(3):
                P.add("pool", lambda e, lay_=lay_: e.memset(Vt[:, lay_, :, :, 64:128], 1.0), writes=["V%d" % lay_])
            for item in range(8):
                stage('I%d_%d' % (s, item))
                if os.environ.get('KBAR'):
                    P.add("act", lambda e: e.activation(out=bar[:, 0:1], in_=mhalf[:, 0:1], func=AF.Copy), reads=["mhalf"], writes=["bar_act"])
                    P.add("dve", lambda e: e.tensor_copy(out=bar[:, 1:2], in_=mhalf[:, 0:1]), reads=["mhalf"], writes=["bar_dve"])
                    P.add("pool", lambda e: e.tensor_copy(out=bar[:, 2:3], in_=mhalf[:, 0:1]), reads=["mhalf"], writes=["bar_pool"])
                    for en in ("pe", "act", "dve", "pool", "sp"):
                        P.add(en, lambda e: None, reads=["bar_act", "bar_dve", "bar_pool"], waitonly=True)
                if os.environ.get('KSNAP') and item == 1:
                    P.add("pool", lambda e: e.tensor_copy(out=yT[:, 7, :], in_=yT[:, 0, :]), reads=["yT0_0", "yT0_1"], writes=["yT7_0", "yT7_1"])
                    P.add("pool", lambda e: e.tensor_copy(out=yT[:, 6, :], in_=acc[0][:, :]), reads=["acc0"], writes=["yT6_0", "yT6_1"])
                if s == 0:
                    for ex_ in (2 * item, 2 * item + 1):
                        for nm_, src_, dst_ in (("g", wg_d, wgs_d), ("u", wu_d, wus_d), ("d", wd_d, wds_d)):
                            P.add("pool", lambda e, src_=src_, dst_=dst_, ex_=ex_: e.dma_start(out=dst_[ex_], in_=src_[ex_], max_dma_last_dim=4096),
                                  writes=["cv%s%d" % (nm_, ex_)], dma="cv%s%d" % (nm_, ex_))
                is_dil = item < 4
                j = item % 4
                st = item % 2
                colbase = 0 if is_dil else 1536
                wq, wk, wv = wblk[st]
                for wi, (wb, off) in enumerate(((wq, 0), (wk, 512), (wv, 1024))):
                    c0 = colbase + off + j * 128
                    P.add("pool", lambda e, wb=wb, c0=c0: e.dma_start(out=wb, in_=win_d.rearrange("(c p) n -> p c n", p=128)[:, :, c0:c0 + 128]),
                          writes=["wb%d_%d" % (st, wi)], dma="wb%d_%d" % (st, wi))
                if not is_dil:
                    P.add("pool", lambda e, j=j: e.dma_start(out=nastrip, in_=nas_d[j].rearrange("p (h v n) -> p h v n", h=2, v=2), max_dma_last_dim=3584),
                          writes=["nastrip"], dma="nas")
                for which, (wb, dstT, scl) in enumerate(((wq, QT[st], 0.125), (wk, KT[st], 1.0))):
                    for tc in range(4):
                        bi = ctr["P"] % 2; ctr["P"] += 1
                        bk = bP[bi]
                        for c in range(8):
                            P.add("pe", lambda e, bk=bk, wb=wb, c=c, tc=tc: e.matmul(bk[:, 0:512], lhsT=wb[:, c, :], rhs=hnT[:, c, tc * 512:(tc + 1) * 512],
                                                                                     start=(c == 0), stop=(c == 7)),
                                  reads=["wb%d_%d" % (st, which)] + hn_tokens(range(4 * tc, 4 * tc + 4)), writes=["bk%d" % bi])
                        dst = dstT[:, tc * 512:(tc + 1) * 512] if which == 1 else None
                        tokn = ("QT%d" if which == 0 else "KT%d") % st
                        if which == 0:
                            for hp_ in range(2):
                                rw = slice(hp_ * 64, hp_ * 64 + 64)
                                P.add("dve", lambda e, bk=bk, rw=rw, hp_=hp_, tc=tc, dstT=dstT: e.tensor_scalar(out=dstT[rw, hp_, tc * 512:(tc + 1) * 512], in0=bk[rw, 0:512], scalar1=0.125, scalar2=None, op0=ALU.mult),
                                      reads=["bk%d" % bi], writes=[tokn])
                        else:
                            P.add("dve", lambda e, dst=dst, bk=bk: e.tensor_copy(out=dst, in_=bk[:, 0:512]),
                                  reads=["bk%d" % bi], writes=[tokn])
                for tc in range(4):
                    bi = ctr["P"] % 2; ctr["P"] += 1
                    bk = bP[bi]
                    for c in range(8):
                        P.add("pe", lambda e, bk=bk, wv=wv, c=c, tc=tc: e.matmul(bk[:, 0:512], lhsT=wv[:, c, :], rhs=hnT[:, c, tc * 512:(tc + 1) * 512],
                                                                                 start=(c == 0), stop=(c == 7)),
                              reads=["wb%d_2" % st] + hn_tokens(range(4 * tc, 4 * tc + 4)), writes=["bk%d" % bi])
                    dst = VTb[:, tc * 512:(tc + 1) * 512]
                    P.add("dve", lambda e, dst=dst, bk=bk: e.tensor_copy(out=dst, in_=bk[:, 0:512]),
                          reads=["bk%d" % bi], writes=["VT"])
                layouts = ((0, 1), (1, 4), (2, 16)) if is_dil else ((0, 1),)
                for (lay, d) in layouts:
                    L = S // d; nt = L // 128
                    tiles = [(r, m) for r in range(d) for m in range(nt)]
                    for g8 in range(2):
                        bi = ctr["P"] % 2; ctr["P"] += 1
                        bkb = bPbf[bi]
                        for q in range(8):
                            r, m = tiles[g8 * 8 + q]
                            tsl_ = tsl(r, d, 128 * m, 128)
                            P.add("pe", lambda e, bkb=bkb, q=q, tsl_=tsl_: e.transpose(bkb[:, q * 128:(q + 1) * 128], VTb[:, tsl_], ident),
                                  reads=["VT", "ident"], writes=["bk%d" % bi])
                        dst = Vt[:, lay, g8 * 8:(g8 + 1) * 8, :, 0:64]
                        src = bkb[:, 0:1024].rearrange("p (t h c) -> p t h c", t=8, h=2)
                        P.add("dve", lambda e, dst=dst, src=src: e.tensor_copy(out=dst, in_=src),
                              reads=["bk%d" % bi], writes=["V%d" % lay])

                tasks = []
                qz = QT[st]; kTp = KT[st]
                ATOK = ["acc0", "acc1"]
                v2 = lambda ap, lo, n: ap[:, lo:lo + 2 * n].rearrange("p (h q) -> p h q", h=2)
                if is_dil:
                    for pi, d in enumerate((1, 4)):
                        L = S // d; nt = L // 128
                        biasP = dilb[:, (j * 2 + pi) * 512:(j * 2 + pi + 1) * 512]
                        for r in range(d):
                            for b in range(nt + 1):
                                def s_part(b=b, r=r, d=d, nt=nt, biasP=biasP, qz=qz, kTp=kTp, st=st):
                                    si = ctr["S"] % NS; ctr["S"] += 1
                                    bkS = bS[si]; pt = PT[:, si * 512:(si + 1) * 512]
                                    tk = "bk%d" % SBK[si]
                                    if b == 0:
                                        ov = v2(bkS, 256, 128)[:, :, 64:128]; bv = v2(biasP, 256, 128)[:, :, 64:128]; pv_ = v2(pt, 256, 128)[:, :, 64:128]
                                        P.add("pe", lambda e: e.matmul(ov, lhsT=ident, rhs=bv, start=True, stop=False), reads=["ident", "dilb"], writes=[tk])
                                        P.add("pe", lambda e: e.matmul(ov, lhsT=kTp[:, tsl(r, d, 0, 128)], rhs=qz[:, :, tsl(r, d, 0, 64)], start=False, stop=True),
                                              reads=["QT%d" % st, "KT%d" % st], writes=[tk])
                                        P.add("act", lambda e: e.activation(out=pv_, in_=ov, func=AF.Exp), reads=[tk], writes=["PT%d" % si])
                                    elif b == nt:
                                        ov = v2(bkS, 0, 128)[:, :, 0:64]; bv = v2(biasP, 0, 128)[:, :, 0:64]; pv_ = v2(pt, 0, 128)[:, :, 0:64]
                                        P.add("pe", lambda e: e.matmul(ov, lhsT=ident, rhs=bv, start=True, stop=False), reads=["ident", "dilb"], writes=[tk])
                                        P.add("pe", lambda e: e.matmul(ov, lhsT=kTp[:, tsl(r, d, 128 * (nt - 1), 128)], rhs=qz[:, :, tsl(r, d, 128 * nt - 64, 64)], start=False, stop=True),
                                              reads=["QT%d" % st, "KT%d" % st], writes=[tk])
                                        P.add("act", lambda e: e.activation(out=pv_, in_=ov, func=AF.Exp), reads=[tk], writes=["PT%d" % si])
                                    else:
                                        qv = qz[:, :, tsl(r, d, 128 * b - 64, 128)]
                                        P.add("pe", lambda e: e.matmul(bkS[:, 0:512], lhsT=ident, rhs=biasP, start=True, stop=False), reads=["ident", "dilb"], writes=[tk])
                                        P.add("pe", lambda e: e.matmul(v2(bkS, 0, 128), lhsT=kTp[:, tsl(r, d, 128 * (b - 1), 128)], rhs=qv, start=False, stop=False),
                                              reads=["QT%d" % st, "KT%d" % st], writes=[tk])
                                        P.add("pe", lambda e: e.matmul(v2(bkS, 256, 128), lhsT=kTp[:, tsl(r, d, 128 * b, 128)], rhs=qv, start=False, stop=True),
                                              reads=["QT%d" % st, "KT%d" % st], writes=[tk])
                                        P.add("act", lambda e: e.activation(out=pt[:, 0:512], in_=bkS[:, 0:512], func=AF.Exp), reads=[tk], writes=["PT%d" % si])
                                    return si

                                def pv_part(si, b=b, r=r, d=d, nt=nt, pi=pi):
                                    oi = ctr["O"] % NO; ctr["O"] += 1
                                    bkO = bO[oi]; pt = PT[:, si * 512:(si + 1) * 512]
                                    tk = "bk%d" % (5 + oi)
                                    first = [True]
                                    for hp in range(2):
                                        def mm(o_, l_, r_):
                                            stt = first[0]; first[0] = False
                                            P.add("pe", lambda e: e.matmul(o_, lhsT=l_, rhs=r_, start=stt, stop=True), reads=["PT%d" % si, "V%d" % pi], writes=[tk])
                                        ob = hp * 128
                                        if b == 0:
                                            mm(bkO[:, ob + 64:ob + 128], Vt[:, pi, r * nt + 0, hp, :], pt[:, 256 + hp * 128 + 64:256 + hp * 128 + 128])
                                        elif b == nt:
                                            mm(bkO[:, ob:ob + 64], Vt[:, pi, r * nt + nt - 1, hp, :], pt[:, hp * 128:hp * 128 + 64])
                                        else:
                                            mm(bkO[:, ob:ob + 128], Vt[:, pi, r * nt + b - 1, hp, :], pt[:, hp * 128:hp * 128 + 128])
                                            mm(bkO[:, ob:ob + 128], Vt[:, pi, r * nt + b, hp, :], pt[:, 256 + hp * 128:256 + hp * 128 + 128])
                                    c_lo = 64 if b == 0 else 0
                                    c_hi = 64 if b == nt else 128
                                    p_lo = 128 * b - 64 + c_lo
                                    dsta = acc2[:, :, tsl(r, d, p_lo, c_hi - c_lo)]
                                    srca = v2(bkO, 0, 128)[:, :, c_lo:c_hi]
                                    if pi == 0:
                                        P.add("dve", lambda e: e.tensor_copy(out=dsta, in_=srca), reads=[tk], writes=ATOK)
                                    else:
                                        P.add("dve", lambda e: e.tensor_tensor(out=dsta, in0=srca, in1=dsta, op=ALU.add), reads=[tk] + ATOK, writes=ATOK)
                                tasks.append((s_part, pv_part))
                    bias3P = dilb[:, 4096 + j * 256:4096 + (j + 1) * 256]
                    for r0 in range(0, 16, 2):
                        def s_part(r0=r0, bias3P=bias3P, qz=qz, kTp=kTp, st=st):
                            si = ctr["S"] % NS; ctr["S"] += 1
                            bkS = bS[si]; pt = PT[:, si * 512:(si + 1) * 512]
                            tk = "bk%d" % SBK[si]
                            for rr in range(2):
                                r_ = r0 + rr
                                P.add("pe", lambda e, rr=rr: e.matmul(bkS[:, rr * 256:(rr + 1) * 256], lhsT=ident, rhs=bias3P, start=True, stop=False),
                                      reads=["ident", "dilb"], writes=[tk])
                                P.add("pe", lambda e, rr=rr, r_=r_: e.matmul(v2(bkS, rr * 256, 128), lhsT=kTp[:, tsl(r_, 16, 0, 128)], rhs=qz[:, :, tsl(r_, 16, 0, 128)], start=False, stop=True),
                                      reads=["QT%d" % st, "KT%d" % st], writes=[tk])
                            P.add("act", lambda e: e.activation(out=pt[:, 0:512], in_=bkS[:, 0:512], func=AF.Exp), reads=[tk], writes=["PT%d" % si])
                            return si

                        def pv_part(si, r0=r0):
                            oi = ctr["O"] % NO; ctr["O"] += 1
                            bkO = bO[oi]; pt = PT[:, si * 512:(si + 1) * 512]
                            tk = "bk%d" % (5 + oi)
                            for rr in range(2):
                                for hp in range(2):
                                    cs_ = slice(rr * 256 + hp * 128, rr * 256 + hp * 128 + 128)
                                    P.add("pe", lambda e, rr=rr, hp=hp, cs_=cs_: e.matmul(bkO[:, cs_], lhsT=Vt[:, 2, r0 + rr, hp, :], rhs=pt[:, cs_], start=(rr == 0 and hp == 0), stop=True),
                                          reads=["PT%d" % si, "V2"], writes=[tk])
                            dsta = acc2.rearrange("p h (q r) -> p h q r", r=16)[:, :, :, r0:r0 + 2]
                            srca = bkO[:, 0:512].rearrange("p (r h q) -> p h q r", r=2, h=2)
                            P.add("dve", lambda e: e.tensor_tensor(out=dsta, in0=srca, in1=dsta, op=ALU.add), reads=[tk] + ATOK, writes=ATOK)
                        tasks.append((s_part, pv_part))
                else:
                    for g in range(8):
                        if g == 0:
                            ms_, var = [0, 1, 2, 3], 1
                        elif g == 7:
                            ms_, var = [12, 13, 14, 15], 1
                        else:
                            ms_, var = list(range(2 * g - 2, 2 * g + 4)), 0
                        ostate = {}
                        for ki, m in enumerate(ms_):
                            def s_part(m=m, g=g, var=var, qz=qz, kTp=kTp, st=st):
                                si = ctr["S"] % NS; ctr["S"] += 1
                                bkS = bS[si]; pt = PT[:, si * 512:(si + 1) * 512]
                                tk = "bk%d" % SBK[si]
                                sft = 6 - (2 * m - 4 * g)
                                assert 0 <= sft and sft * 64 + 256 <= 896
                                P.add("pe", lambda e: e.matmul(v2(bkS, 0, 256), lhsT=ident, rhs=nastrip[:, :, var, sft * 64:sft * 64 + 256], start=True, stop=False),
                                      reads=["ident", "nastrip"], writes=[tk])
                                P.add("pe", lambda e: e.matmul(v2(bkS, 0, 256), lhsT=kTp[:, m * 128:(m + 1) * 128], rhs=qz[:, :, g * 256:(g + 1) * 256], start=False, stop=True),
                                      reads=["QT%d" % st, "KT%d" % st], writes=[tk])
                                P.add("act", lambda e: e.activation(out=pt[:, 0:512], in_=bkS[:, 0:512], func=AF.Exp), reads=[tk], writes=["PT%d" % si])
                                return si

                            def pv_part(si, m=m, ki=ki, g=g, ostate=ostate, nms=len(ms_)):
                                if ki == 0:
                                    ostate["oi"] = ctr["O"] % NO; ctr["O"] += 1
                                oi = ostate["oi"]
                                bkO = bO[oi]; pt = PT[:, si * 512:(si + 1) * 512]
                                tk = "bk%d" % (5 + oi)
                                for hp in range(2):
                                    P.add("pe", lambda e, hp=hp: e.matmul(bkO[:, hp * 256:(hp + 1) * 256], lhsT=Vt[:, 0, m, hp, :], rhs=pt[:, hp * 256:(hp + 1) * 256],
                                                                         start=(ki == 0 and hp == 0), stop=(ki == nms - 1)),
                                          reads=["PT%d" % si, "V0"], writes=[tk])
                                if ki == nms - 1:
                                    dsta = acc2[:, :, g * 256:(g + 1) * 256]
                                    P.add("dve", lambda e: e.tensor_copy(out=dsta, in_=v2(bkO, 0, 256)), reads=[tk], writes=ATOK)
                            tasks.append((s_part, pv_part))

                for hp in range(2):
                    def fin_part(_si, hp=hp, chunk=(j if is_dil else 4 + j)):
                        rows = slice(hp * 64, hp * 64 + 64)
                        P.add("act", lambda e: e.activation(out=rden[0:64, :], in_=acc2[64:128, hp, :], func=AF.Ln), reads=ATOK, writes=["rden"])
                        P.add("act", lambda e: e.activation(out=rden[0:64, :], in_=rden[0:64, :], func=AF.Exp, scale=-1.0), reads=["rden"], writes=["rden"])
                        P.add("dve", lambda e: e.tensor_tensor(out=yT[rows, chunk, :], in0=acc2[0:64, hp, :], in1=rden[0:64, :], op=ALU.mult),
                              reads=ATOK + ["rden"], writes=["yT%d_%d" % (chunk, hp)])
                    tasks.append((None, fin_part))

                LOOK = int(os.environ.get('KLOOK', '2'))
                sis = []
                for ti_, (sp_, pv_) in enumerate(tasks):
                    sis.append(sp_() if sp_ is not None else None)
                    if ti_ >= LOOK:
                        tasks[ti_ - LOOK][1](sis[ti_ - LOOK])
                for ti_ in range(max(0, len(tasks) - LOOK), len(tasks)):
                    tasks[ti_][1](sis[ti_])


            stage('P%d' % s)
            ytoks = ["yT%d_%d" % (c, hp) for c in range(8) for hp in range(2)]
            P.add("pool", lambda e: e.dma_start(out=wout_bf, in_=wout_d.rearrange("(c p) n -> p c n", p=128)),
                  writes=["V0", "V1", "V2", "wout"], dma="wout")
            for c in range(8):
                P.add("dve", lambda e, c=c: e.tensor_scalar(out=wout_bf[:, c, :], in0=wout_bf[:, c, :], scalar1=gout[:, c:c + 1], scalar2=None, op0=ALU.mult),
                      reads=["wout", "vecs"], writes=["wout"])
            for tt in range(16):
                row0 = tb + tt * 128
                tsl_ = slice(tt * 128, (tt + 1) * 128)
                xi = ctr["x"] % 2; ctr["x"] += 1
                xtile = xt[xi]
                P.add("sp", lambda e, xtile=xtile, row0=row0: e.dma_start(out=xtile, in_=x_d[row0:row0 + 128, :]),
                      writes=["xt%d_src" % xi], dma="x%d" % xi)
                P.add("pool", lambda e, tsl_=tsl_: e.tensor_tensor(out=sqt, in0=yT[:, :, tsl_], in1=yT[:, :, tsl_], op=ALU.mult),
                      reads=ytoks, writes=["sqt"])
                bq = bS[2]
                for c in range(8):
                    col = c // 4
                    P.add("pe", lambda e, c=c, col=col, bq=bq: e.matmul(bq[:, col:col + 1], lhsT=sqt[:, c, :], rhs=ones1[:, 0:1], start=(c % 4 == 0), stop=(c % 4 == 3)),
                          reads=["sqt", "ones1"], writes=["bk4"])
                ms2 = small[:, 16 + xi * 4:16 + xi * 4 + 2]; rs2 = small[:, 16 + xi * 4 + 2:16 + xi * 4 + 4]
                P.add("dve", lambda e, ms2=ms2, bq=bq: e.tensor_scalar(out=ms2, in0=bq[:, 0:2], scalar1=1.0 / 512, scalar2=EPS, op0=ALU.mult, op1=ALU.add),
                      reads=["bk4"], writes=["ms2_%d" % xi])
                P.add("pool", lambda e, ms2=ms2, rs2=rs2: e.tensor_tensor(out=rs2, in0=ms2, in1=mhalf[:, 0:2], op=ALU.pow),
                      reads=["ms2_%d" % xi, "mhalf"], writes=["rs2_%d" % xi])
                ht = acc[xi][:, 0:D]
                for half in range(2):
                    hs_ = slice(half * 512, (half + 1) * 512)
                    bA = [bP[0], bP[1]][half]; bB = [bS[0], bS[1]][half]
                    for c in range(4):
                        P.add("pe", lambda e, c=c, bA=bA, tsl_=tsl_, hs_=hs_: e.matmul(bA[:, 0:512], lhsT=yT[:, c, tsl_], rhs=wout_bf[:, c, hs_], start=(c == 0), stop=(c == 3)),
                              reads=ytoks + ["wout", "V0", "V1", "V2"], writes=["bk%d" % half])
                    for c in range(4, 8):
                        P.add("pe", lambda e, c=c, bB=bB, tsl_=tsl_, hs_=hs_: e.matmul(bB[:, 0:512], lhsT=yT[:, c, tsl_], rhs=wout_bf[:, c, hs_], start=(c == 4), stop=(c == 7)),
                              reads=ytoks + ["wout", "V0", "V1", "V2"], writes=["bk%d" % (2 + half)])
                    P.add("dve", lambda e, ht=ht, hs_=hs_, bA=bA, rs2=rs2, xtile=xtile: e.scalar_tensor_tensor(out=ht[:, hs_], in0=bA[:, 0:512], scalar=rs2[:, 0:1], in1=xtile[:, hs_], op0=ALU.mult, op1=ALU.add),
                          reads=["bk%d" % half, "rs2_%d" % xi, "xt%d_src" % xi], writes=["acc%d" % xi])
                    P.add("dve", lambda e, ht=ht, hs_=hs_, bB=bB, rs2=rs2: e.scalar_tensor_tensor(out=ht[:, hs_], in0=bB[:, 0:512], scalar=rs2[:, 1:2], in1=ht[:, hs_], op0=ALU.mult, op1=ALU.add),
                          reads=["bk%d" % (2 + half), "rs2_%d" % xi, "acc%d" % xi], writes=["acc%d" % xi])
                P.add("sp", lambda e, ht=ht, row0=row0: e.dma_start(out=hs_d[row0:row0 + 128, :], in_=ht),
                      reads=["acc%d" % xi], writes=["hs%d" % (row0 // 128)], dma="hst%d" % xi)

        stage('O')
        P.add("act", lambda e: e.activation(out=bar[:, 0:1], in_=mhalf[:, 0:1], func=AF.Copy), reads=["mhalf"], writes=["bar_act"])
        P.add("dve", lambda e: e.tensor_copy(out=bar[:, 1:2], in_=mhalf[:, 0:1]), reads=["mhalf"], writes=["bar_dve"])
        P.add("pool", lambda e: e.tensor_copy(out=bar[:, 2:3], in_=mhalf[:, 0:1]), reads=["mhalf"], writes=["bar_pool"])
        for en in ("pe", "act", "dve", "pool", "sp"):
            P.add(en, lambda e: None, reads=["bar_act", "bar_dve", "bar_pool"] + ["hs%d" % i_ for i_ in range(32)], waitonly=True)

        ptr[0] = persist_end
        wd_bf = alloc(32 * D, BF16, (32, D))
        hn2T = alloc(8 * 1024, BF16, (8, 1024))
        actT = alloc(32 * 1024, BF16, (32, 1024))
        wgu = [[alloc(8 * 256, BF16, (8, 256)) for _ in range(2)] for _ in range(3)]
        hbA = alloc(D); hbD = [alloc(D) for _ in range(2)]
        xsA = alloc(D, BF16); xsD = alloc(D, BF16)
        combT = alloc(1024, BF16)
        Cs = [alloc(512, BF16) for _ in range(2)]
        ssb = [alloc(512, BF16) for _ in range(2)]
        s2b = [alloc(512, BF16) for _ in range(2)]
        rt = alloc(960)
        comb_bf = alloc(8 * 16, BF16, (8, 16))
        small2 = alloc(32)

        bG = [banks[0], banks[1]]; bU = [banks[2], banks[3]]; bC = banks[4]; bD = [banks[5], banks[6]]

        for k in range(4):
            P.add("sp", lambda e, k=k: e.dma_start(out=wd_bf[:, 8 * k:8 * k + 8, :],
                                                   in_=wds_d[4 * k:4 * k + 4].rearrange("e (f p) n -> p (e f) n", p=128)),
                  reads=["cvd%d" % ee for ee in range(4 * k, 4 * k + 4)], writes=["wd%d" % k], dma="wd%d" % k)
        wd_toks = ["wd%d" % k for k in range(4)]

        def load_gu(n):
            if n >= 64:
                return
            ex_ = n % 16; wi_ = n % 3
            wgb_, wub_ = wgu[wi_]
            P.add("sp", lambda e: e.dma_start(out=wgb_, in_=wgs_d[ex_].rearrange("(c p) n -> p c n", p=128)),
                  reads=["cvg%d" % ex_], writes=["wg%d" % wi_], dma="wg%d" % wi_)
            P.add("sp", lambda e: e.dma_start(out=wub_, in_=wus_d[ex_].rearrange("(c p) n -> p c n", p=128)),
                  reads=["cvu%d" % ex_], writes=["wu%d" % wi_], dma="wu%d" % wi_)
        load_gu(0)
        load_gu(1)

        c2 = {"h": 0, "G": 0, "C": 0, "D": 0, "w": 0, "y": 0}
        h2_all = [("h2_%d_a" % st) for st in range(8)] + [("h2_%d_b" % st) for st in range(8)]

        hpool = [(hbA, "hbA"), (hbD[0], "hbD0"), (hbD[1], "hbD1")]
        xpool = [(xsA, "xsA"), (xsD, "xsD")]
        tbank2 = [(pb_t, "bk7"), (banks[2].bitcast(BF16), "bk2")]
        bD4 = [(banks[5], "bk5"), (banks[6], "bk6"), (banks[0], "bk0"), (banks[1], "bk1")]

        def prep_hn2(T, st):
            row0 = T * 1024 + st * 128
            if T == 0:
                htile, htok = hpool[st % 3]; xsb, xtok = xpool[st % 2]
            else:
                htile, htok = hpool[0]; xsb, xtok = xpool[0]
            si_ = c2["h"] % 4; c2["h"] += 1
            P.add("sp", lambda e: e.dma_start(out=htile, in_=hs_d[row0:row0 + 128, :]),
                  reads=["hs%d" % (row0 // 128)], writes=[htok], dma=htok)
            ss = small2[:, si_ * 4:si_ * 4 + 1]; ms = small2[:, si_ * 4 + 1:si_ * 4 + 2]; rstd = small2[:, si_ * 4 + 2:si_ * 4 + 3]
            P.add("act", lambda e: e.activation(out=xsb, in_=htile, func=AF.Square, accum_out=ss),
                  reads=[htok], writes=[xtok, "h_ss%d" % si_])
            P.add("dve", lambda e: e.tensor_scalar(out=ms, in0=ss, scalar1=1.0 / D, scalar2=EPS, op0=ALU.mult, op1=ALU.add),
                  reads=["h_ss%d" % si_], writes=["h_ms%d" % si_])
            P.add("pool", lambda e: e.tensor_tensor(out=rstd, in0=ms, in1=mhalf[:, 0:1], op=ALU.pow),
                  reads=["h_ms%d" % si_, "mhalf"], writes=["h_rstd%d" % si_])
            P.add("dve", lambda e: e.tensor_scalar(out=xsb, in0=htile, scalar1=rstd, scalar2=None, op0=ALU.mult),
                  reads=[htok, "h_rstd%d" % si_], writes=[xtok])
            return (xsb, xtok)

        def trans_hn2(ctx, T, st):
            xsb, xtok = ctx
            tbk, ttok = tbank2[st % 2]
            for c in range(8):
                P.add("pe", lambda e, c=c: e.transpose(tbk[:, c * 128:(c + 1) * 128], xsb[:, c * 128:(c + 1) * 128], ident),
                      reads=[xtok, "ident"], writes=[ttok])
            for c in range(8):
                dst = hn2T[:, c, st * 128:(st + 1) * 128]
                src = tbk[:, c * 128:(c + 1) * 128]
                if st % 2 == 0:
                    P.add("dve", lambda e, dst=dst, src=src, c=c: e.tensor_scalar(out=dst, in0=src, scalar1=gffn[:, c:c + 1], scalar2=None, op0=ALU.mult),
                          reads=[ttok, "vecs"], writes=["h2_%d_a" % st])
                else:
                    P.add("act", lambda e, dst=dst, src=src, c=c: e.activation(out=dst, in_=src, func=AF.Copy, scale=gffn[:, c:c + 1]),
                          reads=[ttok, "vecs"], writes=["h2_%d_b" % st])

        def emit_router(T):
            if True:
                for st in range(8):
                    for c in range(8):
                        P.add("pe", lambda e, st=st, c=c: e.matmul(bC[:, st * 32:st * 32 + 20], lhsT=hn2T[:, c, st * 128:(st + 1) * 128], rhs=wr_bf[:, c, :],
                                                                   start=(c == 0), stop=(c == 7)),
                              reads=["h2_%d_a" % st, "h2_%d_b" % st, "wr_bf"], writes=["bk4"])
                lg = rt[:, 0:160].rearrange("p (s n) -> p s n", s=8)
                bc3 = bC[:, 0:256].rearrange("p (s n) -> p s n", s=8)[:, :, 0:20]
                RT = "rt"
                r3 = lambda lo, n: rt[:, lo:lo + 8 * n].rearrange("p (s n) -> p s n", s=8)
                gl = lg[:, :, 0:4]; el = lg[:, :, 4:20]
                gmax = rt[:, 160:168]; g1 = r3(168, 4); gex = r3(200, 4); gsum = rt[:, 232:240]; gw = rt[:, 240:248]
                pen = r3(248, 16); ml = r3(376, 16); m1 = rt[:, 504:512]; m2 = rt[:, 512:520]; k1 = r3(520, 16)
                dm = rt[:, 648:656]; w1 = rt[:, 656:664]; w2 = rt[:, 664:672]; k2 = r3(672, 16); ml2 = r3(800, 16)
                bcast = lambda ap, n: ap.unsqueeze(2).to_broadcast([128, 8, n])
                P.add("dve", lambda e: e.tensor_tensor(out=lg, in0=bc3, in1=brep.unsqueeze(1).to_broadcast([128, 8, 20]), op=ALU.add),
                      reads=["bk4", "vecs"], writes=[RT])
                P.add("dve", lambda e: e.tensor_reduce(out=gmax, in_=gl, axis=AX.X, op=ALU.max), reads=[RT], writes=[RT + "a"])
                P.add("dve", lambda e: e.tensor_tensor(out=g1, in0=gl, in1=bcast(gmax, 4), op=ALU.is_equal), reads=[RT, RT + "a"], writes=[RT + "b"])
                P.add("dve", lambda e: e.tensor_tensor(out=gex, in0=gl, in1=bcast(gmax, 4), op=ALU.subtract), reads=[RT, RT + "a"], writes=[RT + "c"])
                P.add("act", lambda e: e.activation(out=gex, in_=gex, func=AF.Exp), reads=[RT + "c"], writes=[RT + "d"])
                P.add("dve", lambda e: e.tensor_reduce(out=gsum, in_=gex, axis=AX.X, op=ALU.add), reads=[RT + "d"], writes=[RT + "e"])
                P.add("dve", lambda e: e.reciprocal(out=gw, in_=gsum), reads=[RT + "e"], writes=[RT + "f"])
                P.add("dve", lambda e: e.tensor_scalar(out=pen.rearrange("p s (g n) -> p s g n", g=4), in0=g1.unsqueeze(3).to_broadcast([128, 8, 4, 4]),
                                                       scalar1=-1.0, scalar2=30000.0, op0=ALU.add, op1=ALU.mult), reads=[RT + "b"], writes=[RT + "g"])
                P.add("dve", lambda e: e.tensor_tensor(out=ml, in0=el, in1=pen, op=ALU.add), reads=[RT, RT + "g"], writes=[RT + "h"])
                P.add("dve", lambda e: e.tensor_reduce(out=m1, in_=ml, axis=AX.X, op=ALU.max), reads=[RT + "h"], writes=[RT + "i"])
                P.add("dve", lambda e: e.tensor_tensor(out=k1, in0=ml, in1=bcast(m1, 16), op=ALU.is_equal), reads=[RT + "h", RT + "i"], writes=[RT + "j"])
                P.add("dve", lambda e: e.scalar_tensor_tensor(out=ml2, in0=k1, scalar=-60000.0, in1=ml, op0=ALU.mult, op1=ALU.add), reads=[RT + "h", RT + "j"], writes=[RT + "k"])
                P.add("dve", lambda e: e.tensor_reduce(out=m2, in_=ml2, axis=AX.X, op=ALU.max), reads=[RT + "k"], writes=[RT + "l"])
                P.add("dve", lambda e: e.tensor_tensor(out=k2, in0=ml2, in1=bcast(m2, 16), op=ALU.is_equal), reads=[RT + "k", RT + "l"], writes=[RT + "m"])
                P.add("dve", lambda e: e.tensor_tensor(out=dm, in0=m2, in1=m1, op=ALU.subtract), reads=[RT + "l", RT + "i"], writes=[RT + "n"])
                P.add("act", lambda e: e.activation(out=dm, in_=dm, func=AF.Exp), reads=[RT + "n"], writes=[RT + "o"])
                P.add("dve", lambda e: e.tensor_scalar(out=dm, in0=dm, scalar1=1.0, scalar2=None, op0=ALU.add), reads=[RT + "o"], writes=[RT + "p"])
                P.add("dve", lambda e: e.reciprocal(out=w1, in_=dm), reads=[RT + "p"], writes=[RT + "q"])
                P.add("dve", lambda e: e.tensor_scalar(out=w2, in0=w1, scalar1=-1.0, scalar2=1.0, op0=ALU.mult, op1=ALU.add), reads=[RT + "q"], writes=[RT + "r"])
                P.add("dve", lambda e: e.tensor_tensor(out=w1, in0=w1, in1=gw, op=ALU.mult), reads=[RT + "q", RT + "r", RT + "f"], writes=[RT + "s"])
                P.add("dve", lambda e: e.tensor_tensor(out=w2, in0=w2, in1=gw, op=ALU.mult), reads=[RT + "r", RT + "f"], writes=[RT + "t"])
                P.add("dve", lambda e: e.tensor_tensor(out=k1, in0=k1, in1=bcast(w1, 16), op=ALU.mult), reads=[RT + "j", RT + "s", RT + "k"], writes=[RT + "u"])
                P.add("dve", lambda e: e.tensor_tensor(out=k2, in0=k2, in1=bcast(w2, 16), op=ALU.mult), reads=[RT + "m", RT + "t"], writes=[RT + "v"])
                P.add("dve", lambda e: e.tensor_tensor(out=comb_bf, in0=k1, in1=k2, op=ALU.add), reads=[RT + "u", RT + "v"], writes=["comb_bf"])
                for st in range(8):
                    P.add("pe", lambda e, st=st: e.transpose(pb_t[0:16, st * 128:(st + 1) * 128], comb_bf[:, st, :], ident),
                          reads=["comb_bf", "ident"] + h2_all, writes=["bk7"])
                P.add("dve", lambda e: e.tensor_copy(out=combT[0:16, :], in_=pb_t[0:16, 0:1024]), reads=["bk7"], writes=["combT"])


        def emit_experts(T):
            if True:
                for ex in range(16):
                    nflat = T * 16 + ex
                    wi = nflat % 3
                    wgb, wub = wgu[wi]
                    load_gu(nflat + 2)
                    for half in range(2):
                        hs_ = slice(half * 512, (half + 1) * 512)
                        ci = c2["C"] % 2; c2["C"] += 1
                        P.add("pe", lambda e, ex=ex, hs_=hs_: e.matmul(bC[:, 0:512], lhsT=sel[0:16, ex, :], rhs=combT[0:16, hs_], start=True, stop=True),
                              reads=["sel", "combT"], writes=["bk4"])
                        P.add("dve", lambda e, ci=ci: e.tensor_copy(out=Cs[ci], in_=bC[:, 0:512]), reads=["bk4"], writes=["Cs%d" % ci])
                        for fc in range(2):
                            gi = c2["G"] % 2; c2["G"] += 1
                            for c in range(8):
                                P.add("pe", lambda e, gi=gi, c=c, fc=fc, hs_=hs_, wgb=wgb: e.matmul(bG[gi][:, 0:512], lhsT=wgb[:, c, fc * 128:(fc + 1) * 128], rhs=hn2T[:, c, hs_],
                                                                                                  start=(c == 0), stop=(c == 7)),
                                      reads=["wg%d" % wi] + h2_all, writes=["bk%d" % gi])
                            for c in range(8):
                                P.add("pe", lambda e, gi=gi, c=c, fc=fc, hs_=hs_, wub=wub: e.matmul(bU[gi][:, 0:512], lhsT=wub[:, c, fc * 128:(fc + 1) * 128], rhs=hn2T[:, c, hs_],
                                                                                                  start=(c == 0), stop=(c == 7)),
                                      reads=["wu%d" % wi] + h2_all, writes=["bk%d" % (2 + gi)])
                            P.add("act", lambda e, gi=gi: e.activation(out=ssb[gi], in_=bG[gi][:, 0:512], func=AF.Silu), reads=["bk%d" % gi], writes=["ssb%d" % gi])
                            P.add("pool", lambda e, gi=gi, ci=ci: e.tensor_tensor(out=s2b[gi], in0=ssb[gi], in1=Cs[ci], op=ALU.mult),
                                  reads=["ssb%d" % gi, "Cs%d" % ci], writes=["s2b%d" % gi])
                            P.add("dve", lambda e, gi=gi, ex=ex, fc=fc, hs_=hs_: e.tensor_tensor(out=actT[:, ex * 2 + fc, hs_], in0=bU[gi][:, 0:512], in1=s2b[gi], op=ALU.mult),
                                  reads=["bk%d" % (2 + gi), "s2b%d" % gi], writes=["actT"])


        def down_mm(T, st):
            row0 = T * 1024 + st * 128
            hi = c2["y"] % 2; c2["y"] += 1
            htile, htok = hpool[1 + hi]
            P.add("sp", lambda e: e.dma_start(out=htile, in_=hs_d[row0:row0 + 128, :]),
                  reads=["hs%d" % (row0 // 128)], writes=[htok], dma=htok)
            for dh in range(2):
                bk_, btok = bD4[c2["D"] % 4]; c2["D"] += 1
                ds_ = slice(dh * 512, (dh + 1) * 512)
                for kk in range(32):
                    P.add("pe", lambda e, bk_=bk_, kk=kk, ds_=ds_: e.matmul(bk_[:, 0:512], lhsT=actT[:, kk, st * 128:(st + 1) * 128], rhs=wd_bf[:, kk, ds_],
                                                                          start=(kk == 0), stop=(kk == 31)),
                          reads=["actT"] + wd_toks, writes=[btok])
                P.add("dve", lambda e, bk_=bk_, ds_=ds_: e.tensor_tensor(out=htile[:, ds_], in0=bk_[:, 0:512], in1=htile[:, ds_], op=ALU.add),
                      reads=[btok, htok], writes=[htok])
            return (htile, htok, row0, hi)

        def down_fin(ctx):
            htile, htok, row0, yi = ctx
            junk, jtok = xpool[1]
            ss = small2[:, 16 + yi * 4:16 + yi * 4 + 1]; ms = small2[:, 16 + yi * 4 + 1:16 + yi * 4 + 2]; rstd = small2[:, 16 + yi * 4 + 2:16 + yi * 4 + 3]
            P.add("act", lambda e: e.activation(out=junk, in_=htile, func=AF.Square, accum_out=ss),
                  reads=[htok], writes=[jtok, "f_ss%d" % yi])
            P.add("dve", lambda e: e.tensor_scalar(out=ms, in0=ss, scalar1=1.0 / D, scalar2=EPS, op0=ALU.mult, op1=ALU.add),
                  reads=["f_ss%d" % yi], writes=["f_ms%d" % yi])
            P.add("pool", lambda e: e.tensor_tensor(out=rstd, in0=ms, in1=mhalf[:, 0:1], op=ALU.pow),
                  reads=["f_ms%d" % yi, "mhalf"], writes=["f_rstd%d" % yi])
            P.add("dve", lambda e: e.scalar_tensor_tensor(out=htile, in0=htile, scalar=rstd, in1=gfin, op0=ALU.mult, op1=ALU.mult),
                  reads=[htok, "f_rstd%d" % yi, "gfin"], writes=[htok])
            P.add("sp", lambda e: e.dma_start(out=y_d[row0:row0 + 128, :], in_=htile),
                  reads=[htok], dma="yst%d" % yi)

        for st in range(8):
            trans_hn2(prep_hn2(0, st), 0, st)
        emit_router(0)
        for T in range(4):
            emit_experts(T)
            for st in range(8):
                cx = prep_hn2(T + 1, st) if T < 3 else None
                dx = down_mm(T, st)
                if cx is not None:
                    trans_hn2(cx, T + 1, st)
                down_fin(dx)
            if T < 3:
                emit_router(T + 1)

    except StopBuild:
        pass

    sems = {}
    for en in Prog.ENGS:
        sems["eng:" + en] = es.enter_context(nc.semaphore("s_" + en))
    for sl in sorted(P.dma_slots):
        sems["dma:" + sl] = es.enter_context(nc.semaphore("d_" + sl))
    if os.environ.get('KMAXOPS'):
        P.ops = P.ops[:int(os.environ['KMAXOPS'])]
        for i_, o_ in enumerate(P.ops[-3:]):
            print('LASTOPS', o_.eng, o_.idx)
    body = P.emit(sems)
    if os.environ.get('KSTATS'):
        print('PROG stats', P.stats, 'ndma_slots', len(P.dma_slots))
    with nc.Block() as block:
        block.sync(body("sp"))
        block.tensor(body("pe"))
        block.scalar(body("act"))
        block.vector(body("dve"))
        block.gpsimd(body("pool"))
    es.close()
    return nc


def _dil_bias():
    out = np.full((128, 4 * 2 * 512 + 4 * 256), NEG, np.float32)
    k = np.arange(128)[:, None]; q = np.arange(128)[None, :]

    def f(delta, slope, d):
        return np.where(np.abs(delta) <= 64, -slope * d * np.abs(delta), NEG).astype(np.float32)
    for j in range(4):
        for hp in range(2):
            slope = 2.0 ** (-(2 * j + hp + 1))
            for pi, d in enumerate((1, 4)):
                c0 = (j * 2 + pi) * 512
                out[:, c0 + hp * 128:c0 + (hp + 1) * 128] = f(k - 64 - q, slope, d)
                out[:, c0 + 256 + hp * 128:c0 + 256 + (hp + 1) * 128] = f(k + 64 - q, slope, d)
            c0 = 4096 + j * 256
            out[:, c0 + hp * 128:c0 + (hp + 1) * 128] = f(k - q, slope, 16)
    return out


def _na_strips(rpb):
    kc = np.arange(64)[:, None]; qc = np.arange(64)[None, :]
    cs = np.clip(qc - 8, 0, 48)
    col_ok = (kc >= cs) & (kc < cs + 16)
    dcidx = np.clip(kc - qc + 15, 0, 30)
    out = np.full((4, 128, 2, 2, 896), NEG, np.float32)
    for h in range(8):
        j, hp = divmod(h, 2)
        for var in range(2):
            for i in range(2):
                for u in range(14):
                    dlt = 6 + i - u
                    ok = (-4 <= dlt <= 3) if var == 0 else (-7 <= dlt <= 7)
                    if not ok:
                        continue
                    blk = np.where(col_ok, rpb[h, dlt + 7][dcidx], NEG).astype(np.float32)
                    out[j, i * 64:(i + 1) * 64, hp, var, u * 64:(u + 1) * 64] = blk
    return out.reshape(4, 128, 4 * 896)


_NC_CACHE = {}


def kernel(x, norm_mix_g, w_in, rpb, g_out_dil, g_out_na, w_out, norm_ffn_g, w_group, b_group, w_router, b_router,
           w_gate, w_up, w_down, norm_final_g):
    f = lambda a: np.ascontiguousarray(np.asarray(a, dtype=np.float32))
    x = f(x).reshape(16 * S, D)
    col = lambda g: f(g).reshape(8, 128).T
    vecs = np.concatenate([col(norm_mix_g[0]), col(norm_ffn_g[0]),
                           col(np.concatenate([f(g_out_dil[0]), f(g_out_na[0])])),
                           np.broadcast_to(np.concatenate([f(b_group[0]), f(b_router[0])])[None, :], (128, 20))], axis=1)
    vecs = np.ascontiguousarray(vecs, dtype=np.float32)
    gfin = np.ascontiguousarray(np.broadcast_to(f(norm_final_g)[None, :], (128, D)))
    gmixr = np.ascontiguousarray(np.broadcast_to(f(norm_mix_g[0])[None, :], (128, D)))
    wr = np.ascontiguousarray(np.concatenate([f(w_group[0]), f(w_router[0])], axis=1))
    shared = {
        "w_in": f(w_in[0]), "w_out": f(w_out[0]), "w_gate": f(w_gate[0]), "w_up": f(w_up[0]), "w_down": f(w_down[0]),
        "wr": wr, "vecs": vecs, "gfin": gfin, "gmixr": gmixr, "dilb": _dil_bias(), "nas": _na_strips(f(rpb[0])),
    }
    if "nc" not in _NC_CACHE:
        _NC_CACHE["nc"] = build_program()
    nc = _NC_CACHE["nc"]
    in_maps = []
    for i in range(NCORES):
        m = dict(shared)
        m["x"] = np.ascontiguousarray(x[i * TOK:(i + 1) * TOK])
        in_maps.append(m)
    res = run_bass_kernel_spmd(nc, in_maps, core_ids=list(range(NCORES)))
    y = np.concatenate([r["y"] for r in res.results], axis=0)
    if DEBUG:
        kernel.dbg = [r.get("dbg") for r in res.results]
    return y.reshape(16, S, D).astype(np.float32)
```

```python
import numpy as np
from contextlib import ExitStack
import concourse.bass as bass
import concourse.mybir as mybir
from concourse.bass_utils import run_bass_kernel_spmd

F32 = mybir.dt.float32
BF16 = mybir.dt.bfloat16
AF = mybir.ActivationFunctionType
ALU = mybir.AluOpType
AX = mybir.AxisListType

NCORES = 8
S = 2048
D = 1024
TOK = 4096
NEG = -30000.0
EPS = 1e-6
import os
DEBUG = bool(os.environ.get('KSTOP', ''))
STOP = os.environ.get('KSTOP', '')


class StopBuild(Exception):
    pass


DUMPS = {}


def stage(name):
    if STOP and name == STOP:
        if name in DUMPS:
            DUMPS[name]()
        raise StopBuild()


class Op:
    __slots__ = ("eng", "fn", "deps", "is_dma", "sem", "semval", "needs_signal", "sigcount", "eidx", "idx", "wo")


class Prog:
    ENGS = ("pe", "act", "dve", "pool", "sp")

    def __init__(self):
        self.ops = []
        self.last_writer = {}
        self.readers = {}
        self.eng_count = {e: 0 for e in self.ENGS}
        self.dma_slots = set()

    def add(self, eng, fn, reads=(), writes=(), dma=None, waitonly=False):
        op = Op()
        op.eng = eng; op.fn = fn; op.idx = len(self.ops)
        op.is_dma = dma is not None
        op.wo = waitonly
        op.sem = dma; op.semval = 0; op.needs_signal = False; op.sigcount = 0
        op.eidx = self.eng_count[eng]; self.eng_count[eng] += 1
        if dma is not None:
            self.dma_slots.add(dma)
        deps = {}
        for t in reads:
            w = self.last_writer.get(t)
            if w is not None:
                deps[w] = "raw"
        for t in writes:
            w = self.last_writer.get(t)
            if w is not None and w not in deps:
                deps[w] = "waw"
            for r in self.readers.get(t, ()):
                if r not in deps and r != op.idx:
                    deps[r] = "war"
        for t in (() if waitonly else reads):
            lst = self.readers.setdefault(t, [])
            if not op.is_dma:
                lst[:] = [r for r in lst if self.ops[r].is_dma or self.ops[r].eng != eng]
            lst.append(op.idx)
        for t in writes:
            self.last_writer[t] = op.idx
            self.readers[t] = []
        op.deps = deps
        self.ops.append(op)
        return op

    def _need_wait(self, op, p, kind):
        if p.is_dma:
            return True
        if p.eng != op.eng:
            return True
        if op.is_dma:
            return True
        if p.eng == "pe":
            return False
        if kind == "raw" and ((op.eidx - p.eidx) <= 3 or p.eng == "pool"):
            return True
        return False

    def emit(self, sem_ctx):
        ops = self.ops
        for op in ops:
            for d, kind in op.deps.items():
                p = ops[d]
                if (not p.is_dma) and self._need_wait(op, p, kind):
                    p.needs_signal = True
        if os.environ.get('KALLSIG'):
            for op in ops:
                if not op.is_dma and op.fn(None) if False else (not op.is_dma and not getattr(op, "wo", False)):
                    op.needs_signal = True
        cnt = {e: 0 for e in self.ENGS}
        dcnt = {}
        for op in ops:
            if op.is_dma:
                dcnt[op.sem] = dcnt.get(op.sem, 0) + 16
                op.semval = dcnt[op.sem]
            elif op.needs_signal:
                cnt[op.eng] += 1
                op.sigcount = cnt[op.eng]
        by_eng = {e: [o for o in ops if o.eng == e] for e in self.ENGS}
        self.stats = (dict(cnt), {e: len(v) for e, v in by_eng.items()})

        def body(ename):
            def run(eng):
                waited = {}
                for op in by_eng[ename]:
                    for d, kind in op.deps.items():
                        p = ops[d]
                        if not self._need_wait(op, p, kind):
                            continue
                        if p.is_dma:
                            key = "dma:" + p.sem; val = p.semval
                        else:
                            key = "eng:" + p.eng; val = p.sigcount
                        if waited.get(key, 0) >= val:
                            continue
                        waited[key] = val
                        eng.wait_ge(sem_ctx[key], val)
                    ins = op.fn(eng)
                    if op.is_dma:
                        ins.then_inc(sem_ctx["dma:" + op.sem], 16)
                    elif op.needs_signal:
                        ins.then_inc(sem_ctx["eng:" + ename], 1)
                for op in by_eng[ename]:
                    if op.is_dma:
                        key = "dma:" + op.sem
                        if waited.get(key, 0) < dcnt[op.sem]:
                            waited[key] = dcnt[op.sem]
                            eng.wait_ge(sem_ctx[key], dcnt[op.sem])
            return run
        return body


def tsl(r, d, p0, n):
    return slice(r + d * p0, r + d * (p0 + n - 1) + 1, d)


def build_program():
    nc = bass.Bass("TRN2", target_bir_lowering=False)
    dt_in = lambda n, s, dt=F32: nc.dram_tensor(n, s, dt, kind="ExternalInput").ap()
    x_d = dt_in("x", [TOK, D])
    win_d = dt_in("w_in", [D, 3072])
    wout_d = dt_in("w_out", [D, D])
    wg_d = dt_in("w_gate", [16, D, 256])
    wu_d = dt_in("w_up", [16, D, 256])
    wd_d = dt_in("w_down", [16, 256, D])
    wr_d = dt_in("wr", [D, 20])
    vec_d = dt_in("vecs", [128, 24 + 20])
    gfin_d = dt_in("gfin", [128, D])
    gmixr_d = dt_in("gmixr", [128, D])
    dilb_d = dt_in("dilb", [128, 5120])
    nas_d = dt_in("nas", [4, 128, 4 * 896])
    y_d = nc.dram_tensor("y", [TOK, D], F32, kind="ExternalOutput").ap()
    hs_d = nc.dram_tensor("hscr", [TOK, D], F32, kind="Internal").ap()
    wgs_d = nc.dram_tensor("wg_bf", [16, D, 256], BF16, kind="Internal").ap()
    wus_d = nc.dram_tensor("wu_bf", [16, D, 256], BF16, kind="Internal").ap()
    wds_d = nc.dram_tensor("wd_bf", [16, 256, D], BF16, kind="Internal").ap()
    dbg_d = None
    if DEBUG:
        dbg_d = nc.dram_tensor("dbg", [128, 8 * 2048], F32, kind="ExternalOutput").ap()

    P = Prog()
    es = ExitStack()
    ARENA = 53200
    arena = es.enter_context(nc.sbuf_tensor("arena", [128, ARENA], F32))
    arena_bf = arena.bitcast(BF16)
    ptr = [0]

    def alloc(ncols, dt=F32, shape=None):
        n32 = ncols if dt == F32 else (ncols + 1) // 2
        a = ptr[0]
        ptr[0] += n32
        assert ptr[0] <= ARENA, ("SBUF overflow", ptr[0])
        if dt == F32:
            ap = arena[:, a:a + ncols]
        else:
            ap = arena_bf[:, 2 * a:2 * a + ncols]
        if shape is not None:
            names = " ".join("d%d" % i for i in range(len(shape)))
            kw = {"d%d" % i: shape[i] for i in range(len(shape))}
            ap = ap.rearrange("p (%s) -> p %s" % (names, names), **kw)
        return ap

    pb_t = es.enter_context(nc.psum_tensor("pb_t", [128, 1024], BF16))
    banks = [es.enter_context(nc.psum_tensor("bk%d" % i, [128, 512], F32)) for i in range(7)]
    tbank = [(pb_t, "bk7"), (banks[6].bitcast(BF16), "bk6")]

    ident = alloc(128, BF16)
    sel = alloc(16 * 128, BF16, (16, 128))
    vecs = alloc(44)
    gfin = alloc(D)
    wr_bf = alloc(8 * 20, BF16, (8, 20))
    mhalf = alloc(4)
    ones1 = alloc(2, BF16)
    bar = alloc(4)
    persist_end = ptr[0]

    try:
        P.add("pool", lambda e: e.memset(ident, 0.0), writes=["ident"])
        P.add("pool", lambda e: e.affine_select(out=ident, in_=ident, pattern=[[-1, 128]], compare_op=ALU.not_equal,
                                                fill=1.0, base=0, channel_multiplier=1), reads=["ident"], writes=["ident"])
        P.add("pool", lambda e: e.memset(sel, 0.0), writes=["sel"])
        P.add("pool", lambda e: e.affine_select(out=sel[0:16], in_=sel[0:16], pattern=[[-1, 16], [0, 128]],
                                                compare_op=ALU.not_equal, fill=1.0, base=0, channel_multiplier=1),
              reads=["sel"], writes=["sel"])
        P.add("pool", lambda e: e.memset(mhalf, -0.5), writes=["mhalf"])
        P.add("pool", lambda e: e.memset(ones1, 1.0), writes=["ones1"])
        P.add("sp", lambda e: e.dma_start(out=vecs, in_=vec_d), writes=["vecs"], dma="c0")
        P.add("sp", lambda e: e.dma_start(out=gfin, in_=gfin_d), writes=["gfin"], dma="c1")
        P.add("pool", lambda e: e.dma_start(out=wr_bf, in_=wr_d.rearrange("(c p) n -> p c n", p=128)),
              writes=["wr_bf"], dma="c2")
        gmix = vecs[:, 0:8]; gffn = vecs[:, 8:16]; gout = vecs[:, 16:24]; brep = vecs[:, 24:44]

        def rms_rstd(src, ss, ms, rstd, junk, tag, ncols=D):
            P.add("act", lambda e: e.activation(out=junk, in_=src, func=AF.Square, accum_out=ss),
                  reads=[tag + "_src"], writes=[tag + "_junk", tag + "_ss"])
            P.add("dve", lambda e: e.tensor_scalar(out=ms, in0=ss, scalar1=1.0 / ncols, scalar2=EPS, op0=ALU.mult, op1=ALU.add),
                  reads=[tag + "_ss"], writes=[tag + "_ms"])
            P.add("pool", lambda e: e.tensor_tensor(out=rstd, in0=ms, in1=mhalf[:, 0:1], op=ALU.pow),
                  reads=[tag + "_ms", "mhalf"], writes=[tag + "_rstd"])

        hnT = alloc(8 * S, BF16, (8, S))
        yT = alloc(8 * S, BF16, (8, S))
        dilb = alloc(5120, BF16)
        wblk = [[alloc(8 * 128, BF16, (8, 128)) for _ in range(3)] for _ in range(2)]
        QT = [alloc(2 * S, BF16, (2, S)) for _ in range(2)]
        KT = [alloc(S, BF16) for _ in range(2)]
        Vt_raw = alloc(3 * 16 * 256, BF16)
        Vt = Vt_raw.rearrange("p (l t h c) -> p l t h c", l=3, t=16, h=2, c=128)
        wout_bf = Vt_raw[:, 0:8 * D].rearrange("p (c n) -> p c n", c=8)
        acc2 = alloc(2 * S, F32, (2, S))
        acc = [acc2[:, 0, :], acc2[:, 1, :]]
        VTb = alloc(S, BF16)
        gmixr = alloc(D)
        rden = alloc(S)
        NS = int(os.environ.get('KNS', '3')); NO = int(os.environ.get('KNO', '2'))
        PT = alloc(NS * 512, BF16)
        nastrip = alloc(4 * 896, BF16, (2, 2, 896))
        xs = [alloc(D, BF16) for _ in range(3)]
        xt = [alloc(D) for _ in range(3)]
        sqt2 = [alloc(8 * 128, BF16, (8, 128)) for _ in range(2)]
        small = alloc(64)
        p1_end = ptr[0]


        def dump(items):
            col = [0]
            for ap, toks in items:
                n = ap.shape[-1] if len(ap.shape) == 2 else None
                assert n is not None
                a = col[0]; col[0] += n
                P.add("pool", lambda e, ap=ap, a=a, n=n: e.dma_start(out=dbg_d[0:ap.shape[0], a:a + n], in_=ap, max_dma_last_dim=2048),
                      reads=toks, dma="dbg")
        alltok = lambda: list(P.last_writer.keys())
        DUMPS['H0'] = lambda: dump([(hnT[:, c, :], alltok()) for c in range(8)])
        DUMPS['I0_1'] = lambda: dump([(QT[0], alltok()), (KT[0], alltok()), (acc[0], alltok()), (acc[1], alltok()), (yT[:, 0, :], alltok()),
                                      (Vt_raw[:, 0:4096], alltok())])
        DUMPS['I0_5'] = lambda: dump([(QT[0], alltok()), (KT[0], alltok()), (acc[0], alltok()), (acc[1], alltok()), (yT[:, 4, :], alltok()),
                                      (Vt_raw[:, 0:4096], alltok())])
        for k_ in (2, 3, 4, 6, 7):
            DUMPS['I0_%d' % k_] = lambda: dump([(yT[:, c, :], alltok()) for c in range(8)])
        if os.environ.get('KD2'):
            DUMPS['I0_2'] = lambda: dump([(QT[0], alltok()), (KT[0], alltok()), (QT[1], alltok()), (KT[1], alltok()), (wblk[0][0][:, 0, :], alltok()), (wblk[0][2][:, 0, :], alltok()), (wblk[1][0][:, 0, :], alltok())])
        DUMPS['P0'] = lambda: dump([(yT[:, c, :], alltok()) for c in range(8)])
        bP = [banks[0], banks[1]]
        bPbf = [banks[0].bitcast(BF16), banks[1].bitcast(BF16)]
        bS = [banks[2], banks[3], banks[4], banks[0], banks[1]][:NS]
        SBK = [2, 3, 4, 0, 1]
        bO = [banks[5], banks[6], pb_t.bitcast(F32)][:NO]

        P.add("pool", lambda e: e.dma_start(out=dilb, in_=dilb_d, max_dma_last_dim=4096), writes=["dilb"], dma="c3")
        if DEBUG:
            P.add("pool", lambda e: e.memset(yT, 0.0), writes=["yT%d_%d" % (c, hp) for c in range(8) for hp in range(2)])

        for st_ in range(2):
            P.add("pool", lambda e, st_=st_: e.memset(QT[st_], 0.0), writes=["QT%d" % st_])
        P.add("sp", lambda e: e.dma_start(out=gmixr, in_=gmixr_d), writes=["gmixr"], dma="c4")
        MASKMUL = os.environ.get('KMASK', '0') == '1'
        if MASKMUL:
            for c_ in range(0, 5120, 512):
                P.add("act", lambda e, c_=c_: e.activation(out=dilb[:, c_:c_ + 512], in_=dilb[:, c_:c_ + 512], func=AF.Exp), reads=["dilb"], writes=["dilb"])
        mctr = {"n": 0}

        def mask_mul(ptv, mv, si, mtok):
            eng = "pool" if (mctr["n"] % int(os.environ.get('KMASKDVE', '3'))) != 0 else "dve"
            mctr["n"] += 1
            P.add(eng, lambda e: e.tensor_tensor(out=ptv, in0=ptv, in1=mv, op=ALU.mult), reads=["PT%d" % si, mtok], writes=["PT%d" % si])
        ctr = {"S": 0, "O": 0, "P": 0, "x": 0}

        for s in range(2):
            tb = s * S
            def h_prep(tt):
                xi = ctr["x"] % 3; ctr["x"] += 1
                xtile = xt[xi]; xsb = xs[xi]
                row0 = tb + tt * 128
                P.add("sp", lambda e: e.dma_start(out=xtile, in_=x_d[row0:row0 + 128, :]),
                      writes=["xt%d_src" % xi], dma="x%d" % xi)
                ss = small[:, xi * 4:xi * 4 + 1]; ms = small[:, xi * 4 + 1:xi * 4 + 2]; rstd = small[:, xi * 4 + 2:xi * 4 + 3]
                P.add("act", lambda e: e.activation(out=xsb, in_=xtile, func=AF.Square, accum_out=ss),
                      reads=["xt%d_src" % xi], writes=["xs%d" % xi, "xt%d_ss" % xi])
                P.add("dve", lambda e: e.tensor_scalar(out=ms, in0=ss, scalar1=1.0 / D, scalar2=EPS, op0=ALU.mult, op1=ALU.add),
                      reads=["xt%d_ss" % xi], writes=["xt%d_ms" % xi])
                P.add("pool", lambda e: e.tensor_tensor(out=rstd, in0=ms, in1=mhalf[:, 0:1], op=ALU.pow),
                      reads=["xt%d_ms" % xi, "mhalf"], writes=["xt%d_rstd" % xi])
                P.add("dve", lambda e: e.scalar_tensor_tensor(out=xsb, in0=xtile, scalar=rstd, in1=gmixr, op0=ALU.mult, op1=ALU.mult),
                      reads=["xt%d_src" % xi, "xt%d_rstd" % xi, "gmixr"], writes=["xs%d" % xi])
                return (xsb, xi)

            def h_trans(ctx, tt):
                xsb, xi = ctx
                tbk, ttok = tbank[tt % 2]
                for c in range(8):
                    P.add("pe", lambda e, c=c: e.transpose(tbk[:, c * 128:(c + 1) * 128], xsb[:, c * 128:(c + 1) * 128], ident),
                          reads=["xs%d" % xi, "ident"], writes=[ttok])
                dst = hnT[:, :, tt * 128:(tt + 1) * 128]
                src = tbk[:, 0:1024].rearrange("p (c q) -> p c q", c=8)
                if tt % 2 == 0:
                    P.add("dve", lambda e: e.tensor_copy(out=dst, in_=src), reads=[ttok], writes=["hn%d_a" % tt])
                else:
                    P.add("act", lambda e: e.activation(out=dst, in_=src, func=AF.Copy), reads=[ttok], writes=["hn%d_b" % tt])

            hctx = [h_prep(0), h_prep(1)]
            for tt in range(16):
                if tt + 2 < 16:
                    hctx.append(h_prep(tt + 2))
                h_trans(hctx[tt], tt)

            def hn_tokens(tiles):
                out = []
                for t in tiles:
                    out += ["hn%d_a" % t, "hn%d_b" % t]
                return out

            stage('H%d' % s)
            for lay_ in range(3):
                P.add("pool", lambda e, lay_=lay_: e.memset(Vt[:, lay_, :, :, 64:128], 1.0), writes=["V%d" % lay_])
            for item in range(8):
                stage('I%d_%d' % (s, item))
                if os.environ.get('KBAR'):
                    P.add("act", lambda e: e.activation(out=bar[:, 0:1], in_=mhalf[:, 0:1], func=AF.Copy), reads=["mhalf"], writes=["bar_act"])
                    P.add("dve", lambda e: e.tensor_copy(out=bar[:, 1:2], in_=mhalf[:, 0:1]), reads=["mhalf"], writes=["bar_dve"])
                    P.add("pool", lambda e: e.tensor_copy(out=bar[:, 2:3], in_=mhalf[:, 0:1]), reads=["mhalf"], writes=["bar_pool"])
                    for en in ("pe", "act", "dve", "pool", "sp"):
                        P.add(en, lambda e: None, reads=["bar_act", "bar_dve", "bar_pool"], waitonly=True)
                if os.environ.get('KSNAP') and item == 1:
                    P.add("pool", lambda e: e.tensor_copy(out=yT[:, 7, :], in_=yT[:, 0, :]), reads=["yT0_0", "yT0_1"], writes=["yT7_0", "yT7_1"])
                    P.add("pool", lambda e: e.tensor_copy(out=yT[:, 6, :], in_=acc[0][:, :]), reads=["acc0"], writes=["yT6_0", "yT6_1"])
                if s == 0:
                    for ex_ in (2 * item, 2 * item + 1):
                        for nm_, src_, dst_ in (("g", wg_d, wgs_d), ("u", wu_d, wus_d), ("d", wd_d, wds_d)):
                            P.add("pool", lambda e, src_=src_, dst_=dst_, ex_=ex_: e.dma_start(out=dst_[ex_], in_=src_[ex_], max_dma_last_dim=4096),
                                  writes=["cv%s%d" % (nm_, ex_)], dma="cv%s%d" % (nm_, ex_))
                is_dil = item < 4
                j = item % 4
                st = item % 2
                colbase = 0 if is_dil else 1536
                wq, wk, wv = wblk[st]
                for wi, (wb, off) in enumerate(((wq, 0), (wk, 512), (wv, 1024))):
                    c0 = colbase + off + j * 128
                    P.add("pool", lambda e, wb=wb, c0=c0: e.dma_start(out=wb, in_=win_d.rearrange("(c p) n -> p c n", p=128)[:, :, c0:c0 + 128]),
                          writes=["wb%d_%d" % (st, wi)], dma="wb%d_%d" % (st, wi))
                if not is_dil:
                    P.add("pool", lambda e, j=j: e.dma_start(out=nastrip, in_=nas_d[j].rearrange("p (h v n) -> p h v n", h=2, v=2), max_dma_last_dim=3584),
                          writes=["nastrip"], dma="nas")
                    if MASKMUL:
                        nflat = nastrip.rearrange("p h v n -> p (h v n)")
                        for c_ in range(0, 3584, 512):
                            P.add("act", lambda e, c_=c_, nflat=nflat: e.activation(out=nflat[:, c_:c_ + 512], in_=nflat[:, c_:c_ + 512], func=AF.Exp), reads=["nastrip"], writes=["nastrip"])
                for which, (wb, dstT, scl) in enumerate(((wq, QT[st], 0.125), (wk, KT[st], 1.0))):
                    for tc in range(4):
                        bi = ctr["P"] % 2; ctr["P"] += 1
                        bk = bP[bi]
                        for c in range(8):
                            P.add("pe", lambda e, bk=bk, wb=wb, c=c, tc=tc: e.matmul(bk[:, 0:512], lhsT=wb[:, c, :], rhs=hnT[:, c, tc * 512:(tc + 1) * 512],
                                                                                     start=(c == 0), stop=(c == 7)),
                                  reads=["wb%d_%d" % (st, which)] + hn_tokens(range(4 * tc, 4 * tc + 4)), writes=["bk%d" % bi])
                        dst = dstT[:, tc * 512:(tc + 1) * 512] if which == 1 else None
                        tokn = ("QT%d" if which == 0 else "KT%d") % st
                        if which == 0:
                            for hp_ in range(2):
                                rw = slice(hp_ * 64, hp_ * 64 + 64)
                                P.add("dve", lambda e, bk=bk, rw=rw, hp_=hp_, tc=tc, dstT=dstT: e.tensor_scalar(out=dstT[rw, hp_, tc * 512:(tc + 1) * 512], in0=bk[rw, 0:512], scalar1=0.125, scalar2=None, op0=ALU.mult),
                                      reads=["bk%d" % bi], writes=[tokn])
                        else:
                            P.add("dve", lambda e, dst=dst, bk=bk: e.tensor_copy(out=dst, in_=bk[:, 0:512]),
                                  reads=["bk%d" % bi], writes=[tokn])
                for tc in range(4):
                    bi = ctr["P"] % 2; ctr["P"] += 1
                    bk = bP[bi]
                    for c in range(8):
                        P.add("pe", lambda e, bk=bk, wv=wv, c=c, tc=tc: e.matmul(bk[:, 0:512], lhsT=wv[:, c, :], rhs=hnT[:, c, tc * 512:(tc + 1) * 512],
                                                                                 start=(c == 0), stop=(c == 7)),
                              reads=["wb%d_2" % st] + hn_tokens(range(4 * tc, 4 * tc + 4)), writes=["bk%d" % bi])
                    dst = VTb[:, tc * 512:(tc + 1) * 512]
                    P.add("dve", lambda e, dst=dst, bk=bk: e.tensor_copy(out=dst, in_=bk[:, 0:512]),
                          reads=["bk%d" % bi], writes=["VT"])
                layouts = ((0, 1), (1, 4), (2, 16)) if is_dil else ((0, 1),)
                for (lay, d) in layouts:
                    L = S // d; nt = L // 128
                    tiles = [(r, m) for r in range(d) for m in range(nt)]
                    for g8 in range(2):
                        bi = ctr["P"] % 2; ctr["P"] += 1
                        bkb = bPbf[bi]
                        for q in range(8):
                            r, m = tiles[g8 * 8 + q]
                            tsl_ = tsl(r, d, 128 * m, 128)
                            P.add("pe", lambda e, bkb=bkb, q=q, tsl_=tsl_: e.transpose(bkb[:, q * 128:(q + 1) * 128], VTb[:, tsl_], ident),
                                  reads=["VT", "ident"], writes=["bk%d" % bi])
                        dst = Vt[:, lay, g8 * 8:(g8 + 1) * 8, :, 0:64]
                        src = bkb[:, 0:1024].rearrange("p (t h c) -> p t h c", t=8, h=2)
                        P.add("dve", lambda e, dst=dst, src=src: e.tensor_copy(out=dst, in_=src),
                              reads=["bk%d" % bi], writes=["V%d" % lay])

                tasks = []
                qz = QT[st]; kTp = KT[st]
                ATOK = ["acc0", "acc1"]
                v2 = lambda ap, lo, n: ap[:, lo:lo + 2 * n].rearrange("p (h q) -> p h q", h=2)
                if is_dil:
                    for pi, d in enumerate((1, 4)):
                        L = S // d; nt = L // 128
                        biasP = dilb[:, (j * 2 + pi) * 512:(j * 2 + pi + 1) * 512]
                        for r in range(d):
                            for b in range(nt + 1):
                                def s_part(b=b, r=r, d=d, nt=nt, biasP=biasP, qz=qz, kTp=kTp, st=st):
                                    si = ctr["S"] % NS; ctr["S"] += 1
                                    bkS = bS[si]; pt = PT[:, si * 512:(si + 1) * 512]
                                    tk = "bk%d" % SBK[si]
                                    if b == 0:
                                        ov = v2(bkS, 256, 128)[:, :, 64:128]; bv = v2(biasP, 256, 128)[:, :, 64:128]; pv_ = v2(pt, 256, 128)[:, :, 64:128]
                                        if not MASKMUL:
                                            P.add("pe", lambda e: e.matmul(ov, lhsT=ident, rhs=bv, start=True, stop=False), reads=["ident", "dilb"], writes=[tk])
                                        P.add("pe", lambda e: e.matmul(ov, lhsT=kTp[:, tsl(r, d, 0, 128)], rhs=qz[:, :, tsl(r, d, 0, 64)], start=MASKMUL, stop=True),
                                              reads=["QT%d" % st, "KT%d" % st], writes=[tk])
                                        P.add("act", lambda e: e.activation(out=pv_, in_=ov, func=AF.Exp), reads=[tk], writes=["PT%d" % si])
                                        if MASKMUL:
                                            mask_mul(pv_, bv, si, "dilb")
                                    elif b == nt:
                                        ov = v2(bkS, 0, 128)[:, :, 0:64]; bv = v2(biasP, 0, 128)[:, :, 0:64]; pv_ = v2(pt, 0, 128)[:, :, 0:64]
                                        if not MASKMUL:
                                            P.add("pe", lambda e: e.matmul(ov, lhsT=ident, rhs=bv, start=True, stop=False), reads=["ident", "dilb"], writes=[tk])
                                        P.add("pe", lambda e: e.matmul(ov, lhsT=kTp[:, tsl(r, d, 128 * (nt - 1), 128)], rhs=qz[:, :, tsl(r, d, 128 * nt - 64, 64)], start=MASKMUL, stop=True),
                                              reads=["QT%d" % st, "KT%d" % st], writes=[tk])
                                        P.add("act", lambda e: e.activation(out=pv_, in_=ov, func=AF.Exp), reads=[tk], writes=["PT%d" % si])
                                        if MASKMUL:
                                            mask_mul(pv_, bv, si, "dilb")
                                    else:
                                        qv = qz[:, :, tsl(r, d, 128 * b - 64, 128)]
                                        if not MASKMUL:
                                            P.add("pe", lambda e: e.matmul(bkS[:, 0:512], lhsT=ident, rhs=biasP, start=True, stop=False), reads=["ident", "dilb"], writes=[tk])
                                        P.add("pe", lambda e: e.matmul(v2(bkS, 0, 128), lhsT=kTp[:, tsl(r, d, 128 * (b - 1), 128)], rhs=qv, start=MASKMUL, stop=False),
                                              reads=["QT%d" % st, "KT%d" % st], writes=[tk])
                                        P.add("pe", lambda e: e.matmul(v2(bkS, 256, 128), lhsT=kTp[:, tsl(r, d, 128 * b, 128)], rhs=qv, start=False, stop=True),
                                              reads=["QT%d" % st, "KT%d" % st], writes=[tk])
                                        P.add("act", lambda e: e.activation(out=pt[:, 0:512], in_=bkS[:, 0:512], func=AF.Exp), reads=[tk], writes=["PT%d" % si])
                                        if MASKMUL:
                                            mask_mul(pt[:, 0:512], biasP, si, "dilb")
                                    return si

                                def pv_part(si, b=b, r=r, d=d, nt=nt, pi=pi):
                                    oi = ctr["O"] % NO; ctr["O"] += 1
                                    bkO = bO[oi]; pt = PT[:, si * 512:(si + 1) * 512]
                                    tk = "bk%d" % (5 + oi)
                                    first = [True]
                                    for hp in range(2):
                                        def mm(o_, l_, r_):
                                            stt = first[0]; first[0] = False
                                            P.add("pe", lambda e: e.matmul(o_, lhsT=l_, rhs=r_, start=stt, stop=True), reads=["PT%d" % si, "V%d" % pi], writes=[tk])
                                        ob = hp * 128
                                        if b == 0:
                                            mm(bkO[:, ob + 64:ob + 128], Vt[:, pi, r * nt + 0, hp, :], pt[:, 256 + hp * 128 + 64:256 + hp * 128 + 128])
                                        elif b == nt:
                                            mm(bkO[:, ob:ob + 64], Vt[:, pi, r * nt + nt - 1, hp, :], pt[:, hp * 128:hp * 128 + 64])
                                        else:
                                            mm(bkO[:, ob:ob + 128], Vt[:, pi, r * nt + b - 1, hp, :], pt[:, hp * 128:hp * 128 + 128])
                                            mm(bkO[:, ob:ob + 128], Vt[:, pi, r * nt + b, hp, :], pt[:, 256 + hp * 128:256 + hp * 128 + 128])
                                    c_lo = 64 if b == 0 else 0
                                    c_hi = 64 if b == nt else 128
                                    p_lo = 128 * b - 64 + c_lo
                                    dsta = acc2[:, :, tsl(r, d, p_lo, c_hi - c_lo)]
                                    srca = v2(bkO, 0, 128)[:, :, c_lo:c_hi]
                                    if pi == 0:
                                        P.add("dve", lambda e: e.tensor_copy(out=dsta, in_=srca), reads=[tk], writes=ATOK)
                                    else:
                                        P.add("dve", lambda e: e.tensor_tensor(out=dsta, in0=srca, in1=dsta, op=ALU.add), reads=[tk] + ATOK, writes=ATOK)
                                tasks.append((s_part, pv_part))
                    bias3P = dilb[:, 4096 + j * 256:4096 + (j + 1) * 256]
                    for r0 in range(0, 16, 2):
                        def s_part(r0=r0, bias3P=bias3P, qz=qz, kTp=kTp, st=st):
                            si = ctr["S"] % NS; ctr["S"] += 1
                            bkS = bS[si]; pt = PT[:, si * 512:(si + 1) * 512]
                            tk = "bk%d" % SBK[si]
                            for rr in range(2):
                                r_ = r0 + rr
                                if not MASKMUL:
                                    P.add("pe", lambda e, rr=rr: e.matmul(bkS[:, rr * 256:(rr + 1) * 256], lhsT=ident, rhs=bias3P, start=True, stop=False),
                                          reads=["ident", "dilb"], writes=[tk])
                                P.add("pe", lambda e, rr=rr, r_=r_: e.matmul(v2(bkS, rr * 256, 128), lhsT=kTp[:, tsl(r_, 16, 0, 128)], rhs=qz[:, :, tsl(r_, 16, 0, 128)], start=(MASKMUL and rr == 0), stop=True),
                                      reads=["QT%d" % st, "KT%d" % st], writes=[tk])
                            P.add("act", lambda e: e.activation(out=pt[:, 0:512], in_=bkS[:, 0:512], func=AF.Exp), reads=[tk], writes=["PT%d" % si])
                            if MASKMUL:
                                mask_mul(pt[:, 0:512].rearrange("p (r c) -> p r c", r=2), bias3P.unsqueeze(1).to_broadcast([128, 2, 256]), si, "dilb")
                            return si

                        def pv_part(si, r0=r0):
                            oi = ctr["O"] % NO; ctr["O"] += 1
                            bkO = bO[oi]; pt = PT[:, si * 512:(si + 1) * 512]
                            tk = "bk%d" % (5 + oi)
                            for rr in range(2):
                                for hp in range(2):
                                    cs_ = slice(rr * 256 + hp * 128, rr * 256 + hp * 128 + 128)
                                    P.add("pe", lambda e, rr=rr, hp=hp, cs_=cs_: e.matmul(bkO[:, cs_], lhsT=Vt[:, 2, r0 + rr, hp, :], rhs=pt[:, cs_], start=(rr == 0 and hp == 0), stop=True),
                                          reads=["PT%d" % si, "V2"], writes=[tk])
                            dsta = acc2.rearrange("p h (q r) -> p h q r", r=16)[:, :, :, r0:r0 + 2]
                            srca = bkO[:, 0:512].rearrange("p (r h q) -> p h q r", r=2, h=2)
                            P.add("dve", lambda e: e.tensor_tensor(out=dsta, in0=srca, in1=dsta, op=ALU.add), reads=[tk] + ATOK, writes=ATOK)
                        tasks.append((s_part, pv_part))
                else:
                    for g in range(8):
                        if g == 0:
                            ms_, var = [0, 1, 2, 3], 1
                        elif g == 7:
                            ms_, var = [12, 13, 14, 15], 1
                        else:
                            ms_, var = list(range(2 * g - 2, 2 * g + 4)), 0
                        ostate = {}
                        for ki, m in enumerate(ms_):
                            def s_part(m=m, g=g, var=var, qz=qz, kTp=kTp, st=st):
                                si = ctr["S"] % NS; ctr["S"] += 1
                                bkS = bS[si]; pt = PT[:, si * 512:(si + 1) * 512]
                                tk = "bk%d" % SBK[si]
                                sft = 6 - (2 * m - 4 * g)
                                assert 0 <= sft and sft * 64 + 256 <= 896
                                if not MASKMUL:
                                    P.add("pe", lambda e: e.matmul(v2(bkS, 0, 256), lhsT=ident, rhs=nastrip[:, :, var, sft * 64:sft * 64 + 256], start=True, stop=False),
                                          reads=["ident", "nastrip"], writes=[tk])
                                P.add("pe", lambda e: e.matmul(v2(bkS, 0, 256), lhsT=kTp[:, m * 128:(m + 1) * 128], rhs=qz[:, :, g * 256:(g + 1) * 256], start=MASKMUL, stop=True),
                                      reads=["QT%d" % st, "KT%d" % st], writes=[tk])
                                P.add("act", lambda e: e.activation(out=pt[:, 0:512], in_=bkS[:, 0:512], func=AF.Exp), reads=[tk], writes=["PT%d" % si])
                                if MASKMUL:
                                    mask_mul(v2(pt, 0, 256), nastrip[:, :, var, sft * 64:sft * 64 + 256], si, "nastrip")
                                return si

                            def pv_part(si, m=m, ki=ki, g=g, ostate=ostate, nms=len(ms_)):
                                if ki == 0:
                                    ostate["oi"] = ctr["O"] % NO; ctr["O"] += 1
                                oi = ostate["oi"]
                                bkO = bO[oi]; pt = PT[:, si * 512:(si + 1) * 512]
                                tk = "bk%d" % (5 + oi)
                                for hp in range(2):
                                    P.add("pe", lambda e, hp=hp: e.matmul(bkO[:, hp * 256:(hp + 1) * 256], lhsT=Vt[:, 0, m, hp, :], rhs=pt[:, hp * 256:(hp + 1) * 256],
                                                                         start=(ki == 0 and hp == 0), stop=(ki == nms - 1)),
                                          reads=["PT%d" % si, "V0"], writes=[tk])
                                if ki == nms - 1:
                                    dsta = acc2[:, :, g * 256:(g + 1) * 256]
                                    P.add("dve", lambda e: e.tensor_copy(out=dsta, in_=v2(bkO, 0, 256)), reads=[tk], writes=ATOK)
                            tasks.append((s_part, pv_part))

                for hp in range(2):
                    def fin_part(_si, hp=hp, chunk=(j if is_dil else 4 + j)):
                        rows = slice(hp * 64, hp * 64 + 64)
                        P.add("act", lambda e: e.activation(out=rden[0:64, :], in_=acc2[64:128, hp, :], func=AF.Ln), reads=ATOK, writes=["rden"])
                        P.add("act", lambda e: e.activation(out=rden[0:64, :], in_=rden[0:64, :], func=AF.Exp, scale=-1.0), reads=["rden"], writes=["rden"])
                        P.add("dve", lambda e: e.tensor_tensor(out=yT[rows, chunk, :], in0=acc2[0:64, hp, :], in1=rden[0:64, :], op=ALU.mult),
                              reads=ATOK + ["rden"], writes=["yT%d_%d" % (chunk, hp)])
                    tasks.append((None, fin_part))

                LOOK = int(os.environ.get('KLOOK', '2'))
                sis = []
                for ti_, (sp_, pv_) in enumerate(tasks):
                    sis.append(sp_() if sp_ is not None else None)
                    if ti_ >= LOOK:
                        tasks[ti_ - LOOK][1](sis[ti_ - LOOK])
                for ti_ in range(max(0, len(tasks) - LOOK), len(tasks)):
                    tasks[ti_][1](sis[ti_])


            stage('P%d' % s)
            ytoks = ["yT%d_%d" % (c, hp) for c in range(8) for hp in range(2)]
            P.add("pool", lambda e: e.dma_start(out=wout_bf, in_=wout_d.rearrange("(c p) n -> p c n", p=128)),
                  writes=["V0", "V1", "V2", "wout"], dma="wout")
            for c in range(8):
                P.add("dve", lambda e, c=c: e.tensor_scalar(out=wout_bf[:, c, :], in0=wout_bf[:, c, :], scalar1=gout[:, c:c + 1], scalar2=None, op0=ALU.mult),
                      reads=["wout", "vecs"], writes=["wout"])
            def o_prep(tt):
                row0 = tb + tt * 128
                tsl_ = slice(tt * 128, (tt + 1) * 128)
                xi = ctr["x"] % 3; ctr["x"] += 1
                qi = tt % 2
                xtile = xt[xi]; sq_ = sqt2[qi]
                P.add("sp", lambda e: e.dma_start(out=xtile, in_=x_d[row0:row0 + 128, :]),
                      writes=["xt%d_src" % xi], dma="x%d" % xi)
                P.add("pool", lambda e: e.tensor_tensor(out=sq_, in0=yT[:, :, tsl_], in1=yT[:, :, tsl_], op=ALU.mult),
                      reads=ytoks, writes=["sqt%d" % qi])
                bq = bS[2]
                for c in range(8):
                    col = qi * 2 + c // 4
                    P.add("pe", lambda e, c=c, col=col: e.matmul(bq[:, col:col + 1], lhsT=sq_[:, c, :], rhs=ones1[:, 0:1], start=(c % 4 == 0), stop=(c % 4 == 3)),
                          reads=["sqt%d" % qi, "ones1"], writes=["bk4"])
                ms2 = small[:, 16 + xi * 4:16 + xi * 4 + 2]; rs2 = small[:, 16 + xi * 4 + 2:16 + xi * 4 + 4]
                P.add("dve", lambda e: e.tensor_scalar(out=ms2, in0=bq[:, qi * 2:qi * 2 + 2], scalar1=1.0 / 512, scalar2=EPS, op0=ALU.mult, op1=ALU.add),
                      reads=["bk4"], writes=["ms2_%d" % xi])
                P.add("pool", lambda e: e.tensor_tensor(out=rs2, in0=ms2, in1=mhalf[:, 0:2], op=ALU.pow),
                      reads=["ms2_%d" % xi, "mhalf"], writes=["rs2_%d" % xi])
                return (xtile, xi, rs2, row0, tsl_)

            def o_main(ctx, tt):
                xtile, xi, rs2, row0, tsl_ = ctx
                hi_ = tt % 2
                ht = acc[hi_][:, 0:D]
                for half in range(2):
                    hs_ = slice(half * 512, (half + 1) * 512)
                    bA = [bP[0], bP[1]][half]; bB = [bS[0], bS[1]][half]
                    for c in range(4):
                        P.add("pe", lambda e, c=c, bA=bA, hs_=hs_: e.matmul(bA[:, 0:512], lhsT=yT[:, c, tsl_], rhs=wout_bf[:, c, hs_], start=(c == 0), stop=(c == 3)),
                              reads=ytoks + ["wout", "V0", "V1", "V2"], writes=["bk%d" % half])
                    for c in range(4, 8):
                        P.add("pe", lambda e, c=c, bB=bB, hs_=hs_: e.matmul(bB[:, 0:512], lhsT=yT[:, c, tsl_], rhs=wout_bf[:, c, hs_], start=(c == 4), stop=(c == 7)),
                              reads=ytoks + ["wout", "V0", "V1", "V2"], writes=["bk%d" % (2 + half)])
                    P.add("dve", lambda e, hs_=hs_, bA=bA: e.scalar_tensor_tensor(out=ht[:, hs_], in0=bA[:, 0:512], scalar=rs2[:, 0:1], in1=xtile[:, hs_], op0=ALU.mult, op1=ALU.add),
                          reads=["bk%d" % half, "rs2_%d" % xi, "xt%d_src" % xi], writes=["acc%d" % hi_])
                    P.add("dve", lambda e, hs_=hs_, bB=bB: e.scalar_tensor_tensor(out=ht[:, hs_], in0=bB[:, 0:512], scalar=rs2[:, 1:2], in1=ht[:, hs_], op0=ALU.mult, op1=ALU.add),
                          reads=["bk%d" % (2 + half), "rs2_%d" % xi, "acc%d" % hi_], writes=["acc%d" % hi_])
                P.add("sp", lambda e: e.dma_start(out=hs_d[row0:row0 + 128, :], in_=ht),
                      reads=["acc%d" % hi_], writes=["hs%d" % (row0 // 128)], dma="hst%d" % hi_)

            octx = [o_prep(0)]
            for tt in range(16):
                if tt + 1 < 16:
                    octx.append(o_prep(tt + 1))
                o_main(octx[tt], tt)

        stage('O')
        P.add("act", lambda e: e.activation(out=bar[:, 0:1], in_=mhalf[:, 0:1], func=AF.Copy), reads=["mhalf"], writes=["bar_act"])
        P.add("dve", lambda e: e.tensor_copy(out=bar[:, 1:2], in_=mhalf[:, 0:1]), reads=["mhalf"], writes=["bar_dve"])
        P.add("pool", lambda e: e.tensor_copy(out=bar[:, 2:3], in_=mhalf[:, 0:1]), reads=["mhalf"], writes=["bar_pool"])
        for en in ("pe", "act", "dve", "pool", "sp"):
            P.add(en, lambda e: None, reads=["bar_act", "bar_dve", "bar_pool"] + ["hs%d" % i_ for i_ in range(32)], waitonly=True)

        ptr[0] = persist_end
        wd_bf = alloc(32 * D, BF16, (32, D))
        hn2T = alloc(8 * 1024, BF16, (8, 1024))
        actT = alloc(32 * 1024, BF16, (32, 1024))
        wgu = [[alloc(8 * 256, BF16, (8, 256)) for _ in range(2)] for _ in range(3)]
        hbA = alloc(D); hbD = [alloc(D) for _ in range(2)]
        xsA = alloc(D, BF16); xsD = alloc(D, BF16)
        combT = alloc(1024, BF16)
        Cs = [alloc(512, BF16) for _ in range(2)]
        ssb = [alloc(512, BF16) for _ in range(2)]
        s2b = [alloc(512, BF16) for _ in range(2)]
        rt = alloc(960)
        comb_bf = alloc(8 * 16, BF16, (8, 16))
        small2 = alloc(32)

        bG = [banks[0], banks[1]]; bU = [banks[2], banks[3]]; bC = banks[4]; bD = [banks[5], banks[6]]

        for k in range(4):
            P.add("sp", lambda e, k=k: e.dma_start(out=wd_bf[:, 8 * k:8 * k + 8, :],
                                                   in_=wds_d[4 * k:4 * k + 4].rearrange("e (f p) n -> p (e f) n", p=128)),
                  reads=["cvd%d" % ee for ee in range(4 * k, 4 * k + 4)], writes=["wd%d" % k], dma="wd%d" % k)
        wd_toks = ["wd%d" % k for k in range(4)]

        def load_gu(n):
            if n >= 64:
                return
            ex_ = n % 16; wi_ = n % 3
            wgb_, wub_ = wgu[wi_]
            P.add("sp", lambda e: e.dma_start(out=wgb_, in_=wgs_d[ex_].rearrange("(c p) n -> p c n", p=128)),
                  reads=["cvg%d" % ex_], writes=["wg%d" % wi_], dma="wg%d" % wi_)
            P.add("sp", lambda e: e.dma_start(out=wub_, in_=wus_d[ex_].rearrange("(c p) n -> p c n", p=128)),
                  reads=["cvu%d" % ex_], writes=["wu%d" % wi_], dma="wu%d" % wi_)
        load_gu(0)
        load_gu(1)

        c2 = {"h": 0, "G": 0, "C": 0, "D": 0, "w": 0, "y": 0}
        h2_all = [("h2_%d_a" % st) for st in range(8)] + [("h2_%d_b" % st) for st in range(8)]

        hpool = [(hbA, "hbA"), (hbD[0], "hbD0"), (hbD[1], "hbD1")]
        xpool = [(xsA, "xsA"), (xsD, "xsD")]
        tbank2 = [(pb_t, "bk7"), (banks[2].bitcast(BF16), "bk2")]
        bD4 = [(banks[5], "bk5"), (banks[6], "bk6"), (banks[0], "bk0"), (banks[1], "bk1")]

        def prep_hn2(T, st):
            row0 = T * 1024 + st * 128
            if T == 0:
                htile, htok = hpool[st % 3]; xsb, xtok = xpool[st % 2]
            else:
                htile, htok = hpool[0]; xsb, xtok = xpool[0]
            si_ = c2["h"] % 4; c2["h"] += 1
            P.add("sp", lambda e: e.dma_start(out=htile, in_=hs_d[row0:row0 + 128, :]),
                  reads=["hs%d" % (row0 // 128)], writes=[htok], dma=htok)
            ss = small2[:, si_ * 4:si_ * 4 + 1]; ms = small2[:, si_ * 4 + 1:si_ * 4 + 2]; rstd = small2[:, si_ * 4 + 2:si_ * 4 + 3]
            P.add("act", lambda e: e.activation(out=xsb, in_=htile, func=AF.Square, accum_out=ss),
                  reads=[htok], writes=[xtok, "h_ss%d" % si_])
            P.add("dve", lambda e: e.tensor_scalar(out=ms, in0=ss, scalar1=1.0 / D, scalar2=EPS, op0=ALU.mult, op1=ALU.add),
                  reads=["h_ss%d" % si_], writes=["h_ms%d" % si_])
            P.add("pool", lambda e: e.tensor_tensor(out=rstd, in0=ms, in1=mhalf[:, 0:1], op=ALU.pow),
                  reads=["h_ms%d" % si_, "mhalf"], writes=["h_rstd%d" % si_])
            P.add("dve", lambda e: e.tensor_scalar(out=xsb, in0=htile, scalar1=rstd, scalar2=None, op0=ALU.mult),
                  reads=[htok, "h_rstd%d" % si_], writes=[xtok])
            return (xsb, xtok)

        def trans_hn2(ctx, T, st):
            xsb, xtok = ctx
            tbk, ttok = tbank2[st % 2]
            for c in range(8):
                P.add("pe", lambda e, c=c: e.transpose(tbk[:, c * 128:(c + 1) * 128], xsb[:, c * 128:(c + 1) * 128], ident),
                      reads=[xtok, "ident"], writes=[ttok])
            for c in range(8):
                dst = hn2T[:, c, st * 128:(st + 1) * 128]
                src = tbk[:, c * 128:(c + 1) * 128]
                if st % 2 == 0:
                    P.add("dve", lambda e, dst=dst, src=src, c=c: e.tensor_scalar(out=dst, in0=src, scalar1=gffn[:, c:c + 1], scalar2=None, op0=ALU.mult),
                          reads=[ttok, "vecs"], writes=["h2_%d_a" % st])
                else:
                    P.add("act", lambda e, dst=dst, src=src, c=c: e.activation(out=dst, in_=src, func=AF.Copy, scale=gffn[:, c:c + 1]),
                          reads=[ttok, "vecs"], writes=["h2_%d_b" % st])

        def emit_router(T):
            if True:
                for st in range(8):
                    for c in range(8):
                        P.add("pe", lambda e, st=st, c=c: e.matmul(bC[:, st * 32:st * 32 + 20], lhsT=hn2T[:, c, st * 128:(st + 1) * 128], rhs=wr_bf[:, c, :],
                                                                   start=(c == 0), stop=(c == 7)),
                              reads=["h2_%d_a" % st, "h2_%d_b" % st, "wr_bf"], writes=["bk4"])
                lg = rt[:, 0:160].rearrange("p (s n) -> p s n", s=8)
                bc3 = bC[:, 0:256].rearrange("p (s n) -> p s n", s=8)[:, :, 0:20]
                RT = "rt"
                r3 = lambda lo, n: rt[:, lo:lo + 8 * n].rearrange("p (s n) -> p s n", s=8)
                gl = lg[:, :, 0:4]; el = lg[:, :, 4:20]
                gmax = rt[:, 160:168]; g1 = r3(168, 4); gex = r3(200, 4); gsum = rt[:, 232:240]; gw = rt[:, 240:248]
                pen = r3(248, 16); ml = r3(376, 16); m1 = rt[:, 504:512]; m2 = rt[:, 512:520]; k1 = r3(520, 16)
                dm = rt[:, 648:656]; w1 = rt[:, 656:664]; w2 = rt[:, 664:672]; k2 = r3(672, 16); ml2 = r3(800, 16)
                bcast = lambda ap, n: ap.unsqueeze(2).to_broadcast([128, 8, n])
                P.add("dve", lambda e: e.tensor_tensor(out=lg, in0=bc3, in1=brep.unsqueeze(1).to_broadcast([128, 8, 20]), op=ALU.add),
                      reads=["bk4", "vecs"], writes=[RT])
                P.add("dve", lambda e: e.tensor_reduce(out=gmax, in_=gl, axis=AX.X, op=ALU.max), reads=[RT], writes=[RT + "a"])
                P.add("dve", lambda e: e.tensor_tensor(out=g1, in0=gl, in1=bcast(gmax, 4), op=ALU.is_equal), reads=[RT, RT + "a"], writes=[RT + "b"])
                P.add("dve", lambda e: e.tensor_tensor(out=gex, in0=gl, in1=bcast(gmax, 4), op=ALU.subtract), reads=[RT, RT + "a"], writes=[RT + "c"])
                P.add("act", lambda e: e.activation(out=gex, in_=gex, func=AF.Exp), reads=[RT + "c"], writes=[RT + "d"])
                P.add("dve", lambda e: e.tensor_reduce(out=gsum, in_=gex, axis=AX.X, op=ALU.add), reads=[RT + "d"], writes=[RT + "e"])
                P.add("dve", lambda e: e.reciprocal(out=gw, in_=gsum), reads=[RT + "e"], writes=[RT + "f"])
                P.add("dve", lambda e: e.tensor_scalar(out=pen.rearrange("p s (g n) -> p s g n", g=4), in0=g1.unsqueeze(3).to_broadcast([128, 8, 4, 4]),
                                                       scalar1=-1.0, scalar2=30000.0, op0=ALU.add, op1=ALU.mult), reads=[RT + "b"], writes=[RT + "g"])
                P.add("dve", lambda e: e.tensor_tensor(out=ml, in0=el, in1=pen, op=ALU.add), reads=[RT, RT + "g"], writes=[RT + "h"])
                P.add("dve", lambda e: e.tensor_reduce(out=m1, in_=ml, axis=AX.X, op=ALU.max), reads=[RT + "h"], writes=[RT + "i"])
                P.add("dve", lambda e: e.tensor_tensor(out=k1, in0=ml, in1=bcast(m1, 16), op=ALU.is_equal), reads=[RT + "h", RT + "i"], writes=[RT + "j"])
                P.add("dve", lambda e: e.scalar_tensor_tensor(out=ml2, in0=k1, scalar=-60000.0, in1=ml, op0=ALU.mult, op1=ALU.add), reads=[RT + "h", RT + "j"], writes=[RT + "k"])
                P.add("dve", lambda e: e.tensor_reduce(out=m2, in_=ml2, axis=AX.X, op=ALU.max), reads=[RT + "k"], writes=[RT + "l"])
                P.add("dve", lambda e: e.tensor_tensor(out=k2, in0=ml2, in1=bcast(m2, 16), op=ALU.is_equal), reads=[RT + "k", RT + "l"], writes=[RT + "m"])
                P.add("dve", lambda e: e.tensor_tensor(out=dm, in0=m2, in1=m1, op=ALU.subtract), reads=[RT + "l", RT + "i"], writes=[RT + "n"])
                P.add("act", lambda e: e.activation(out=dm, in_=dm, func=AF.Exp), reads=[RT + "n"], writes=[RT + "o"])
                P.add("dve", lambda e: e.tensor_scalar(out=dm, in0=dm, scalar1=1.0, scalar2=None, op0=ALU.add), reads=[RT + "o"], writes=[RT + "p"])
                P.add("dve", lambda e: e.reciprocal(out=w1, in_=dm), reads=[RT + "p"], writes=[RT + "q"])
                P.add("dve", lambda e: e.tensor_scalar(out=w2, in0=w1, scalar1=-1.0, scalar2=1.0, op0=ALU.mult, op1=ALU.add), reads=[RT + "q"], writes=[RT + "r"])
                P.add("dve", lambda e: e.tensor_tensor(out=w1, in0=w1, in1=gw, op=ALU.mult), reads=[RT + "q", RT + "r", RT + "f"], writes=[RT + "s"])
                P.add("dve", lambda e: e.tensor_tensor(out=w2, in0=w2, in1=gw, op=ALU.mult), reads=[RT + "r", RT + "f"], writes=[RT + "t"])
                P.add("dve", lambda e: e.tensor_tensor(out=k1, in0=k1, in1=bcast(w1, 16), op=ALU.mult), reads=[RT + "j", RT + "s", RT + "k"], writes=[RT + "u"])
                P.add("dve", lambda e: e.tensor_tensor(out=k2, in0=k2, in1=bcast(w2, 16), op=ALU.mult), reads=[RT + "m", RT + "t"], writes=[RT + "v"])
                P.add("dve", lambda e: e.tensor_tensor(out=comb_bf, in0=k1, in1=k2, op=ALU.add), reads=[RT + "u", RT + "v"], writes=["comb_bf"])
                for st in range(8):
                    P.add("pe", lambda e, st=st: e.transpose(pb_t[0:16, st * 128:(st + 1) * 128], comb_bf[:, st, :], ident),
                          reads=["comb_bf", "ident"] + h2_all, writes=["bk7"])
                P.add("dve", lambda e: e.tensor_copy(out=combT[0:16, :], in_=pb_t[0:16, 0:1024]), reads=["bk7"], writes=["combT"])


        def emit_experts(T):
            if True:
                for ex in range(16):
                    nflat = T * 16 + ex
                    wi = nflat % 3
                    wgb, wub = wgu[wi]
                    load_gu(nflat + 2)
                    for half in range(2):
                        hs_ = slice(half * 512, (half + 1) * 512)
                        ci = c2["C"] % 2; c2["C"] += 1
                        P.add("pe", lambda e, ex=ex, hs_=hs_: e.matmul(bC[:, 0:512], lhsT=sel[0:16, ex, :], rhs=combT[0:16, hs_], start=True, stop=True),
                              reads=["sel", "combT"], writes=["bk4"])
                        P.add("dve", lambda e, ci=ci: e.tensor_copy(out=Cs[ci], in_=bC[:, 0:512]), reads=["bk4"], writes=["Cs%d" % ci])
                        for fc in range(2):
                            gi = c2["G"] % 2; c2["G"] += 1
                            for c in range(8):
                                P.add("pe", lambda e, gi=gi, c=c, fc=fc, hs_=hs_, wgb=wgb: e.matmul(bG[gi][:, 0:512], lhsT=wgb[:, c, fc * 128:(fc + 1) * 128], rhs=hn2T[:, c, hs_],
                                                                                                  start=(c == 0), stop=(c == 7)),
                                      reads=["wg%d" % wi] + h2_all, writes=["bk%d" % gi])
                            for c in range(8):
                                P.add("pe", lambda e, gi=gi, c=c, fc=fc, hs_=hs_, wub=wub: e.matmul(bU[gi][:, 0:512], lhsT=wub[:, c, fc * 128:(fc + 1) * 128], rhs=hn2T[:, c, hs_],
                                                                                                  start=(c == 0), stop=(c == 7)),
                                      reads=["wu%d" % wi] + h2_all, writes=["bk%d" % (2 + gi)])
                            P.add("act", lambda e, gi=gi: e.activation(out=ssb[gi], in_=bG[gi][:, 0:512], func=AF.Silu), reads=["bk%d" % gi], writes=["ssb%d" % gi])
                            P.add("pool", lambda e, gi=gi, ci=ci: e.tensor_tensor(out=s2b[gi], in0=ssb[gi], in1=Cs[ci], op=ALU.mult),
                                  reads=["ssb%d" % gi, "Cs%d" % ci], writes=["s2b%d" % gi])
                            P.add("dve", lambda e, gi=gi, ex=ex, fc=fc, hs_=hs_: e.tensor_tensor(out=actT[:, ex * 2 + fc, hs_], in0=bU[gi][:, 0:512], in1=s2b[gi], op=ALU.mult),
                                  reads=["bk%d" % (2 + gi), "s2b%d" % gi], writes=["actT"])


        def down_mm(T, st):
            row0 = T * 1024 + st * 128
            hi = c2["y"] % 2; c2["y"] += 1
            htile, htok = hpool[1 + hi]
            P.add("sp", lambda e: e.dma_start(out=htile, in_=hs_d[row0:row0 + 128, :]),
                  reads=["hs%d" % (row0 // 128)], writes=[htok], dma=htok)
            for dh in range(2):
                bk_, btok = bD4[c2["D"] % 4]; c2["D"] += 1
                ds_ = slice(dh * 512, (dh + 1) * 512)
                for kk in range(32):
                    P.add("pe", lambda e, bk_=bk_, kk=kk, ds_=ds_: e.matmul(bk_[:, 0:512], lhsT=actT[:, kk, st * 128:(st + 1) * 128], rhs=wd_bf[:, kk, ds_],
                                                                          start=(kk == 0), stop=(kk == 31)),
                          reads=["actT"] + wd_toks, writes=[btok])
                P.add("dve", lambda e, bk_=bk_, ds_=ds_: e.tensor_tensor(out=htile[:, ds_], in0=bk_[:, 0:512], in1=htile[:, ds_], op=ALU.add),
                      reads=[btok, htok], writes=[htok])
            return (htile, htok, row0, hi)

        def down_fin(ctx):
            htile, htok, row0, yi = ctx
            junk, jtok = xpool[1]
            ss = small2[:, 16 + yi * 4:16 + yi * 4 + 1]; ms = small2[:, 16 + yi * 4 + 1:16 + yi * 4 + 2]; rstd = small2[:, 16 + yi * 4 + 2:16 + yi * 4 + 3]
            P.add("act", lambda e: e.activation(out=junk, in_=htile, func=AF.Square, accum_out=ss),
                  reads=[htok], writes=[jtok, "f_ss%d" % yi])
            P.add("dve", lambda e: e.tensor_scalar(out=ms, in0=ss, scalar1=1.0 / D, scalar2=EPS, op0=ALU.mult, op1=ALU.add),
                  reads=["f_ss%d" % yi], writes=["f_ms%d" % yi])
            P.add("pool", lambda e: e.tensor_tensor(out=rstd, in0=ms, in1=mhalf[:, 0:1], op=ALU.pow),
                  reads=["f_ms%d" % yi, "mhalf"], writes=["f_rstd%d" % yi])
            P.add("dve", lambda e: e.scalar_tensor_tensor(out=htile, in0=htile, scalar=rstd, in1=gfin, op0=ALU.mult, op1=ALU.mult),
                  reads=[htok, "f_rstd%d" % yi, "gfin"], writes=[htok])
            P.add("sp", lambda e: e.dma_start(out=y_d[row0:row0 + 128, :], in_=htile),
                  reads=[htok], dma="yst%d" % yi)

        for st in range(8):
            trans_hn2(prep_hn2(0, st), 0, st)
        emit_router(0)
        for T in range(4):
            emit_experts(T)
            for st in range(8):
                cx = prep_hn2(T + 1, st) if T < 3 else None
                dx = down_mm(T, st)
                if cx is not None:
                    trans_hn2(cx, T + 1, st)
                down_fin(dx)
            if T < 3:
                emit_router(T + 1)

    except StopBuild:
        pass

    sems = {}
    for en in Prog.ENGS:
        sems["eng:" + en] = es.enter_context(nc.semaphore("s_" + en))
    for sl in sorted(P.dma_slots):
        sems["dma:" + sl] = es.enter_context(nc.semaphore("d_" + sl))
    if os.environ.get('KMAXOPS'):
        P.ops = P.ops[:int(os.environ['KMAXOPS'])]
        for i_, o_ in enumerate(P.ops[-3:]):
            print('LASTOPS', o_.eng, o_.idx)
    body = P.emit(sems)
    if os.environ.get('KSTATS'):
        print('PROG stats', P.stats, 'ndma_slots', len(P.dma_slots))
    with nc.Block() as block:
        block.sync(body("sp"))
        block.tensor(body("pe"))
        block.scalar(body("act"))
        block.vector(body("dve"))
        block.gpsimd(body("pool"))
    es.close()
    return nc


def _dil_bias():
    out = np.full((128, 4 * 2 * 512 + 4 * 256), NEG, np.float32)
    k = np.arange(128)[:, None]; q = np.arange(128)[None, :]

    def f(delta, slope, d):
        return np.where(np.abs(delta) <= 64, -slope * d * np.abs(delta), NEG).astype(np.float32)
    for j in range(4):
        for hp in range(2):
            slope = 2.0 ** (-(2 * j + hp + 1))
            for pi, d in enumerate((1, 4)):
                c0 = (j * 2 + pi) * 512
                out[:, c0 + hp * 128:c0 + (hp + 1) * 128] = f(k - 64 - q, slope, d)
                out[:, c0 + 256 + hp * 128:c0 + 256 + (hp + 1) * 128] = f(k + 64 - q, slope, d)
            c0 = 4096 + j * 256
            out[:, c0 + hp * 128:c0 + (hp + 1) * 128] = f(k - q, slope, 16)
    return out


def _na_strips(rpb):
    kc = np.arange(64)[:, None]; qc = np.arange(64)[None, :]
    cs = np.clip(qc - 8, 0, 48)
    col_ok = (kc >= cs) & (kc < cs + 16)
    dcidx = np.clip(kc - qc + 15, 0, 30)
    out = np.full((4, 128, 2, 2, 896), NEG, np.float32)
    for h in range(8):
        j, hp = divmod(h, 2)
        for var in range(2):
            for i in range(2):
                for u in range(14):
                    dlt = 6 + i - u
                    ok = (-4 <= dlt <= 3) if var == 0 else (-7 <= dlt <= 7)
                    if not ok:
                        continue
                    blk = np.where(col_ok, rpb[h, dlt + 7][dcidx], NEG).astype(np.float32)
                    out[j, i * 64:(i + 1) * 64, hp, var, u * 64:(u + 1) * 64] = blk
    return out.reshape(4, 128, 4 * 896)


_NC_CACHE = {}


def kernel(x, norm_mix_g, w_in, rpb, g_out_dil, g_out_na, w_out, norm_ffn_g, w_group, b_group, w_router, b_router,
           w_gate, w_up, w_down, norm_final_g):
    f = lambda a: np.ascontiguousarray(np.asarray(a, dtype=np.float32))
    x = f(x).reshape(16 * S, D)
    col = lambda g: f(g).reshape(8, 128).T
    vecs = np.concatenate([col(norm_mix_g[0]), col(norm_ffn_g[0]),
                           col(np.concatenate([f(g_out_dil[0]), f(g_out_na[0])])),
                           np.broadcast_to(np.concatenate([f(b_group[0]), f(b_router[0])])[None, :], (128, 20))], axis=1)
    vecs = np.ascontiguousarray(vecs, dtype=np.float32)
    gfin = np.ascontiguousarray(np.broadcast_to(f(norm_final_g)[None, :], (128, D)))
    gmixr = np.ascontiguousarray(np.broadcast_to(f(norm_mix_g[0])[None, :], (128, D)))
    wr = np.ascontiguousarray(np.concatenate([f(w_group[0]), f(w_router[0])], axis=1))
    shared = {
        "w_in": f(w_in[0]), "w_out": f(w_out[0]), "w_gate": f(w_gate[0]), "w_up": f(w_up[0]), "w_down": f(w_down[0]),
        "wr": wr, "vecs": vecs, "gfin": gfin, "gmixr": gmixr, "dilb": _dil_bias(), "nas": _na_strips(f(rpb[0])),
    }
    if "nc" not in _NC_CACHE:
        _NC_CACHE["nc"] = build_program()
    nc = _NC_CACHE["nc"]
    in_maps = []
    for i in range(NCORES):
        m = dict(shared)
        m["x"] = np.ascontiguousarray(x[i * TOK:(i + 1) * TOK])
        in_maps.append(m)
    res = run_bass_kernel_spmd(nc, in_maps, core_ids=list(range(NCORES)))
    y = np.concatenate([r["y"] for r in res.results], axis=0)
    if DEBUG:
        kernel.dbg = [r.get("dbg") for r in res.results]
    return y.reshape(16, S, D).astype(np.float32)
```

```python
import numpy as np
from contextlib import ExitStack
import concourse.bass as bass
import concourse.mybir as mybir
from concourse.bass_utils import run_bass_kernel_spmd

F32 = mybir.dt.float32
BF16 = mybir.dt.bfloat16
AF = mybir.ActivationFunctionType
ALU = mybir.AluOpType
AX = mybir.AxisListType

NCORES = 8
S = 2048
D = 1024
TOK = 4096
NEG = -30000.0
EPS = 1e-6
import os
DEBUG = bool(os.environ.get('KSTOP', ''))
STOP = os.environ.get('KSTOP', '')


class StopBuild(Exception):
    pass


DUMPS = {}


def stage(name):
    if STOP and name == STOP:
        if name in DUMPS:
            DUMPS[name]()
        raise StopBuild()


class Op:
    __slots__ = ("eng", "fn", "deps", "is_dma", "sem", "semval", "needs_signal", "sigcount", "eidx", "idx", "wo")


class Prog:
    ENGS = ("pe", "act", "dve", "pool", "sp")

    def __init__(self):
        self.ops = []
        self.last_writer = {}
        self.readers = {}
        self.eng_count = {e: 0 for e in self.ENGS}
        self.dma_slots = set()

    def add(self, eng, fn, reads=(), writes=(), dma=None, waitonly=False):
        op = Op()
        op.eng = eng; op.fn = fn; op.idx = len(self.ops)
        op.is_dma = dma is not None
        op.wo = waitonly
        op.sem = dma; op.semval = 0; op.needs_signal = False; op.sigcount = 0
        op.eidx = self.eng_count[eng]; self.eng_count[eng] += 1
        if dma is not None:
            self.dma_slots.add(dma)
        deps = {}
        for t in reads:
            w = self.last_writer.get(t)
            if w is not None:
                deps[w] = "raw"
        for t in writes:
            w = self.last_writer.get(t)
            if w is not None and w not in deps:
                deps[w] = "waw"
            for r in self.readers.get(t, ()):
                if r not in deps and r != op.idx:
                    deps[r] = "war"
        for t in (() if waitonly else reads):
            lst = self.readers.setdefault(t, [])
            if not op.is_dma:
                lst[:] = [r for r in lst if self.ops[r].is_dma or self.ops[r].eng != eng]
            lst.append(op.idx)
        for t in writes:
            self.last_writer[t] = op.idx
            self.readers[t] = []
        op.deps = deps
        self.ops.append(op)
        return op

    def _need_wait(self, op, p, kind):
        if p.is_dma:
            return True
        if p.eng != op.eng:
            return True
        if op.is_dma:
            return True
        if p.eng == "pe":
            return False
        if kind == "raw" and ((op.eidx - p.eidx) <= 3 or p.eng == "pool"):
            return True
        return False

    def emit(self, sem_ctx):
        ops = self.ops
        for op in ops:
            for d, kind in op.deps.items():
                p = ops[d]
                if (not p.is_dma) and self._need_wait(op, p, kind):
                    p.needs_signal = True
        if os.environ.get('KALLSIG'):
            for op in ops:
                if not op.is_dma and op.fn(None) if False else (not op.is_dma and not getattr(op, "wo", False)):
                    op.needs_signal = True
        cnt = {e: 0 for e in self.ENGS}
        dcnt = {}
        for op in ops:
            if op.is_dma:
                dcnt[op.sem] = dcnt.get(op.sem, 0) + 16
                op.semval = dcnt[op.sem]
            elif op.needs_signal:
                cnt[op.eng] += 1
                op.sigcount = cnt[op.eng]
        by_eng = {e: [o for o in ops if o.eng == e] for e in self.ENGS}
        self.stats = (dict(cnt), {e: len(v) for e, v in by_eng.items()})

        def body(ename):
            def run(eng):
                waited = {}
                for op in by_eng[ename]:
                    for d, kind in op.deps.items():
                        p = ops[d]
                        if not self._need_wait(op, p, kind):
                            continue
                        if p.is_dma:
                            key = "dma:" + p.sem; val = p.semval
                        else:
                            key = "eng:" + p.eng; val = p.sigcount
                        if waited.get(key, 0) >= val:
                            continue
                        waited[key] = val
                        eng.wait_ge(sem_ctx[key], val)
                    ins = op.fn(eng)
                    if op.is_dma:
                        ins.then_inc(sem_ctx["dma:" + op.sem], 16)
                    elif op.needs_signal:
                        ins.then_inc(sem_ctx["eng:" + ename], 1)
                for op in by_eng[ename]:
                    if op.is_dma:
                        key = "dma:" + op.sem
                        if waited.get(key, 0) < dcnt[op.sem]:
                            waited[key] = dcnt[op.sem]
                            eng.wait_ge(sem_ctx[key], dcnt[op.sem])
            return run
        return body


def tsl(r, d, p0, n):
    return slice(r + d * p0, r + d * (p0 + n - 1) + 1, d)


def build_program():
    nc = bass.Bass("TRN2", target_bir_lowering=False)
    dt_in = lambda n, s, dt=F32: nc.dram_tensor(n, s, dt, kind="ExternalInput").ap()
    x_d = dt_in("x", [TOK, D])
    win_d = dt_in("w_in", [D, 3072])
    wout_d = dt_in("w_out", [D, D])
    wg_d = dt_in("w_gate", [16, D, 256])
    wu_d = dt_in("w_up", [16, D, 256])
    wd_d = dt_in("w_down", [16, 256, D])
    wr_d = dt_in("wr", [D, 20])
    vec_d = dt_in("vecs", [128, 24 + 20])
    gfin_d = dt_in("gfin", [128, D])
    gmixr_d = dt_in("gmixr", [128, D])
    dilb_d = dt_in("dilb", [128, 5120])
    nas_d = dt_in("nas", [4, 128, 4 * 896])
    y_d = nc.dram_tensor("y", [TOK, D], F32, kind="ExternalOutput").ap()
    hs_d = nc.dram_tensor("hscr", [TOK, D], F32, kind="Internal").ap()
    wgs_d = nc.dram_tensor("wg_bf", [16, D, 256], BF16, kind="Internal").ap()
    wus_d = nc.dram_tensor("wu_bf", [16, D, 256], BF16, kind="Internal").ap()
    wds_d = nc.dram_tensor("wd_bf", [16, 256, D], BF16, kind="Internal").ap()
    dbg_d = None
    if DEBUG:
        dbg_d = nc.dram_tensor("dbg", [128, 8 * 2048], F32, kind="ExternalOutput").ap()

    P = Prog()
    es = ExitStack()
    ARENA = 53200
    arena = es.enter_context(nc.sbuf_tensor("arena", [128, ARENA], F32))
    arena_bf = arena.bitcast(BF16)
    ptr = [0]

    def alloc(ncols, dt=F32, shape=None):
        n32 = ncols if dt == F32 else (ncols + 1) // 2
        a = ptr[0]
        ptr[0] += n32
        assert ptr[0] <= ARENA, ("SBUF overflow", ptr[0])
        if dt == F32:
            ap = arena[:, a:a + ncols]
        else:
            ap = arena_bf[:, 2 * a:2 * a + ncols]
        if shape is not None:
            names = " ".join("d%d" % i for i in range(len(shape)))
            kw = {"d%d" % i: shape[i] for i in range(len(shape))}
            ap = ap.rearrange("p (%s) -> p %s" % (names, names), **kw)
        return ap

    pb_t = es.enter_context(nc.psum_tensor("pb_t", [128, 1024], BF16))
    banks = [es.enter_context(nc.psum_tensor("bk%d" % i, [128, 512], F32)) for i in range(7)]
    tbank = [(pb_t, "bk7"), (banks[6].bitcast(BF16), "bk6")]

    ident = alloc(128, BF16)
    sel = alloc(16 * 128, BF16, (16, 128))
    vecs = alloc(44)
    gfin = alloc(D)
    wr_bf = alloc(8 * 20, BF16, (8, 20))
    mhalf = alloc(4)
    ones1 = alloc(2, BF16)
    bar = alloc(4)
    persist_end = ptr[0]

    try:
        P.add("pool", lambda e: e.memset(ident, 0.0), writes=["ident"])
        P.add("pool", lambda e: e.affine_select(out=ident, in_=ident, pattern=[[-1, 128]], compare_op=ALU.not_equal,
                                                fill=1.0, base=0, channel_multiplier=1), reads=["ident"], writes=["ident"])
        P.add("pool", lambda e: e.memset(sel, 0.0), writes=["sel"])
        P.add("pool", lambda e: e.affine_select(out=sel[0:16], in_=sel[0:16], pattern=[[-1, 16], [0, 128]],
                                                compare_op=ALU.not_equal, fill=1.0, base=0, channel_multiplier=1),
              reads=["sel"], writes=["sel"])
        P.add("pool", lambda e: e.memset(mhalf, -0.5), writes=["mhalf"])
        P.add("pool", lambda e: e.memset(ones1, 1.0), writes=["ones1"])
        P.add("sp", lambda e: e.dma_start(out=vecs, in_=vec_d), writes=["vecs"], dma="c0")
        P.add("sp", lambda e: e.dma_start(out=gfin, in_=gfin_d), writes=["gfin"], dma="c1")
        P.add("pool", lambda e: e.dma_start(out=wr_bf, in_=wr_d.rearrange("(c p) n -> p c n", p=128)),
              writes=["wr_bf"], dma="c2")
        gmix = vecs[:, 0:8]; gffn = vecs[:, 8:16]; gout = vecs[:, 16:24]; brep = vecs[:, 24:44]

        def rms_rstd(src, ss, ms, rstd, junk, tag, ncols=D):
            P.add("act", lambda e: e.activation(out=junk, in_=src, func=AF.Square, accum_out=ss),
                  reads=[tag + "_src"], writes=[tag + "_junk", tag + "_ss"])
            P.add("dve", lambda e: e.tensor_scalar(out=ms, in0=ss, scalar1=1.0 / ncols, scalar2=EPS, op0=ALU.mult, op1=ALU.add),
                  reads=[tag + "_ss"], writes=[tag + "_ms"])
            P.add("pool", lambda e: e.tensor_tensor(out=rstd, in0=ms, in1=mhalf[:, 0:1], op=ALU.pow),
                  reads=[tag + "_ms", "mhalf"], writes=[tag + "_rstd"])

        hnT = alloc(8 * S, BF16, (8, S))
        yT = alloc(8 * S, BF16, (8, S))
        dilb = alloc(5120, BF16)
        wblk = [[alloc(8 * 128, BF16, (8, 128)) for _ in range(3)] for _ in range(2)]
        QT = [alloc(2 * S, BF16, (2, S)) for _ in range(2)]
        KT = [alloc(S, BF16) for _ in range(2)]
        Vt_raw = alloc(3 * 16 * 256, BF16)
        Vt = Vt_raw.rearrange("p (l t h c) -> p l t h c", l=3, t=16, h=2, c=128)
        wout_bf = Vt_raw[:, 0:8 * D].rearrange("p (c n) -> p c n", c=8)
        acc2 = alloc(2 * S, F32, (2, S))
        acc = [acc2[:, 0, :], acc2[:, 1, :]]
        VTb = alloc(S, BF16)
        gmixr = alloc(D)
        rden = alloc(S)
        NS = int(os.environ.get('KNS', '3')); NO = int(os.environ.get('KNO', '2'))
        PT = alloc(NS * 512, BF16)
        nastrip = alloc(4 * 896, BF16, (2, 2, 896))
        xs = [alloc(D, BF16) for _ in range(3)]
        xt = [alloc(D) for _ in range(3)]
        sqt2 = [alloc(8 * 128, BF16, (8, 128)) for _ in range(2)]
        small = alloc(64)
        p1_end = ptr[0]


        def dump(items):
            col = [0]
            for ap, toks in items:
                n = ap.shape[-1] if len(ap.shape) == 2 else None
                assert n is not None
                a = col[0]; col[0] += n
                P.add("pool", lambda e, ap=ap, a=a, n=n: e.dma_start(out=dbg_d[0:ap.shape[0], a:a + n], in_=ap, max_dma_last_dim=2048),
                      reads=toks, dma="dbg")
        alltok = lambda: list(P.last_writer.keys())
        DUMPS['H0'] = lambda: dump([(hnT[:, c, :], alltok()) for c in range(8)])
        DUMPS['I0_1'] = lambda: dump([(QT[0], alltok()), (KT[0], alltok()), (acc[0], alltok()), (acc[1], alltok()), (yT[:, 0, :], alltok()),
                                      (Vt_raw[:, 0:4096], alltok())])
        DUMPS['I0_5'] = lambda: dump([(QT[0], alltok()), (KT[0], alltok()), (acc[0], alltok()), (acc[1], alltok()), (yT[:, 4, :], alltok()),
                                      (Vt_raw[:, 0:4096], alltok())])
        for k_ in (2, 3, 4, 6, 7):
            DUMPS['I0_%d' % k_] = lambda: dump([(yT[:, c, :], alltok()) for c in range(8)])
        if os.environ.get('KD2'):
            DUMPS['I0_2'] = lambda: dump([(QT[0], alltok()), (KT[0], alltok()), (QT[1], alltok()), (KT[1], alltok()), (wblk[0][0][:, 0, :], alltok()), (wblk[0][2][:, 0, :], alltok()), (wblk[1][0][:, 0, :], alltok())])
        DUMPS['P0'] = lambda: dump([(yT[:, c, :], alltok()) for c in range(8)])
        bP = [banks[0], banks[1]]
        bPbf = [banks[0].bitcast(BF16), banks[1].bitcast(BF16)]
        bS = [banks[2], banks[3], banks[4], banks[0], banks[1]][:NS]
        SBK = [2, 3, 4, 0, 1]
        bO = [banks[5], banks[6], pb_t.bitcast(F32)][:NO]

        P.add("pool", lambda e: e.dma_start(out=dilb, in_=dilb_d, max_dma_last_dim=4096), writes=["dilb"], dma="c3")
        if DEBUG:
            P.add("pool", lambda e: e.memset(yT, 0.0), writes=["yT%d_%d" % (c, hp) for c in range(8) for hp in range(2)])

        for st_ in range(2):
            P.add("pool", lambda e, st_=st_: e.memset(QT[st_], 0.0), writes=["QT%d" % st_])
        P.add("sp", lambda e: e.dma_start(out=gmixr, in_=gmixr_d), writes=["gmixr"], dma="c4")
        MASKMUL = os.environ.get('KMASK', '0') == '1'
        if MASKMUL:
            for c_ in range(0, 5120, 512):
                P.add("act", lambda e, c_=c_: e.activation(out=dilb[:, c_:c_ + 512], in_=dilb[:, c_:c_ + 512], func=AF.Exp), reads=["dilb"], writes=["dilb"])
        mctr = {"n": 0}

        def mask_mul(ptv, mv, si, mtok):
            eng = "pool" if (mctr["n"] % int(os.environ.get('KMASKDVE', '3'))) != 0 else "dve"
            mctr["n"] += 1
            P.add(eng, lambda e: e.tensor_tensor(out=ptv, in0=ptv, in1=mv, op=ALU.mult), reads=["PT%d" % si, mtok], writes=["PT%d" % si])
        ctr = {"S": 0, "O": 0, "P": 0, "x": 0}

        for s in range(2):
            tb = s * S
            def h_prep(tt):
                xi = ctr["x"] % 3; ctr["x"] += 1
                xtile = xt[xi]; xsb = xs[xi]
                row0 = tb + tt * 128
                P.add("sp", lambda e: e.dma_start(out=xtile, in_=x_d[row0:row0 + 128, :]),
                      writes=["xt%d_src" % xi], dma="x%d" % xi)
                ss = small[:, xi * 4:xi * 4 + 1]; ms = small[:, xi * 4 + 1:xi * 4 + 2]; rstd = small[:, xi * 4 + 2:xi * 4 + 3]
                P.add("act", lambda e: e.activation(out=xsb, in_=xtile, func=AF.Square, accum_out=ss),
                      reads=["xt%d_src" % xi], writes=["xs%d" % xi, "xt%d_ss" % xi])
                P.add("dve", lambda e: e.tensor_scalar(out=ms, in0=ss, scalar1=1.0 / D, scalar2=EPS, op0=ALU.mult, op1=ALU.add),
                      reads=["xt%d_ss" % xi], writes=["xt%d_ms" % xi])
                P.add("pool", lambda e: e.tensor_tensor(out=rstd, in0=ms, in1=mhalf[:, 0:1], op=ALU.pow),
                      reads=["xt%d_ms" % xi, "mhalf"], writes=["xt%d_rstd" % xi])
                P.add("dve", lambda e: e.scalar_tensor_tensor(out=xsb, in0=xtile, scalar=rstd, in1=gmixr, op0=ALU.mult, op1=ALU.mult),
                      reads=["xt%d_src" % xi, "xt%d_rstd" % xi, "gmixr"], writes=["xs%d" % xi])
                return (xsb, xi)

            def h_trans(ctx, tt):
                xsb, xi = ctx
                tbk, ttok = tbank[tt % 2]
                for c in range(8):
                    P.add("pe", lambda e, c=c: e.transpose(tbk[:, c * 128:(c + 1) * 128], xsb[:, c * 128:(c + 1) * 128], ident),
                          reads=["xs%d" % xi, "ident"], writes=[ttok])
                dst = hnT[:, :, tt * 128:(tt + 1) * 128]
                src = tbk[:, 0:1024].rearrange("p (c q) -> p c q", c=8)
                if tt % 2 == 0:
                    P.add("dve", lambda e: e.tensor_copy(out=dst, in_=src), reads=[ttok], writes=["hn%d_a" % tt])
                else:
                    P.add("act", lambda e: e.activation(out=dst, in_=src, func=AF.Copy), reads=[ttok], writes=["hn%d_b" % tt])

            hctx = [h_prep(0), h_prep(1)]
            for tt in range(16):
                if tt + 2 < 16:
                    hctx.append(h_prep(tt + 2))
                h_trans(hctx[tt], tt)

            def hn_tokens(tiles):
                out = []
                for t in tiles:
                    out += ["hn%d_a" % t, "hn%d_b" % t]
                return out

            stage('H%d' % s)
            for lay_ in range(3):
                P.add("pool", lambda e, lay_=lay_: e.memset(Vt[:, lay_, :, :, 64:128], 1.0), writes=["V%d" % lay_])
            for item in range(8):
                stage('I%d_%d' % (s, item))
                if os.environ.get('KBAR'):
                    P.add("act", lambda e: e.activation(out=bar[:, 0:1], in_=mhalf[:, 0:1], func=AF.Copy), reads=["mhalf"], writes=["bar_act"])
                    P.add("dve", lambda e: e.tensor_copy(out=bar[:, 1:2], in_=mhalf[:, 0:1]), reads=["mhalf"], writes=["bar_dve"])
                    P.add("pool", lambda e: e.tensor_copy(out=bar[:, 2:3], in_=mhalf[:, 0:1]), reads=["mhalf"], writes=["bar_pool"])
                    for en in ("pe", "act", "dve", "pool", "sp"):
                        P.add(en, lambda e: None, reads=["bar_act", "bar_dve", "bar_pool"], waitonly=True)
                if os.environ.get('KSNAP') and item == 1:
                    P.add("pool", lambda e: e.tensor_copy(out=yT[:, 7, :], in_=yT[:, 0, :]), reads=["yT0_0", "yT0_1"], writes=["yT7_0", "yT7_1"])
                    P.add("pool", lambda e: e.tensor_copy(out=yT[:, 6, :], in_=acc[0][:, :]), reads=["acc0"], writes=["yT6_0", "yT6_1"])
                is_dil = item < 4
                j = item % 4
                st = item % 2
                colbase = 0 if is_dil else 1536
                wq, wk, wv = wblk[st]
                for wi, (wb, off) in enumerate(((wq, 0), (wk, 512), (wv, 1024))):
                    c0 = colbase + off + j * 128
                    P.add("pool", lambda e, wb=wb, c0=c0: e.dma_start(out=wb, in_=win_d.rearrange("(c p) n -> p c n", p=128)[:, :, c0:c0 + 128]),
                          writes=["wb%d_%d" % (st, wi)], dma="wb%d_%d" % (st, wi))
                if not is_dil:
                    P.add("pool", lambda e, j=j: e.dma_start(out=nastrip, in_=nas_d[j].rearrange("p (h v n) -> p h v n", h=2, v=2), max_dma_last_dim=3584),
                          writes=["nastrip"], dma="nas")
                    if MASKMUL:
                        nflat = nastrip.rearrange("p h v n -> p (h v n)")
                        for c_ in range(0, 3584, 512):
                            P.add("act", lambda e, c_=c_, nflat=nflat: e.activation(out=nflat[:, c_:c_ + 512], in_=nflat[:, c_:c_ + 512], func=AF.Exp), reads=["nastrip"], writes=["nastrip"])
                if s == 0:
                    for ex_ in (2 * item, 2 * item + 1):
                        for nm_, src_, dst_ in (("g", wg_d, wgs_d), ("u", wu_d, wus_d), ("d", wd_d, wds_d)):
                            P.add("pool", lambda e, src_=src_, dst_=dst_, ex_=ex_: e.dma_start(out=dst_[ex_], in_=src_[ex_], max_dma_last_dim=4096),
                                  writes=["cv%s%d" % (nm_, ex_)], dma="cv%s%d" % (nm_, ex_))
                for which, (wb, dstT, scl) in enumerate(((wq, QT[st], 0.125), (wk, KT[st], 1.0))):
                    for tc in range(4):
                        bi = ctr["P"] % 2; ctr["P"] += 1
                        bk = bP[bi]
                        for c in range(8):
                            P.add("pe", lambda e, bk=bk, wb=wb, c=c, tc=tc: e.matmul(bk[:, 0:512], lhsT=wb[:, c, :], rhs=hnT[:, c, tc * 512:(tc + 1) * 512],
                                                                                     start=(c == 0), stop=(c == 7)),
                                  reads=["wb%d_%d" % (st, which)] + hn_tokens(range(4 * tc, 4 * tc + 4)), writes=["bk%d" % bi])
                        dst = dstT[:, tc * 512:(tc + 1) * 512] if which == 1 else None
                        tokn = ("QT%d" if which == 0 else "KT%d") % st
                        if which == 0:
                            for hp_ in range(2):
                                rw = slice(hp_ * 64, hp_ * 64 + 64)
                                P.add("dve", lambda e, bk=bk, rw=rw, hp_=hp_, tc=tc, dstT=dstT: e.tensor_scalar(out=dstT[rw, hp_, tc * 512:(tc + 1) * 512], in0=bk[rw, 0:512], scalar1=0.125, scalar2=None, op0=ALU.mult),
                                      reads=["bk%d" % bi], writes=[tokn])
                        else:
                            P.add("dve", lambda e, dst=dst, bk=bk: e.tensor_copy(out=dst, in_=bk[:, 0:512]),
                                  reads=["bk%d" % bi], writes=[tokn])
                for tc in range(4):
                    bi = ctr["P"] % 2; ctr["P"] += 1
                    bk = bP[bi]
                    for c in range(8):
                        P.add("pe", lambda e, bk=bk, wv=wv, c=c, tc=tc: e.matmul(bk[:, 0:512], lhsT=wv[:, c, :], rhs=hnT[:, c, tc * 512:(tc + 1) * 512],
                                                                                 start=(c == 0), stop=(c == 7)),
                              reads=["wb%d_2" % st] + hn_tokens(range(4 * tc, 4 * tc + 4)), writes=["bk%d" % bi])
                    dst = VTb[:, tc * 512:(tc + 1) * 512]
                    P.add("dve", lambda e, dst=dst, bk=bk: e.tensor_copy(out=dst, in_=bk[:, 0:512]),
                          reads=["bk%d" % bi], writes=["VT"])
                layouts = ((0, 1), (1, 4), (2, 16)) if is_dil else ((0, 1),)
                for (lay, d) in layouts:
                    L = S // d; nt = L // 128
                    tiles = [(r, m) for r in range(d) for m in range(nt)]
                    for g8 in range(2):
                        bi = ctr["P"] % 2; ctr["P"] += 1
                        bkb = bPbf[bi]
                        for q in range(8):
                            r, m = tiles[g8 * 8 + q]
                            tsl_ = tsl(r, d, 128 * m, 128)
                            P.add("pe", lambda e, bkb=bkb, q=q, tsl_=tsl_: e.transpose(bkb[:, q * 128:(q + 1) * 128], VTb[:, tsl_], ident),
                                  reads=["VT", "ident"], writes=["bk%d" % bi])
                        dst = Vt[:, lay, g8 * 8:(g8 + 1) * 8, :, 0:64]
                        src = bkb[:, 0:1024].rearrange("p (t h c) -> p t h c", t=8, h=2)
                        P.add("dve", lambda e, dst=dst, src=src: e.tensor_copy(out=dst, in_=src),
                              reads=["bk%d" % bi], writes=["V%d" % lay])

                tasks = []
                qz = QT[st]; kTp = KT[st]
                ATOK = ["acc0", "acc1"]
                v2 = lambda ap, lo, n: ap[:, lo:lo + 2 * n].rearrange("p (h q) -> p h q", h=2)
                if is_dil:
                    for pi, d in enumerate((1, 4)):
                        L = S // d; nt = L // 128
                        biasP = dilb[:, (j * 2 + pi) * 512:(j * 2 + pi + 1) * 512]
                        for r in range(d):
                            for b in range(nt + 1):
                                def s_part(b=b, r=r, d=d, nt=nt, biasP=biasP, qz=qz, kTp=kTp, st=st):
                                    si = ctr["S"] % NS; ctr["S"] += 1
                                    bkS = bS[si]; pt = PT[:, si * 512:(si + 1) * 512]
                                    tk = "bk%d" % SBK[si]
                                    if b == 0:
                                        ov = v2(bkS, 256, 128)[:, :, 64:128]; bv = v2(biasP, 256, 128)[:, :, 64:128]; pv_ = v2(pt, 256, 128)[:, :, 64:128]
                                        if not MASKMUL:
                                            P.add("pe", lambda e: e.matmul(ov, lhsT=ident, rhs=bv, start=True, stop=False), reads=["ident", "dilb"], writes=[tk])
                                        P.add("pe", lambda e: e.matmul(ov, lhsT=kTp[:, tsl(r, d, 0, 128)], rhs=qz[:, :, tsl(r, d, 0, 64)], start=MASKMUL, stop=True),
                                              reads=["QT%d" % st, "KT%d" % st], writes=[tk])
                                        P.add("act", lambda e: e.activation(out=pv_, in_=ov, func=AF.Exp), reads=[tk], writes=["PT%d" % si])
                                        if MASKMUL:
                                            mask_mul(pv_, bv, si, "dilb")
                                    elif b == nt:
                                        ov = v2(bkS, 0, 128)[:, :, 0:64]; bv = v2(biasP, 0, 128)[:, :, 0:64]; pv_ = v2(pt, 0, 128)[:, :, 0:64]
                                        if not MASKMUL:
                                            P.add("pe", lambda e: e.matmul(ov, lhsT=ident, rhs=bv, start=True, stop=False), reads=["ident", "dilb"], writes=[tk])
                                        P.add("pe", lambda e: e.matmul(ov, lhsT=kTp[:, tsl(r, d, 128 * (nt - 1), 128)], rhs=qz[:, :, tsl(r, d, 128 * nt - 64, 64)], start=MASKMUL, stop=True),
                                              reads=["QT%d" % st, "KT%d" % st], writes=[tk])
                                        P.add("act", lambda e: e.activation(out=pv_, in_=ov, func=AF.Exp), reads=[tk], writes=["PT%d" % si])
                                        if MASKMUL:
                                            mask_mul(pv_, bv, si, "dilb")
                                    else:
                                        qv = qz[:, :, tsl(r, d, 128 * b - 64, 128)]
                                        if not MASKMUL:
                                            P.add("pe", lambda e: e.matmul(bkS[:, 0:512], lhsT=ident, rhs=biasP, start=True, stop=False), reads=["ident", "dilb"], writes=[tk])
                                        P.add("pe", lambda e: e.matmul(v2(bkS, 0, 128), lhsT=kTp[:, tsl(r, d, 128 * (b - 1), 128)], rhs=qv, start=MASKMUL, stop=False),
                                              reads=["QT%d" % st, "KT%d" % st], writes=[tk])
                                        P.add("pe", lambda e: e.matmul(v2(bkS, 256, 128), lhsT=kTp[:, tsl(r, d, 128 * b, 128)], rhs=qv, start=False, stop=True),
                                              reads=["QT%d" % st, "KT%d" % st], writes=[tk])
                                        P.add("act", lambda e: e.activation(out=pt[:, 0:512], in_=bkS[:, 0:512], func=AF.Exp), reads=[tk], writes=["PT%d" % si])
                                        if MASKMUL:
                                            mask_mul(pt[:, 0:512], biasP, si, "dilb")
                                    return si

                                def pv_part(si, b=b, r=r, d=d, nt=nt, pi=pi):
                                    oi = ctr["O"] % NO; ctr["O"] += 1
                                    bkO = bO[oi]; pt = PT[:, si * 512:(si + 1) * 512]
                                    tk = "bk%d" % (5 + oi)
                                    first = [True]
                                    for hp in range(2):
                                        def mm(o_, l_, r_):
                                            stt = first[0]; first[0] = False
                                            P.add("pe", lambda e: e.matmul(o_, lhsT=l_, rhs=r_, start=stt, stop=True), reads=["PT%d" % si, "V%d" % pi], writes=[tk])
                                        ob = hp * 128
                                        if b == 0:
                                            mm(bkO[:, ob + 64:ob + 128], Vt[:, pi, r * nt + 0, hp, :], pt[:, 256 + hp * 128 + 64:256 + hp * 128 + 128])
                                        elif b == nt:
                                            mm(bkO[:, ob:ob + 64], Vt[:, pi, r * nt + nt - 1, hp, :], pt[:, hp * 128:hp * 128 + 64])
                                        else:
                                            mm(bkO[:, ob:ob + 128], Vt[:, pi, r * nt + b - 1, hp, :], pt[:, hp * 128:hp * 128 + 128])
                                            mm(bkO[:, ob:ob + 128], Vt[:, pi, r * nt + b, hp, :], pt[:, 256 + hp * 128:256 + hp * 128 + 128])
                                    c_lo = 64 if b == 0 else 0
                                    c_hi = 64 if b == nt else 128
                                    p_lo = 128 * b - 64 + c_lo
                                    dsta = acc2[:, :, tsl(r, d, p_lo, c_hi - c_lo)]
                                    srca = v2(bkO, 0, 128)[:, :, c_lo:c_hi]
                                    if pi == 0:
                                        P.add("dve", lambda e: e.tensor_copy(out=dsta, in_=srca), reads=[tk], writes=ATOK)
                                    else:
                                        P.add("dve", lambda e: e.tensor_tensor(out=dsta, in0=srca, in1=dsta, op=ALU.add), reads=[tk] + ATOK, writes=ATOK)
                                tasks.append((s_part, pv_part))
                    bias3P = dilb[:, 4096 + j * 256:4096 + (j + 1) * 256]
                    for r0 in range(0, 16, 2):
                        def s_part(r0=r0, bias3P=bias3P, qz=qz, kTp=kTp, st=st):
                            si = ctr["S"] % NS; ctr["S"] += 1
                            bkS = bS[si]; pt = PT[:, si * 512:(si + 1) * 512]
                            tk = "bk%d" % SBK[si]
                            for rr in range(2):
                                r_ = r0 + rr
                                if not MASKMUL:
                                    P.add("pe", lambda e, rr=rr: e.matmul(bkS[:, rr * 256:(rr + 1) * 256], lhsT=ident, rhs=bias3P, start=True, stop=False),
                                          reads=["ident", "dilb"], writes=[tk])
                                P.add("pe", lambda e, rr=rr, r_=r_: e.matmul(v2(bkS, rr * 256, 128), lhsT=kTp[:, tsl(r_, 16, 0, 128)], rhs=qz[:, :, tsl(r_, 16, 0, 128)], start=(MASKMUL and rr == 0), stop=True),
                                      reads=["QT%d" % st, "KT%d" % st], writes=[tk])
                            P.add("act", lambda e: e.activation(out=pt[:, 0:512], in_=bkS[:, 0:512], func=AF.Exp), reads=[tk], writes=["PT%d" % si])
                            if MASKMUL:
                                mask_mul(pt[:, 0:512].rearrange("p (r c) -> p r c", r=2), bias3P.unsqueeze(1).to_broadcast([128, 2, 256]), si, "dilb")
                            return si

                        def pv_part(si, r0=r0):
                            oi = ctr["O"] % NO; ctr["O"] += 1
                            bkO = bO[oi]; pt = PT[:, si * 512:(si + 1) * 512]
                            tk = "bk%d" % (5 + oi)
                            for rr in range(2):
                                for hp in range(2):
                                    cs_ = slice(rr * 256 + hp * 128, rr * 256 + hp * 128 + 128)
                                    P.add("pe", lambda e, rr=rr, hp=hp, cs_=cs_: e.matmul(bkO[:, cs_], lhsT=Vt[:, 2, r0 + rr, hp, :], rhs=pt[:, cs_], start=(rr == 0 and hp == 0), stop=True),
                                          reads=["PT%d" % si, "V2"], writes=[tk])
                            dsta = acc2.rearrange("p h (q r) -> p h q r", r=16)[:, :, :, r0:r0 + 2]
                            srca = bkO[:, 0:512].rearrange("p (r h q) -> p h q r", r=2, h=2)
                            P.add("dve", lambda e: e.tensor_tensor(out=dsta, in0=srca, in1=dsta, op=ALU.add), reads=[tk] + ATOK, writes=ATOK)
                        tasks.append((s_part, pv_part))
                else:
                    for g in range(8):
                        if g == 0:
                            ms_, var = [0, 1, 2, 3], 1
                        elif g == 7:
                            ms_, var = [12, 13, 14, 15], 1
                        else:
                            ms_, var = list(range(2 * g - 2, 2 * g + 4)), 0
                        ostate = {}
                        for ki, m in enumerate(ms_):
                            def s_part(m=m, g=g, var=var, qz=qz, kTp=kTp, st=st):
                                si = ctr["S"] % NS; ctr["S"] += 1
                                bkS = bS[si]; pt = PT[:, si * 512:(si + 1) * 512]
                                tk = "bk%d" % SBK[si]
                                sft = 6 - (2 * m - 4 * g)
                                assert 0 <= sft and sft * 64 + 256 <= 896
                                if not MASKMUL:
                                    P.add("pe", lambda e: e.matmul(v2(bkS, 0, 256), lhsT=ident, rhs=nastrip[:, :, var, sft * 64:sft * 64 + 256], start=True, stop=False),
                                          reads=["ident", "nastrip"], writes=[tk])
                                P.add("pe", lambda e: e.matmul(v2(bkS, 0, 256), lhsT=kTp[:, m * 128:(m + 1) * 128], rhs=qz[:, :, g * 256:(g + 1) * 256], start=MASKMUL, stop=True),
                                      reads=["QT%d" % st, "KT%d" % st], writes=[tk])
                                P.add("act", lambda e: e.activation(out=pt[:, 0:512], in_=bkS[:, 0:512], func=AF.Exp), reads=[tk], writes=["PT%d" % si])
                                if MASKMUL:
                                    mask_mul(v2(pt, 0, 256), nastrip[:, :, var, sft * 64:sft * 64 + 256], si, "nastrip")
                                return si

                            def pv_part(si, m=m, ki=ki, g=g, ostate=ostate, nms=len(ms_)):
                                if ki == 0:
                                    ostate["oi"] = ctr["O"] % NO; ctr["O"] += 1
                                oi = ostate["oi"]
                                bkO = bO[oi]; pt = PT[:, si * 512:(si + 1) * 512]
                                tk = "bk%d" % (5 + oi)
                                for hp in range(2):
                                    P.add("pe", lambda e, hp=hp: e.matmul(bkO[:, hp * 256:(hp + 1) * 256], lhsT=Vt[:, 0, m, hp, :], rhs=pt[:, hp * 256:(hp + 1) * 256],
                                                                         start=(ki == 0 and hp == 0), stop=(ki == nms - 1)),
                                          reads=["PT%d" % si, "V0"], writes=[tk])
                                if ki == nms - 1:
                                    dsta = acc2[:, :, g * 256:(g + 1) * 256]
                                    P.add("dve", lambda e: e.tensor_copy(out=dsta, in_=v2(bkO, 0, 256)), reads=[tk], writes=ATOK)
                            tasks.append((s_part, pv_part))

                for hp in range(2):
                    def fin_part(_si, hp=hp, chunk=(j if is_dil else 4 + j)):
                        rows = slice(hp * 64, hp * 64 + 64)
                        P.add("act", lambda e: e.activation(out=rden[0:64, :], in_=acc2[64:128, hp, :], func=AF.Ln), reads=ATOK, writes=["rden"])
                        P.add("act", lambda e: e.activation(out=rden[0:64, :], in_=rden[0:64, :], func=AF.Exp, scale=-1.0), reads=["rden"], writes=["rden"])
                        P.add("dve", lambda e: e.tensor_tensor(out=yT[rows, chunk, :], in0=acc2[0:64, hp, :], in1=rden[0:64, :], op=ALU.mult),
                              reads=ATOK + ["rden"], writes=["yT%d_%d" % (chunk, hp)])
                    tasks.append((None, fin_part))

                LOOK = int(os.environ.get('KLOOK', '2'))
                sis = []
                for ti_, (sp_, pv_) in enumerate(tasks):
                    sis.append(sp_() if sp_ is not None else None)
                    if ti_ >= LOOK:
                        tasks[ti_ - LOOK][1](sis[ti_ - LOOK])
                for ti_ in range(max(0, len(tasks) - LOOK), len(tasks)):
                    tasks[ti_][1](sis[ti_])


            stage('P%d' % s)
            ytoks = ["yT%d_%d" % (c, hp) for c in range(8) for hp in range(2)]
            P.add("pool", lambda e: e.dma_start(out=wout_bf, in_=wout_d.rearrange("(c p) n -> p c n", p=128)),
                  writes=["V0", "V1", "V2", "wout"], dma="wout")
            for c in range(8):
                P.add("dve", lambda e, c=c: e.tensor_scalar(out=wout_bf[:, c, :], in0=wout_bf[:, c, :], scalar1=gout[:, c:c + 1], scalar2=None, op0=ALU.mult),
                      reads=["wout", "vecs"], writes=["wout"])
            def o_prep(tt):
                row0 = tb + tt * 128
                tsl_ = slice(tt * 128, (tt + 1) * 128)
                xi = ctr["x"] % 3; ctr["x"] += 1
                qi = tt % 2
                xtile = xt[xi]; sq_ = sqt2[qi]
                P.add("sp", lambda e: e.dma_start(out=xtile, in_=x_d[row0:row0 + 128, :]),
                      writes=["xt%d_src" % xi], dma="x%d" % xi)
                P.add("pool", lambda e: e.tensor_tensor(out=sq_, in0=yT[:, :, tsl_], in1=yT[:, :, tsl_], op=ALU.mult),
                      reads=ytoks, writes=["sqt%d" % qi])
                bq = bS[2]
                for c in range(8):
                    col = qi * 2 + c // 4
                    P.add("pe", lambda e, c=c, col=col: e.matmul(bq[:, col:col + 1], lhsT=sq_[:, c, :], rhs=ones1[:, 0:1], start=(c % 4 == 0), stop=(c % 4 == 3)),
                          reads=["sqt%d" % qi, "ones1"], writes=["bk4"])
                ms2 = small[:, 16 + xi * 4:16 + xi * 4 + 2]; rs2 = small[:, 16 + xi * 4 + 2:16 + xi * 4 + 4]
                P.add("dve", lambda e: e.tensor_scalar(out=ms2, in0=bq[:, qi * 2:qi * 2 + 2], scalar1=1.0 / 512, scalar2=EPS, op0=ALU.mult, op1=ALU.add),
                      reads=["bk4"], writes=["ms2_%d" % xi])
                P.add("pool", lambda e: e.tensor_tensor(out=rs2, in0=ms2, in1=mhalf[:, 0:2], op=ALU.pow),
                      reads=["ms2_%d" % xi, "mhalf"], writes=["rs2_%d" % xi])
                return (xtile, xi, rs2, row0, tsl_)

            def o_main(ctx, tt):
                xtile, xi, rs2, row0, tsl_ = ctx
                hi_ = tt % 2
                ht = acc[hi_][:, 0:D]
                for half in range(2):
                    hs_ = slice(half * 512, (half + 1) * 512)
                    bA = [bP[0], bP[1]][half]; bB = [bS[0], bS[1]][half]
                    for c in range(4):
                        P.add("pe", lambda e, c=c, bA=bA, hs_=hs_: e.matmul(bA[:, 0:512], lhsT=yT[:, c, tsl_], rhs=wout_bf[:, c, hs_], start=(c == 0), stop=(c == 3)),
                              reads=ytoks + ["wout", "V0", "V1", "V2"], writes=["bk%d" % half])
                    for c in range(4, 8):
                        P.add("pe", lambda e, c=c, bB=bB, hs_=hs_: e.matmul(bB[:, 0:512], lhsT=yT[:, c, tsl_], rhs=wout_bf[:, c, hs_], start=(c == 4), stop=(c == 7)),
                              reads=ytoks + ["wout", "V0", "V1", "V2"], writes=["bk%d" % (2 + half)])
                    P.add("dve", lambda e, hs_=hs_, bA=bA: e.scalar_tensor_tensor(out=ht[:, hs_], in0=bA[:, 0:512], scalar=rs2[:, 0:1], in1=xtile[:, hs_], op0=ALU.mult, op1=ALU.add),
                          reads=["bk%d" % half, "rs2_%d" % xi, "xt%d_src" % xi], writes=["acc%d" % hi_])
                    P.add("dve", lambda e, hs_=hs_, bB=bB: e.scalar_tensor_tensor(out=ht[:, hs_], in0=bB[:, 0:512], scalar=rs2[:, 1:2], in1=ht[:, hs_], op0=ALU.mult, op1=ALU.add),
                          reads=["bk%d" % (2 + half), "rs2_%d" % xi, "acc%d" % hi_], writes=["acc%d" % hi_])
                P.add("sp", lambda e: e.dma_start(out=hs_d[row0:row0 + 128, :], in_=ht),
                      reads=["acc%d" % hi_], writes=["hs%d" % (row0 // 128)], dma="hst%d" % hi_)

            octx = [o_prep(0)]
            for tt in range(16):
                if tt + 1 < 16:
                    octx.append(o_prep(tt + 1))
                o_main(octx[tt], tt)

        stage('O')
        P.add("act", lambda e: e.activation(out=bar[:, 0:1], in_=mhalf[:, 0:1], func=AF.Copy), reads=["mhalf"], writes=["bar_act"])
        P.add("dve", lambda e: e.tensor_copy(out=bar[:, 1:2], in_=mhalf[:, 0:1]), reads=["mhalf"], writes=["bar_dve"])
        P.add("pool", lambda e: e.tensor_copy(out=bar[:, 2:3], in_=mhalf[:, 0:1]), reads=["mhalf"], writes=["bar_pool"])
        for en in ("pe", "act", "dve", "pool", "sp"):
            P.add(en, lambda e: None, reads=["bar_act", "bar_dve", "bar_pool"] + ["hs%d" % i_ for i_ in range(32)], waitonly=True)

        ptr[0] = persist_end
        wd_bf = alloc(32 * D, BF16, (32, D))
        hn2T = alloc(8 * 1024, BF16, (8, 1024))
        actT = alloc(32 * 1024, BF16, (32, 1024))
        wgu = [[alloc(8 * 256, BF16, (8, 256)) for _ in range(2)] for _ in range(3)]
        hbA = alloc(D); hbD = [alloc(D) for _ in range(2)]
        xsA = alloc(D, BF16); xsD = alloc(D, BF16)
        combT = alloc(1024, BF16)
        Cs = [alloc(512, BF16) for _ in range(2)]
        ssb = [alloc(512, BF16) for _ in range(2)]
        s2b = [alloc(512, BF16) for _ in range(2)]
        rt = alloc(960)
        comb_bf = alloc(8 * 16, BF16, (8, 16))
        small2 = alloc(32)

        bG = [banks[0], banks[1]]; bU = [banks[2], banks[3]]; bC = banks[4]; bD = [banks[5], banks[6]]

        def load_wd():
            for k in range(4):
                P.add("sp", lambda e, k=k: e.dma_start(out=wd_bf[:, 8 * k:8 * k + 8, :],
                                                       in_=wds_d[4 * k:4 * k + 4].rearrange("e (f p) n -> p (e f) n", p=128)),
                      reads=["cvd%d" % ee for ee in range(4 * k, 4 * k + 4)], writes=["wd%d" % k], dma="wd%d" % k)
        wd_toks = ["wd%d" % k for k in range(4)]

        def load_gu(n):
            if n >= 64:
                return
            ex_ = n % 16; wi_ = n % 3
            wgb_, wub_ = wgu[wi_]
            P.add("sp", lambda e: e.dma_start(out=wgb_, in_=wgs_d[ex_].rearrange("(c p) n -> p c n", p=128)),
                  reads=["cvg%d" % ex_], writes=["wg%d" % wi_], dma="wg%d" % wi_)
            P.add("sp", lambda e: e.dma_start(out=wub_, in_=wus_d[ex_].rearrange("(c p) n -> p c n", p=128)),
                  reads=["cvu%d" % ex_], writes=["wu%d" % wi_], dma="wu%d" % wi_)
        load_gu(0)
        load_gu(1)

        c2 = {"h": 0, "G": 0, "C": 0, "D": 0, "w": 0, "y": 0}
        h2_all = [("h2_%d_a" % st) for st in range(8)] + [("h2_%d_b" % st) for st in range(8)]

        hpool = [(hbA, "hbA"), (hbD[0], "hbD0"), (hbD[1], "hbD1")]
        xpool = [(xsA, "xsA"), (xsD, "xsD")]
        tbank2 = [(pb_t, "bk7"), (banks[2].bitcast(BF16), "bk2")]
        bD4 = [(banks[5], "bk5"), (banks[6], "bk6"), (banks[0], "bk0"), (banks[1], "bk1")]

        def prep_hn2(T, st):
            row0 = T * 1024 + st * 128
            if T == 0:
                htile, htok = hpool[st % 3]; xsb, xtok = xpool[st % 2]
            else:
                htile, htok = hpool[0]; xsb, xtok = xpool[0]
            si_ = c2["h"] % 4; c2["h"] += 1
            P.add("sp", lambda e: e.dma_start(out=htile, in_=hs_d[row0:row0 + 128, :]),
                  reads=["hs%d" % (row0 // 128)], writes=[htok], dma=htok)
            ss = small2[:, si_ * 4:si_ * 4 + 1]; ms = small2[:, si_ * 4 + 1:si_ * 4 + 2]; rstd = small2[:, si_ * 4 + 2:si_ * 4 + 3]
            P.add("act", lambda e: e.activation(out=xsb, in_=htile, func=AF.Square, accum_out=ss),
                  reads=[htok], writes=[xtok, "h_ss%d" % si_])
            P.add("dve", lambda e: e.tensor_scalar(out=ms, in0=ss, scalar1=1.0 / D, scalar2=EPS, op0=ALU.mult, op1=ALU.add),
                  reads=["h_ss%d" % si_], writes=["h_ms%d" % si_])
            P.add("pool", lambda e: e.tensor_tensor(out=rstd, in0=ms, in1=mhalf[:, 0:1], op=ALU.pow),
                  reads=["h_ms%d" % si_, "mhalf"], writes=["h_rstd%d" % si_])
            P.add("dve", lambda e: e.tensor_scalar(out=xsb, in0=htile, scalar1=rstd, scalar2=None, op0=ALU.mult),
                  reads=[htok, "h_rstd%d" % si_], writes=[xtok])
            return (xsb, xtok)

        def trans_hn2(ctx, T, st):
            xsb, xtok = ctx
            tbk, ttok = tbank2[st % 2]
            for c in range(8):
                P.add("pe", lambda e, c=c: e.transpose(tbk[:, c * 128:(c + 1) * 128], xsb[:, c * 128:(c + 1) * 128], ident),
                      reads=[xtok, "ident"], writes=[ttok])
            for c in range(8):
                dst = hn2T[:, c, st * 128:(st + 1) * 128]
                src = tbk[:, c * 128:(c + 1) * 128]
                if st % 2 == 0:
                    P.add("dve", lambda e, dst=dst, src=src, c=c: e.tensor_scalar(out=dst, in0=src, scalar1=gffn[:, c:c + 1], scalar2=None, op0=ALU.mult),
                          reads=[ttok, "vecs"], writes=["h2_%d_a" % st])
                else:
                    P.add("act", lambda e, dst=dst, src=src, c=c: e.activation(out=dst, in_=src, func=AF.Copy, scale=gffn[:, c:c + 1]),
                          reads=[ttok, "vecs"], writes=["h2_%d_b" % st])

        def emit_router(T):
            if True:
                for st in range(8):
                    for c in range(8):
                        P.add("pe", lambda e, st=st, c=c: e.matmul(bC[:, st * 32:st * 32 + 20], lhsT=hn2T[:, c, st * 128:(st + 1) * 128], rhs=wr_bf[:, c, :],
                                                                   start=(c == 0), stop=(c == 7)),
                              reads=["h2_%d_a" % st, "h2_%d_b" % st, "wr_bf"], writes=["bk4"])
                lg = rt[:, 0:160].rearrange("p (s n) -> p s n", s=8)
                bc3 = bC[:, 0:256].rearrange("p (s n) -> p s n", s=8)[:, :, 0:20]
                RT = "rt"
                r3 = lambda lo, n: rt[:, lo:lo + 8 * n].rearrange("p (s n) -> p s n", s=8)
                gl = lg[:, :, 0:4]; el = lg[:, :, 4:20]
                gmax = rt[:, 160:168]; g1 = r3(168, 4); gex = r3(200, 4); gsum = rt[:, 232:240]; gw = rt[:, 240:248]
                pen = r3(248, 16); ml = r3(376, 16); m1 = rt[:, 504:512]; m2 = rt[:, 512:520]; k1 = r3(520, 16)
                dm = rt[:, 648:656]; w1 = rt[:, 656:664]; w2 = rt[:, 664:672]; k2 = r3(672, 16); ml2 = r3(800, 16)
                bcast = lambda ap, n: ap.unsqueeze(2).to_broadcast([128, 8, n])
                P.add("dve", lambda e: e.tensor_tensor(out=lg, in0=bc3, in1=brep.unsqueeze(1).to_broadcast([128, 8, 20]), op=ALU.add),
                      reads=["bk4", "vecs"], writes=[RT])
                P.add("dve", lambda e: e.tensor_reduce(out=gmax, in_=gl, axis=AX.X, op=ALU.max), reads=[RT], writes=[RT + "a"])
                P.add("dve", lambda e: e.tensor_tensor(out=g1, in0=gl, in1=bcast(gmax, 4), op=ALU.is_equal), reads=[RT, RT + "a"], writes=[RT + "b"])
                P.add("dve", lambda e: e.tensor_tensor(out=gex, in0=gl, in1=bcast(gmax, 4), op=ALU.subtract), reads=[RT, RT + "a"], writes=[RT + "c"])
                P.add("act", lambda e: e.activation(out=gex, in_=gex, func=AF.Exp), reads=[RT + "c"], writes=[RT + "d"])
                P.add("dve", lambda e: e.tensor_reduce(out=gsum, in_=gex, axis=AX.X, op=ALU.add), reads=[RT + "d"], writes=[RT + "e"])
                P.add("dve", lambda e: e.reciprocal(out=gw, in_=gsum), reads=[RT + "e"], writes=[RT + "f"])
                P.add("dve", lambda e: e.tensor_scalar(out=pen.rearrange("p s (g n) -> p s g n", g=4), in0=g1.unsqueeze(3).to_broadcast([128, 8, 4, 4]),
                                                       scalar1=-1.0, scalar2=30000.0, op0=ALU.add, op1=ALU.mult), reads=[RT + "b"], writes=[RT + "g"])
                P.add("dve", lambda e: e.tensor_tensor(out=ml, in0=el, in1=pen, op=ALU.add), reads=[RT, RT + "g"], writes=[RT + "h"])
                P.add("dve", lambda e: e.tensor_reduce(out=m1, in_=ml, axis=AX.X, op=ALU.max), reads=[RT + "h"], writes=[RT + "i"])
                P.add("dve", lambda e: e.tensor_tensor(out=k1, in0=ml, in1=bcast(m1, 16), op=ALU.is_equal), reads=[RT + "h", RT + "i"], writes=[RT + "j"])
                P.add("dve", lambda e: e.scalar_tensor_tensor(out=ml2, in0=k1, scalar=-60000.0, in1=ml, op0=ALU.mult, op1=ALU.add), reads=[RT + "h", RT + "j"], writes=[RT + "k"])
                P.add("dve", lambda e: e.tensor_reduce(out=m2, in_=ml2, axis=AX.X, op=ALU.max), reads=[RT + "k"], writes=[RT + "l"])
                P.add("dve", lambda e: e.tensor_tensor(out=k2, in0=ml2, in1=bcast(m2, 16), op=ALU.is_equal), reads=[RT + "k", RT + "l"], writes=[RT + "m"])
                P.add("dve", lambda e: e.tensor_tensor(out=dm, in0=m2, in1=m1, op=ALU.subtract), reads=[RT + "l", RT + "i"], writes=[RT + "n"])
                P.add("act", lambda e: e.activation(out=dm, in_=dm, func=AF.Exp), reads=[RT + "n"], writes=[RT + "o"])
                P.add("dve", lambda e: e.tensor_scalar(out=dm, in0=dm, scalar1=1.0, scalar2=None, op0=ALU.add), reads=[RT + "o"], writes=[RT + "p"])
                P.add("dve", lambda e: e.reciprocal(out=w1, in_=dm), reads=[RT + "p"], writes=[RT + "q"])
                P.add("dve", lambda e: e.tensor_scalar(out=w2, in0=w1, scalar1=-1.0, scalar2=1.0, op0=ALU.mult, op1=ALU.add), reads=[RT + "q"], writes=[RT + "r"])
                P.add("dve", lambda e: e.tensor_tensor(out=w1, in0=w1, in1=gw, op=ALU.mult), reads=[RT + "q", RT + "r", RT + "f"], writes=[RT + "s"])
                P.add("dve", lambda e: e.tensor_tensor(out=w2, in0=w2, in1=gw, op=ALU.mult), reads=[RT + "r", RT + "f"], writes=[RT + "t"])
                P.add("dve", lambda e: e.tensor_tensor(out=k1, in0=k1, in1=bcast(w1, 16), op=ALU.mult), reads=[RT + "j", RT + "s", RT + "k"], writes=[RT + "u"])
                P.add("dve", lambda e: e.tensor_tensor(out=k2, in0=k2, in1=bcast(w2, 16), op=ALU.mult), reads=[RT + "m", RT + "t"], writes=[RT + "v"])
                P.add("dve", lambda e: e.tensor_tensor(out=comb_bf, in0=k1, in1=k2, op=ALU.add), reads=[RT + "u", RT + "v"], writes=["comb_bf"])
                for st in range(8):
                    P.add("pe", lambda e, st=st: e.transpose(pb_t[0:16, st * 128:(st + 1) * 128], comb_bf[:, st, :], ident),
                          reads=["comb_bf", "ident"] + h2_all, writes=["bk7"])
                P.add("dve", lambda e: e.tensor_copy(out=combT[0:16, :], in_=pb_t[0:16, 0:1024]), reads=["bk7"], writes=["combT"])


        def emit_experts(T):
            if True:
                for ex in range(16):
                    nflat = T * 16 + ex
                    wi = nflat % 3
                    wgb, wub = wgu[wi]
                    load_gu(nflat + 2)
                    for half in range(2):
                        hs_ = slice(half * 512, (half + 1) * 512)
                        ci = c2["C"] % 2; c2["C"] += 1
                        P.add("pe", lambda e, ex=ex, hs_=hs_: e.matmul(bC[:, 0:512], lhsT=sel[0:16, ex, :], rhs=combT[0:16, hs_], start=True, stop=True),
                              reads=["sel", "combT"], writes=["bk4"])
                        P.add("dve", lambda e, ci=ci: e.tensor_copy(out=Cs[ci], in_=bC[:, 0:512]), reads=["bk4"], writes=["Cs%d" % ci])
                        for fc in range(2):
                            gi = c2["G"] % 2; c2["G"] += 1
                            for c in range(8):
                                P.add("pe", lambda e, gi=gi, c=c, fc=fc, hs_=hs_, wgb=wgb: e.matmul(bG[gi][:, 0:512], lhsT=wgb[:, c, fc * 128:(fc + 1) * 128], rhs=hn2T[:, c, hs_],
                                                                                                  start=(c == 0), stop=(c == 7)),
                                      reads=["wg%d" % wi] + h2_all, writes=["bk%d" % gi])
                            for c in range(8):
                                P.add("pe", lambda e, gi=gi, c=c, fc=fc, hs_=hs_, wub=wub: e.matmul(bU[gi][:, 0:512], lhsT=wub[:, c, fc * 128:(fc + 1) * 128], rhs=hn2T[:, c, hs_],
                                                                                                  start=(c == 0), stop=(c == 7)),
                                      reads=["wu%d" % wi] + h2_all, writes=["bk%d" % (2 + gi)])
                            P.add("act", lambda e, gi=gi: e.activation(out=ssb[gi], in_=bG[gi][:, 0:512], func=AF.Silu), reads=["bk%d" % gi], writes=["ssb%d" % gi])
                            P.add("pool", lambda e, gi=gi, ci=ci: e.tensor_tensor(out=s2b[gi], in0=ssb[gi], in1=Cs[ci], op=ALU.mult),
                                  reads=["ssb%d" % gi, "Cs%d" % ci], writes=["s2b%d" % gi])
                            P.add("dve", lambda e, gi=gi, ex=ex, fc=fc, hs_=hs_: e.tensor_tensor(out=actT[:, ex * 2 + fc, hs_], in0=bU[gi][:, 0:512], in1=s2b[gi], op=ALU.mult),
                                  reads=["bk%d" % (2 + gi), "s2b%d" % gi], writes=["actT"])


        def down_mm(T, st):
            row0 = T * 1024 + st * 128
            hi = c2["y"] % 2; c2["y"] += 1
            htile, htok = hpool[1 + hi]
            P.add("sp", lambda e: e.dma_start(out=htile, in_=hs_d[row0:row0 + 128, :]),
                  reads=["hs%d" % (row0 // 128)], writes=[htok], dma=htok)
            for dh in range(2):
                bk_, btok = bD4[c2["D"] % 4]; c2["D"] += 1
                ds_ = slice(dh * 512, (dh + 1) * 512)
                for kk in range(32):
                    P.add("pe", lambda e, bk_=bk_, kk=kk, ds_=ds_: e.matmul(bk_[:, 0:512], lhsT=actT[:, kk, st * 128:(st + 1) * 128], rhs=wd_bf[:, kk, ds_],
                                                                          start=(kk == 0), stop=(kk == 31)),
                          reads=["actT"] + wd_toks, writes=[btok])
                P.add("dve", lambda e, bk_=bk_, ds_=ds_: e.tensor_tensor(out=htile[:, ds_], in0=bk_[:, 0:512], in1=htile[:, ds_], op=ALU.add),
                      reads=[btok, htok], writes=[htok])
            return (htile, htok, row0, hi)

        def down_fin(ctx):
            htile, htok, row0, yi = ctx
            junk, jtok = xpool[1]
            ss = small2[:, 16 + yi * 4:16 + yi * 4 + 1]; ms = small2[:, 16 + yi * 4 + 1:16 + yi * 4 + 2]; rstd = small2[:, 16 + yi * 4 + 2:16 + yi * 4 + 3]
            P.add("act", lambda e: e.activation(out=junk, in_=htile, func=AF.Square, accum_out=ss),
                  reads=[htok], writes=[jtok, "f_ss%d" % yi])
            P.add("dve", lambda e: e.tensor_scalar(out=ms, in0=ss, scalar1=1.0 / D, scalar2=EPS, op0=ALU.mult, op1=ALU.add),
                  reads=["f_ss%d" % yi], writes=["f_ms%d" % yi])
            P.add("pool", lambda e: e.tensor_tensor(out=rstd, in0=ms, in1=mhalf[:, 0:1], op=ALU.pow),
                  reads=["f_ms%d" % yi, "mhalf"], writes=["f_rstd%d" % yi])
            P.add("dve", lambda e: e.scalar_tensor_tensor(out=htile, in0=htile, scalar=rstd, in1=gfin, op0=ALU.mult, op1=ALU.mult),
                  reads=[htok, "f_rstd%d" % yi, "gfin"], writes=[htok])
            P.add("sp", lambda e: e.dma_start(out=y_d[row0:row0 + 128, :], in_=htile),
                  reads=[htok], dma="yst%d" % yi)

        for st in range(8):
            trans_hn2(prep_hn2(0, st), 0, st)
        emit_router(0)
        load_wd()
        for T in range(4):
            emit_experts(T)
            for st in range(8):
                cx = prep_hn2(T + 1, st) if T < 3 else None
                dx = down_mm(T, st)
                if cx is not None:
                    trans_hn2(cx, T + 1, st)
                down_fin(dx)
            if T < 3:
                emit_router(T + 1)

    except StopBuild:
        pass

    sems = {}
    for en in Prog.ENGS:
        sems["eng:" + en] = es.enter_context(nc.semaphore("s_" + en))
    for sl in sorted(P.dma_slots):
        sems["dma:" + sl] = es.enter_context(nc.semaphore("d_" + sl))
    if os.environ.get('KMAXOPS'):
        P.ops = P.ops[:int(os.environ['KMAXOPS'])]
        for i_, o_ in enumerate(P.ops[-3:]):
            print('LASTOPS', o_.eng, o_.idx)
    body = P.emit(sems)
    if os.environ.get('KSTATS'):
        print('PROG stats', P.stats, 'ndma_slots', len(P.dma_slots))
    with nc.Block() as block:
        block.sync(body("sp"))
        block.tensor(body("pe"))
        block.scalar(body("act"))
        block.vector(body("dve"))
        block.gpsimd(body("pool"))
    es.close()
    return nc


def _dil_bias():
    out = np.full((128, 4 * 2 * 512 + 4 * 256), NEG, np.float32)
    k = np.arange(128)[:, None]; q = np.arange(128)[None, :]

    def f(delta, slope, d):
        return np.where(np.abs(delta) <= 64, -slope * d * np.abs(delta), NEG).astype(np.float32)
    for j in range(4):
        for hp in range(2):
            slope = 2.0 ** (-(2 * j + hp + 1))
            for pi, d in enumerate((1, 4)):
                c0 = (j * 2 + pi) * 512
                out[:, c0 + hp * 128:c0 + (hp + 1) * 128] = f(k - 64 - q, slope, d)
                out[:, c0 + 256 + hp * 128:c0 + 256 + (hp + 1) * 128] = f(k + 64 - q, slope, d)
            c0 = 4096 + j * 256
            out[:, c0 + hp * 128:c0 + (hp + 1) * 128] = f(k - q, slope, 16)
    return out


def _na_strips(rpb):
    kc = np.arange(64)[:, None]; qc = np.arange(64)[None, :]
    cs = np.clip(qc - 8, 0, 48)
    col_ok = (kc >= cs) & (kc < cs + 16)
    dcidx = np.clip(kc - qc + 15, 0, 30)
    out = np.full((4, 128, 2, 2, 896), NEG, np.float32)
    for h in range(8):
        j, hp = divmod(h, 2)
        for var in range(2):
            for i in range(2):
                for u in range(14):
                    dlt = 6 + i - u
                    ok = (-4 <= dlt <= 3) if var == 0 else (-7 <= dlt <= 7)
                    if not ok:
                        continue
                    blk = np.where(col_ok, rpb[h, dlt + 7][dcidx], NEG).astype(np.float32)
                    out[j, i * 64:(i + 1) * 64, hp, var, u * 64:(u + 1) * 64] = blk
    return out.reshape(4, 128, 4 * 896)


_NC_CACHE = {}


def kernel(x, norm_mix_g, w_in, rpb, g_out_dil, g_out_na, w_out, norm_ffn_g, w_group, b_group, w_router, b_router,
           w_gate, w_up, w_down, norm_final_g):
    f = lambda a: np.ascontiguousarray(np.asarray(a, dtype=np.float32))
    x = f(x).reshape(16 * S, D)
    col = lambda g: f(g).reshape(8, 128).T
    vecs = np.concatenate([col(norm_mix_g[0]), col(norm_ffn_g[0]),
                           col(np.concatenate([f(g_out_dil[0]), f(g_out_na[0])])),
                           np.broadcast_to(np.concatenate([f(b_group[0]), f(b_router[0])])[None, :], (128, 20))], axis=1)
    vecs = np.ascontiguousarray(vecs, dtype=np.float32)
    gfin = np.ascontiguousarray(np.broadcast_to(f(norm_final_g)[None, :], (128, D)))
    gmixr = np.ascontiguousarray(np.broadcast_to(f(norm_mix_g[0])[None, :], (128, D)))
    wr = np.ascontiguousarray(np.concatenate([f(w_group[0]), f(w_router[0])], axis=1))
    shared = {
        "w_in": f(w_in[0]), "w_out": f(w_out[0]), "w_gate": f(w_gate[0]), "w_up": f(w_up[0]), "w_down": f(w_down[0]),
        "wr": wr, "vecs": vecs, "gfin": gfin, "gmixr": gmixr, "dilb": _dil_bias(), "nas": _na_strips(f(rpb[0])),
    }
    if "nc" not in _NC_CACHE:
        _NC_CACHE["nc"] = build_program()
    nc = _NC_CACHE["nc"]
    in_maps = []
    for i in range(NCORES):
        m = dict(shared)
        m["x"] = np.ascontiguousarray(x[i * TOK:(i + 1) * TOK])
        in_maps.append(m)
    res = run_bass_kernel_spmd(nc, in_maps, core_ids=list(range(NCORES)))
    y = np.concatenate([r["y"] for r in res.results], axis=0)
    if DEBUG:
        kernel.dbg = [r.get("dbg") for r in res.results]
    return y.reshape(16, S, D).astype(np.float32)
```

```python
import numpy as np
from contextlib import ExitStack
import concourse.bass as bass
import concourse.mybir as mybir
from concourse.bass_utils import run_bass_kernel_spmd

F32 = mybir.dt.float32
BF16 = mybir.dt.bfloat16
AF = mybir.ActivationFunctionType
ALU = mybir.AluOpType
AX = mybir.AxisListType

NCORES = 8
S = 2048
D = 1024
TOK = 4096
NEG = -30000.0
EPS = 1e-6
import os
DEBUG = bool(os.environ.get('KSTOP', ''))
STOP = os.environ.get('KSTOP', '')


class StopBuild(Exception):
    pass


DUMPS = {}


def stage(name):
    if STOP and name == STOP:
        if name in DUMPS:
            DUMPS[name]()
        raise StopBuild()


class Op:
    __slots__ = ("eng", "fn", "deps", "is_dma", "sem", "semval", "needs_signal", "sigcount", "eidx", "idx", "wo")


class Prog:
    ENGS = ("pe", "act", "dve", "pool", "sp")

    def __init__(self):
        self.ops = []
        self.last_writer = {}
        self.readers = {}
        self.eng_count = {e: 0 for e in self.ENGS}
        self.dma_slots = set()

    def add(self, eng, fn, reads=(), writes=(), dma=None, waitonly=False):
        op = Op()
        op.eng = eng; op.fn = fn; op.idx = len(self.ops)
        op.is_dma = dma is not None
        op.wo = waitonly
        op.sem = dma; op.semval = 0; op.needs_signal = False; op.sigcount = 0
        op.eidx = self.eng_count[eng]; self.eng_count[eng] += 1
        if dma is not None:
            self.dma_slots.add(dma)
        deps = {}
        for t in reads:
            w = self.last_writer.get(t)
            if w is not None:
                deps[w] = "raw"
        for t in writes:
            w = self.last_writer.get(t)
            if w is not None and w not in deps:
                deps[w] = "waw"
            for r in self.readers.get(t, ()):
                if r not in deps and r != op.idx:
                    deps[r] = "war"
        for t in (() if waitonly else reads):
            lst = self.readers.setdefault(t, [])
            if not op.is_dma:
                lst[:] = [r for r in lst if self.ops[r].is_dma or self.ops[r].eng != eng]
            lst.append(op.idx)
        for t in writes:
            self.last_writer[t] = op.idx
            self.readers[t] = []
        op.deps = deps
        self.ops.append(op)
        return op

    def _need_wait(self, op, p, kind):
        if p.is_dma:
            return True
        if p.eng != op.eng:
            return True
        if op.is_dma:
            return True
        if p.eng == "pe":
            return False
        if kind == "raw" and ((op.eidx - p.eidx) <= 3 or p.eng == "pool"):
            return True
        return False

    def emit(self, sem_ctx):
        ops = self.ops
        for op in ops:
            for d, kind in op.deps.items():
                p = ops[d]
                if (not p.is_dma) and self._need_wait(op, p, kind):
                    p.needs_signal = True
        if os.environ.get('KALLSIG'):
            for op in ops:
                if not op.is_dma and op.fn(None) if False else (not op.is_dma and not getattr(op, "wo", False)):
                    op.needs_signal = True
        cnt = {e: 0 for e in self.ENGS}
        dcnt = {}
        for op in ops:
            if op.is_dma:
                dcnt[op.sem] = dcnt.get(op.sem, 0) + 16
                op.semval = dcnt[op.sem]
            elif op.needs_signal:
                cnt[op.eng] += 1
                op.sigcount = cnt[op.eng]
        by_eng = {e: [o for o in ops if o.eng == e] for e in self.ENGS}
        self.stats = (dict(cnt), {e: len(v) for e, v in by_eng.items()})

        def body(ename):
            def run(eng):
                waited = {}
                for op in by_eng[ename]:
                    for d, kind in op.deps.items():
                        p = ops[d]
                        if not self._need_wait(op, p, kind):
                            continue
                        if p.is_dma:
                            key = "dma:" + p.sem; val = p.semval
                        else:
                            key = "eng:" + p.eng; val = p.sigcount
                        if waited.get(key, 0) >= val:
                            continue
                        waited[key] = val
                        eng.wait_ge(sem_ctx[key], val)
                    ins = op.fn(eng)
                    if op.is_dma:
                        ins.then_inc(sem_ctx["dma:" + op.sem], 16)
                    elif op.needs_signal:
                        ins.then_inc(sem_ctx["eng:" + ename], 1)
                for op in by_eng[ename]:
                    if op.is_dma:
                        key = "dma:" + op.sem
                        if waited.get(key, 0) < dcnt[op.sem]:
                            waited[key] = dcnt[op.sem]
                            eng.wait_ge(sem_ctx[key], dcnt[op.sem])
            return run
        return body


def tsl(r, d, p0, n):
    return slice(r + d * p0, r + d * (p0 + n - 1) + 1, d)


def build_program():
    nc = bass.Bass("TRN2", target_bir_lowering=False)
    dt_in = lambda n, s, dt=F32: nc.dram_tensor(n, s, dt, kind="ExternalInput").ap()
    x_d = dt_in("x", [TOK, D])
    win_d = dt_in("w_in", [D, 3072])
    wout_d = dt_in("w_out", [D, D])
    wg_d = dt_in("w_gate", [16, D, 256])
    wu_d = dt_in("w_up", [16, D, 256])
    wd_d = dt_in("w_down", [16, 256, D])
    wr_d = dt_in("wr", [D, 20])
    vec_d = dt_in("vecs", [128, 24 + 20])
    gfin_d = dt_in("gfin", [128, D])
    gmixr_d = dt_in("gmixr", [128, D])
    dilb_d = dt_in("dilb", [128, 5120])
    nas_d = dt_in("nas", [4, 128, 4 * 896])
    y_d = nc.dram_tensor("y", [TOK, D], F32, kind="ExternalOutput").ap()
    hs_d = nc.dram_tensor("hscr", [TOK, D], F32, kind="Internal").ap()
    wgs_d = nc.dram_tensor("wg_bf", [16, D, 256], BF16, kind="Internal").ap()
    wus_d = nc.dram_tensor("wu_bf", [16, D, 256], BF16, kind="Internal").ap()
    wds_d = nc.dram_tensor("wd_bf", [16, 256, D], BF16, kind="Internal").ap()
    dbg_d = None
    if DEBUG:
        dbg_d = nc.dram_tensor("dbg", [128, 8 * 2048], F32, kind="ExternalOutput").ap()

    P = Prog()
    es = ExitStack()
    ARENA = 53200
    arena = es.enter_context(nc.sbuf_tensor("arena", [128, ARENA], F32))
    arena_bf = arena.bitcast(BF16)
    ptr = [0]

    def alloc(ncols, dt=F32, shape=None):
        n32 = ncols if dt == F32 else (ncols + 1) // 2
        a = ptr[0]
        ptr[0] += n32
        assert ptr[0] <= ARENA, ("SBUF overflow", ptr[0])
        if dt == F32:
            ap = arena[:, a:a + ncols]
        else:
            ap = arena_bf[:, 2 * a:2 * a + ncols]
        if shape is not None:
            names = " ".join("d%d" % i for i in range(len(shape)))
            kw = {"d%d" % i: shape[i] for i in range(len(shape))}
            ap = ap.rearrange("p (%s) -> p %s" % (names, names), **kw)
        return ap

    pb_t = es.enter_context(nc.psum_tensor("pb_t", [128, 1024], BF16))
    banks = [es.enter_context(nc.psum_tensor("bk%d" % i, [128, 512], F32)) for i in range(7)]
    tbank = [(pb_t, "bk7"), (banks[6].bitcast(BF16), "bk6")]

    ident = alloc(128, BF16)
    sel = alloc(16 * 128, BF16, (16, 128))
    vecs = alloc(44)
    gfin = alloc(D)
    wr_bf = alloc(8 * 20, BF16, (8, 20))
    mhalf = alloc(4)
    ones1 = alloc(2, BF16)
    bar = alloc(4)
    persist_end = ptr[0]

    try:
        P.add("pool", lambda e: e.memset(ident, 0.0), writes=["ident"])
        P.add("pool", lambda e: e.affine_select(out=ident, in_=ident, pattern=[[-1, 128]], compare_op=ALU.not_equal,
                                                fill=1.0, base=0, channel_multiplier=1), reads=["ident"], writes=["ident"])
        P.add("pool", lambda e: e.memset(sel, 0.0), writes=["sel"])
        P.add("pool", lambda e: e.affine_select(out=sel[0:16], in_=sel[0:16], pattern=[[-1, 16], [0, 128]],
                                                compare_op=ALU.not_equal, fill=1.0, base=0, channel_multiplier=1),
              reads=["sel"], writes=["sel"])
        P.add("pool", lambda e: e.memset(mhalf, -0.5), writes=["mhalf"])
        P.add("pool", lambda e: e.memset(ones1, 1.0), writes=["ones1"])
        P.add("sp", lambda e: e.dma_start(out=vecs, in_=vec_d), writes=["vecs"], dma="c0")
        P.add("sp", lambda e: e.dma_start(out=gfin, in_=gfin_d), writes=["gfin"], dma="c1")
        P.add("pool", lambda e: e.dma_start(out=wr_bf, in_=wr_d.rearrange("(c p) n -> p c n", p=128)),
              writes=["wr_bf"], dma="c2")
        gmix = vecs[:, 0:8]; gffn = vecs[:, 8:16]; gout = vecs[:, 16:24]; brep = vecs[:, 24:44]

        def rms_rstd(src, ss, ms, rstd, junk, tag, ncols=D):
            P.add("act", lambda e: e.activation(out=junk, in_=src, func=AF.Square, accum_out=ss),
                  reads=[tag + "_src"], writes=[tag + "_junk", tag + "_ss"])
            P.add("dve", lambda e: e.tensor_scalar(out=ms, in0=ss, scalar1=1.0 / ncols, scalar2=EPS, op0=ALU.mult, op1=ALU.add),
                  reads=[tag + "_ss"], writes=[tag + "_ms"])
            P.add("pool", lambda e: e.tensor_tensor(out=rstd, in0=ms, in1=mhalf[:, 0:1], op=ALU.pow),
                  reads=[tag + "_ms", "mhalf"], writes=[tag + "_rstd"])

        hnT = alloc(8 * S, BF16, (8, S))
        yT = alloc(8 * S, BF16, (8, S))
        dilb = alloc(5120, BF16)
        wblk = [[alloc(8 * 128, BF16, (8, 128)) for _ in range(3)] for _ in range(2)]
        QT = [alloc(2 * S, BF16, (2, S)) for _ in range(2)]
        KT = [alloc(S, BF16) for _ in range(2)]
        Vt_raw = alloc(3 * 16 * 256, BF16)
        Vt = Vt_raw.rearrange("p (l t h c) -> p l t h c", l=3, t=16, h=2, c=128)
        wout_bf = Vt_raw[:, 0:8 * D].rearrange("p (c n) -> p c n", c=8)
        acc2 = alloc(2 * S, F32, (2, S))
        acc = [acc2[:, 0, :], acc2[:, 1, :]]
        VTb = alloc(S, BF16)
        gmixr = alloc(D)
        rden = alloc(S)
        NS = int(os.environ.get('KNS', '3')); NO = int(os.environ.get('KNO', '2'))
        PT = alloc(NS * 512, BF16)
        nastrip = alloc(4 * 896, BF16, (2, 2, 896))
        xs = [alloc(D, BF16) for _ in range(3)]
        xt = [alloc(D) for _ in range(3)]
        sqt2 = [alloc(8 * 128, BF16, (8, 128)) for _ in range(2)]
        small = alloc(64)
        p1_end = ptr[0]


        def dump(items):
            col = [0]
            for ap, toks in items:
                n = ap.shape[-1] if len(ap.shape) == 2 else None
                assert n is not None
                a = col[0]; col[0] += n
                P.add("pool", lambda e, ap=ap, a=a, n=n: e.dma_start(out=dbg_d[0:ap.shape[0], a:a + n], in_=ap, max_dma_last_dim=2048),
                      reads=toks, dma="dbg")
        alltok = lambda: list(P.last_writer.keys())
        DUMPS['H0'] = lambda: dump([(hnT[:, c, :], alltok()) for c in range(8)])
        DUMPS['I0_1'] = lambda: dump([(QT[0], alltok()), (KT[0], alltok()), (acc[0], alltok()), (acc[1], alltok()), (yT[:, 0, :], alltok()),
                                      (Vt_raw[:, 0:4096], alltok())])
        DUMPS['I0_5'] = lambda: dump([(QT[0], alltok()), (KT[0], alltok()), (acc[0], alltok()), (acc[1], alltok()), (yT[:, 4, :], alltok()),
                                      (Vt_raw[:, 0:4096], alltok())])
        for k_ in (2, 3, 4, 6, 7):
            DUMPS['I0_%d' % k_] = lambda: dump([(yT[:, c, :], alltok()) for c in range(8)])
        if os.environ.get('KD2'):
            DUMPS['I0_2'] = lambda: dump([(QT[0], alltok()), (KT[0], alltok()), (QT[1], alltok()), (KT[1], alltok()), (wblk[0][0][:, 0, :], alltok()), (wblk[0][2][:, 0, :], alltok()), (wblk[1][0][:, 0, :], alltok())])
        DUMPS['P0'] = lambda: dump([(yT[:, c, :], alltok()) for c in range(8)])
        bP = [banks[0], banks[1]]
        bPbf = [banks[0].bitcast(BF16), banks[1].bitcast(BF16)]
        bS = [banks[2], banks[3], banks[4], banks[0], banks[1]][:NS]
        SBK = [2, 3, 4, 0, 1]
        bO = [banks[5], banks[6], pb_t.bitcast(F32)][:NO]

        P.add("pool", lambda e: e.dma_start(out=dilb, in_=dilb_d, max_dma_last_dim=4096), writes=["dilb"], dma="c3")
        if DEBUG:
            P.add("pool", lambda e: e.memset(yT, 0.0), writes=["yT%d_%d" % (c, hp) for c in range(8) for hp in range(2)])

        for st_ in range(2):
            P.add("pool", lambda e, st_=st_: e.memset(QT[st_], 0.0), writes=["QT%d" % st_])
        P.add("sp", lambda e: e.dma_start(out=gmixr, in_=gmixr_d), writes=["gmixr"], dma="c4")
        MASKMUL = os.environ.get('KMASK', '0') == '1'
        if MASKMUL:
            for c_ in range(0, 5120, 512):
                P.add("act", lambda e, c_=c_: e.activation(out=dilb[:, c_:c_ + 512], in_=dilb[:, c_:c_ + 512], func=AF.Exp), reads=["dilb"], writes=["dilb"])
        mctr = {"n": 0}

        def mask_mul(ptv, mv, si, mtok):
            eng = "pool" if (mctr["n"] % int(os.environ.get('KMASKDVE', '3'))) != 0 else "dve"
            mctr["n"] += 1
            P.add(eng, lambda e: e.tensor_tensor(out=ptv, in0=ptv, in1=mv, op=ALU.mult), reads=["PT%d" % si, mtok], writes=["PT%d" % si])
        ctr = {"S": 0, "O": 0, "P": 0, "x": 0}

        for s in range(2):
            tb = s * S
            def h_prep(tt):
                xi = ctr["x"] % 3; ctr["x"] += 1
                xtile = xt[xi]; xsb = xs[xi]
                row0 = tb + tt * 128
                P.add("sp", lambda e: e.dma_start(out=xtile, in_=x_d[row0:row0 + 128, :]),
                      writes=["xt%d_src" % xi], dma="x%d" % xi)
                ss = small[:, xi * 4:xi * 4 + 1]; ms = small[:, xi * 4 + 1:xi * 4 + 2]; rstd = small[:, xi * 4 + 2:xi * 4 + 3]
                P.add("act", lambda e: e.activation(out=xsb, in_=xtile, func=AF.Square, accum_out=ss),
                      reads=["xt%d_src" % xi], writes=["xs%d" % xi, "xt%d_ss" % xi])
                P.add("dve", lambda e: e.tensor_scalar(out=ms, in0=ss, scalar1=1.0 / D, scalar2=EPS, op0=ALU.mult, op1=ALU.add),
                      reads=["xt%d_ss" % xi], writes=["xt%d_ms" % xi])
                P.add("pool", lambda e: e.tensor_tensor(out=rstd, in0=ms, in1=mhalf[:, 0:1], op=ALU.pow),
                      reads=["xt%d_ms" % xi, "mhalf"], writes=["xt%d_rstd" % xi])
                P.add("dve", lambda e: e.scalar_tensor_tensor(out=xsb, in0=xtile, scalar=rstd, in1=gmixr, op0=ALU.mult, op1=ALU.mult),
                      reads=["xt%d_src" % xi, "xt%d_rstd" % xi, "gmixr"], writes=["xs%d" % xi])
                return (xsb, xi)

            def h_trans(ctx, tt):
                xsb, xi = ctx
                tbk, ttok = tbank[tt % 2]
                for c in range(8):
                    P.add("pe", lambda e, c=c: e.transpose(tbk[:, c * 128:(c + 1) * 128], xsb[:, c * 128:(c + 1) * 128], ident),
                          reads=["xs%d" % xi, "ident"], writes=[ttok])
                dst = hnT[:, :, tt * 128:(tt + 1) * 128]
                src = tbk[:, 0:1024].rearrange("p (c q) -> p c q", c=8)
                if tt % 2 == 0:
                    P.add("dve", lambda e: e.tensor_copy(out=dst, in_=src), reads=[ttok], writes=["hn%d_a" % tt])
                else:
                    P.add("act", lambda e: e.activation(out=dst, in_=src, func=AF.Copy), reads=[ttok], writes=["hn%d_b" % tt])

            hctx = [h_prep(0), h_prep(1)]
            for tt in range(16):
                if tt + 2 < 16:
                    hctx.append(h_prep(tt + 2))
                h_trans(hctx[tt], tt)

            def hn_tokens(tiles):
                out = []
                for t in tiles:
                    out += ["hn%d_a" % t, "hn%d_b" % t]
                return out

            stage('H%d' % s)
            for lay_ in range(3):
                P.add("pool", lambda e, lay_=lay_: e.memset(Vt[:, lay_, :, :, 64:128], 1.0), writes=["V%d" % lay_])
            for item in range(8):
                stage('I%d_%d' % (s, item))
                if os.environ.get('KBAR'):
                    P.add("act", lambda e: e.activation(out=bar[:, 0:1], in_=mhalf[:, 0:1], func=AF.Copy), reads=["mhalf"], writes=["bar_act"])
                    P.add("dve", lambda e: e.tensor_copy(out=bar[:, 1:2], in_=mhalf[:, 0:1]), reads=["mhalf"], writes=["bar_dve"])
                    P.add("pool", lambda e: e.tensor_copy(out=bar[:, 2:3], in_=mhalf[:, 0:1]), reads=["mhalf"], writes=["bar_pool"])
                    for en in ("pe", "act", "dve", "pool", "sp"):
                        P.add(en, lambda e: None, reads=["bar_act", "bar_dve", "bar_pool"], waitonly=True)
                if os.environ.get('KSNAP') and item == 1:
                    P.add("pool", lambda e: e.tensor_copy(out=yT[:, 7, :], in_=yT[:, 0, :]), reads=["yT0_0", "yT0_1"], writes=["yT7_0", "yT7_1"])
                    P.add("pool", lambda e: e.tensor_copy(out=yT[:, 6, :], in_=acc[0][:, :]), reads=["acc0"], writes=["yT6_0", "yT6_1"])
                is_dil = item < 4
                j = item % 4
                st = item % 2
                colbase = 0 if is_dil else 1536
                wq, wk, wv = wblk[st]
                for wi, (wb, off) in enumerate(((wq, 0), (wk, 512), (wv, 1024))):
                    c0 = colbase + off + j * 128
                    P.add("pool", lambda e, wb=wb, c0=c0: e.dma_start(out=wb, in_=win_d.rearrange("(c p) n -> p c n", p=128)[:, :, c0:c0 + 128]),
                          writes=["wb%d_%d" % (st, wi)], dma="wb%d_%d" % (st, wi))
                if not is_dil:
                    P.add("pool", lambda e, j=j: e.dma_start(out=nastrip, in_=nas_d[j].rearrange("p (h v n) -> p h v n", h=2, v=2), max_dma_last_dim=3584),
                          writes=["nastrip"], dma="nas")
                    if MASKMUL:
                        nflat = nastrip.rearrange("p h v n -> p (h v n)")
                        for c_ in range(0, 3584, 512):
                            P.add("act", lambda e, c_=c_, nflat=nflat: e.activation(out=nflat[:, c_:c_ + 512], in_=nflat[:, c_:c_ + 512], func=AF.Exp), reads=["nastrip"], writes=["nastrip"])
                if s == 0:
                    for ex_ in (2 * item, 2 * item + 1):
                        for nm_, src_, dst_ in (("g", wg_d, wgs_d), ("u", wu_d, wus_d), ("d", wd_d, wds_d)):
                            P.add("pool", lambda e, src_=src_, dst_=dst_, ex_=ex_: e.dma_start(out=dst_[ex_], in_=src_[ex_], max_dma_last_dim=4096),
                                  writes=["cv%s%d" % (nm_, ex_)], dma="cv%s%d" % (nm_, ex_))
                for which, (wb, dstT, scl) in enumerate(((wq, QT[st], 0.125), (wk, KT[st], 1.0))):
                    for tc in range(4):
                        bi = ctr["P"] % 2; ctr["P"] += 1
                        bk = bP[bi]
                        for c in range(8):
                            P.add("pe", lambda e, bk=bk, wb=wb, c=c, tc=tc: e.matmul(bk[:, 0:512], lhsT=wb[:, c, :], rhs=hnT[:, c, tc * 512:(tc + 1) * 512],
                                                                                     start=(c == 0), stop=(c == 7)),
                                  reads=["wb%d_%d" % (st, which)] + hn_tokens(range(4 * tc, 4 * tc + 4)), writes=["bk%d" % bi])
                        dst = dstT[:, tc * 512:(tc + 1) * 512] if which == 1 else None
                        tokn = ("QT%d" if which == 0 else "KT%d") % st
                        if which == 0:
                            for hp_ in range(2):
                                rw = slice(hp_ * 64, hp_ * 64 + 64)
                                P.add("dve", lambda e, bk=bk, rw=rw, hp_=hp_, tc=tc, dstT=dstT: e.tensor_scalar(out=dstT[rw, hp_, tc * 512:(tc + 1) * 512], in0=bk[rw, 0:512], scalar1=0.125, scalar2=None, op0=ALU.mult),
                                      reads=["bk%d" % bi], writes=[tokn])
                        else:
                            P.add("dve", lambda e, dst=dst, bk=bk: e.tensor_copy(out=dst, in_=bk[:, 0:512]),
                                  reads=["bk%d" % bi], writes=[tokn])
                for tc in range(4):
                    bi = ctr["P"] % 2; ctr["P"] += 1
                    bk = bP[bi]
                    for c in range(8):
                        P.add("pe", lambda e, bk=bk, wv=wv, c=c, tc=tc: e.matmul(bk[:, 0:512], lhsT=wv[:, c, :], rhs=hnT[:, c, tc * 512:(tc + 1) * 512],
                                                                                 start=(c == 0), stop=(c == 7)),
                              reads=["wb%d_2" % st] + hn_tokens(range(4 * tc, 4 * tc + 4)), writes=["bk%d" % bi])
                    dst = VTb[:, tc * 512:(tc + 1) * 512]
                    P.add("dve", lambda e, dst=dst, bk=bk: e.tensor_copy(out=dst, in_=bk[:, 0:512]),
                          reads=["bk%d" % bi], writes=["VT"])
                layouts = ((0, 1), (1, 4), (2, 16)) if is_dil else ((0, 1),)
                for (lay, d) in layouts:
                    L = S // d; nt = L // 128
                    tiles = [(r, m) for r in range(d) for m in range(nt)]
                    for g8 in range(2):
                        bi = ctr["P"] % 2; ctr["P"] += 1
                        bkb = bPbf[bi]
                        for q in range(8):
                            r, m = tiles[g8 * 8 + q]
                            tsl_ = tsl(r, d, 128 * m, 128)
                            P.add("pe", lambda e, bkb=bkb, q=q, tsl_=tsl_: e.transpose(bkb[:, q * 128:(q + 1) * 128], VTb[:, tsl_], ident),
                                  reads=["VT", "ident"], writes=["bk%d" % bi])
                        dst = Vt[:, lay, g8 * 8:(g8 + 1) * 8, :, 0:64]
                        src = bkb[:, 0:1024].rearrange("p (t h c) -> p t h c", t=8, h=2)
                        P.add("dve", lambda e, dst=dst, src=src: e.tensor_copy(out=dst, in_=src),
                              reads=["bk%d" % bi], writes=["V%d" % lay])

                tasks = []
                qz = QT[st]; kTp = KT[st]
                ATOK = ["acc0", "acc1"]
                v2 = lambda ap, lo, n: ap[:, lo:lo + 2 * n].rearrange("p (h q) -> p h q", h=2)
                if is_dil:
                    for pi, d in enumerate((1, 4)):
                        L = S // d; nt = L // 128
                        biasP = dilb[:, (j * 2 + pi) * 512:(j * 2 + pi + 1) * 512]
                        for r in range(d):
                            for b in range(nt + 1):
                                def s_part(b=b, r=r, d=d, nt=nt, biasP=biasP, qz=qz, kTp=kTp, st=st):
                                    si = ctr["S"] % NS; ctr["S"] += 1
                                    bkS = bS[si]; pt = PT[:, si * 512:(si + 1) * 512]
                                    tk = "bk%d" % SBK[si]
                                    if b == 0:
                                        ov = v2(bkS, 256, 128)[:, :, 64:128]; bv = v2(biasP, 256, 128)[:, :, 64:128]; pv_ = v2(pt, 256, 128)[:, :, 64:128]
                                        for hp_ in range(2):
                                            c0_ = 256 + hp_ * 128 + 64
                                            if not MASKMUL:
                                                P.add("pe", lambda e, c0_=c0_: e.matmul(bkS[:, c0_:c0_ + 64], lhsT=ident, rhs=biasP[:, c0_:c0_ + 64], start=(c0_ == 320), stop=False),
                                                      reads=["ident", "dilb"], writes=[tk])
                                            P.add("pe", lambda e, c0_=c0_, hp_=hp_: e.matmul(bkS[:, c0_:c0_ + 64], lhsT=kTp[:, tsl(r, d, 0, 128)], rhs=qz[:, hp_, tsl(r, d, 0, 64)], start=(MASKMUL and hp_ == 0), stop=True),
                                                  reads=["QT%d" % st, "KT%d" % st], writes=[tk])
                                        P.add("act", lambda e: e.activation(out=pv_, in_=ov, func=AF.Exp), reads=[tk], writes=["PT%d" % si])
                                        if MASKMUL:
                                            mask_mul(pv_, bv, si, "dilb")
                                    elif b == nt:
                                        ov = v2(bkS, 0, 128)[:, :, 0:64]; bv = v2(biasP, 0, 128)[:, :, 0:64]; pv_ = v2(pt, 0, 128)[:, :, 0:64]
                                        for hp_ in range(2):
                                            c0_ = hp_ * 128
                                            if not MASKMUL:
                                                P.add("pe", lambda e, c0_=c0_: e.matmul(bkS[:, c0_:c0_ + 64], lhsT=ident, rhs=biasP[:, c0_:c0_ + 64], start=(c0_ == 0), stop=False),
                                                      reads=["ident", "dilb"], writes=[tk])
                                            P.add("pe", lambda e, c0_=c0_, hp_=hp_: e.matmul(bkS[:, c0_:c0_ + 64], lhsT=kTp[:, tsl(r, d, 128 * (nt - 1), 128)], rhs=qz[:, hp_, tsl(r, d, 128 * nt - 64, 64)], start=(MASKMUL and hp_ == 0), stop=True),
                                                  reads=["QT%d" % st, "KT%d" % st], writes=[tk])
                                        P.add("act", lambda e: e.activation(out=pv_, in_=ov, func=AF.Exp), reads=[tk], writes=["PT%d" % si])
                                        if MASKMUL:
                                            mask_mul(pv_, bv, si, "dilb")
                                    else:
                                        qv = qz[:, :, tsl(r, d, 128 * b - 64, 128)]
                                        if not MASKMUL:
                                            P.add("pe", lambda e: e.matmul(bkS[:, 0:512], lhsT=ident, rhs=biasP, start=True, stop=False), reads=["ident", "dilb"], writes=[tk])
                                        P.add("pe", lambda e: e.matmul(bkS[:, 0:256], lhsT=kTp[:, tsl(r, d, 128 * (b - 1), 128)], rhs=qv, start=MASKMUL, stop=False),
                                              reads=["QT%d" % st, "KT%d" % st], writes=[tk])
                                        P.add("pe", lambda e: e.matmul(bkS[:, 256:512], lhsT=kTp[:, tsl(r, d, 128 * b, 128)], rhs=qv, start=False, stop=True),
                                              reads=["QT%d" % st, "KT%d" % st], writes=[tk])
                                        P.add("act", lambda e: e.activation(out=pt[:, 0:512], in_=bkS[:, 0:512], func=AF.Exp), reads=[tk], writes=["PT%d" % si])
                                        if MASKMUL:
                                            mask_mul(pt[:, 0:512], biasP, si, "dilb")
                                    return si

                                def pv_part(si, b=b, r=r, d=d, nt=nt, pi=pi):
                                    oi = ctr["O"] % NO; ctr["O"] += 1
                                    bkO = bO[oi]; pt = PT[:, si * 512:(si + 1) * 512]
                                    tk = "bk%d" % (5 + oi)
                                    first = [True]
                                    for hp in range(2):
                                        def mm(o_, l_, r_):
                                            stt = first[0]; first[0] = False
                                            P.add("pe", lambda e: e.matmul(o_, lhsT=l_, rhs=r_, start=stt, stop=True), reads=["PT%d" % si, "V%d" % pi], writes=[tk])
                                        ob = hp * 128
                                        if b == 0:
                                            mm(bkO[:, ob + 64:ob + 128], Vt[:, pi, r * nt + 0, hp, :], pt[:, 256 + hp * 128 + 64:256 + hp * 128 + 128])
                                        elif b == nt:
                                            mm(bkO[:, ob:ob + 64], Vt[:, pi, r * nt + nt - 1, hp, :], pt[:, hp * 128:hp * 128 + 64])
                                        else:
                                            mm(bkO[:, ob:ob + 128], Vt[:, pi, r * nt + b - 1, hp, :], pt[:, hp * 128:hp * 128 + 128])
                                            mm(bkO[:, ob:ob + 128], Vt[:, pi, r * nt + b, hp, :], pt[:, 256 + hp * 128:256 + hp * 128 + 128])
                                    c_lo = 64 if b == 0 else 0
                                    c_hi = 64 if b == nt else 128
                                    p_lo = 128 * b - 64 + c_lo
                                    dsta = acc2[:, :, tsl(r, d, p_lo, c_hi - c_lo)]
                                    srca = v2(bkO, 0, 128)[:, :, c_lo:c_hi]
                                    if pi == 0:
                                        P.add("dve", lambda e: e.tensor_copy(out=dsta, in_=srca), reads=[tk], writes=ATOK)
                                    else:
                                        P.add("dve", lambda e: e.tensor_tensor(out=dsta, in0=srca, in1=dsta, op=ALU.add), reads=[tk] + ATOK, writes=ATOK)
                                tasks.append((s_part, pv_part))
                    bias3P = dilb[:, 4096 + j * 256:4096 + (j + 1) * 256]
                    for r0 in range(0, 16, 2):
                        def s_part(r0=r0, bias3P=bias3P, qz=qz, kTp=kTp, st=st):
                            si = ctr["S"] % NS; ctr["S"] += 1
                            bkS = bS[si]; pt = PT[:, si * 512:(si + 1) * 512]
                            tk = "bk%d" % SBK[si]
                            for rr in range(2):
                                r_ = r0 + rr
                                if not MASKMUL:
                                    P.add("pe", lambda e, rr=rr: e.matmul(bkS[:, rr * 256:(rr + 1) * 256], lhsT=ident, rhs=bias3P, start=True, stop=False),
                                          reads=["ident", "dilb"], writes=[tk])
                                P.add("pe", lambda e, rr=rr, r_=r_: e.matmul(bkS[:, rr * 256:(rr + 1) * 256], lhsT=kTp[:, tsl(r_, 16, 0, 128)], rhs=qz[:, :, tsl(r_, 16, 0, 128)], start=(MASKMUL and rr == 0), stop=True),
                                      reads=["QT%d" % st, "KT%d" % st], writes=[tk])
                            P.add("act", lambda e: e.activation(out=pt[:, 0:512], in_=bkS[:, 0:512], func=AF.Exp), reads=[tk], writes=["PT%d" % si])
                            if MASKMUL:
                                mask_mul(pt[:, 0:512].rearrange("p (r c) -> p r c", r=2), bias3P.unsqueeze(1).to_broadcast([128, 2, 256]), si, "dilb")
                            return si

                        def pv_part(si, r0=r0):
                            oi = ctr["O"] % NO; ctr["O"] += 1
                            bkO = bO[oi]; pt = PT[:, si * 512:(si + 1) * 512]
                            tk = "bk%d" % (5 + oi)
                            for rr in range(2):
                                for hp in range(2):
                                    cs_ = slice(rr * 256 + hp * 128, rr * 256 + hp * 128 + 128)
                                    P.add("pe", lambda e, rr=rr, hp=hp, cs_=cs_: e.matmul(bkO[:, cs_], lhsT=Vt[:, 2, r0 + rr, hp, :], rhs=pt[:, cs_], start=(rr == 0 and hp == 0), stop=True),
                                          reads=["PT%d" % si, "V2"], writes=[tk])
                            dsta = acc2.rearrange("p h (q r) -> p h q r", r=16)[:, :, :, r0:r0 + 2]
                            srca = bkO[:, 0:512].rearrange("p (r h q) -> p h q r", r=2, h=2)
                            P.add("dve", lambda e: e.tensor_tensor(out=dsta, in0=srca, in1=dsta, op=ALU.add), reads=[tk] + ATOK, writes=ATOK)
                        tasks.append((s_part, pv_part))
                else:
                    for g in range(8):
                        if g == 0:
                            ms_, var = [0, 1, 2, 3], 1
                        elif g == 7:
                            ms_, var = [12, 13, 14, 15], 1
                        else:
                            ms_, var = list(range(2 * g - 2, 2 * g + 4)), 0
                        ostate = {}
                        for ki, m in enumerate(ms_):
                            def s_part(m=m, g=g, var=var, qz=qz, kTp=kTp, st=st):
                                si = ctr["S"] % NS; ctr["S"] += 1
                                bkS = bS[si]; pt = PT[:, si * 512:(si + 1) * 512]
                                tk = "bk%d" % SBK[si]
                                sft = 6 - (2 * m - 4 * g)
                                assert 0 <= sft and sft * 64 + 256 <= 896
                                if not MASKMUL:
                                    P.add("pe", lambda e: e.matmul(bkS[:, 0:512], lhsT=ident, rhs=nastrip[:, :, var, sft * 64:sft * 64 + 256], start=True, stop=False),
                                          reads=["ident", "nastrip"], writes=[tk])
                                P.add("pe", lambda e: e.matmul(bkS[:, 0:512], lhsT=kTp[:, m * 128:(m + 1) * 128], rhs=qz[:, :, g * 256:(g + 1) * 256], start=MASKMUL, stop=True),
                                      reads=["QT%d" % st, "KT%d" % st], writes=[tk])
                                P.add("act", lambda e: e.activation(out=pt[:, 0:512], in_=bkS[:, 0:512], func=AF.Exp), reads=[tk], writes=["PT%d" % si])
                                if MASKMUL:
                                    mask_mul(v2(pt, 0, 256), nastrip[:, :, var, sft * 64:sft * 64 + 256], si, "nastrip")
                                return si

                            def pv_part(si, m=m, ki=ki, g=g, ostate=ostate, nms=len(ms_)):
                                if ki == 0:
                                    ostate["oi"] = ctr["O"] % NO; ctr["O"] += 1
                                oi = ostate["oi"]
                                bkO = bO[oi]; pt = PT[:, si * 512:(si + 1) * 512]
                                tk = "bk%d" % (5 + oi)
                                for hp in range(2):
                                    P.add("pe", lambda e, hp=hp: e.matmul(bkO[:, hp * 256:(hp + 1) * 256], lhsT=Vt[:, 0, m, hp, :], rhs=pt[:, hp * 256:(hp + 1) * 256],
                                                                         start=(ki == 0 and hp == 0), stop=(ki == nms - 1)),
                                          reads=["PT%d" % si, "V0"], writes=[tk])
                                if ki == nms - 1:
                                    dsta = acc2[:, :, g * 256:(g + 1) * 256]
                                    P.add("dve", lambda e: e.tensor_copy(out=dsta, in_=v2(bkO, 0, 256)), reads=[tk], writes=ATOK)
                            tasks.append((s_part, pv_part))

                for hp in range(2):
                    def fin_part(_si, hp=hp, chunk=(j if is_dil else 4 + j)):
                        rows = slice(hp * 64, hp * 64 + 64)
                        P.add("act", lambda e: e.activation(out=rden[0:64, :], in_=acc2[64:128, hp, :], func=AF.Ln), reads=ATOK, writes=["rden"])
                        P.add("act", lambda e: e.activation(out=rden[0:64, :], in_=rden[0:64, :], func=AF.Exp, scale=-1.0), reads=["rden"], writes=["rden"])
                        P.add("dve", lambda e: e.tensor_tensor(out=yT[rows, chunk, :], in0=acc2[0:64, hp, :], in1=rden[0:64, :], op=ALU.mult),
                              reads=ATOK + ["rden"], writes=["yT%d_%d" % (chunk, hp)])
                    tasks.append((None, fin_part))

                LOOK = int(os.environ.get('KLOOK', '2'))
                sis = []
                for ti_, (sp_, pv_) in enumerate(tasks):
                    sis.append(sp_() if sp_ is not None else None)
                    if ti_ >= LOOK:
                        tasks[ti_ - LOOK][1](sis[ti_ - LOOK])
                for ti_ in range(max(0, len(tasks) - LOOK), len(tasks)):
                    tasks[ti_][1](sis[ti_])


            stage('P%d' % s)
            ytoks = ["yT%d_%d" % (c, hp) for c in range(8) for hp in range(2)]
            P.add("pool", lambda e: e.dma_start(out=wout_bf, in_=wout_d.rearrange("(c p) n -> p c n", p=128)),
                  writes=["V0", "V1", "V2", "wout"], dma="wout")
            for c in range(8):
                P.add("dve", lambda e, c=c: e.tensor_scalar(out=wout_bf[:, c, :], in0=wout_bf[:, c, :], scalar1=gout[:, c:c + 1], scalar2=None, op0=ALU.mult),
                      reads=["wout", "vecs"], writes=["wout"])
            def o_prep(tt):
                row0 = tb + tt * 128
                tsl_ = slice(tt * 128, (tt + 1) * 128)
                xi = ctr["x"] % 3; ctr["x"] += 1
                qi = tt % 2
                xtile = xt[xi]; sq_ = sqt2[qi]
                P.add("sp", lambda e: e.dma_start(out=xtile, in_=x_d[row0:row0 + 128, :]),
                      writes=["xt%d_src" % xi], dma="x%d" % xi)
                P.add("pool", lambda e: e.tensor_tensor(out=sq_, in0=yT[:, :, tsl_], in1=yT[:, :, tsl_], op=ALU.mult),
                      reads=ytoks, writes=["sqt%d" % qi])
                bq = bS[2]
                for c in range(8):
                    col = qi * 2 + c // 4
                    P.add("pe", lambda e, c=c, col=col: e.matmul(bq[:, col:col + 1], lhsT=sq_[:, c, :], rhs=ones1[:, 0:1], start=(c % 4 == 0), stop=(c % 4 == 3)),
                          reads=["sqt%d" % qi, "ones1"], writes=["bk4"])
                ms2 = small[:, 16 + xi * 4:16 + xi * 4 + 2]; rs2 = small[:, 16 + xi * 4 + 2:16 + xi * 4 + 4]
                P.add("dve", lambda e: e.tensor_scalar(out=ms2, in0=bq[:, qi * 2:qi * 2 + 2], scalar1=1.0 / 512, scalar2=EPS, op0=ALU.mult, op1=ALU.add),
                      reads=["bk4"], writes=["ms2_%d" % xi])
                P.add("pool", lambda e: e.tensor_tensor(out=rs2, in0=ms2, in1=mhalf[:, 0:2], op=ALU.pow),
                      reads=["ms2_%d" % xi, "mhalf"], writes=["rs2_%d" % xi])
                return (xtile, xi, rs2, row0, tsl_)

            def o_main(ctx, tt):
                xtile, xi, rs2, row0, tsl_ = ctx
                hi_ = tt % 2
                ht = acc[hi_][:, 0:D]
                for half in range(2):
                    hs_ = slice(half * 512, (half + 1) * 512)
                    bA = [bP[0], bP[1]][half]; bB = [bS[0], bS[1]][half]
                    for c in range(4):
                        P.add("pe", lambda e, c=c, bA=bA, hs_=hs_: e.matmul(bA[:, 0:512], lhsT=yT[:, c, tsl_], rhs=wout_bf[:, c, hs_], start=(c == 0), stop=(c == 3)),
                              reads=ytoks + ["wout", "V0", "V1", "V2"], writes=["bk%d" % half])
                    for c in range(4, 8):
                        P.add("pe", lambda e, c=c, bB=bB, hs_=hs_: e.matmul(bB[:, 0:512], lhsT=yT[:, c, tsl_], rhs=wout_bf[:, c, hs_], start=(c == 4), stop=(c == 7)),
                              reads=ytoks + ["wout", "V0", "V1", "V2"], writes=["bk%d" % (2 + half)])
                    P.add("dve", lambda e, hs_=hs_, bA=bA: e.scalar_tensor_tensor(out=ht[:, hs_], in0=bA[:, 0:512], scalar=rs2[:, 0:1], in1=xtile[:, hs_], op0=ALU.mult, op1=ALU.add),
                          reads=["bk%d" % half, "rs2_%d" % xi, "xt%d_src" % xi], writes=["acc%d" % hi_])
                    P.add("dve", lambda e, hs_=hs_, bB=bB: e.scalar_tensor_tensor(out=ht[:, hs_], in0=bB[:, 0:512], scalar=rs2[:, 1:2], in1=ht[:, hs_], op0=ALU.mult, op1=ALU.add),
                          reads=["bk%d" % (2 + half), "rs2_%d" % xi, "acc%d" % hi_], writes=["acc%d" % hi_])
                P.add("sp", lambda e: e.dma_start(out=hs_d[row0:row0 + 128, :], in_=ht),
                      reads=["acc%d" % hi_], writes=["hs%d" % (row0 // 128)], dma="hst%d" % hi_)

            octx = [o_prep(0)]
            for tt in range(16):
                if tt + 1 < 16:
                    octx.append(o_prep(tt + 1))
                o_main(octx[tt], tt)

        stage('O')
        P.add("act", lambda e: e.activation(out=bar[:, 0:1], in_=mhalf[:, 0:1], func=AF.Copy), reads=["mhalf"], writes=["bar_act"])
        P.add("dve", lambda e: e.tensor_copy(out=bar[:, 1:2], in_=mhalf[:, 0:1]), reads=["mhalf"], writes=["bar_dve"])
        P.add("pool", lambda e: e.tensor_copy(out=bar[:, 2:3], in_=mhalf[:, 0:1]), reads=["mhalf"], writes=["bar_pool"])
        for en in ("pe", "act", "dve", "pool", "sp"):
            P.add(en, lambda e: None, reads=["bar_act", "bar_dve", "bar_pool"] + ["hs%d" % i_ for i_ in range(32)], waitonly=True)

        ptr[0] = persist_end
        wd_bf = alloc(32 * D, BF16, (32, D))
        hn2T = alloc(8 * 1024, BF16, (8, 1024))
        actT = alloc(32 * 1024, BF16, (32, 1024))
        wgu = [[alloc(8 * 256, BF16, (8, 256)) for _ in range(2)] for _ in range(3)]
        hbA = alloc(D); hbD = [alloc(D) for _ in range(2)]
        xsA = alloc(D, BF16); xsD = alloc(D, BF16)
        combT = alloc(1024, BF16)
        Cs = [alloc(512, BF16) for _ in range(2)]
        ssb = [alloc(512, BF16) for _ in range(2)]
        s2b = [alloc(512, BF16) for _ in range(2)]
        rt = alloc(960)
        comb_bf = alloc(8 * 16, BF16, (8, 16))
        small2 = alloc(32)

        bG = [banks[0], banks[1]]; bU = [banks[2], banks[3]]; bC = banks[4]; bD = [banks[5], banks[6]]

        def load_wd():
            for k in range(4):
                P.add("sp", lambda e, k=k: e.dma_start(out=wd_bf[:, 8 * k:8 * k + 8, :],
                                                       in_=wds_d[4 * k:4 * k + 4].rearrange("e (f p) n -> p (e f) n", p=128)),
                      reads=["cvd%d" % ee for ee in range(4 * k, 4 * k + 4)], writes=["wd%d" % k], dma="wd%d" % k)
        wd_toks = ["wd%d" % k for k in range(4)]

        def load_gu(n):
            if n >= 64:
                return
            ex_ = n % 16; wi_ = n % 3
            wgb_, wub_ = wgu[wi_]
            P.add("sp", lambda e: e.dma_start(out=wgb_, in_=wgs_d[ex_].rearrange("(c p) n -> p c n", p=128)),
                  reads=["cvg%d" % ex_], writes=["wg%d" % wi_], dma="wg%d" % wi_)
            P.add("sp", lambda e: e.dma_start(out=wub_, in_=wus_d[ex_].rearrange("(c p) n -> p c n", p=128)),
                  reads=["cvu%d" % ex_], writes=["wu%d" % wi_], dma="wu%d" % wi_)
        load_gu(0)
        load_gu(1)

        c2 = {"h": 0, "G": 0, "C": 0, "D": 0, "w": 0, "y": 0}
        h2_all = [("h2_%d_a" % st) for st in range(8)] + [("h2_%d_b" % st) for st in range(8)]

        hpool = [(hbA, "hbA"), (hbD[0], "hbD0"), (hbD[1], "hbD1")]
        xpool = [(xsA, "xsA"), (xsD, "xsD")]
        tbank2 = [(pb_t, "bk7"), (banks[2].bitcast(BF16), "bk2")]
        bD4 = [(banks[5], "bk5"), (banks[6], "bk6"), (banks[0], "bk0"), (banks[1], "bk1")]

        def prep_hn2(T, st):
            row0 = T * 1024 + st * 128
            if T == 0:
                htile, htok = hpool[st % 3]; xsb, xtok = xpool[st % 2]
            else:
                htile, htok = hpool[0]; xsb, xtok = xpool[0]
            si_ = c2["h"] % 4; c2["h"] += 1
            P.add("sp", lambda e: e.dma_start(out=htile, in_=hs_d[row0:row0 + 128, :]),
                  reads=["hs%d" % (row0 // 128)], writes=[htok], dma=htok)
            ss = small2[:, si_ * 4:si_ * 4 + 1]; ms = small2[:, si_ * 4 + 1:si_ * 4 + 2]; rstd = small2[:, si_ * 4 + 2:si_ * 4 + 3]
            P.add("act", lambda e: e.activation(out=xsb, in_=htile, func=AF.Square, accum_out=ss),
                  reads=[htok], writes=[xtok, "h_ss%d" % si_])
            P.add("dve", lambda e: e.tensor_scalar(out=ms, in0=ss, scalar1=1.0 / D, scalar2=EPS, op0=ALU.mult, op1=ALU.add),
                  reads=["h_ss%d" % si_], writes=["h_ms%d" % si_])
            P.add("pool", lambda e: e.tensor_tensor(out=rstd, in0=ms, in1=mhalf[:, 0:1], op=ALU.pow),
                  reads=["h_ms%d" % si_, "mhalf"], writes=["h_rstd%d" % si_])
            P.add("dve", lambda e: e.tensor_scalar(out=xsb, in0=htile, scalar1=rstd, scalar2=None, op0=ALU.mult),
                  reads=[htok, "h_rstd%d" % si_], writes=[xtok])
            return (xsb, xtok)

        def trans_hn2(ctx, T, st):
            xsb, xtok = ctx
            tbk, ttok = tbank2[st % 2]
            for c in range(8):
                P.add("pe", lambda e, c=c: e.transpose(tbk[:, c * 128:(c + 1) * 128], xsb[:, c * 128:(c + 1) * 128], ident),
                      reads=[xtok, "ident"], writes=[ttok])
            for c in range(8):
                dst = hn2T[:, c, st * 128:(st + 1) * 128]
                src = tbk[:, c * 128:(c + 1) * 128]
                if st % 2 == 0:
                    P.add("dve", lambda e, dst=dst, src=src, c=c: e.tensor_scalar(out=dst, in0=src, scalar1=gffn[:, c:c + 1], scalar2=None, op0=ALU.mult),
                          reads=[ttok, "vecs"], writes=["h2_%d_a" % st])
                else:
                    P.add("act", lambda e, dst=dst, src=src, c=c: e.activation(out=dst, in_=src, func=AF.Copy, scale=gffn[:, c:c + 1]),
                          reads=[ttok, "vecs"], writes=["h2_%d_b" % st])

        def emit_router(T):
            if True:
                for st in range(8):
                    for c in range(8):
                        P.add("pe", lambda e, st=st, c=c: e.matmul(bC[:, st * 32:st * 32 + 20], lhsT=hn2T[:, c, st * 128:(st + 1) * 128], rhs=wr_bf[:, c, :],
                                                                   start=(c == 0), stop=(c == 7)),
                              reads=["h2_%d_a" % st, "h2_%d_b" % st, "wr_bf"], writes=["bk4"])
                lg = rt[:, 0:160].rearrange("p (s n) -> p s n", s=8)
                bc3 = bC[:, 0:256].rearrange("p (s n) -> p s n", s=8)[:, :, 0:20]
                RT = "rt"
                r3 = lambda lo, n: rt[:, lo:lo + 8 * n].rearrange("p (s n) -> p s n", s=8)
                gl = lg[:, :, 0:4]; el = lg[:, :, 4:20]
                gmax = rt[:, 160:168]; g1 = r3(168, 4); gex = r3(200, 4); gsum = rt[:, 232:240]; gw = rt[:, 240:248]
                pen = r3(248, 16); ml = r3(376, 16); m1 = rt[:, 504:512]; m2 = rt[:, 512:520]; k1 = r3(520, 16)
                dm = rt[:, 648:656]; w1 = rt[:, 656:664]; w2 = rt[:, 664:672]; k2 = r3(672, 16); ml2 = r3(800, 16)
                bcast = lambda ap, n: ap.unsqueeze(2).to_broadcast([128, 8, n])
                P.add("dve", lambda e: e.tensor_tensor(out=lg, in0=bc3, in1=brep.unsqueeze(1).to_broadcast([128, 8, 20]), op=ALU.add),
                      reads=["bk4", "vecs"], writes=[RT])
                P.add("dve", lambda e: e.tensor_reduce(out=gmax, in_=gl, axis=AX.X, op=ALU.max), reads=[RT], writes=[RT + "a"])
                P.add("dve", lambda e: e.tensor_tensor(out=g1, in0=gl, in1=bcast(gmax, 4), op=ALU.is_equal), reads=[RT, RT + "a"], writes=[RT + "b"])
                P.add("dve", lambda e: e.tensor_tensor(out=gex, in0=gl, in1=bcast(gmax, 4), op=ALU.subtract), reads=[RT, RT + "a"], writes=[RT + "c"])
                P.add("act", lambda e: e.activation(out=gex, in_=gex, func=AF.Exp), reads=[RT + "c"], writes=[RT + "d"])
                P.add("dve", lambda e: e.tensor_reduce(out=gsum, in_=gex, axis=AX.X, op=ALU.add), reads=[RT + "d"], writes=[RT + "e"])
                P.add("dve", lambda e: e.reciprocal(out=gw, in_=gsum), reads=[RT + "e"], writes=[RT + "f"])
                P.add("dve", lambda e: e.tensor_scalar(out=pen.rearrange("p s (g n) -> p s g n", g=4), in0=g1.unsqueeze(3).to_broadcast([128, 8, 4, 4]),
                                                       scalar1=-1.0, scalar2=30000.0, op0=ALU.add, op1=ALU.mult), reads=[RT + "b"], writes=[RT + "g"])
                P.add("dve", lambda e: e.tensor_tensor(out=ml, in0=el, in1=pen, op=ALU.add), reads=[RT, RT + "g"], writes=[RT + "h"])
                P.add("dve", lambda e: e.tensor_reduce(out=m1, in_=ml, axis=AX.X, op=ALU.max), reads=[RT + "h"], writes=[RT + "i"])
                P.add("dve", lambda e: e.tensor_tensor(out=k1, in0=ml, in1=bcast(m1, 16), op=ALU.is_equal), reads=[RT + "h", RT + "i"], writes=[RT + "j"])
                P.add("dve", lambda e: e.scalar_tensor_tensor(out=ml2, in0=k1, scalar=-60000.0, in1=ml, op0=ALU.mult, op1=ALU.add), reads=[RT + "h", RT + "j"], writes=[RT + "k"])
                P.add("dve", lambda e: e.tensor_reduce(out=m2, in_=ml2, axis=AX.X, op=ALU.max), reads=[RT + "k"], writes=[RT + "l"])
                P.add("dve", lambda e: e.tensor_tensor(out=k2, in0=ml2, in1=bcast(m2, 16), op=ALU.is_equal), reads=[RT + "k", RT + "l"], writes=[RT + "m"])
                P.add("dve", lambda e: e.tensor_tensor(out=dm, in0=m2, in1=m1, op=ALU.subtract), reads=[RT + "l", RT + "i"], writes=[RT + "n"])
                P.add("act", lambda e: e.activation(out=dm, in_=dm, func=AF.Exp), reads=[RT + "n"], writes=[RT + "o"])
                P.add("dve", lambda e: e.tensor_scalar(out=dm, in0=dm, scalar1=1.0, scalar2=None, op0=ALU.add), reads=[RT + "o"], writes=[RT + "p"])
                P.add("dve", lambda e: e.reciprocal(out=w1, in_=dm), reads=[RT + "p"], writes=[RT + "q"])
                P.add("dve", lambda e: e.tensor_scalar(out=w2, in0=w1, scalar1=-1.0, scalar2=1.0, op0=ALU.mult, op1=ALU.add), reads=[RT + "q"], writes=[RT + "r"])
                P.add("dve", lambda e: e.tensor_tensor(out=w1, in0=w1, in1=gw, op=ALU.mult), reads=[RT + "q", RT + "r", RT + "f"], writes=[RT + "s"])
                P.add("dve", lambda e: e.tensor_tensor(out=w2, in0=w2, in1=gw, op=ALU.mult), reads=[RT + "r", RT + "f"], writes=[RT + "t"])
                P.add("dve", lambda e: e.tensor_tensor(out=k1, in0=k1, in1=bcast(w1, 16), op=ALU.mult), reads=[RT + "j", RT + "s", RT + "k"], writes=[RT + "u"])
                P.add("dve", lambda e: e.tensor_tensor(out=k2, in0=k2, in1=bcast(w2, 16), op=ALU.mult), reads=[RT + "m", RT + "t"], writes=[RT + "v"])
                P.add("dve", lambda e: e.tensor_tensor(out=comb_bf, in0=k1, in1=k2, op=ALU.add), reads=[RT + "u", RT + "v"], writes=["comb_bf"])
                for st in range(8):
                    P.add("pe", lambda e, st=st: e.transpose(pb_t[0:16, st * 128:(st + 1) * 128], comb_bf[:, st, :], ident),
                          reads=["comb_bf", "ident"] + h2_all, writes=["bk7"])
                P.add("dve", lambda e: e.tensor_copy(out=combT[0:16, :], in_=pb_t[0:16, 0:1024]), reads=["bk7"], writes=["combT"])


        def emit_experts(T):
            if True:
                for ex in range(16):
                    nflat = T * 16 + ex
                    wi = nflat % 3
                    wgb, wub = wgu[wi]
                    load_gu(nflat + 2)
                    for half in range(2):
                        hs_ = slice(half * 512, (half + 1) * 512)
                        ci = c2["C"] % 2; c2["C"] += 1
                        P.add("pe", lambda e, ex=ex, hs_=hs_: e.matmul(bC[:, 0:512], lhsT=sel[0:16, ex, :], rhs=combT[0:16, hs_], start=True, stop=True),
                              reads=["sel", "combT"], writes=["bk4"])
                        P.add("dve", lambda e, ci=ci: e.tensor_copy(out=Cs[ci], in_=bC[:, 0:512]), reads=["bk4"], writes=["Cs%d" % ci])
                        for fc in range(2):
                            gi = c2["G"] % 2; c2["G"] += 1
                            for c in range(8):
                                P.add("pe", lambda e, gi=gi, c=c, fc=fc, hs_=hs_, wgb=wgb: e.matmul(bG[gi][:, 0:512], lhsT=wgb[:, c, fc * 128:(fc + 1) * 128], rhs=hn2T[:, c, hs_],
                                                                                                  start=(c == 0), stop=(c == 7)),
                                      reads=["wg%d" % wi] + h2_all, writes=["bk%d" % gi])
                            for c in range(8):
                                P.add("pe", lambda e, gi=gi, c=c, fc=fc, hs_=hs_, wub=wub: e.matmul(bU[gi][:, 0:512], lhsT=wub[:, c, fc * 128:(fc + 1) * 128], rhs=hn2T[:, c, hs_],
                                                                                                  start=(c == 0), stop=(c == 7)),
                                      reads=["wu%d" % wi] + h2_all, writes=["bk%d" % (2 + gi)])
                            P.add("act", lambda e, gi=gi: e.activation(out=ssb[gi], in_=bG[gi][:, 0:512], func=AF.Silu), reads=["bk%d" % gi], writes=["ssb%d" % gi])
                            P.add("pool", lambda e, gi=gi, ci=ci: e.tensor_tensor(out=s2b[gi], in0=ssb[gi], in1=Cs[ci], op=ALU.mult),
                                  reads=["ssb%d" % gi, "Cs%d" % ci], writes=["s2b%d" % gi])
                            P.add("dve", lambda e, gi=gi, ex=ex, fc=fc, hs_=hs_: e.tensor_tensor(out=actT[:, ex * 2 + fc, hs_], in0=bU[gi][:, 0:512], in1=s2b[gi], op=ALU.mult),
                                  reads=["bk%d" % (2 + gi), "s2b%d" % gi], writes=["actT"])


        def down_mm(T, st):
            row0 = T * 1024 + st * 128
            hi = c2["y"] % 2; c2["y"] += 1
            htile, htok = hpool[1 + hi]
            P.add("sp", lambda e: e.dma_start(out=htile, in_=hs_d[row0:row0 + 128, :]),
                  reads=["hs%d" % (row0 // 128)], writes=[htok], dma=htok)
            for dh in range(2):
                bk_, btok = bD4[c2["D"] % 4]; c2["D"] += 1
                ds_ = slice(dh * 512, (dh + 1) * 512)
                for kk in range(32):
                    P.add("pe", lambda e, bk_=bk_, kk=kk, ds_=ds_: e.matmul(bk_[:, 0:512], lhsT=actT[:, kk, st * 128:(st + 1) * 128], rhs=wd_bf[:, kk, ds_],
                                                                          start=(kk == 0), stop=(kk == 31)),
                          reads=["actT"] + wd_toks, writes=[btok])
                P.add("dve", lambda e, bk_=bk_, ds_=ds_: e.tensor_tensor(out=htile[:, ds_], in0=bk_[:, 0:512], in1=htile[:, ds_], op=ALU.add),
                      reads=[btok, htok], writes=[htok])
            return (htile, htok, row0, hi)

        def down_fin(ctx):
            htile, htok, row0, yi = ctx
            junk, jtok = xpool[1]
            ss = small2[:, 16 + yi * 4:16 + yi * 4 + 1]; ms = small2[:, 16 + yi * 4 + 1:16 + yi * 4 + 2]; rstd = small2[:, 16 + yi * 4 + 2:16 + yi * 4 + 3]
            P.add("act", lambda e: e.activation(out=junk, in_=htile, func=AF.Square, accum_out=ss),
                  reads=[htok], writes=[jtok, "f_ss%d" % yi])
            P.add("dve", lambda e: e.tensor_scalar(out=ms, in0=ss, scalar1=1.0 / D, scalar2=EPS, op0=ALU.mult, op1=ALU.add),
                  reads=["f_ss%d" % yi], writes=["f_ms%d" % yi])
            P.add("pool", lambda e: e.tensor_tensor(out=rstd, in0=ms, in1=mhalf[:, 0:1], op=ALU.pow),
                  reads=["f_ms%d" % yi, "mhalf"], writes=["f_rstd%d" % yi])
            P.add("dve", lambda e: e.scalar_tensor_tensor(out=htile, in0=htile, scalar=rstd, in1=gfin, op0=ALU.mult, op1=ALU.mult),
                  reads=[htok, "f_rstd%d" % yi, "gfin"], writes=[htok])
            P.add("sp", lambda e: e.dma_start(out=y_d[row0:row0 + 128, :], in_=htile),
                  reads=[htok], dma="yst%d" % yi)

        for st in range(8):
            trans_hn2(prep_hn2(0, st), 0, st)
        emit_router(0)
        load_wd()
        for T in range(4):
            emit_experts(T)
            for st in range(8):
                cx = prep_hn2(T + 1, st) if T < 3 else None
                dx = down_mm(T, st)
                if cx is not None:
                    trans_hn2(cx, T + 1, st)
                down_fin(dx)
            if T < 3:
                emit_router(T + 1)

    except StopBuild:
        pass

    sems = {}
    for en in Prog.ENGS:
        sems["eng:" + en] = es.enter_context(nc.semaphore("s_" + en))
    for sl in sorted(P.dma_slots):
        sems["dma:" + sl] = es.enter_context(nc.semaphore("d_" + sl))
    if os.environ.get('KMAXOPS'):
        P.ops = P.ops[:int(os.environ['KMAXOPS'])]
        for i_, o_ in enumerate(P.ops[-3:]):
            print('LASTOPS', o_.eng, o_.idx)
    body = P.emit(sems)
    if os.environ.get('KSTATS'):
        print('PROG stats', P.stats, 'ndma_slots', len(P.dma_slots))
    with nc.Block() as block:
        block.sync(body("sp"))
        block.tensor(body("pe"))
        block.scalar(body("act"))
        block.vector(body("dve"))
        block.gpsimd(body("pool"))
    es.close()
    return nc


def _dil_bias():
    out = np.full((128, 4 * 2 * 512 + 4 * 256), NEG, np.float32)
    k = np.arange(128)[:, None]; q = np.arange(128)[None, :]

    def f(delta, slope, d):
        return np.where(np.abs(delta) <= 64, -slope * d * np.abs(delta), NEG).astype(np.float32)
    for j in range(4):
        for hp in range(2):
            slope = 2.0 ** (-(2 * j + hp + 1))
            for pi, d in enumerate((1, 4)):
                c0 = (j * 2 + pi) * 512
                out[:, c0 + hp * 128:c0 + (hp + 1) * 128] = f(k - 64 - q, slope, d)
                out[:, c0 + 256 + hp * 128:c0 + 256 + (hp + 1) * 128] = f(k + 64 - q, slope, d)
            c0 = 4096 + j * 256
            out[:, c0 + hp * 128:c0 + (hp + 1) * 128] = f(k - q, slope, 16)
    return out


def _na_strips(rpb):
    kc = np.arange(64)[:, None]; qc = np.arange(64)[None, :]
    cs = np.clip(qc - 8, 0, 48)
    col_ok = (kc >= cs) & (kc < cs + 16)
    dcidx = np.clip(kc - qc + 15, 0, 30)
    out = np.full((4, 128, 2, 2, 896), NEG, np.float32)
    for h in range(8):
        j, hp = divmod(h, 2)
        for var in range(2):
            for i in range(2):
                for u in range(14):
                    dlt = 6 + i - u
                    ok = (-4 <= dlt <= 3) if var == 0 else (-7 <= dlt <= 7)
                    if not ok:
                        continue
                    blk = np.where(col_ok, rpb[h, dlt + 7][dcidx], NEG).astype(np.float32)
                    out[j, i * 64:(i + 1) * 64, hp, var, u * 64:(u + 1) * 64] = blk
    return out.reshape(4, 128, 4 * 896)


_NC_CACHE = {}


def kernel(x, norm_mix_g, w_in, rpb, g_out_dil, g_out_na, w_out, norm_ffn_g, w_group, b_group, w_router, b_router,
           w_gate, w_up, w_down, norm_final_g):
    f = lambda a: np.ascontiguousarray(np.asarray(a, dtype=np.float32))
    x = f(x).reshape(16 * S, D)
    col = lambda g: f(g).reshape(8, 128).T
    vecs = np.concatenate([col(norm_mix_g[0]), col(norm_ffn_g[0]),
                           col(np.concatenate([f(g_out_dil[0]), f(g_out_na[0])])),
                           np.broadcast_to(np.concatenate([f(b_group[0]), f(b_router[0])])[None, :], (128, 20))], axis=1)
    vecs = np.ascontiguousarray(vecs, dtype=np.float32)
    gfin = np.ascontiguousarray(np.broadcast_to(f(norm_final_g)[None, :], (128, D)))
    gmixr = np.ascontiguousarray(np.broadcast_to(f(norm_mix_g[0])[None, :], (128, D)))
    wr = np.ascontiguousarray(np.concatenate([f(w_group[0]), f(w_router[0])], axis=1))
    shared = {
        "w_in": f(w_in[0]), "w_out": f(w_out[0]), "w_gate": f(w_gate[0]), "w_up": f(w_up[0]), "w_down": f(w_down[0]),
        "wr": wr, "vecs": vecs, "gfin": gfin, "gmixr": gmixr, "dilb": _dil_bias(), "nas": _na_strips(f(rpb[0])),
    }
    if "nc" not in _NC_CACHE:
        _NC_CACHE["nc"] = build_program()
    nc = _NC_CACHE["nc"]
    in_maps = []
    for i in range(NCORES):
        m = dict(shared)
        m["x"] = np.ascontiguousarray(x[i * TOK:(i + 1) * TOK])
        in_maps.append(m)
    res = run_bass_kernel_spmd(nc, in_maps, core_ids=list(range(NCORES)))
    y = np.concatenate([r["y"] for r in res.results], axis=0)
    if DEBUG:
        kernel.dbg = [r.get("dbg") for r in res.results]
    return y.reshape(16, S, D).astype(np.float32)
```

```python
import numpy as np
from contextlib import ExitStack
import concourse.bass as bass
import concourse.mybir as mybir
from concourse.bass_utils import run_bass_kernel_spmd

F32 = mybir.dt.float32
BF16 = mybir.dt.bfloat16
AF = mybir.ActivationFunctionType
ALU = mybir.AluOpType
AX = mybir.AxisListType

NCORES = 8
S = 2048
D = 1024
TOK = 4096
NEG = -30000.0
EPS = 1e-6
import os
DEBUG = bool(os.environ.get('KSTOP', ''))
STOP = os.environ.get('KSTOP', '')


class StopBuild(Exception):
    pass


DUMPS = {}


def stage(name):
    if STOP and name == STOP:
        if name in DUMPS:
            DUMPS[name]()
        raise StopBuild()


class Op:
    __slots__ = ("eng", "fn", "deps", "is_dma", "sem", "semval", "needs_signal", "sigcount", "eidx", "idx", "wo")


class Prog:
    ENGS = ("pe", "act", "dve", "pool", "sp")

    def __init__(self):
        self.ops = []
        self.last_writer = {}
        self.readers = {}
        self.eng_count = {e: 0 for e in self.ENGS}
        self.dma_slots = set()

    def add(self, eng, fn, reads=(), writes=(), dma=None, waitonly=False):
        op = Op()
        op.eng = eng; op.fn = fn; op.idx = len(self.ops)
        op.is_dma = dma is not None
        op.wo = waitonly
        op.sem = dma; op.semval = 0; op.needs_signal = False; op.sigcount = 0
        op.eidx = self.eng_count[eng]; self.eng_count[eng] += 1
        if dma is not None:
            self.dma_slots.add(dma)
        deps = {}
        for t in reads:
            w = self.last_writer.get(t)
            if w is not None:
                deps[w] = "raw"
        for t in writes:
            w = self.last_writer.get(t)
            if w is not None and w not in deps:
                deps[w] = "waw"
            for r in self.readers.get(t, ()):
                if r not in deps and r != op.idx:
                    deps[r] = "war"
        for t in (() if waitonly else reads):
            lst = self.readers.setdefault(t, [])
            if not op.is_dma:
                lst[:] = [r for r in lst if self.ops[r].is_dma or self.ops[r].eng != eng]
            lst.append(op.idx)
        for t in writes:
            self.last_writer[t] = op.idx
            self.readers[t] = []
        op.deps = deps
        self.ops.append(op)
        return op

    def _need_wait(self, op, p, kind):
        if p.is_dma:
            return True
        if p.eng != op.eng:
            return True
        if op.is_dma:
            return True
        if p.eng == "pe":
            return False
        if kind == "raw" and ((op.eidx - p.eidx) <= 3 or p.eng == "pool"):
            return True
        return False

    def emit(self, sem_ctx):
        ops = self.ops
        for op in ops:
            for d, kind in op.deps.items():
                p = ops[d]
                if (not p.is_dma) and self._need_wait(op, p, kind):
                    p.needs_signal = True
        if os.environ.get('KALLSIG'):
            for op in ops:
                if not op.is_dma and op.fn(None) if False else (not op.is_dma and not getattr(op, "wo", False)):
                    op.needs_signal = True
        cnt = {e: 0 for e in self.ENGS}
        dcnt = {}
        for op in ops:
            if op.is_dma:
                dcnt[op.sem] = dcnt.get(op.sem, 0) + 16
                op.semval = dcnt[op.sem]
            elif op.needs_signal:
                cnt[op.eng] += 1
                op.sigcount = cnt[op.eng]
        by_eng = {e: [o for o in ops if o.eng == e] for e in self.ENGS}
        self.stats = (dict(cnt), {e: len(v) for e, v in by_eng.items()})

        def body(ename):
            def run(eng):
                waited = {}
                for op in by_eng[ename]:
                    for d, kind in op.deps.items():
                        p = ops[d]
                        if not self._need_wait(op, p, kind):
                            continue
                        if p.is_dma:
                            key = "dma:" + p.sem; val = p.semval
                        else:
                            key = "eng:" + p.eng; val = p.sigcount
                        if waited.get(key, 0) >= val:
                            continue
                        waited[key] = val
                        eng.wait_ge(sem_ctx[key], val)
                    ins = op.fn(eng)
                    if op.is_dma:
                        ins.then_inc(sem_ctx["dma:" + op.sem], 16)
                    elif op.needs_signal:
                        ins.then_inc(sem_ctx["eng:" + ename], 1)
                for op in by_eng[ename]:
                    if op.is_dma:
                        key = "dma:" + op.sem
                        if waited.get(key, 0) < dcnt[op.sem]:
                            waited[key] = dcnt[op.sem]
                            eng.wait_ge(sem_ctx[key], dcnt[op.sem])
            return run
        return body


def tsl(r, d, p0, n):
    return slice(r + d * p0, r + d * (p0 + n - 1) + 1, d)


def build_program():
    nc = bass.Bass("TRN2", target_bir_lowering=False)
    dt_in = lambda n, s, dt=F32: nc.dram_tensor(n, s, dt, kind="ExternalInput").ap()
    x_d = dt_in("x", [TOK, D])
    win_d = dt_in("w_in", [D, 3072])
    wout_d = dt_in("w_out", [D, D])
    wg_d = dt_in("w_gate", [16, D, 256])
    wu_d = dt_in("w_up", [16, D, 256])
    wd_d = dt_in("w_down", [16, 256, D])
    wr_d = dt_in("wr", [D, 20])
    vec_d = dt_in("vecs", [128, 24 + 20])
    gfin_d = dt_in("gfin", [128, D])
    gmixr_d = dt_in("gmixr", [128, D])
    dilb_d = dt_in("dilb", [128, 5120])
    nas_d = dt_in("nas", [4, 128, 4 * 896])
    y_d = nc.dram_tensor("y", [TOK, D], F32, kind="ExternalOutput").ap()
    hs_d = nc.dram_tensor("hscr", [TOK, D], F32, kind="Internal").ap()
    wgs_d = nc.dram_tensor("wg_bf", [16, D, 256], BF16, kind="Internal").ap()
    wus_d = nc.dram_tensor("wu_bf", [16, D, 256], BF16, kind="Internal").ap()
    wds_d = nc.dram_tensor("wd_bf", [16, 256, D], BF16, kind="Internal").ap()
    dbg_d = None
    if DEBUG:
        dbg_d = nc.dram_tensor("dbg", [128, 8 * 2048], F32, kind="ExternalOutput").ap()

    P = Prog()
    es = ExitStack()
    ARENA = 53200
    arena = es.enter_context(nc.sbuf_tensor("arena", [128, ARENA], F32))
    arena_bf = arena.bitcast(BF16)
    ptr = [0]

    def alloc(ncols, dt=F32, shape=None):
        n32 = ncols if dt == F32 else (ncols + 1) // 2
        a = ptr[0]
        ptr[0] += n32
        assert ptr[0] <= ARENA, ("SBUF overflow", ptr[0])
        if dt == F32:
            ap = arena[:, a:a + ncols]
        else:
            ap = arena_bf[:, 2 * a:2 * a + ncols]
        if shape is not None:
            names = " ".join("d%d" % i for i in range(len(shape)))
            kw = {"d%d" % i: shape[i] for i in range(len(shape))}
            ap = ap.rearrange("p (%s) -> p %s" % (names, names), **kw)
        return ap

    pb_t = es.enter_context(nc.psum_tensor("pb_t", [128, 1024], BF16))
    banks = [es.enter_context(nc.psum_tensor("bk%d" % i, [128, 512], F32)) for i in range(7)]
    tbank = [(pb_t, "bk7"), (banks[6].bitcast(BF16), "bk6")]

    ident = alloc(128, BF16)
    sel = alloc(16 * 128, BF16, (16, 128))
    vecs = alloc(44)
    gfin = alloc(D)
    wr_bf = alloc(8 * 20, BF16, (8, 20))
    mhalf = alloc(4)
    ones1 = alloc(2, BF16)
    bar = alloc(4)
    persist_end = ptr[0]

    try:
        P.add("pool", lambda e: e.memset(ident, 0.0), writes=["ident"])
        P.add("pool", lambda e: e.affine_select(out=ident, in_=ident, pattern=[[-1, 128]], compare_op=ALU.not_equal,
                                                fill=1.0, base=0, channel_multiplier=1), reads=["ident"], writes=["ident"])
        P.add("pool", lambda e: e.memset(sel, 0.0), writes=["sel"])
        P.add("pool", lambda e: e.affine_select(out=sel[0:16], in_=sel[0:16], pattern=[[-1, 16], [0, 128]],
                                                compare_op=ALU.not_equal, fill=1.0, base=0, channel_multiplier=1),
              reads=["sel"], writes=["sel"])
        P.add("pool", lambda e: e.memset(mhalf, -0.5), writes=["mhalf"])
        P.add("pool", lambda e: e.memset(ones1, 1.0), writes=["ones1"])
        P.add("sp", lambda e: e.dma_start(out=vecs, in_=vec_d), writes=["vecs"], dma="c0")
        P.add("sp", lambda e: e.dma_start(out=gfin, in_=gfin_d), writes=["gfin"], dma="c1")
        P.add("pool", lambda e: e.dma_start(out=wr_bf, in_=wr_d.rearrange("(c p) n -> p c n", p=128)),
              writes=["wr_bf"], dma="c2")
        gmix = vecs[:, 0:8]; gffn = vecs[:, 8:16]; gout = vecs[:, 16:24]; brep = vecs[:, 24:44]

        def rms_rstd(src, ss, ms, rstd, junk, tag, ncols=D):
            P.add("act", lambda e: e.activation(out=junk, in_=src, func=AF.Square, accum_out=ss),
                  reads=[tag + "_src"], writes=[tag + "_junk", tag + "_ss"])
            P.add("dve", lambda e: e.tensor_scalar(out=ms, in0=ss, scalar1=1.0 / ncols, scalar2=EPS, op0=ALU.mult, op1=ALU.add),
                  reads=[tag + "_ss"], writes=[tag + "_ms"])
            P.add("pool", lambda e: e.tensor_tensor(out=rstd, in0=ms, in1=mhalf[:, 0:1], op=ALU.pow),
                  reads=[tag + "_ms", "mhalf"], writes=[tag + "_rstd"])

        hnT = alloc(8 * S, BF16, (8, S))
        yT = alloc(8 * S, BF16, (8, S))
        dilb = alloc(5120, BF16)
        wblk = [[alloc(8 * 128, BF16, (8, 128)) for _ in range(3)] for _ in range(2)]
        QT = [alloc(2 * S, BF16, (2, S)) for _ in range(2)]
        KT = [alloc(S, BF16) for _ in range(2)]
        Vt_raw = alloc(3 * 16 * 256, BF16)
        Vt = Vt_raw.rearrange("p (l t h c) -> p l t h c", l=3, t=16, h=2, c=128)
        wout_bf = Vt_raw[:, 0:8 * D].rearrange("p (c n) -> p c n", c=8)
        acc2 = alloc(2 * S, F32, (2, S))
        acc = [acc2[:, 0, :], acc2[:, 1, :]]
        VTb = alloc(S, BF16)
        gmixr = alloc(D)
        rden = alloc(S)
        NS = int(os.environ.get('KNS', '3')); NO = int(os.environ.get('KNO', '2'))
        PT = alloc(NS * 512, BF16)
        nastrip = alloc(4 * 896, BF16, (2, 2, 896))
        xs = [alloc(D, BF16) for _ in range(3)]
        xt = [alloc(D) for _ in range(3)]
        sqt2 = [alloc(8 * 128, BF16, (8, 128)) for _ in range(2)]
        small = alloc(64)
        p1_end = ptr[0]


        def dump(items):
            col = [0]
            for ap, toks in items:
                n = ap.shape[-1] if len(ap.shape) == 2 else None
                assert n is not None
                a = col[0]; col[0] += n
                P.add("pool", lambda e, ap=ap, a=a, n=n: e.dma_start(out=dbg_d[0:ap.shape[0], a:a + n], in_=ap, max_dma_last_dim=2048),
                      reads=toks, dma="dbg")
        alltok = lambda: list(P.last_writer.keys())
        DUMPS['H0'] = lambda: dump([(hnT[:, c, :], alltok()) for c in range(8)])
        DUMPS['I0_1'] = lambda: dump([(QT[0], alltok()), (KT[0], alltok()), (acc[0], alltok()), (acc[1], alltok()), (yT[:, 0, :], alltok()),
                                      (Vt_raw[:, 0:4096], alltok())])
        DUMPS['I0_5'] = lambda: dump([(QT[0], alltok()), (KT[0], alltok()), (acc[0], alltok()), (acc[1], alltok()), (yT[:, 4, :], alltok()),
                                      (Vt_raw[:, 0:4096], alltok())])
        for k_ in (2, 3, 4, 6, 7):
            DUMPS['I0_%d' % k_] = lambda: dump([(yT[:, c, :], alltok()) for c in range(8)])
        if os.environ.get('KD2'):
            DUMPS['I0_2'] = lambda: dump([(QT[0], alltok()), (KT[0], alltok()), (QT[1], alltok()), (KT[1], alltok()), (wblk[0][0][:, 0, :], alltok()), (wblk[0][2][:, 0, :], alltok()), (wblk[1][0][:, 0, :], alltok())])
        DUMPS['P0'] = lambda: dump([(yT[:, c, :], alltok()) for c in range(8)])
        bP = [banks[0], banks[1]]
        bPbf = [banks[0].bitcast(BF16), banks[1].bitcast(BF16)]
        bS = [banks[2], banks[3], banks[4], banks[0], banks[1]][:NS]
        SBK = [2, 3, 4, 0, 1]
        bO = [banks[5], banks[6], pb_t.bitcast(F32)][:NO]

        P.add("pool", lambda e: e.dma_start(out=dilb, in_=dilb_d, max_dma_last_dim=4096), writes=["dilb"], dma="c3")
        if DEBUG:
            P.add("pool", lambda e: e.memset(yT, 0.0), writes=["yT%d_%d" % (c, hp) for c in range(8) for hp in range(2)])

        for st_ in range(2):
            P.add("pool", lambda e, st_=st_: e.memset(QT[st_], 0.0), writes=["QT%d" % st_])
        P.add("sp", lambda e: e.dma_start(out=gmixr, in_=gmixr_d), writes=["gmixr"], dma="c4")
        MASKMUL = os.environ.get('KMASK', '0') == '1'
        if MASKMUL:
            for c_ in range(0, 5120, 512):
                P.add("act", lambda e, c_=c_: e.activation(out=dilb[:, c_:c_ + 512], in_=dilb[:, c_:c_ + 512], func=AF.Exp), reads=["dilb"], writes=["dilb"])
        mctr = {"n": 0}

        def mask_mul(ptv, mv, si, mtok):
            eng = "pool" if (mctr["n"] % int(os.environ.get('KMASKDVE', '3'))) != 0 else "dve"
            mctr["n"] += 1
            P.add(eng, lambda e: e.tensor_tensor(out=ptv, in0=ptv, in1=mv, op=ALU.mult), reads=["PT%d" % si, mtok], writes=["PT%d" % si])
        ctr = {"S": 0, "O": 0, "P": 0, "x": 0}

        for s in range(2):
            tb = s * S
            def h_prep(tt):
                xi = ctr["x"] % 3; ctr["x"] += 1
                xtile = xt[xi]; xsb = xs[xi]
                row0 = tb + tt * 128
                P.add("sp", lambda e: e.dma_start(out=xtile, in_=x_d[row0:row0 + 128, :]),
                      writes=["xt%d_src" % xi], dma="x%d" % xi)
                ss = small[:, xi * 4:xi * 4 + 1]; ms = small[:, xi * 4 + 1:xi * 4 + 2]; rstd = small[:, xi * 4 + 2:xi * 4 + 3]
                P.add("act", lambda e: e.activation(out=xsb, in_=xtile, func=AF.Square, accum_out=ss),
                      reads=["xt%d_src" % xi], writes=["xs%d" % xi, "xt%d_ss" % xi])
                P.add("dve", lambda e: e.tensor_scalar(out=ms, in0=ss, scalar1=1.0 / D, scalar2=EPS, op0=ALU.mult, op1=ALU.add),
                      reads=["xt%d_ss" % xi], writes=["xt%d_ms" % xi])
                P.add("pool", lambda e: e.tensor_tensor(out=rstd, in0=ms, in1=mhalf[:, 0:1], op=ALU.pow),
                      reads=["xt%d_ms" % xi, "mhalf"], writes=["xt%d_rstd" % xi])
                P.add("dve", lambda e: e.scalar_tensor_tensor(out=xsb, in0=xtile, scalar=rstd, in1=gmixr, op0=ALU.mult, op1=ALU.mult),
                      reads=["xt%d_src" % xi, "xt%d_rstd" % xi, "gmixr"], writes=["xs%d" % xi])
                return (xsb, xi)

            def h_trans(ctx, tt):
                xsb, xi = ctx
                tbk, ttok = tbank[tt % 2]
                for c in range(8):
                    P.add("pe", lambda e, c=c: e.transpose(tbk[:, c * 128:(c + 1) * 128], xsb[:, c * 128:(c + 1) * 128], ident),
                          reads=["xs%d" % xi, "ident"], writes=[ttok])
                dst = hnT[:, :, tt * 128:(tt + 1) * 128]
                src = tbk[:, 0:1024].rearrange("p (c q) -> p c q", c=8)
                if tt % 2 == 0:
                    P.add("dve", lambda e: e.tensor_copy(out=dst, in_=src), reads=[ttok], writes=["hn%d_a" % tt])
                else:
                    P.add("act", lambda e: e.activation(out=dst, in_=src, func=AF.Copy), reads=[ttok], writes=["hn%d_b" % tt])

            hctx = [h_prep(0), h_prep(1)]
            for tt in range(16):
                if tt + 2 < 16:
                    hctx.append(h_prep(tt + 2))
                h_trans(hctx[tt], tt)

            def hn_tokens(tiles):
                out = []
                for t in tiles:
                    out += ["hn%d_a" % t, "hn%d_b" % t]
                return out

            stage('H%d' % s)
            pending_fins = []
            for lay_ in range(3):
                P.add("pool", lambda e, lay_=lay_: e.memset(Vt[:, lay_, :, :, 64:128], 1.0), writes=["V%d" % lay_])
            for item in range(8):
                stage('I%d_%d' % (s, item))
                if os.environ.get('KBAR'):
                    P.add("act", lambda e: e.activation(out=bar[:, 0:1], in_=mhalf[:, 0:1], func=AF.Copy), reads=["mhalf"], writes=["bar_act"])
                    P.add("dve", lambda e: e.tensor_copy(out=bar[:, 1:2], in_=mhalf[:, 0:1]), reads=["mhalf"], writes=["bar_dve"])
                    P.add("pool", lambda e: e.tensor_copy(out=bar[:, 2:3], in_=mhalf[:, 0:1]), reads=["mhalf"], writes=["bar_pool"])
                    for en in ("pe", "act", "dve", "pool", "sp"):
                        P.add(en, lambda e: None, reads=["bar_act", "bar_dve", "bar_pool"], waitonly=True)
                if os.environ.get('KSNAP') and item == 1:
                    P.add("pool", lambda e: e.tensor_copy(out=yT[:, 7, :], in_=yT[:, 0, :]), reads=["yT0_0", "yT0_1"], writes=["yT7_0", "yT7_1"])
                    P.add("pool", lambda e: e.tensor_copy(out=yT[:, 6, :], in_=acc[0][:, :]), reads=["acc0"], writes=["yT6_0", "yT6_1"])
                is_dil = item < 4
                j = item % 4
                st = item % 2
                colbase = 0 if is_dil else 1536
                wq, wk, wv = wblk[st]
                for wi, (wb, off) in enumerate(((wq, 0), (wk, 512), (wv, 1024))):
                    c0 = colbase + off + j * 128
                    P.add("pool", lambda e, wb=wb, c0=c0: e.dma_start(out=wb, in_=win_d.rearrange("(c p) n -> p c n", p=128)[:, :, c0:c0 + 128]),
                          writes=["wb%d_%d" % (st, wi)], dma="wb%d_%d" % (st, wi))
                if not is_dil:
                    P.add("pool", lambda e, j=j: e.dma_start(out=nastrip, in_=nas_d[j].rearrange("p (h v n) -> p h v n", h=2, v=2), max_dma_last_dim=3584),
                          writes=["nastrip"], dma="nas")
                    if MASKMUL:
                        nflat = nastrip.rearrange("p h v n -> p (h v n)")
                        for c_ in range(0, 3584, 512):
                            P.add("act", lambda e, c_=c_, nflat=nflat: e.activation(out=nflat[:, c_:c_ + 512], in_=nflat[:, c_:c_ + 512], func=AF.Exp), reads=["nastrip"], writes=["nastrip"])
                if s == 0:
                    for ex_ in (2 * item, 2 * item + 1):
                        for nm_, src_, dst_ in (("g", wg_d, wgs_d), ("u", wu_d, wus_d), ("d", wd_d, wds_d)):
                            P.add("pool", lambda e, src_=src_, dst_=dst_, ex_=ex_: e.dma_start(out=dst_[ex_], in_=src_[ex_], max_dma_last_dim=4096),
                                  writes=["cv%s%d" % (nm_, ex_)], dma="cv%s%d" % (nm_, ex_))
                for which, (wb, dstT, scl) in enumerate(((wq, QT[st], 0.125), (wk, KT[st], 1.0))):
                    for tc in range(4):
                        bi = ctr["P"] % 2; ctr["P"] += 1
                        bk = bP[bi]
                        for c in range(8):
                            P.add("pe", lambda e, bk=bk, wb=wb, c=c, tc=tc: e.matmul(bk[:, 0:512], lhsT=wb[:, c, :], rhs=hnT[:, c, tc * 512:(tc + 1) * 512],
                                                                                     start=(c == 0), stop=(c == 7)),
                                  reads=["wb%d_%d" % (st, which)] + hn_tokens(range(4 * tc, 4 * tc + 4)), writes=["bk%d" % bi])
                        dst = dstT[:, tc * 512:(tc + 1) * 512] if which == 1 else None
                        tokn = ("QT%d" if which == 0 else "KT%d") % st
                        if which == 0:
                            for hp_ in range(2):
                                rw = slice(hp_ * 64, hp_ * 64 + 64)
                                P.add("dve", lambda e, bk=bk, rw=rw, hp_=hp_, tc=tc, dstT=dstT: e.tensor_scalar(out=dstT[rw, hp_, tc * 512:(tc + 1) * 512], in0=bk[rw, 0:512], scalar1=0.125, scalar2=None, op0=ALU.mult),
                                      reads=["bk%d" % bi], writes=[tokn])
                        else:
                            P.add("dve", lambda e, dst=dst, bk=bk: e.tensor_copy(out=dst, in_=bk[:, 0:512]),
                                  reads=["bk%d" % bi], writes=[tokn])
                if pending_fins:
                    pending_fins[0](None)
                for tc in range(4):
                    bi = ctr["P"] % 2; ctr["P"] += 1
                    bk = bP[bi]
                    for c in range(8):
                        P.add("pe", lambda e, bk=bk, wv=wv, c=c, tc=tc: e.matmul(bk[:, 0:512], lhsT=wv[:, c, :], rhs=hnT[:, c, tc * 512:(tc + 1) * 512],
                                                                                 start=(c == 0), stop=(c == 7)),
                              reads=["wb%d_2" % st] + hn_tokens(range(4 * tc, 4 * tc + 4)), writes=["bk%d" % bi])
                    dst = VTb[:, tc * 512:(tc + 1) * 512]
                    P.add("dve", lambda e, dst=dst, bk=bk: e.tensor_copy(out=dst, in_=bk[:, 0:512]),
                          reads=["bk%d" % bi], writes=["VT"])
                if pending_fins:
                    pending_fins[1](None)
                pending_fins = []
                layouts = ((0, 1), (1, 4), (2, 16)) if is_dil else ((0, 1),)
                for (lay, d) in layouts:
                    L = S // d; nt = L // 128
                    tiles = [(r, m) for r in range(d) for m in range(nt)]
                    for g8 in range(2):
                        bi = ctr["P"] % 2; ctr["P"] += 1
                        bkb = bPbf[bi]
                        for q in range(8):
                            r, m = tiles[g8 * 8 + q]
                            tsl_ = tsl(r, d, 128 * m, 128)
                            P.add("pe", lambda e, bkb=bkb, q=q, tsl_=tsl_: e.transpose(bkb[:, q * 128:(q + 1) * 128], VTb[:, tsl_], ident),
                                  reads=["VT", "ident"], writes=["bk%d" % bi])
                        dst = Vt[:, lay, g8 * 8:(g8 + 1) * 8, :, 0:64]
                        src = bkb[:, 0:1024].rearrange("p (t h c) -> p t h c", t=8, h=2)
                        P.add("dve", lambda e, dst=dst, src=src: e.tensor_copy(out=dst, in_=src),
                              reads=["bk%d" % bi], writes=["V%d" % lay])

                new_fins = []
                tasks = []
                qz = QT[st]; kTp = KT[st]
                ATOK = ["acc0", "acc1"]
                v2 = lambda ap, lo, n: ap[:, lo:lo + 2 * n].rearrange("p (h q) -> p h q", h=2)
                if is_dil:
                    for pi, d in enumerate((1, 4)):
                        L = S // d; nt = L // 128
                        biasP = dilb[:, (j * 2 + pi) * 512:(j * 2 + pi + 1) * 512]
                        for r in range(d):
                            for b in range(nt + 1):
                                def s_part(b=b, r=r, d=d, nt=nt, biasP=biasP, qz=qz, kTp=kTp, st=st):
                                    si = ctr["S"] % NS; ctr["S"] += 1
                                    bkS = bS[si]; pt = PT[:, si * 512:(si + 1) * 512]
                                    tk = "bk%d" % SBK[si]
                                    if b == 0:
                                        ov = v2(bkS, 256, 128)[:, :, 64:128]; bv = v2(biasP, 256, 128)[:, :, 64:128]; pv_ = v2(pt, 256, 128)[:, :, 64:128]
                                        for hp_ in range(2):
                                            c0_ = 256 + hp_ * 128 + 64
                                            if not MASKMUL:
                                                P.add("pe", lambda e, c0_=c0_: e.matmul(bkS[:, c0_:c0_ + 64], lhsT=ident, rhs=biasP[:, c0_:c0_ + 64], start=(c0_ == 320), stop=False),
                                                      reads=["ident", "dilb"], writes=[tk])
                                            P.add("pe", lambda e, c0_=c0_, hp_=hp_: e.matmul(bkS[:, c0_:c0_ + 64], lhsT=kTp[:, tsl(r, d, 0, 128)], rhs=qz[:, hp_, tsl(r, d, 0, 64)], start=(MASKMUL and hp_ == 0), stop=True),
                                                  reads=["QT%d" % st, "KT%d" % st], writes=[tk])
                                        P.add("act", lambda e: e.activation(out=pv_, in_=ov, func=AF.Exp), reads=[tk], writes=["PT%d" % si])
                                        if MASKMUL:
                                            mask_mul(pv_, bv, si, "dilb")
                                    elif b == nt:
                                        ov = v2(bkS, 0, 128)[:, :, 0:64]; bv = v2(biasP, 0, 128)[:, :, 0:64]; pv_ = v2(pt, 0, 128)[:, :, 0:64]
                                        for hp_ in range(2):
                                            c0_ = hp_ * 128
                                            if not MASKMUL:
                                                P.add("pe", lambda e, c0_=c0_: e.matmul(bkS[:, c0_:c0_ + 64], lhsT=ident, rhs=biasP[:, c0_:c0_ + 64], start=(c0_ == 0), stop=False),
                                                      reads=["ident", "dilb"], writes=[tk])
                                            P.add("pe", lambda e, c0_=c0_, hp_=hp_: e.matmul(bkS[:, c0_:c0_ + 64], lhsT=kTp[:, tsl(r, d, 128 * (nt - 1), 128)], rhs=qz[:, hp_, tsl(r, d, 128 * nt - 64, 64)], start=(MASKMUL and hp_ == 0), stop=True),
                                                  reads=["QT%d" % st, "KT%d" % st], writes=[tk])
                                        P.add("act", lambda e: e.activation(out=pv_, in_=ov, func=AF.Exp), reads=[tk], writes=["PT%d" % si])
                                        if MASKMUL:
                                            mask_mul(pv_, bv, si, "dilb")
                                    else:
                                        qv = qz[:, :, tsl(r, d, 128 * b - 64, 128)]
                                        if not MASKMUL:
                                            P.add("pe", lambda e: e.matmul(bkS[:, 0:512], lhsT=ident, rhs=biasP, start=True, stop=False), reads=["ident", "dilb"], writes=[tk])
                                        P.add("pe", lambda e: e.matmul(bkS[:, 0:256], lhsT=kTp[:, tsl(r, d, 128 * (b - 1), 128)], rhs=qv, start=MASKMUL, stop=False),
                                              reads=["QT%d" % st, "KT%d" % st], writes=[tk])
                                        P.add("pe", lambda e: e.matmul(bkS[:, 256:512], lhsT=kTp[:, tsl(r, d, 128 * b, 128)], rhs=qv, start=False, stop=True),
                                              reads=["QT%d" % st, "KT%d" % st], writes=[tk])
                                        P.add("act", lambda e: e.activation(out=pt[:, 0:512], in_=bkS[:, 0:512], func=AF.Exp), reads=[tk], writes=["PT%d" % si])
                                        if MASKMUL:
                                            mask_mul(pt[:, 0:512], biasP, si, "dilb")
                                    return si

                                def pv_part(si, b=b, r=r, d=d, nt=nt, pi=pi):
                                    oi = ctr["O"] % NO; ctr["O"] += 1
                                    bkO = bO[oi]; pt = PT[:, si * 512:(si + 1) * 512]
                                    tk = "bk%d" % (5 + oi)
                                    first = [True]
                                    for hp in range(2):
                                        def mm(o_, l_, r_):
                                            stt = first[0]; first[0] = False
                                            P.add("pe", lambda e: e.matmul(o_, lhsT=l_, rhs=r_, start=stt, stop=True), reads=["PT%d" % si, "V%d" % pi], writes=[tk])
                                        ob = hp * 128
                                        if b == 0:
                                            mm(bkO[:, ob + 64:ob + 128], Vt[:, pi, r * nt + 0, hp, :], pt[:, 256 + hp * 128 + 64:256 + hp * 128 + 128])
                                        elif b == nt:
                                            mm(bkO[:, ob:ob + 64], Vt[:, pi, r * nt + nt - 1, hp, :], pt[:, hp * 128:hp * 128 + 64])
                                        else:
                                            mm(bkO[:, ob:ob + 128], Vt[:, pi, r * nt + b - 1, hp, :], pt[:, hp * 128:hp * 128 + 128])
                                            mm(bkO[:, ob:ob + 128], Vt[:, pi, r * nt + b, hp, :], pt[:, 256 + hp * 128:256 + hp * 128 + 128])
                                    c_lo = 64 if b == 0 else 0
                                    c_hi = 64 if b == nt else 128
                                    p_lo = 128 * b - 64 + c_lo
                                    dsta = acc2[:, :, tsl(r, d, p_lo, c_hi - c_lo)]
                                    srca = v2(bkO, 0, 128)[:, :, c_lo:c_hi]
                                    if pi == 0:
                                        P.add("dve", lambda e: e.tensor_copy(out=dsta, in_=srca), reads=[tk], writes=ATOK)
                                    else:
                                        P.add("dve", lambda e: e.tensor_tensor(out=dsta, in0=srca, in1=dsta, op=ALU.add), reads=[tk] + ATOK, writes=ATOK)
                                tasks.append((s_part, pv_part))
                    bias3P = dilb[:, 4096 + j * 256:4096 + (j + 1) * 256]
                    for r0 in range(0, 16, 2):
                        def s_part(r0=r0, bias3P=bias3P, qz=qz, kTp=kTp, st=st):
                            si = ctr["S"] % NS; ctr["S"] += 1
                            bkS = bS[si]; pt = PT[:, si * 512:(si + 1) * 512]
                            tk = "bk%d" % SBK[si]
                            for rr in range(2):
                                r_ = r0 + rr
                                if not MASKMUL:
                                    P.add("pe", lambda e, rr=rr: e.matmul(bkS[:, rr * 256:(rr + 1) * 256], lhsT=ident, rhs=bias3P, start=True, stop=False),
                                          reads=["ident", "dilb"], writes=[tk])
                                P.add("pe", lambda e, rr=rr, r_=r_: e.matmul(bkS[:, rr * 256:(rr + 1) * 256], lhsT=kTp[:, tsl(r_, 16, 0, 128)], rhs=qz[:, :, tsl(r_, 16, 0, 128)], start=(MASKMUL and rr == 0), stop=True),
                                      reads=["QT%d" % st, "KT%d" % st], writes=[tk])
                            P.add("act", lambda e: e.activation(out=pt[:, 0:512], in_=bkS[:, 0:512], func=AF.Exp), reads=[tk], writes=["PT%d" % si])
                            if MASKMUL:
                                mask_mul(pt[:, 0:512].rearrange("p (r c) -> p r c", r=2), bias3P.unsqueeze(1).to_broadcast([128, 2, 256]), si, "dilb")
                            return si

                        def pv_part(si, r0=r0):
                            oi = ctr["O"] % NO; ctr["O"] += 1
                            bkO = bO[oi]; pt = PT[:, si * 512:(si + 1) * 512]
                            tk = "bk%d" % (5 + oi)
                            for rr in range(2):
                                for hp in range(2):
                                    cs_ = slice(rr * 256 + hp * 128, rr * 256 + hp * 128 + 128)
                                    P.add("pe", lambda e, rr=rr, hp=hp, cs_=cs_: e.matmul(bkO[:, cs_], lhsT=Vt[:, 2, r0 + rr, hp, :], rhs=pt[:, cs_], start=(rr == 0 and hp == 0), stop=True),
                                          reads=["PT%d" % si, "V2"], writes=[tk])
                            dsta = acc2.rearrange("p h (q r) -> p h q r", r=16)[:, :, :, r0:r0 + 2]
                            srca = bkO[:, 0:512].rearrange("p (r h q) -> p h q r", r=2, h=2)
                            P.add("dve", lambda e: e.tensor_tensor(out=dsta, in0=srca, in1=dsta, op=ALU.add), reads=[tk] + ATOK, writes=ATOK)
                        tasks.append((s_part, pv_part))
                else:
                    for g in range(8):
                        if g == 0:
                            ms_, var = [0, 1, 2, 3], 1
                        elif g == 7:
                            ms_, var = [12, 13, 14, 15], 1
                        else:
                            ms_, var = list(range(2 * g - 2, 2 * g + 4)), 0
                        ostate = {}
                        for ki, m in enumerate(ms_):
                            def s_part(m=m, g=g, var=var, qz=qz, kTp=kTp, st=st):
                                si = ctr["S"] % NS; ctr["S"] += 1
                                bkS = bS[si]; pt = PT[:, si * 512:(si + 1) * 512]
                                tk = "bk%d" % SBK[si]
                                sft = 6 - (2 * m - 4 * g)
                                assert 0 <= sft and sft * 64 + 256 <= 896
                                if not MASKMUL:
                                    P.add("pe", lambda e: e.matmul(bkS[:, 0:512], lhsT=ident, rhs=nastrip[:, :, var, sft * 64:sft * 64 + 256], start=True, stop=False),
                                          reads=["ident", "nastrip"], writes=[tk])
                                P.add("pe", lambda e: e.matmul(bkS[:, 0:512], lhsT=kTp[:, m * 128:(m + 1) * 128], rhs=qz[:, :, g * 256:(g + 1) * 256], start=MASKMUL, stop=True),
                                      reads=["QT%d" % st, "KT%d" % st], writes=[tk])
                                P.add("act", lambda e: e.activation(out=pt[:, 0:512], in_=bkS[:, 0:512], func=AF.Exp), reads=[tk], writes=["PT%d" % si])
                                if MASKMUL:
                                    mask_mul(v2(pt, 0, 256), nastrip[:, :, var, sft * 64:sft * 64 + 256], si, "nastrip")
                                return si

                            def pv_part(si, m=m, ki=ki, g=g, ostate=ostate, nms=len(ms_)):
                                if ki == 0:
                                    ostate["oi"] = ctr["O"] % NO; ctr["O"] += 1
                                oi = ostate["oi"]
                                bkO = bO[oi]; pt = PT[:, si * 512:(si + 1) * 512]
                                tk = "bk%d" % (5 + oi)
                                for hp in range(2):
                                    P.add("pe", lambda e, hp=hp: e.matmul(bkO[:, hp * 256:(hp + 1) * 256], lhsT=Vt[:, 0, m, hp, :], rhs=pt[:, hp * 256:(hp + 1) * 256],
                                                                         start=(ki == 0 and hp == 0), stop=(ki == nms - 1)),
                                          reads=["PT%d" % si, "V0"], writes=[tk])
                                if ki == nms - 1:
                                    dsta = acc2[:, :, g * 256:(g + 1) * 256]
                                    P.add("dve", lambda e: e.tensor_copy(out=dsta, in_=v2(bkO, 0, 256)), reads=[tk], writes=ATOK)
                            tasks.append((s_part, pv_part))

                for hp in range(2):
                    def fin_part(_si, hp=hp, chunk=(j if is_dil else 4 + j)):
                        rows = slice(hp * 64, hp * 64 + 64)
                        P.add("act", lambda e: e.activation(out=rden[0:64, :], in_=acc2[64:128, hp, :], func=AF.Ln), reads=ATOK, writes=["rden"])
                        P.add("act", lambda e: e.activation(out=rden[0:64, :], in_=rden[0:64, :], func=AF.Exp, scale=-1.0), reads=["rden"], writes=["rden"])
                        P.add("dve", lambda e: e.tensor_tensor(out=yT[rows, chunk, :], in0=acc2[0:64, hp, :], in1=rden[0:64, :], op=ALU.mult),
                              reads=ATOK + ["rden"], writes=["yT%d_%d" % (chunk, hp)])
                    new_fins.append(fin_part)

                LOOK = int(os.environ.get('KLOOK', '2'))
                sis = []
                for ti_, (sp_, pv_) in enumerate(tasks):
                    sis.append(sp_() if sp_ is not None else None)
                    if ti_ >= LOOK:
                        tasks[ti_ - LOOK][1](sis[ti_ - LOOK])
                for ti_ in range(max(0, len(tasks) - LOOK), len(tasks)):
                    tasks[ti_][1](sis[ti_])
                pending_fins = new_fins


            for fp_ in pending_fins:
                fp_(None)
            pending_fins = []
            stage('P%d' % s)
            ytoks = ["yT%d_%d" % (c, hp) for c in range(8) for hp in range(2)]
            P.add("pool", lambda e: e.dma_start(out=wout_bf, in_=wout_d.rearrange("(c p) n -> p c n", p=128)),
                  writes=["V0", "V1", "V2", "wout"], dma="wout")
            for c in range(8):
                P.add("dve", lambda e, c=c: e.tensor_scalar(out=wout_bf[:, c, :], in0=wout_bf[:, c, :], scalar1=gout[:, c:c + 1], scalar2=None, op0=ALU.mult),
                      reads=["wout", "vecs"], writes=["wout"])
            def o_prep(tt):
                row0 = tb + tt * 128
                tsl_ = slice(tt * 128, (tt + 1) * 128)
                xi = ctr["x"] % 3; ctr["x"] += 1
                qi = tt % 2
                xtile = xt[xi]; sq_ = sqt2[qi]
                P.add("sp", lambda e: e.dma_start(out=xtile, in_=x_d[row0:row0 + 128, :]),
                      writes=["xt%d_src" % xi], dma="x%d" % xi)
                P.add("pool", lambda e: e.tensor_tensor(out=sq_, in0=yT[:, :, tsl_], in1=yT[:, :, tsl_], op=ALU.mult),
                      reads=ytoks, writes=["sqt%d" % qi])
                bq = bS[2]
                for c in range(8):
                    col = qi * 2 + c // 4
                    P.add("pe", lambda e, c=c, col=col: e.matmul(bq[:, col:col + 1], lhsT=sq_[:, c, :], rhs=ones1[:, 0:1], start=(c % 4 == 0), stop=(c % 4 == 3)),
                          reads=["sqt%d" % qi, "ones1"], writes=["bk4"])
                ms2 = small[:, 16 + xi * 4:16 + xi * 4 + 2]; rs2 = small[:, 16 + xi * 4 + 2:16 + xi * 4 + 4]
                P.add("dve", lambda e: e.tensor_scalar(out=ms2, in0=bq[:, qi * 2:qi * 2 + 2], scalar1=1.0 / 512, scalar2=EPS, op0=ALU.mult, op1=ALU.add),
                      reads=["bk4"], writes=["ms2_%d" % xi])
                P.add("pool", lambda e: e.tensor_tensor(out=rs2, in0=ms2, in1=mhalf[:, 0:2], op=ALU.pow),
                      reads=["ms2_%d" % xi, "mhalf"], writes=["rs2_%d" % xi])
                return (xtile, xi, rs2, row0, tsl_)

            def o_main(ctx, tt):
                xtile, xi, rs2, row0, tsl_ = ctx
                hi_ = tt % 2
                ht = acc[hi_][:, 0:D]
                for half in range(2):
                    hs_ = slice(half * 512, (half + 1) * 512)
                    bA = [bP[0], bP[1]][half]; bB = [bS[0], bS[1]][half]
                    for c in range(4):
                        P.add("pe", lambda e, c=c, bA=bA, hs_=hs_: e.matmul(bA[:, 0:512], lhsT=yT[:, c, tsl_], rhs=wout_bf[:, c, hs_], start=(c == 0), stop=(c == 3)),
                              reads=ytoks + ["wout", "V0", "V1", "V2"], writes=["bk%d" % half])
                    for c in range(4, 8):
                        P.add("pe", lambda e, c=c, bB=bB, hs_=hs_: e.matmul(bB[:, 0:512], lhsT=yT[:, c, tsl_], rhs=wout_bf[:, c, hs_], start=(c == 4), stop=(c == 7)),
                              reads=ytoks + ["wout", "V0", "V1", "V2"], writes=["bk%d" % (2 + half)])
                    P.add("dve", lambda e, hs_=hs_, bA=bA: e.scalar_tensor_tensor(out=ht[:, hs_], in0=bA[:, 0:512], scalar=rs2[:, 0:1], in1=xtile[:, hs_], op0=ALU.mult, op1=ALU.add),
                          reads=["bk%d" % half, "rs2_%d" % xi, "xt%d_src" % xi], writes=["acc%d" % hi_])
                    P.add("dve", lambda e, hs_=hs_, bB=bB: e.scalar_tensor_tensor(out=ht[:, hs_], in0=bB[:, 0:512], scalar=rs2[:, 1:2], in1=ht[:, hs_], op0=ALU.mult, op1=ALU.add),
                          reads=["bk%d" % (2 + half), "rs2_%d" % xi, "acc%d" % hi_], writes=["acc%d" % hi_])
                P.add("sp", lambda e: e.dma_start(out=hs_d[row0:row0 + 128, :], in_=ht),
                      reads=["acc%d" % hi_], writes=["hs%d" % (row0 // 128)], dma="hst%d" % hi_)

            octx = [o_prep(0)]
            for tt in range(16):
                if tt + 1 < 16:
                    octx.append(o_prep(tt + 1))
                o_main(octx[tt], tt)

        stage('O')
        P.add("act", lambda e: e.activation(out=bar[:, 0:1], in_=mhalf[:, 0:1], func=AF.Copy), reads=["mhalf"], writes=["bar_act"])
        P.add("dve", lambda e: e.tensor_copy(out=bar[:, 1:2], in_=mhalf[:, 0:1]), reads=["mhalf"], writes=["bar_dve"])
        P.add("pool", lambda e: e.tensor_copy(out=bar[:, 2:3], in_=mhalf[:, 0:1]), reads=["mhalf"], writes=["bar_pool"])
        for en in ("pe", "act", "dve", "pool", "sp"):
            P.add(en, lambda e: None, reads=["bar_act", "bar_dve", "bar_pool"] + ["hs%d" % i_ for i_ in range(32)], waitonly=True)

        ptr[0] = persist_end
        wd_bf = alloc(32 * D, BF16, (32, D))
        hn2T = alloc(8 * 1024, BF16, (8, 1024))
        actT = alloc(32 * 1024, BF16, (32, 1024))
        wgu = [[alloc(8 * 256, BF16, (8, 256)) for _ in range(2)] for _ in range(3)]
        hbA = alloc(D); hbD = [alloc(D) for _ in range(2)]
        xsA = alloc(D, BF16); xsD = alloc(D, BF16)
        combT = alloc(1024, BF16)
        Cs = [alloc(512, BF16) for _ in range(2)]
        ssb = [alloc(512, BF16) for _ in range(2)]
        s2b = [alloc(512, BF16) for _ in range(2)]
        rt = alloc(960)
        comb_bf = alloc(8 * 16, BF16, (8, 16))
        small2 = alloc(32)

        bG = [banks[0], banks[1]]; bU = [banks[2], banks[3]]; bC = banks[4]; bD = [banks[5], banks[6]]

        def load_wd():
            for k in range(4):
                P.add("sp", lambda e, k=k: e.dma_start(out=wd_bf[:, 8 * k:8 * k + 8, :],
                                                       in_=wds_d[4 * k:4 * k + 4].rearrange("e (f p) n -> p (e f) n", p=128)),
                      reads=["cvd%d" % ee for ee in range(4 * k, 4 * k + 4)], writes=["wd%d" % k], dma="wd%d" % k)
        wd_toks = ["wd%d" % k for k in range(4)]

        def load_gu(n):
            if n >= 64:
                return
            ex_ = n % 16; wi_ = n % 3
            wgb_, wub_ = wgu[wi_]
            P.add("sp", lambda e: e.dma_start(out=wgb_, in_=wgs_d[ex_].rearrange("(c p) n -> p c n", p=128)),
                  reads=["cvg%d" % ex_], writes=["wg%d" % wi_], dma="wg%d" % wi_)
            P.add("sp", lambda e: e.dma_start(out=wub_, in_=wus_d[ex_].rearrange("(c p) n -> p c n", p=128)),
                  reads=["cvu%d" % ex_], writes=["wu%d" % wi_], dma="wu%d" % wi_)
        load_gu(0)
        load_gu(1)

        c2 = {"h": 0, "G": 0, "C": 0, "D": 0, "w": 0, "y": 0}
        h2_all = [("h2_%d_a" % st) for st in range(8)] + [("h2_%d_b" % st) for st in range(8)]

        hpool = [(hbA, "hbA"), (hbD[0], "hbD0"), (hbD[1], "hbD1")]
        xpool = [(xsA, "xsA"), (xsD, "xsD")]
        tbank2 = [(pb_t, "bk7"), (banks[2].bitcast(BF16), "bk2")]
        bD4 = [(banks[5], "bk5"), (banks[6], "bk6"), (banks[0], "bk0"), (banks[1], "bk1")]

        def prep_hn2(T, st):
            row0 = T * 1024 + st * 128
            if T == 0:
                htile, htok = hpool[st % 3]; xsb, xtok = xpool[st % 2]
            else:
                htile, htok = hpool[0]; xsb, xtok = xpool[0]
            si_ = c2["h"] % 4; c2["h"] += 1
            P.add("sp", lambda e: e.dma_start(out=htile, in_=hs_d[row0:row0 + 128, :]),
                  reads=["hs%d" % (row0 // 128)], writes=[htok], dma=htok)
            ss = small2[:, si_ * 4:si_ * 4 + 1]; ms = small2[:, si_ * 4 + 1:si_ * 4 + 2]; rstd = small2[:, si_ * 4 + 2:si_ * 4 + 3]
            P.add("act", lambda e: e.activation(out=xsb, in_=htile, func=AF.Square, accum_out=ss),
                  reads=[htok], writes=[xtok, "h_ss%d" % si_])
            P.add("dve", lambda e: e.tensor_scalar(out=ms, in0=ss, scalar1=1.0 / D, scalar2=EPS, op0=ALU.mult, op1=ALU.add),
                  reads=["h_ss%d" % si_], writes=["h_ms%d" % si_])
            P.add("pool", lambda e: e.tensor_tensor(out=rstd, in0=ms, in1=mhalf[:, 0:1], op=ALU.pow),
                  reads=["h_ms%d" % si_, "mhalf"], writes=["h_rstd%d" % si_])
            P.add("dve", lambda e: e.tensor_scalar(out=xsb, in0=htile, scalar1=rstd, scalar2=None, op0=ALU.mult),
                  reads=[htok, "h_rstd%d" % si_], writes=[xtok])
            return (xsb, xtok)

        def trans_hn2(ctx, T, st):
            xsb, xtok = ctx
            tbk, ttok = tbank2[st % 2]
            for c in range(8):
                P.add("pe", lambda e, c=c: e.transpose(tbk[:, c * 128:(c + 1) * 128], xsb[:, c * 128:(c + 1) * 128], ident),
                      reads=[xtok, "ident"], writes=[ttok])
            for c in range(8):
                dst = hn2T[:, c, st * 128:(st + 1) * 128]
                src = tbk[:, c * 128:(c + 1) * 128]
                if st % 2 == 0:
                    P.add("dve", lambda e, dst=dst, src=src, c=c: e.tensor_scalar(out=dst, in0=src, scalar1=gffn[:, c:c + 1], scalar2=None, op0=ALU.mult),
                          reads=[ttok, "vecs"], writes=["h2_%d_a" % st])
                else:
                    P.add("act", lambda e, dst=dst, src=src, c=c: e.activation(out=dst, in_=src, func=AF.Copy, scale=gffn[:, c:c + 1]),
                          reads=[ttok, "vecs"], writes=["h2_%d_b" % st])

        def emit_router(T):
            if True:
                for st in range(8):
                    for c in range(8):
                        P.add("pe", lambda e, st=st, c=c: e.matmul(bC[:, st * 32:st * 32 + 20], lhsT=hn2T[:, c, st * 128:(st + 1) * 128], rhs=wr_bf[:, c, :],
                                                                   start=(c == 0), stop=(c == 7)),
                              reads=["h2_%d_a" % st, "h2_%d_b" % st, "wr_bf"], writes=["bk4"])
                lg = rt[:, 0:160].rearrange("p (s n) -> p s n", s=8)
                bc3 = bC[:, 0:256].rearrange("p (s n) -> p s n", s=8)[:, :, 0:20]
                RT = "rt"
                r3 = lambda lo, n: rt[:, lo:lo + 8 * n].rearrange("p (s n) -> p s n", s=8)
                gl = lg[:, :, 0:4]; el = lg[:, :, 4:20]
                gmax = rt[:, 160:168]; g1 = r3(168, 4); gex = r3(200, 4); gsum = rt[:, 232:240]; gw = rt[:, 240:248]
                pen = r3(248, 16); ml = r3(376, 16); m1 = rt[:, 504:512]; m2 = rt[:, 512:520]; k1 = r3(520, 16)
                dm = rt[:, 648:656]; w1 = rt[:, 656:664]; w2 = rt[:, 664:672]; k2 = r3(672, 16); ml2 = r3(800, 16)
                bcast = lambda ap, n: ap.unsqueeze(2).to_broadcast([128, 8, n])
                P.add("dve", lambda e: e.tensor_tensor(out=lg, in0=bc3, in1=brep.unsqueeze(1).to_broadcast([128, 8, 20]), op=ALU.add),
                      reads=["bk4", "vecs"], writes=[RT])
                P.add("dve", lambda e: e.tensor_reduce(out=gmax, in_=gl, axis=AX.X, op=ALU.max), reads=[RT], writes=[RT + "a"])
                P.add("dve", lambda e: e.tensor_tensor(out=g1, in0=gl, in1=bcast(gmax, 4), op=ALU.is_equal), reads=[RT, RT + "a"], writes=[RT + "b"])
                P.add("dve", lambda e: e.tensor_tensor(out=gex, in0=gl, in1=bcast(gmax, 4), op=ALU.subtract), reads=[RT, RT + "a"], writes=[RT + "c"])
                P.add("act", lambda e: e.activation(out=gex, in_=gex, func=AF.Exp), reads=[RT + "c"], writes=[RT + "d"])
                P.add("dve", lambda e: e.tensor_reduce(out=gsum, in_=gex, axis=AX.X, op=ALU.add), reads=[RT + "d"], writes=[RT + "e"])
                P.add("dve", lambda e: e.reciprocal(out=gw, in_=gsum), reads=[RT + "e"], writes=[RT + "f"])
                P.add("dve", lambda e: e.tensor_scalar(out=pen.rearrange("p s (g n) -> p s g n", g=4), in0=g1.unsqueeze(3).to_broadcast([128, 8, 4, 4]),
                                                       scalar1=-1.0, scalar2=30000.0, op0=ALU.add, op1=ALU.mult), reads=[RT + "b"], writes=[RT + "g"])
                P.add("dve", lambda e: e.tensor_tensor(out=ml, in0=el, in1=pen, op=ALU.add), reads=[RT, RT + "g"], writes=[RT + "h"])
                P.add("dve", lambda e: e.tensor_reduce(out=m1, in_=ml, axis=AX.X, op=ALU.max), reads=[RT + "h"], writes=[RT + "i"])
                P.add("dve", lambda e: e.tensor_tensor(out=k1, in0=ml, in1=bcast(m1, 16), op=ALU.is_equal), reads=[RT + "h", RT + "i"], writes=[RT + "j"])
                P.add("dve", lambda e: e.scalar_tensor_tensor(out=ml2, in0=k1, scalar=-60000.0, in1=ml, op0=ALU.mult, op1=ALU.add), reads=[RT + "h", RT + "j"], writes=[RT + "k"])
                P.add("dve", lambda e: e.tensor_reduce(out=m2, in_=ml2, axis=AX.X, op=ALU.max), reads=[RT + "k"], writes=[RT + "l"])
                P.add("dve", lambda e: e.tensor_tensor(out=k2, in0=ml2, in1=bcast(m2, 16), op=ALU.is_equal), reads=[RT + "k", RT + "l"], writes=[RT + "m"])
                P.add("dve", lambda e: e.tensor_tensor(out=dm, in0=m2, in1=m1, op=ALU.subtract), reads=[RT + "l", RT + "i"], writes=[RT + "n"])
                P.add("act", lambda e: e.activation(out=dm, in_=dm, func=AF.Exp), reads=[RT + "n"], writes=[RT + "o"])
                P.add("dve", lambda e: e.tensor_scalar(out=dm, in0=dm, scalar1=1.0, scalar2=None, op0=ALU.add), reads=[RT + "o"], writes=[RT + "p"])
                P.add("dve", lambda e: e.reciprocal(out=w1, in_=dm), reads=[RT + "p"], writes=[RT + "q"])
                P.add("dve", lambda e: e.tensor_scalar(out=w2, in0=w1, scalar1=-1.0, scalar2=1.0, op0=ALU.mult, op1=ALU.add), reads=[RT + "q"], writes=[RT + "r"])
                P.add("dve", lambda e: e.tensor_tensor(out=w1, in0=w1, in1=gw, op=ALU.mult), reads=[RT + "q", RT + "r", RT + "f"], writes=[RT + "s"])
                P.add("dve", lambda e: e.tensor_tensor(out=w2, in0=w2, in1=gw, op=ALU.mult), reads=[RT + "r", RT + "f"], writes=[RT + "t"])
                P.add("dve", lambda e: e.tensor_tensor(out=k1, in0=k1, in1=bcast(w1, 16), op=ALU.mult), reads=[RT + "j", RT + "s", RT + "k"], writes=[RT + "u"])
                P.add("dve", lambda e: e.tensor_tensor(out=k2, in0=k2, in1=bcast(w2, 16), op=ALU.mult), reads=[RT + "m", RT + "t"], writes=[RT + "v"])
                P.add("dve", lambda e: e.tensor_tensor(out=comb_bf, in0=k1, in1=k2, op=ALU.add), reads=[RT + "u", RT + "v"], writes=["comb_bf"])
                for st in range(8):
                    P.add("pe", lambda e, st=st: e.transpose(pb_t[0:16, st * 128:(st + 1) * 128], comb_bf[:, st, :], ident),
                          reads=["comb_bf", "ident"] + h2_all, writes=["bk7"])
                P.add("dve", lambda e: e.tensor_copy(out=combT[0:16, :], in_=pb_t[0:16, 0:1024]), reads=["bk7"], writes=["combT"])


        def emit_experts(T):
            if True:
                for ex in range(16):
                    nflat = T * 16 + ex
                    wi = nflat % 3
                    wgb, wub = wgu[wi]
                    load_gu(nflat + 2)
                    for half in range(2):
                        hs_ = slice(half * 512, (half + 1) * 512)
                        ci = c2["C"] % 2; c2["C"] += 1
                        P.add("pe", lambda e, ex=ex, hs_=hs_: e.matmul(bC[:, 0:512], lhsT=sel[0:16, ex, :], rhs=combT[0:16, hs_], start=True, stop=True),
                              reads=["sel", "combT"], writes=["bk4"])
                        P.add("dve", lambda e, ci=ci: e.tensor_copy(out=Cs[ci], in_=bC[:, 0:512]), reads=["bk4"], writes=["Cs%d" % ci])
                        for fc in range(2):
                            gi = c2["G"] % 2; c2["G"] += 1
                            for c in range(8):
                                P.add("pe", lambda e, gi=gi, c=c, fc=fc, hs_=hs_, wgb=wgb: e.matmul(bG[gi][:, 0:512], lhsT=wgb[:, c, fc * 128:(fc + 1) * 128], rhs=hn2T[:, c, hs_],
                                                                                                  start=(c == 0), stop=(c == 7)),
                                      reads=["wg%d" % wi] + h2_all, writes=["bk%d" % gi])
                            for c in range(8):
                                P.add("pe", lambda e, gi=gi, c=c, fc=fc, hs_=hs_, wub=wub: e.matmul(bU[gi][:, 0:512], lhsT=wub[:, c, fc * 128:(fc + 1) * 128], rhs=hn2T[:, c, hs_],
                                                                                                  start=(c == 0), stop=(c == 7)),
                                      reads=["wu%d" % wi] + h2_all, writes=["bk%d" % (2 + gi)])
                            P.add("act", lambda e, gi=gi: e.activation(out=ssb[gi], in_=bG[gi][:, 0:512], func=AF.Silu), reads=["bk%d" % gi], writes=["ssb%d" % gi])
                            P.add("pool", lambda e, gi=gi, ci=ci: e.tensor_tensor(out=s2b[gi], in0=ssb[gi], in1=Cs[ci], op=ALU.mult),
                                  reads=["ssb%d" % gi, "Cs%d" % ci], writes=["s2b%d" % gi])
                            P.add("dve", lambda e, gi=gi, ex=ex, fc=fc, hs_=hs_: e.tensor_tensor(out=actT[:, ex * 2 + fc, hs_], in0=bU[gi][:, 0:512], in1=s2b[gi], op=ALU.mult),
                                  reads=["bk%d" % (2 + gi), "s2b%d" % gi], writes=["actT"])


        def down_mm(T, st):
            row0 = T * 1024 + st * 128
            hi = c2["y"] % 2; c2["y"] += 1
            htile, htok = hpool[1 + hi]
            P.add("sp", lambda e: e.dma_start(out=htile, in_=hs_d[row0:row0 + 128, :]),
                  reads=["hs%d" % (row0 // 128)], writes=[htok], dma=htok)
            for dh in range(2):
                bk_, btok = bD4[c2["D"] % 4]; c2["D"] += 1
                ds_ = slice(dh * 512, (dh + 1) * 512)
                for kk in range(32):
                    P.add("pe", lambda e, bk_=bk_, kk=kk, ds_=ds_: e.matmul(bk_[:, 0:512], lhsT=actT[:, kk, st * 128:(st + 1) * 128], rhs=wd_bf[:, kk, ds_],
                                                                          start=(kk == 0), stop=(kk == 31)),
                          reads=["actT"] + wd_toks, writes=[btok])
                P.add("dve", lambda e, bk_=bk_, ds_=ds_: e.tensor_tensor(out=htile[:, ds_], in0=bk_[:, 0:512], in1=htile[:, ds_], op=ALU.add),
                      reads=[btok, htok], writes=[htok])
            return (htile, htok, row0, hi)

        def down_fin(ctx):
            htile, htok, row0, yi = ctx
            junk, jtok = xpool[1]
            ss = small2[:, 16 + yi * 4:16 + yi * 4 + 1]; ms = small2[:, 16 + yi * 4 + 1:16 + yi * 4 + 2]; rstd = small2[:, 16 + yi * 4 + 2:16 + yi * 4 + 3]
            P.add("act", lambda e: e.activation(out=junk, in_=htile, func=AF.Square, accum_out=ss),
                  reads=[htok], writes=[jtok, "f_ss%d" % yi])
            P.add("dve", lambda e: e.tensor_scalar(out=ms, in0=ss, scalar1=1.0 / D, scalar2=EPS, op0=ALU.mult, op1=ALU.add),
                  reads=["f_ss%d" % yi], writes=["f_ms%d" % yi])
            P.add("pool", lambda e: e.tensor_tensor(out=rstd, in0=ms, in1=mhalf[:, 0:1], op=ALU.pow),
                  reads=["f_ms%d" % yi, "mhalf"], writes=["f_rstd%d" % yi])
            P.add("dve", lambda e: e.scalar_tensor_tensor(out=htile, in0=htile, scalar=rstd, in1=gfin, op0=ALU.mult, op1=ALU.mult),
                  reads=[htok, "f_rstd%d" % yi, "gfin"], writes=[htok])
            P.add("sp", lambda e: e.dma_start(out=y_d[row0:row0 + 128, :], in_=htile),
                  reads=[htok], dma="yst%d" % yi)

        for st in range(8):
            trans_hn2(prep_hn2(0, st), 0, st)
        emit_router(0)
        load_wd()
        for T in range(4):
            emit_experts(T)
            for st in range(8):
                cx = prep_hn2(T + 1, st) if T < 3 else None
                dx = down_mm(T, st)
                if cx is not None:
                    trans_hn2(cx, T + 1, st)
                down_fin(dx)
            if T < 3:
                emit_router(T + 1)

    except StopBuild:
        pass

    sems = {}
    for en in Prog.ENGS:
        sems["eng:" + en] = es.enter_context(nc.semaphore("s_" + en))
    for sl in sorted(P.dma_slots):
        sems["dma:" + sl] = es.enter_context(nc.semaphore("d_" + sl))
    if os.environ.get('KMAXOPS'):
        P.ops = P.ops[:int(os.environ['KMAXOPS'])]
        for i_, o_ in enumerate(P.ops[-3:]):
            print('LASTOPS', o_.eng, o_.idx)
    body = P.emit(sems)
    if os.environ.get('KSTATS'):
        print('PROG stats', P.stats, 'ndma_slots', len(P.dma_slots))
    with nc.Block() as block:
        block.sync(body("sp"))
        block.tensor(body("pe"))
        block.scalar(body("act"))
        block.vector(body("dve"))
        block.gpsimd(body("pool"))
    es.close()
    return nc


def _dil_bias():
    out = np.full((128, 4 * 2 * 512 + 4 * 256), NEG, np.float32)
    k = np.arange(128)[:, None]; q = np.arange(128)[None, :]

    def f(delta, slope, d):
        return np.where(np.abs(delta) <= 64, -slope * d * np.abs(delta), NEG).astype(np.float32)
    for j in range(4):
        for hp in range(2):
            slope = 2.0 ** (-(2 * j + hp + 1))
            for pi, d in enumerate((1, 4)):
                c0 = (j * 2 + pi) * 512
                out[:, c0 + hp * 128:c0 + (hp + 1) * 128] = f(k - 64 - q, slope, d)
                out[:, c0 + 256 + hp * 128:c0 + 256 + (hp + 1) * 128] = f(k + 64 - q, slope, d)
            c0 = 4096 + j * 256
            out[:, c0 + hp * 128:c0 + (hp + 1) * 128] = f(k - q, slope, 16)
    return out


def _na_strips(rpb):
    kc = np.arange(64)[:, None]; qc = np.arange(64)[None, :]
    cs = np.clip(qc - 8, 0, 48)
    col_ok = (kc >= cs) & (kc < cs + 16)
    dcidx = np.clip(kc - qc + 15, 0, 30)
    out = np.full((4, 128, 2, 2, 896), NEG, np.float32)
    for h in range(8):
        j, hp = divmod(h, 2)
        for var in range(2):
            for i in range(2):
                for u in range(14):
                    dlt = 6 + i - u
                    ok = (-4 <= dlt <= 3) if var == 0 else (-7 <= dlt <= 7)
                    if not ok:
                        continue
                    blk = np.where(col_ok, rpb[h, dlt + 7][dcidx], NEG).astype(np.float32)
                    out[j, i * 64:(i + 1) * 64, hp, var, u * 64:(u + 1) * 64] = blk
    return out.reshape(4, 128, 4 * 896)


_NC_CACHE = {}


def kernel(x, norm_mix_g, w_in, rpb, g_out_dil, g_out_na, w_out, norm_ffn_g, w_group, b_group, w_router, b_router,
           w_gate, w_up, w_down, norm_final_g):
    f = lambda a: np.ascontiguousarray(np.asarray(a, dtype=np.float32))
    x = f(x).reshape(16 * S, D)
    col = lambda g: f(g).reshape(8, 128).T
    vecs = np.concatenate([col(norm_mix_g[0]), col(norm_ffn_g[0]),
                           col(np.concatenate([f(g_out_dil[0]), f(g_out_na[0])])),
                           np.broadcast_to(np.concatenate([f(b_group[0]), f(b_router[0])])[None, :], (128, 20))], axis=1)
    vecs = np.ascontiguousarray(vecs, dtype=np.float32)
    gfin = np.ascontiguousarray(np.broadcast_to(f(norm_final_g)[None, :], (128, D)))
    gmixr = np.ascontiguousarray(np.broadcast_to(f(norm_mix_g[0])[None, :], (128, D)))
    wr = np.ascontiguousarray(np.concatenate([f(w_group[0]), f(w_router[0])], axis=1))
    shared = {
        "w_in": f(w_in[0]), "w_out": f(w_out[0]), "w_gate": f(w_gate[0]), "w_up": f(w_up[0]), "w_down": f(w_down[0]),
        "wr": wr, "vecs": vecs, "gfin": gfin, "gmixr": gmixr, "dilb": _dil_bias(), "nas": _na_strips(f(rpb[0])),
    }
    if "nc" not in _NC_CACHE:
        _NC_CACHE["nc"] = build_program()
    nc = _NC_CACHE["nc"]
    in_maps = []
    for i in range(NCORES):
        m = dict(shared)
        m["x"] = np.ascontiguousarray(x[i * TOK:(i + 1) * TOK])
        in_maps.append(m)
    res = run_bass_kernel_spmd(nc, in_maps, core_ids=list(range(NCORES)))
    y = np.concatenate([r["y"] for r in res.results], axis=0)
    if DEBUG:
        kernel.dbg = [r.get("dbg") for r in res.results]
    return y.reshape(16, S, D).astype(np.float32)
```

```python
import numpy as np
from contextlib import ExitStack
import concourse.bass as bass
import concourse.mybir as mybir
from concourse.bass_utils import run_bass_kernel_spmd

F32 = mybir.dt.float32
BF16 = mybir.dt.bfloat16
AF = mybir.ActivationFunctionType
ALU = mybir.AluOpType
AX = mybir.AxisListType

NCORES = 8
S = 2048
D = 1024
TOK = 4096
NEG = -30000.0
EPS = 1e-6
import os
DEBUG = bool(os.environ.get('KSTOP', ''))
STOP = os.environ.get('KSTOP', '')


class StopBuild(Exception):
    pass


DUMPS = {}


def stage(name):
    if STOP and name == STOP:
        if name in DUMPS:
            DUMPS[name]()
        raise StopBuild()


class Op:
    __slots__ = ("eng", "fn", "deps", "is_dma", "sem", "semval", "needs_signal", "sigcount", "eidx", "idx", "wo")


class Prog:
    ENGS = ("pe", "act", "dve", "pool", "sp")

    def __init__(self):
        self.ops = []
        self.last_writer = {}
        self.readers = {}
        self.eng_count = {e: 0 for e in self.ENGS}
        self.dma_slots = set()

    def add(self, eng, fn, reads=(), writes=(), dma=None, waitonly=False):
        op = Op()
        op.eng = eng; op.fn = fn; op.idx = len(self.ops)
        op.is_dma = dma is not None
        op.wo = waitonly
        op.sem = dma; op.semval = 0; op.needs_signal = False; op.sigcount = 0
        op.eidx = self.eng_count[eng]; self.eng_count[eng] += 1
        if dma is not None:
            self.dma_slots.add(dma)
        deps = {}
        for t in reads:
            w = self.last_writer.get(t)
            if w is not None:
                deps[w] = "raw"
        for t in writes:
            w = self.last_writer.get(t)
            if w is not None and w not in deps:
                deps[w] = "waw"
            for r in self.readers.get(t, ()):
                if r not in deps and r != op.idx:
                    deps[r] = "war"
        for t in (() if waitonly else reads):
            lst = self.readers.setdefault(t, [])
            if not op.is_dma:
                lst[:] = [r for r in lst if self.ops[r].is_dma or self.ops[r].eng != eng]
            lst.append(op.idx)
        for t in writes:
            self.last_writer[t] = op.idx
            self.readers[t] = []
        op.deps = deps
        self.ops.append(op)
        return op

    def _need_wait(self, op, p, kind):
        if p.is_dma:
            return True
        if p.eng != op.eng:
            return True
        if op.is_dma:
            return True
        if p.eng == "pe":
            return False
        if kind == "raw" and ((op.eidx - p.eidx) <= 3 or p.eng == "pool"):
            return True
        return False

    def emit(self, sem_ctx):
        ops = self.ops
        for op in ops:
            for d, kind in op.deps.items():
                p = ops[d]
                if (not p.is_dma) and self._need_wait(op, p, kind):
                    p.needs_signal = True
        if os.environ.get('KALLSIG'):
            for op in ops:
                if not op.is_dma and op.fn(None) if False else (not op.is_dma and not getattr(op, "wo", False)):
                    op.needs_signal = True
        cnt = {e: 0 for e in self.ENGS}
        dcnt = {}
        for op in ops:
            if op.is_dma:
                dcnt[op.sem] = dcnt.get(op.sem, 0) + 16
                op.semval = dcnt[op.sem]
            elif op.needs_signal:
                cnt[op.eng] += 1
                op.sigcount = cnt[op.eng]
        by_eng = {e: [o for o in ops if o.eng == e] for e in self.ENGS}
        self.stats = (dict(cnt), {e: len(v) for e, v in by_eng.items()})

        def body(ename):
            def run(eng):
                waited = {}
                for op in by_eng[ename]:
                    for d, kind in op.deps.items():
                        p = ops[d]
                        if not self._need_wait(op, p, kind):
                            continue
                        if p.is_dma:
                            key = "dma:" + p.sem; val = p.semval
                        else:
                            key = "eng:" + p.eng; val = p.sigcount
                        if waited.get(key, 0) >= val:
                            continue
                        waited[key] = val
                        eng.wait_ge(sem_ctx[key], val)
                    ins = op.fn(eng)
                    if op.is_dma:
                        ins.then_inc(sem_ctx["dma:" + op.sem], 16)
                    elif op.needs_signal:
                        ins.then_inc(sem_ctx["eng:" + ename], 1)
                for op in by_eng[ename]:
                    if op.is_dma:
                        key = "dma:" + op.sem
                        if waited.get(key, 0) < dcnt[op.sem]:
                            waited[key] = dcnt[op.sem]
                            eng.wait_ge(sem_ctx[key], dcnt[op.sem])
            return run
        return body


def tsl(r, d, p0, n):
    return slice(r + d * p0, r + d * (p0 + n - 1) + 1, d)


def build_program():
    nc = bass.Bass("TRN2", target_bir_lowering=False)
    dt_in = lambda n, s, dt=F32: nc.dram_tensor(n, s, dt, kind="ExternalInput").ap()
    x_d = dt_in("x", [TOK, D])
    win_d = dt_in("w_in", [D, 3072])
    wout_d = dt_in("w_out", [D, D])
    wg_d = dt_in("w_gate", [16, D, 256])
    wu_d = dt_in("w_up", [16, D, 256])
    wd_d = dt_in("w_down", [16, 256, D])
    wr_d = dt_in("wr", [D, 20])
    vec_d = dt_in("vecs", [128, 24 + 20])
    gfin_d = dt_in("gfin", [128, D])
    gmixr_d = dt_in("gmixr", [128, D])
    dilb_d = dt_in("dilb", [128, 5120])
    nas_d = dt_in("nas", [4, 128, 4 * 896])
    y_d = nc.dram_tensor("y", [TOK, D], F32, kind="ExternalOutput").ap()
    hs_d = nc.dram_tensor("hscr", [TOK, D], F32, kind="Internal").ap()
    wgs_d = nc.dram_tensor("wg_bf", [16, D, 256], BF16, kind="Internal").ap()
    wus_d = nc.dram_tensor("wu_bf", [16, D, 256], BF16, kind="Internal").ap()
    wds_d = nc.dram_tensor("wd_bf", [16, 256, D], BF16, kind="Internal").ap()
    dbg_d = None
    if DEBUG:
        dbg_d = nc.dram_tensor("dbg", [128, 8 * 2048], F32, kind="ExternalOutput").ap()

    P = Prog()
    es = ExitStack()
    ARENA = 53200
    arena = es.enter_context(nc.sbuf_tensor("arena", [128, ARENA], F32))
    arena_bf = arena.bitcast(BF16)
    ptr = [0]

    def alloc(ncols, dt=F32, shape=None):
        n32 = ncols if dt == F32 else (ncols + 1) // 2
        a = ptr[0]
        ptr[0] += n32
        assert ptr[0] <= ARENA, ("SBUF overflow", ptr[0])
        if dt == F32:
            ap = arena[:, a:a + ncols]
        else:
            ap = arena_bf[:, 2 * a:2 * a + ncols]
        if shape is not None:
            names = " ".join("d%d" % i for i in range(len(shape)))
            kw = {"d%d" % i: shape[i] for i in range(len(shape))}
            ap = ap.rearrange("p (%s) -> p %s" % (names, names), **kw)
        return ap

    pb_t = es.enter_context(nc.psum_tensor("pb_t", [128, 1024], BF16))
    banks = [es.enter_context(nc.psum_tensor("bk%d" % i, [128, 512], F32)) for i in range(7)]
    tbank = [(pb_t, "bk7"), (banks[6].bitcast(BF16), "bk6")]

    ident = alloc(128, BF16)
    sel = alloc(16 * 128, BF16, (16, 128))
    vecs = alloc(44)
    gfin = alloc(D)
    wr_bf = alloc(8 * 20, BF16, (8, 20))
    mhalf = alloc(4)
    ones1 = alloc(2, BF16)
    bar = alloc(4)
    persist_end = ptr[0]

    try:
        P.add("pool", lambda e: e.memset(ident, 0.0), writes=["ident"])
        P.add("pool", lambda e: e.affine_select(out=ident, in_=ident, pattern=[[-1, 128]], compare_op=ALU.not_equal,
                                                fill=1.0, base=0, channel_multiplier=1), reads=["ident"], writes=["ident"])
        P.add("pool", lambda e: e.memset(sel, 0.0), writes=["sel"])
        P.add("pool", lambda e: e.affine_select(out=sel[0:16], in_=sel[0:16], pattern=[[-1, 16], [0, 128]],
                                                compare_op=ALU.not_equal, fill=1.0, base=0, channel_multiplier=1),
              reads=["sel"], writes=["sel"])
        P.add("pool", lambda e: e.memset(mhalf, -0.5), writes=["mhalf"])
        P.add("pool", lambda e: e.memset(ones1, 1.0), writes=["ones1"])
        P.add("sp", lambda e: e.dma_start(out=vecs, in_=vec_d), writes=["vecs"], dma="c0")
        P.add("sp", lambda e: e.dma_start(out=gfin, in_=gfin_d), writes=["gfin"], dma="c1")
        P.add("pool", lambda e: e.dma_start(out=wr_bf, in_=wr_d.rearrange("(c p) n -> p c n", p=128)),
              writes=["wr_bf"], dma="c2")
        gmix = vecs[:, 0:8]; gffn = vecs[:, 8:16]; gout = vecs[:, 16:24]; brep = vecs[:, 24:44]

        def rms_rstd(src, ss, ms, rstd, junk, tag, ncols=D):
            P.add("act", lambda e: e.activation(out=junk, in_=src, func=AF.Square, accum_out=ss),
                  reads=[tag + "_src"], writes=[tag + "_junk", tag + "_ss"])
            P.add("dve", lambda e: e.tensor_scalar(out=ms, in0=ss, scalar1=1.0 / ncols, scalar2=EPS, op0=ALU.mult, op1=ALU.add),
                  reads=[tag + "_ss"], writes=[tag + "_ms"])
            P.add("pool", lambda e: e.tensor_tensor(out=rstd, in0=ms, in1=mhalf[:, 0:1], op=ALU.pow),
                  reads=[tag + "_ms", "mhalf"], writes=[tag + "_rstd"])

        hnT = alloc(8 * S, BF16, (8, S))
        yT = alloc(8 * S, BF16, (8, S))
        dilb = alloc(5120, BF16)
        wblk = [[alloc(8 * 128, BF16, (8, 128)) for _ in range(3)] for _ in range(2)]
        QT = [alloc(2 * S, BF16, (2, S)) for _ in range(2)]
        KT = [alloc(S, BF16) for _ in range(2)]
        Vt_raw = alloc(3 * 16 * 256, BF16)
        Vt = Vt_raw.rearrange("p (l t h c) -> p l t h c", l=3, t=16, h=2, c=128)
        wout_bf = Vt_raw[:, 0:8 * D].rearrange("p (c n) -> p c n", c=8)
        acc2 = alloc(2 * S, F32, (2, S))
        acc = [acc2[:, 0, :], acc2[:, 1, :]]
        VTb = alloc(S, BF16)
        gmixr = alloc(D)
        rden = alloc(S)
        NS = int(os.environ.get('KNS', '3')); NO = int(os.environ.get('KNO', '2'))
        PT = alloc(NS * 512, BF16)
        nastrip = alloc(4 * 896, BF16, (2, 2, 896))
        xs = [alloc(D, BF16) for _ in range(3)]
        xt = [alloc(D) for _ in range(3)]
        sqt2 = [alloc(8 * 128, BF16, (8, 128)) for _ in range(2)]
        small = alloc(64)
        p1_end = ptr[0]


        def dump(items):
            col = [0]
            for ap, toks in items:
                n = ap.shape[-1] if len(ap.shape) == 2 else None
                assert n is not None
                a = col[0]; col[0] += n
                P.add("pool", lambda e, ap=ap, a=a, n=n: e.dma_start(out=dbg_d[0:ap.shape[0], a:a + n], in_=ap, max_dma_last_dim=2048),
                      reads=toks, dma="dbg")
        alltok = lambda: list(P.last_writer.keys())
        DUMPS['H0'] = lambda: dump([(hnT[:, c, :], alltok()) for c in range(8)])
        DUMPS['I0_1'] = lambda: dump([(QT[0], alltok()), (KT[0], alltok()), (acc[0], alltok()), (acc[1], alltok()), (yT[:, 0, :], alltok()),
                                      (Vt_raw[:, 0:4096], alltok())])
        DUMPS['I0_5'] = lambda: dump([(QT[0], alltok()), (KT[0], alltok()), (acc[0], alltok()), (acc[1], alltok()), (yT[:, 4, :], alltok()),
                                      (Vt_raw[:, 0:4096], alltok())])
        for k_ in (2, 3, 4, 6, 7):
            DUMPS['I0_%d' % k_] = lambda: dump([(yT[:, c, :], alltok()) for c in range(8)])
        if os.environ.get('KD2'):
            DUMPS['I0_2'] = lambda: dump([(QT[0], alltok()), (KT[0], alltok()), (QT[1], alltok()), (KT[1], alltok()), (wblk[0][0][:, 0, :], alltok()), (wblk[0][2][:, 0, :], alltok()), (wblk[1][0][:, 0, :], alltok())])
        DUMPS['P0'] = lambda: dump([(yT[:, c, :], alltok()) for c in range(8)])
        bP = [banks[0], banks[1]]
        bPbf = [banks[0].bitcast(BF16), banks[1].bitcast(BF16)]
        bS = [banks[2], banks[3], banks[4], banks[0], banks[1]][:NS]
        SBK = [2, 3, 4, 0, 1]
        bO = [banks[5], banks[6], pb_t.bitcast(F32)][:NO]

        P.add("pool", lambda e: e.dma_start(out=dilb, in_=dilb_d, max_dma_last_dim=4096), writes=["dilb"], dma="c3")
        if DEBUG:
            P.add("pool", lambda e: e.memset(yT, 0.0), writes=["yT%d_%d" % (c, hp) for c in range(8) for hp in range(2)])

        for st_ in range(2):
            P.add("pool", lambda e, st_=st_: e.memset(QT[st_], 0.0), writes=["QT%d" % st_])
        P.add("sp", lambda e: e.dma_start(out=gmixr, in_=gmixr_d), writes=["gmixr"], dma="c4")
        MASKMUL = os.environ.get('KMASK', '0') == '1'
        if MASKMUL:
            for c_ in range(0, 5120, 512):
                P.add("act", lambda e, c_=c_: e.activation(out=dilb[:, c_:c_ + 512], in_=dilb[:, c_:c_ + 512], func=AF.Exp), reads=["dilb"], writes=["dilb"])
        mctr = {"n": 0}

        def mask_mul(ptv, mv, si, mtok):
            eng = "pool" if (mctr["n"] % int(os.environ.get('KMASKDVE', '3'))) != 0 else "dve"
            mctr["n"] += 1
            P.add(eng, lambda e: e.tensor_tensor(out=ptv, in0=ptv, in1=mv, op=ALU.mult), reads=["PT%d" % si, mtok], writes=["PT%d" % si])
        ctr = {"S": 0, "O": 0, "P": 0, "x": 0}

        for s in range(2):
            tb = s * S
            def h_prep(tt):
                xi = ctr["x"] % 3; ctr["x"] += 1
                xtile = xt[xi]; xsb = xs[xi]
                row0 = tb + tt * 128
                P.add("sp", lambda e: e.dma_start(out=xtile, in_=x_d[row0:row0 + 128, :]),
                      writes=["xt%d_src" % xi], dma="x%d" % xi)
                ss = small[:, xi * 4:xi * 4 + 1]; ms = small[:, xi * 4 + 1:xi * 4 + 2]; rstd = small[:, xi * 4 + 2:xi * 4 + 3]
                P.add("act", lambda e: e.activation(out=xsb, in_=xtile, func=AF.Square, accum_out=ss),
                      reads=["xt%d_src" % xi], writes=["xs%d" % xi, "xt%d_ss" % xi])
                P.add("dve", lambda e: e.tensor_scalar(out=ms, in0=ss, scalar1=1.0 / D, scalar2=EPS, op0=ALU.mult, op1=ALU.add),
                      reads=["xt%d_ss" % xi], writes=["xt%d_ms" % xi])
                P.add("pool", lambda e: e.tensor_tensor(out=rstd, in0=ms, in1=mhalf[:, 0:1], op=ALU.pow),
                      reads=["xt%d_ms" % xi, "mhalf"], writes=["xt%d_rstd" % xi])
                P.add("dve", lambda e: e.scalar_tensor_tensor(out=xsb, in0=xtile, scalar=rstd, in1=gmixr, op0=ALU.mult, op1=ALU.mult),
                      reads=["xt%d_src" % xi, "xt%d_rstd" % xi, "gmixr"], writes=["xs%d" % xi])
                return (xsb, xi)

            def h_trans(ctx, tt):
                xsb, xi = ctx
                tbk, ttok = tbank[tt % 2]
                for c in range(8):
                    P.add("pe", lambda e, c=c: e.transpose(tbk[:, c * 128:(c + 1) * 128], xsb[:, c * 128:(c + 1) * 128], ident),
                          reads=["xs%d" % xi, "ident"], writes=[ttok])
                dst = hnT[:, :, tt * 128:(tt + 1) * 128]
                src = tbk[:, 0:1024].rearrange("p (c q) -> p c q", c=8)
                if tt % 2 == 0:
                    P.add("dve", lambda e: e.tensor_copy(out=dst, in_=src), reads=[ttok], writes=["hn%d_a" % tt])
                else:
                    P.add("act", lambda e: e.activation(out=dst, in_=src, func=AF.Copy), reads=[ttok], writes=["hn%d_b" % tt])

            hctx = [h_prep(0), h_prep(1)]
            for tt in range(16):
                if tt + 2 < 16:
                    hctx.append(h_prep(tt + 2))
                h_trans(hctx[tt], tt)

            def hn_tokens(tiles):
                out = []
                for t in tiles:
                    out += ["hn%d_a" % t, "hn%d_b" % t]
                return out

            stage('H%d' % s)
            pending_fins = []
            for lay_ in range(3):
                P.add("pool", lambda e, lay_=lay_: e.memset(Vt[:, lay_, :, :, 64:128], 1.0), writes=["V%d" % lay_])
            for item in range(8):
                stage('I%d_%d' % (s, item))
                if os.environ.get('KBAR'):
                    P.add("act", lambda e: e.activation(out=bar[:, 0:1], in_=mhalf[:, 0:1], func=AF.Copy), reads=["mhalf"], writes=["bar_act"])
                    P.add("dve", lambda e: e.tensor_copy(out=bar[:, 1:2], in_=mhalf[:, 0:1]), reads=["mhalf"], writes=["bar_dve"])
                    P.add("pool", lambda e: e.tensor_copy(out=bar[:, 2:3], in_=mhalf[:, 0:1]), reads=["mhalf"], writes=["bar_pool"])
                    for en in ("pe", "act", "dve", "pool", "sp"):
                        P.add(en, lambda e: None, reads=["bar_act", "bar_dve", "bar_pool"], waitonly=True)
                if os.environ.get('KSNAP') and item == 1:
                    P.add("pool", lambda e: e.tensor_copy(out=yT[:, 7, :], in_=yT[:, 0, :]), reads=["yT0_0", "yT0_1"], writes=["yT7_0", "yT7_1"])
                    P.add("pool", lambda e: e.tensor_copy(out=yT[:, 6, :], in_=acc[0][:, :]), reads=["acc0"], writes=["yT6_0", "yT6_1"])
                is_dil = item < 4
                j = item % 4
                st = item % 2
                colbase = 0 if is_dil else 1536
                wq, wk, wv = wblk[st]
                for wi, (wb, off) in enumerate(((wq, 0), (wk, 512), (wv, 1024))):
                    c0 = colbase + off + j * 128
                    P.add("pool", lambda e, wb=wb, c0=c0: e.dma_start(out=wb, in_=win_d.rearrange("(c p) n -> p c n", p=128)[:, :, c0:c0 + 128]),
                          writes=["wb%d_%d" % (st, wi)], dma="wb%d_%d" % (st, wi))
                if not is_dil:
                    P.add("pool", lambda e, j=j: e.dma_start(out=nastrip, in_=nas_d[j].rearrange("p (h v n) -> p h v n", h=2, v=2), max_dma_last_dim=3584),
                          writes=["nastrip"], dma="nas")
                    if MASKMUL:
                        nflat = nastrip.rearrange("p h v n -> p (h v n)")
                        for c_ in range(0, 3584, 512):
                            P.add("act", lambda e, c_=c_, nflat=nflat: e.activation(out=nflat[:, c_:c_ + 512], in_=nflat[:, c_:c_ + 512], func=AF.Exp), reads=["nastrip"], writes=["nastrip"])
                if s == 0:
                    for ex_ in (2 * item, 2 * item + 1):
                        for nm_, src_, dst_ in (("g", wg_d, wgs_d), ("u", wu_d, wus_d), ("d", wd_d, wds_d)):
                            P.add("pool", lambda e, src_=src_, dst_=dst_, ex_=ex_: e.dma_start(out=dst_[ex_], in_=src_[ex_], max_dma_last_dim=4096),
                                  writes=["cv%s%d" % (nm_, ex_)], dma="cv%s%d" % (nm_, ex_))
                for which, (wb, dstT, scl) in enumerate(((wq, QT[st], 0.125), (wk, KT[st], 1.0))):
                    for tc in range(4):
                        bi = ctr["P"] % 2; ctr["P"] += 1
                        bk = bP[bi]
                        for c in range(8):
                            P.add("pe", lambda e, bk=bk, wb=wb, c=c, tc=tc: e.matmul(bk[:, 0:512], lhsT=wb[:, c, :], rhs=hnT[:, c, tc * 512:(tc + 1) * 512],
                                                                                     start=(c == 0), stop=(c == 7)),
                                  reads=["wb%d_%d" % (st, which)] + hn_tokens(range(4 * tc, 4 * tc + 4)), writes=["bk%d" % bi])
                        dst = dstT[:, tc * 512:(tc + 1) * 512] if which == 1 else None
                        tokn = ("QT%d" if which == 0 else "KT%d") % st
                        if which == 0:
                            for hp_ in range(2):
                                rw = slice(hp_ * 64, hp_ * 64 + 64)
                                P.add("dve", lambda e, bk=bk, rw=rw, hp_=hp_, tc=tc, dstT=dstT: e.tensor_scalar(out=dstT[rw, hp_, tc * 512:(tc + 1) * 512], in0=bk[rw, 0:512], scalar1=0.125, scalar2=None, op0=ALU.mult),
                                      reads=["bk%d" % bi], writes=[tokn])
                        else:
                            P.add("dve", lambda e, dst=dst, bk=bk: e.tensor_copy(out=dst, in_=bk[:, 0:512]),
                                  reads=["bk%d" % bi], writes=[tokn])
                if pending_fins:
                    pending_fins[0](None)
                for tc in range(4):
                    bi = ctr["P"] % 2; ctr["P"] += 1
                    bk = bP[bi]
                    for c in range(8):
                        P.add("pe", lambda e, bk=bk, wv=wv, c=c, tc=tc: e.matmul(bk[:, 0:512], lhsT=wv[:, c, :], rhs=hnT[:, c, tc * 512:(tc + 1) * 512],
                                                                                 start=(c == 0), stop=(c == 7)),
                              reads=["wb%d_2" % st] + hn_tokens(range(4 * tc, 4 * tc + 4)), writes=["bk%d" % bi])
                    dst = VTb[:, tc * 512:(tc + 1) * 512]
                    P.add("dve", lambda e, dst=dst, bk=bk: e.tensor_copy(out=dst, in_=bk[:, 0:512]),
                          reads=["bk%d" % bi], writes=["VT"])
                if pending_fins:
                    pending_fins[1](None)
                pending_fins = []
                layouts = ((0, 1), (1, 4), (2, 16)) if is_dil else ((0, 1),)
                for (lay, d) in layouts:
                    L = S // d; nt = L // 128
                    tiles = [(r, m) for r in range(d) for m in range(nt)]
                    for g8 in range(2):
                        bi = ctr["P"] % 2; ctr["P"] += 1
                        bkb = bPbf[bi]
                        for q in range(8):
                            r, m = tiles[g8 * 8 + q]
                            tsl_ = tsl(r, d, 128 * m, 128)
                            P.add("pe", lambda e, bkb=bkb, q=q, tsl_=tsl_: e.transpose(bkb[:, q * 128:(q + 1) * 128], VTb[:, tsl_], ident),
                                  reads=["VT", "ident"], writes=["bk%d" % bi])
                        dst = Vt[:, lay, g8 * 8:(g8 + 1) * 8, :, 0:64]
                        src = bkb[:, 0:1024].rearrange("p (t h c) -> p t h c", t=8, h=2)
                        P.add("dve", lambda e, dst=dst, src=src: e.tensor_copy(out=dst, in_=src),
                              reads=["bk%d" % bi], writes=["V%d" % lay])

                new_fins = []
                tasks = []
                qz = QT[st]; kTp = KT[st]
                ATOK = ["acc0", "acc1"]
                v2 = lambda ap, lo, n: ap[:, lo:lo + 2 * n].rearrange("p (h q) -> p h q", h=2)
                if is_dil:
                    for pi, d in enumerate((1, 4)):
                        L = S // d; nt = L // 128
                        biasP = dilb[:, (j * 2 + pi) * 512:(j * 2 + pi + 1) * 512]
                        for r in range(d):
                            for b in range(nt + 1):
                                def s_part(b=b, r=r, d=d, nt=nt, biasP=biasP, qz=qz, kTp=kTp, st=st):
                                    si = ctr["S"] % NS; ctr["S"] += 1
                                    bkS = bS[si]; pt = PT[:, si * 512:(si + 1) * 512]
                                    tk = "bk%d" % SBK[si]
                                    if b == 0:
                                        ov = v2(bkS, 256, 128)[:, :, 64:128]; bv = v2(biasP, 256, 128)[:, :, 64:128]; pv_ = v2(pt, 256, 128)[:, :, 64:128]
                                        for hp_ in range(2):
                                            c0_ = 256 + hp_ * 128 + 64
                                            if not MASKMUL:
                                                P.add("pe", lambda e, c0_=c0_: e.matmul(bkS[:, c0_:c0_ + 64], lhsT=ident, rhs=biasP[:, c0_:c0_ + 64], start=True, stop=False),
                                                      reads=["ident", "dilb"], writes=[tk])
                                            P.add("pe", lambda e, c0_=c0_, hp_=hp_: e.matmul(bkS[:, c0_:c0_ + 64], lhsT=kTp[:, tsl(r, d, 0, 128)], rhs=qz[:, hp_, tsl(r, d, 0, 64)], start=(MASKMUL and hp_ == 0), stop=True),
                                                  reads=["QT%d" % st, "KT%d" % st], writes=[tk])
                                        P.add("act", lambda e: e.activation(out=pv_, in_=ov, func=AF.Exp), reads=[tk], writes=["PT%d" % si])
                                        if MASKMUL:
                                            mask_mul(pv_, bv, si, "dilb")
                                    elif b == nt:
                                        ov = v2(bkS, 0, 128)[:, :, 0:64]; bv = v2(biasP, 0, 128)[:, :, 0:64]; pv_ = v2(pt, 0, 128)[:, :, 0:64]
                                        for hp_ in range(2):
                                            c0_ = hp_ * 128
                                            if not MASKMUL:
                                                P.add("pe", lambda e, c0_=c0_: e.matmul(bkS[:, c0_:c0_ + 64], lhsT=ident, rhs=biasP[:, c0_:c0_ + 64], start=True, stop=False),
                                                      reads=["ident", "dilb"], writes=[tk])
                                            P.add("pe", lambda e, c0_=c0_, hp_=hp_: e.matmul(bkS[:, c0_:c0_ + 64], lhsT=kTp[:, tsl(r, d, 128 * (nt - 1), 128)], rhs=qz[:, hp_, tsl(r, d, 128 * nt - 64, 64)], start=(MASKMUL and hp_ == 0), stop=True),
                                                  reads=["QT%d" % st, "KT%d" % st], writes=[tk])
                                        P.add("act", lambda e: e.activation(out=pv_, in_=ov, func=AF.Exp), reads=[tk], writes=["PT%d" % si])
                                        if MASKMUL:
                                            mask_mul(pv_, bv, si, "dilb")
                                    else:
                                        qv = qz[:, :, tsl(r, d, 128 * b - 64, 128)]
                                        if not MASKMUL:
                                            P.add("pe", lambda e: e.matmul(bkS[:, 0:512], lhsT=ident, rhs=biasP, start=True, stop=False), reads=["ident", "dilb"], writes=[tk])
                                        P.add("pe", lambda e: e.matmul(bkS[:, 0:256], lhsT=kTp[:, tsl(r, d, 128 * (b - 1), 128)], rhs=qv, start=MASKMUL, stop=False),
                                              reads=["QT%d" % st, "KT%d" % st], writes=[tk])
                                        P.add("pe", lambda e: e.matmul(bkS[:, 256:512], lhsT=kTp[:, tsl(r, d, 128 * b, 128)], rhs=qv, start=False, stop=True),
                                              reads=["QT%d" % st, "KT%d" % st], writes=[tk])
                                        P.add("act", lambda e: e.activation(out=pt[:, 0:512], in_=bkS[:, 0:512], func=AF.Exp), reads=[tk], writes=["PT%d" % si])
                                        if MASKMUL:
                                            mask_mul(pt[:, 0:512], biasP, si, "dilb")
                                    return si

                                def pv_part(si, b=b, r=r, d=d, nt=nt, pi=pi):
                                    oi = ctr["O"] % NO; ctr["O"] += 1
                                    bkO = bO[oi]; pt = PT[:, si * 512:(si + 1) * 512]
                                    tk = "bk%d" % (5 + oi)
                                    for hp in range(2):
                                        def mm(o_, l_, r_, stt, stp):
                                            P.add("pe", lambda e: e.matmul(o_, lhsT=l_, rhs=r_, start=stt, stop=stp), reads=["PT%d" % si, "V%d" % pi], writes=[tk])
                                        ob = hp * 128
                                        if b == 0:
                                            mm(bkO[:, ob + 64:ob + 128], Vt[:, pi, r * nt + 0, hp, :], pt[:, 256 + hp * 128 + 64:256 + hp * 128 + 128], True, True)
                                        elif b == nt:
                                            mm(bkO[:, ob:ob + 64], Vt[:, pi, r * nt + nt - 1, hp, :], pt[:, hp * 128:hp * 128 + 64], True, True)
                                        else:
                                            mm(bkO[:, ob:ob + 128], Vt[:, pi, r * nt + b - 1, hp, :], pt[:, hp * 128:hp * 128 + 128], True, False)
                                            mm(bkO[:, ob:ob + 128], Vt[:, pi, r * nt + b, hp, :], pt[:, 256 + hp * 128:256 + hp * 128 + 128], False, True)
                                    c_lo = 64 if b == 0 else 0
                                    c_hi = 64 if b == nt else 128
                                    p_lo = 128 * b - 64 + c_lo
                                    dsta = acc2[:, :, tsl(r, d, p_lo, c_hi - c_lo)]
                                    srca = v2(bkO, 0, 128)[:, :, c_lo:c_hi]
                                    if pi == 0:
                                        P.add("dve", lambda e: e.tensor_copy(out=dsta, in_=srca), reads=[tk], writes=ATOK)
                                    else:
                                        P.add("dve", lambda e: e.tensor_tensor(out=dsta, in0=srca, in1=dsta, op=ALU.add), reads=[tk] + ATOK, writes=ATOK)
                                tasks.append((s_part, pv_part))
                    bias3P = dilb[:, 4096 + j * 256:4096 + (j + 1) * 256]
                    for r0 in range(0, 16, 2):
                        def s_part(r0=r0, bias3P=bias3P, qz=qz, kTp=kTp, st=st):
                            si = ctr["S"] % NS; ctr["S"] += 1
                            bkS = bS[si]; pt = PT[:, si * 512:(si + 1) * 512]
                            tk = "bk%d" % SBK[si]
                            for rr in range(2):
                                r_ = r0 + rr
                                if not MASKMUL:
                                    P.add("pe", lambda e, rr=rr: e.matmul(bkS[:, rr * 256:(rr + 1) * 256], lhsT=ident, rhs=bias3P, start=True, stop=False),
                                          reads=["ident", "dilb"], writes=[tk])
                                P.add("pe", lambda e, rr=rr, r_=r_: e.matmul(bkS[:, rr * 256:(rr + 1) * 256], lhsT=kTp[:, tsl(r_, 16, 0, 128)], rhs=qz[:, :, tsl(r_, 16, 0, 128)], start=(MASKMUL and rr == 0), stop=True),
                                      reads=["QT%d" % st, "KT%d" % st], writes=[tk])
                            P.add("act", lambda e: e.activation(out=pt[:, 0:512], in_=bkS[:, 0:512], func=AF.Exp), reads=[tk], writes=["PT%d" % si])
                            if MASKMUL:
                                mask_mul(pt[:, 0:512].rearrange("p (r c) -> p r c", r=2), bias3P.unsqueeze(1).to_broadcast([128, 2, 256]), si, "dilb")
                            return si

                        def pv_part(si, r0=r0):
                            oi = ctr["O"] % NO; ctr["O"] += 1
                            bkO = bO[oi]; pt = PT[:, si * 512:(si + 1) * 512]
                            tk = "bk%d" % (5 + oi)
                            for rr in range(2):
                                for hp in range(2):
                                    cs_ = slice(rr * 256 + hp * 128, rr * 256 + hp * 128 + 128)
                                    P.add("pe", lambda e, rr=rr, hp=hp, cs_=cs_: e.matmul(bkO[:, cs_], lhsT=Vt[:, 2, r0 + rr, hp, :], rhs=pt[:, cs_], start=True, stop=True),
                                          reads=["PT%d" % si, "V2"], writes=[tk])
                            dsta = acc2.rearrange("p h (q r) -> p h q r", r=16)[:, :, :, r0:r0 + 2]
                            srca = bkO[:, 0:512].rearrange("p (r h q) -> p h q r", r=2, h=2)
                            P.add("dve", lambda e: e.tensor_tensor(out=dsta, in0=srca, in1=dsta, op=ALU.add), reads=[tk] + ATOK, writes=ATOK)
                        tasks.append((s_part, pv_part))
                else:
                    for g in range(8):
                        if g == 0:
                            ms_, var = [0, 1, 2, 3], 1
                        elif g == 7:
                            ms_, var = [12, 13, 14, 15], 1
                        else:
                            ms_, var = list(range(2 * g - 2, 2 * g + 4)), 0
                        ostate = {}
                        for ki, m in enumerate(ms_):
                            def s_part(m=m, g=g, var=var, qz=qz, kTp=kTp, st=st):
                                si = ctr["S"] % NS; ctr["S"] += 1
                                bkS = bS[si]; pt = PT[:, si * 512:(si + 1) * 512]
                                tk = "bk%d" % SBK[si]
                                sft = 6 - (2 * m - 4 * g)
                                assert 0 <= sft and sft * 64 + 256 <= 896
                                if not MASKMUL:
                                    P.add("pe", lambda e: e.matmul(bkS[:, 0:512], lhsT=ident, rhs=nastrip[:, :, var, sft * 64:sft * 64 + 256], start=True, stop=False),
                                          reads=["ident", "nastrip"], writes=[tk])
                                P.add("pe", lambda e: e.matmul(bkS[:, 0:512], lhsT=kTp[:, m * 128:(m + 1) * 128], rhs=qz[:, :, g * 256:(g + 1) * 256], start=MASKMUL, stop=True),
                                      reads=["QT%d" % st, "KT%d" % st], writes=[tk])
                                P.add("act", lambda e: e.activation(out=pt[:, 0:512], in_=bkS[:, 0:512], func=AF.Exp), reads=[tk], writes=["PT%d" % si])
                                if MASKMUL:
                                    mask_mul(v2(pt, 0, 256), nastrip[:, :, var, sft * 64:sft * 64 + 256], si, "nastrip")
                                return si

                            def pv_part(si, m=m, ki=ki, g=g, ostate=ostate, nms=len(ms_)):
                                if ki == 0:
                                    ostate["oi"] = ctr["O"] % NO; ctr["O"] += 1
                                oi = ostate["oi"]
                                bkO = bO[oi]; pt = PT[:, si * 512:(si + 1) * 512]
                                tk = "bk%d" % (5 + oi)
                                for hp in range(2):
                                    P.add("pe", lambda e, hp=hp: e.matmul(bkO[:, hp * 256:(hp + 1) * 256], lhsT=Vt[:, 0, m, hp, :], rhs=pt[:, hp * 256:(hp + 1) * 256],
                                                                         start=(ki == 0 and hp == 0), stop=(ki == nms - 1), skip_group_check=True),
                                          reads=["PT%d" % si, "V0"], writes=[tk])
                                if ki == nms - 1:
                                    dsta = acc2[:, :, g * 256:(g + 1) * 256]
                                    P.add("dve", lambda e: e.tensor_copy(out=dsta, in_=v2(bkO, 0, 256)), reads=[tk], writes=ATOK)
                            tasks.append((s_part, pv_part))

                for hp in range(2):
                    def fin_part(_si, hp=hp, chunk=(j if is_dil else 4 + j)):
                        rows = slice(hp * 64, hp * 64 + 64)
                        P.add("act", lambda e: e.activation(out=rden[0:64, :], in_=acc2[64:128, hp, :], func=AF.Ln), reads=ATOK, writes=["rden"])
                        P.add("act", lambda e: e.activation(out=rden[0:64, :], in_=rden[0:64, :], func=AF.Exp, scale=-1.0), reads=["rden"], writes=["rden"])
                        P.add("dve", lambda e: e.tensor_tensor(out=yT[rows, chunk, :], in0=acc2[0:64, hp, :], in1=rden[0:64, :], op=ALU.mult),
                              reads=ATOK + ["rden"], writes=["yT%d_%d" % (chunk, hp)])
                    new_fins.append(fin_part)

                LOOK = int(os.environ.get('KLOOK', '2'))
                sis = []
                for ti_, (sp_, pv_) in enumerate(tasks):
                    sis.append(sp_() if sp_ is not None else None)
                    if ti_ >= LOOK:
                        tasks[ti_ - LOOK][1](sis[ti_ - LOOK])
                for ti_ in range(max(0, len(tasks) - LOOK), len(tasks)):
                    tasks[ti_][1](sis[ti_])
                pending_fins = new_fins


            for fp_ in pending_fins:
                fp_(None)
            pending_fins = []
            stage('P%d' % s)
            ytoks = ["yT%d_%d" % (c, hp) for c in range(8) for hp in range(2)]
            P.add("pool", lambda e: e.dma_start(out=wout_bf, in_=wout_d.rearrange("(c p) n -> p c n", p=128)),
                  writes=["V0", "V1", "V2", "wout"], dma="wout")
            for c in range(8):
                P.add("dve", lambda e, c=c: e.tensor_scalar(out=wout_bf[:, c, :], in0=wout_bf[:, c, :], scalar1=gout[:, c:c + 1], scalar2=None, op0=ALU.mult),
                      reads=["wout", "vecs"], writes=["wout"])
            def o_prep(tt):
                row0 = tb + tt * 128
                tsl_ = slice(tt * 128, (tt + 1) * 128)
                xi = ctr["x"] % 3; ctr["x"] += 1
                qi = tt % 2
                xtile = xt[xi]; sq_ = sqt2[qi]
                P.add("sp", lambda e: e.dma_start(out=xtile, in_=x_d[row0:row0 + 128, :]),
                      writes=["xt%d_src" % xi], dma="x%d" % xi)
                P.add("pool", lambda e: e.tensor_tensor(out=sq_, in0=yT[:, :, tsl_], in1=yT[:, :, tsl_], op=ALU.mult),
                      reads=ytoks, writes=["sqt%d" % qi])
                bq = bS[2]
                for c in range(8):
                    col = qi * 2 + c // 4
                    P.add("pe", lambda e, c=c, col=col: e.matmul(bq[:, col:col + 1], lhsT=sq_[:, c, :], rhs=ones1[:, 0:1], start=(c % 4 == 0), stop=(c % 4 == 3)),
                          reads=["sqt%d" % qi, "ones1"], writes=["bk4"])
                ms2 = small[:, 16 + xi * 4:16 + xi * 4 + 2]; rs2 = small[:, 16 + xi * 4 + 2:16 + xi * 4 + 4]
                P.add("dve", lambda e: e.tensor_scalar(out=ms2, in0=bq[:, qi * 2:qi * 2 + 2], scalar1=1.0 / 512, scalar2=EPS, op0=ALU.mult, op1=ALU.add),
                      reads=["bk4"], writes=["ms2_%d" % xi])
                P.add("pool", lambda e: e.tensor_tensor(out=rs2, in0=ms2, in1=mhalf[:, 0:2], op=ALU.pow),
                      reads=["ms2_%d" % xi, "mhalf"], writes=["rs2_%d" % xi])
                return (xtile, xi, rs2, row0, tsl_)

            def o_main(ctx, tt):
                xtile, xi, rs2, row0, tsl_ = ctx
                hi_ = tt % 2
                ht = acc[hi_][:, 0:D]
                for half in range(2):
                    hs_ = slice(half * 512, (half + 1) * 512)
                    bA = [bP[0], bP[1]][half]; bB = [bS[0], bS[1]][half]
                    for c in range(4):
                        P.add("pe", lambda e, c=c, bA=bA, hs_=hs_: e.matmul(bA[:, 0:512], lhsT=yT[:, c, tsl_], rhs=wout_bf[:, c, hs_], start=(c == 0), stop=(c == 3)),
                              reads=ytoks + ["wout", "V0", "V1", "V2"], writes=["bk%d" % half])
                    for c in range(4, 8):
                        P.add("pe", lambda e, c=c, bB=bB, hs_=hs_: e.matmul(bB[:, 0:512], lhsT=yT[:, c, tsl_], rhs=wout_bf[:, c, hs_], start=(c == 4), stop=(c == 7)),
                              reads=ytoks + ["wout", "V0", "V1", "V2"], writes=["bk%d" % (2 + half)])
                    P.add("dve", lambda e, hs_=hs_, bA=bA: e.scalar_tensor_tensor(out=ht[:, hs_], in0=bA[:, 0:512], scalar=rs2[:, 0:1], in1=xtile[:, hs_], op0=ALU.mult, op1=ALU.add),
                          reads=["bk%d" % half, "rs2_%d" % xi, "xt%d_src" % xi], writes=["acc%d" % hi_])
                    P.add("dve", lambda e, hs_=hs_, bB=bB: e.scalar_tensor_tensor(out=ht[:, hs_], in0=bB[:, 0:512], scalar=rs2[:, 1:2], in1=ht[:, hs_], op0=ALU.mult, op1=ALU.add),
                          reads=["bk%d" % (2 + half), "rs2_%d" % xi, "acc%d" % hi_], writes=["acc%d" % hi_])
                P.add("sp", lambda e: e.dma_start(out=hs_d[row0:row0 + 128, :], in_=ht),
                      reads=["acc%d" % hi_], writes=["hs%d" % (row0 // 128)], dma="hst%d" % hi_)

            octx = [o_prep(0)]
            for tt in range(16):
                if tt + 1 < 16:
                    octx.append(o_prep(tt + 1))
                o_main(octx[tt], tt)

        stage('O')
        P.add("act", lambda e: e.activation(out=bar[:, 0:1], in_=mhalf[:, 0:1], func=AF.Copy), reads=["mhalf"], writes=["bar_act"])
        P.add("dve", lambda e: e.tensor_copy(out=bar[:, 1:2], in_=mhalf[:, 0:1]), reads=["mhalf"], writes=["bar_dve"])
        P.add("pool", lambda e: e.tensor_copy(out=bar[:, 2:3], in_=mhalf[:, 0:1]), reads=["mhalf"], writes=["bar_pool"])
        for en in ("pe", "act", "dve", "pool", "sp"):
            P.add(en, lambda e: None, reads=["bar_act", "bar_dve", "bar_pool"] + ["hs%d" % i_ for i_ in range(32)], waitonly=True)

        ptr[0] = persist_end
        wd_bf = alloc(32 * D, BF16, (32, D))
        hn2T = alloc(8 * 1024, BF16, (8, 1024))
        actT = alloc(32 * 1024, BF16, (32, 1024))
        wgu = [[alloc(8 * 256, BF16, (8, 256)) for _ in range(2)] for _ in range(3)]
        hbA = alloc(D); hbD = [alloc(D) for _ in range(2)]
        xsA = alloc(D, BF16); xsD = alloc(D, BF16)
        combT = alloc(1024, BF16)
        Cs = [alloc(512, BF16) for _ in range(2)]
        ssb = [alloc(512, BF16) for _ in range(2)]
        s2b = [alloc(512, BF16) for _ in range(2)]
        rt = alloc(960)
        comb_bf = alloc(8 * 16, BF16, (8, 16))
        small2 = alloc(32)

        bG = [banks[0], banks[1]]; bU = [banks[2], banks[3]]; bC = banks[4]; bD = [banks[5], banks[6]]

        def load_wd():
            for k in range(4):
                P.add("sp", lambda e, k=k: e.dma_start(out=wd_bf[:, 8 * k:8 * k + 8, :],
                                                       in_=wds_d[4 * k:4 * k + 4].rearrange("e (f p) n -> p (e f) n", p=128)),
                      reads=["cvd%d" % ee for ee in range(4 * k, 4 * k + 4)], writes=["wd%d" % k], dma="wd%d" % k)
        wd_toks = ["wd%d" % k for k in range(4)]

        def load_gu(n):
            if n >= 64:
                return
            ex_ = n % 16; wi_ = n % 3
            wgb_, wub_ = wgu[wi_]
            P.add("sp", lambda e: e.dma_start(out=wgb_, in_=wgs_d[ex_].rearrange("(c p) n -> p c n", p=128)),
                  reads=["cvg%d" % ex_], writes=["wg%d" % wi_], dma="wg%d" % wi_)
            P.add("sp", lambda e: e.dma_start(out=wub_, in_=wus_d[ex_].rearrange("(c p) n -> p c n", p=128)),
                  reads=["cvu%d" % ex_], writes=["wu%d" % wi_], dma="wu%d" % wi_)
        load_gu(0)
        load_gu(1)

        c2 = {"h": 0, "G": 0, "C": 0, "D": 0, "w": 0, "y": 0}
        h2_all = [("h2_%d_a" % st) for st in range(8)] + [("h2_%d_b" % st) for st in range(8)]

        hpool = [(hbA, "hbA"), (hbD[0], "hbD0"), (hbD[1], "hbD1")]
        xpool = [(xsA, "xsA"), (xsD, "xsD")]
        tbank2 = [(pb_t, "bk7"), (banks[2].bitcast(BF16), "bk2")]
        bD4 = [(banks[5], "bk5"), (banks[6], "bk6"), (banks[0], "bk0"), (banks[1], "bk1")]

        def prep_hn2(T, st):
            row0 = T * 1024 + st * 128
            if T == 0:
                htile, htok = hpool[st % 3]; xsb, xtok = xpool[st % 2]
            else:
                htile, htok = hpool[0]; xsb, xtok = xpool[0]
            si_ = c2["h"] % 4; c2["h"] += 1
            P.add("sp", lambda e: e.dma_start(out=htile, in_=hs_d[row0:row0 + 128, :]),
                  reads=["hs%d" % (row0 // 128)], writes=[htok], dma=htok)
            ss = small2[:, si_ * 4:si_ * 4 + 1]; ms = small2[:, si_ * 4 + 1:si_ * 4 + 2]; rstd = small2[:, si_ * 4 + 2:si_ * 4 + 3]
            P.add("act", lambda e: e.activation(out=xsb, in_=htile, func=AF.Square, accum_out=ss),
                  reads=[htok], writes=[xtok, "h_ss%d" % si_])
            P.add("dve", lambda e: e.tensor_scalar(out=ms, in0=ss, scalar1=1.0 / D, scalar2=EPS, op0=ALU.mult, op1=ALU.add),
                  reads=["h_ss%d" % si_], writes=["h_ms%d" % si_])
            P.add("pool", lambda e: e.tensor_tensor(out=rstd, in0=ms, in1=mhalf[:, 0:1], op=ALU.pow),
                  reads=["h_ms%d" % si_, "mhalf"], writes=["h_rstd%d" % si_])
            P.add("dve", lambda e: e.tensor_scalar(out=xsb, in0=htile, scalar1=rstd, scalar2=None, op0=ALU.mult),
                  reads=[htok, "h_rstd%d" % si_], writes=[xtok])
            return (xsb, xtok)

        def trans_hn2(ctx, T, st):
            xsb, xtok = ctx
            tbk, ttok = tbank2[st % 2]
            for c in range(8):
                P.add("pe", lambda e, c=c: e.transpose(tbk[:, c * 128:(c + 1) * 128], xsb[:, c * 128:(c + 1) * 128], ident),
                      reads=[xtok, "ident"], writes=[ttok])
            for c in range(8):
                dst = hn2T[:, c, st * 128:(st + 1) * 128]
                src = tbk[:, c * 128:(c + 1) * 128]
                if st % 2 == 0:
                    P.add("dve", lambda e, dst=dst, src=src, c=c: e.tensor_scalar(out=dst, in0=src, scalar1=gffn[:, c:c + 1], scalar2=None, op0=ALU.mult),
                          reads=[ttok, "vecs"], writes=["h2_%d_a" % st])
                else:
                    P.add("act", lambda e, dst=dst, src=src, c=c: e.activation(out=dst, in_=src, func=AF.Copy, scale=gffn[:, c:c + 1]),
                          reads=[ttok, "vecs"], writes=["h2_%d_b" % st])

        def emit_router(T):
            if True:
                for st in range(8):
                    for c in range(8):
                        P.add("pe", lambda e, st=st, c=c: e.matmul(bC[:, st * 32:st * 32 + 20], lhsT=hn2T[:, c, st * 128:(st + 1) * 128], rhs=wr_bf[:, c, :],
                                                                   start=(c == 0), stop=(c == 7)),
                              reads=["h2_%d_a" % st, "h2_%d_b" % st, "wr_bf"], writes=["bk4"])
                lg = rt[:, 0:160].rearrange("p (s n) -> p s n", s=8)
                bc3 = bC[:, 0:256].rearrange("p (s n) -> p s n", s=8)[:, :, 0:20]
                RT = "rt"
                r3 = lambda lo, n: rt[:, lo:lo + 8 * n].rearrange("p (s n) -> p s n", s=8)
                gl = lg[:, :, 0:4]; el = lg[:, :, 4:20]
                gmax = rt[:, 160:168]; g1 = r3(168, 4); gex = r3(200, 4); gsum = rt[:, 232:240]; gw = rt[:, 240:248]
                pen = r3(248, 16); ml = r3(376, 16); m1 = rt[:, 504:512]; m2 = rt[:, 512:520]; k1 = r3(520, 16)
                dm = rt[:, 648:656]; w1 = rt[:, 656:664]; w2 = rt[:, 664:672]; k2 = r3(672, 16); ml2 = r3(800, 16)
                bcast = lambda ap, n: ap.unsqueeze(2).to_broadcast([128, 8, n])
                P.add("dve", lambda e: e.tensor_tensor(out=lg, in0=bc3, in1=brep.unsqueeze(1).to_broadcast([128, 8, 20]), op=ALU.add),
                      reads=["bk4", "vecs"], writes=[RT])
                P.add("dve", lambda e: e.tensor_reduce(out=gmax, in_=gl, axis=AX.X, op=ALU.max), reads=[RT], writes=[RT + "a"])
                P.add("dve", lambda e: e.tensor_tensor(out=g1, in0=gl, in1=bcast(gmax, 4), op=ALU.is_equal), reads=[RT, RT + "a"], writes=[RT + "b"])
                P.add("dve", lambda e: e.tensor_tensor(out=gex, in0=gl, in1=bcast(gmax, 4), op=ALU.subtract), reads=[RT, RT + "a"], writes=[RT + "c"])
                P.add("act", lambda e: e.activation(out=gex, in_=gex, func=AF.Exp), reads=[RT + "c"], writes=[RT + "d"])
                P.add("dve", lambda e: e.tensor_reduce(out=gsum, in_=gex, axis=AX.X, op=ALU.add), reads=[RT + "d"], writes=[RT + "e"])
                P.add("dve", lambda e: e.reciprocal(out=gw, in_=gsum), reads=[RT + "e"], writes=[RT + "f"])
                P.add("dve", lambda e: e.tensor_scalar(out=pen.rearrange("p s (g n) -> p s g n", g=4), in0=g1.unsqueeze(3).to_broadcast([128, 8, 4, 4]),
                                                       scalar1=-1.0, scalar2=30000.0, op0=ALU.add, op1=ALU.mult), reads=[RT + "b"], writes=[RT + "g"])
                P.add("dve", lambda e: e.tensor_tensor(out=ml, in0=el, in1=pen, op=ALU.add), reads=[RT, RT + "g"], writes=[RT + "h"])
                P.add("dve", lambda e: e.tensor_reduce(out=m1, in_=ml, axis=AX.X, op=ALU.max), reads=[RT + "h"], writes=[RT + "i"])
                P.add("dve", lambda e: e.tensor_tensor(out=k1, in0=ml, in1=bcast(m1, 16), op=ALU.is_equal), reads=[RT + "h", RT + "i"], writes=[RT + "j"])
                P.add("dve", lambda e: e.scalar_tensor_tensor(out=ml2, in0=k1, scalar=-60000.0, in1=ml, op0=ALU.mult, op1=ALU.add), reads=[RT + "h", RT + "j"], writes=[RT + "k"])
                P.add("dve", lambda e: e.tensor_reduce(out=m2, in_=ml2, axis=AX.X, op=ALU.max), reads=[RT + "k"], writes=[RT + "l"])
                P.add("dve", lambda e: e.tensor_tensor(out=k2, in0=ml2, in1=bcast(m2, 16), op=ALU.is_equal), reads=[RT + "k", RT + "l"], writes=[RT + "m"])
                P.add("dve", lambda e: e.tensor_tensor(out=dm, in0=m2, in1=m1, op=ALU.subtract), reads=[RT + "l", RT + "i"], writes=[RT + "n"])
                P.add("act", lambda e: e.activation(out=dm, in_=dm, func=AF.Exp), reads=[RT + "n"], writes=[RT + "o"])
                P.add("dve", lambda e: e.tensor_scalar(out=dm, in0=dm, scalar1=1.0, scalar2=None, op0=ALU.add), reads=[RT + "o"], writes=[RT + "p"])
                P.add("dve", lambda e: e.reciprocal(out=w1, in_=dm), reads=[RT + "p"], writes=[RT + "q"])
                P.add("dve", lambda e: e.tensor_scalar(out=w2, in0=w1, scalar1=-1.0, scalar2=1.0, op0=ALU.mult, op1=ALU.add), reads=[RT + "q"], writes=[RT + "r"])
                P.add("dve", lambda e: e.tensor_tensor(out=w1, in0=w1, in1=gw, op=ALU.mult), reads=[RT + "q", RT + "r", RT + "f"], writes=[RT + "s"])
                P.add("dve", lambda e: e.tensor_tensor(out=w2, in0=w2, in1=gw, op=ALU.mult), reads=[RT + "r", RT + "f"], writes=[RT + "t"])
                P.add("dve", lambda e: e.tensor_tensor(out=k1, in0=k1, in1=bcast(w1, 16), op=ALU.mult), reads=[RT + "j", RT + "s", RT + "k"], writes=[RT + "u"])
                P.add("dve", lambda e: e.tensor_tensor(out=k2, in0=k2, in1=bcast(w2, 16), op=ALU.mult), reads=[RT + "m", RT + "t"], writes=[RT + "v"])
                P.add("dve", lambda e: e.tensor_tensor(out=comb_bf, in0=k1, in1=k2, op=ALU.add), reads=[RT + "u", RT + "v"], writes=["comb_bf"])
                for st in range(8):
                    P.add("pe", lambda e, st=st: e.transpose(pb_t[0:16, st * 128:(st + 1) * 128], comb_bf[:, st, :], ident),
                          reads=["comb_bf", "ident"] + h2_all, writes=["bk7"])
                P.add("dve", lambda e: e.tensor_copy(out=combT[0:16, :], in_=pb_t[0:16, 0:1024]), reads=["bk7"], writes=["combT"])


        def emit_experts(T):
            if True:
                for ex in range(16):
                    nflat = T * 16 + ex
                    wi = nflat % 3
                    wgb, wub = wgu[wi]
                    load_gu(nflat + 2)
                    for half in range(2):
                        hs_ = slice(half * 512, (half + 1) * 512)
                        ci = c2["C"] % 2; c2["C"] += 1
                        P.add("pe", lambda e, ex=ex, hs_=hs_: e.matmul(bC[:, 0:512], lhsT=sel[0:16, ex, :], rhs=combT[0:16, hs_], start=True, stop=True),
                              reads=["sel", "combT"], writes=["bk4"])
                        P.add("dve", lambda e, ci=ci: e.tensor_copy(out=Cs[ci], in_=bC[:, 0:512]), reads=["bk4"], writes=["Cs%d" % ci])
                        for fc in range(2):
                            gi = c2["G"] % 2; c2["G"] += 1
                            for c in range(8):
                                P.add("pe", lambda e, gi=gi, c=c, fc=fc, hs_=hs_, wgb=wgb: e.matmul(bG[gi][:, 0:512], lhsT=wgb[:, c, fc * 128:(fc + 1) * 128], rhs=hn2T[:, c, hs_],
                                                                                                  start=(c == 0), stop=(c == 7)),
                                      reads=["wg%d" % wi] + h2_all, writes=["bk%d" % gi])
                            for c in range(8):
                                P.add("pe", lambda e, gi=gi, c=c, fc=fc, hs_=hs_, wub=wub: e.matmul(bU[gi][:, 0:512], lhsT=wub[:, c, fc * 128:(fc + 1) * 128], rhs=hn2T[:, c, hs_],
                                                                                                  start=(c == 0), stop=(c == 7)),
                                      reads=["wu%d" % wi] + h2_all, writes=["bk%d" % (2 + gi)])
                            P.add("act", lambda e, gi=gi: e.activation(out=ssb[gi], in_=bG[gi][:, 0:512], func=AF.Silu), reads=["bk%d" % gi], writes=["ssb%d" % gi])
                            P.add("pool", lambda e, gi=gi, ci=ci: e.tensor_tensor(out=s2b[gi], in0=ssb[gi], in1=Cs[ci], op=ALU.mult),
                                  reads=["ssb%d" % gi, "Cs%d" % ci], writes=["s2b%d" % gi])
                            P.add("dve", lambda e, gi=gi, ex=ex, fc=fc, hs_=hs_: e.tensor_tensor(out=actT[:, ex * 2 + fc, hs_], in0=bU[gi][:, 0:512], in1=s2b[gi], op=ALU.mult),
                                  reads=["bk%d" % (2 + gi), "s2b%d" % gi], writes=["actT"])


        def down_mm(T, st):
            row0 = T * 1024 + st * 128
            hi = c2["y"] % 2; c2["y"] += 1
            htile, htok = hpool[1 + hi]
            P.add("sp", lambda e: e.dma_start(out=htile, in_=hs_d[row0:row0 + 128, :]),
                  reads=["hs%d" % (row0 // 128)], writes=[htok], dma=htok)
            for dh in range(2):
                bk_, btok = bD4[c2["D"] % 4]; c2["D"] += 1
                ds_ = slice(dh * 512, (dh + 1) * 512)
                for kk in range(32):
                    P.add("pe", lambda e, bk_=bk_, kk=kk, ds_=ds_: e.matmul(bk_[:, 0:512], lhsT=actT[:, kk, st * 128:(st + 1) * 128], rhs=wd_bf[:, kk, ds_],
                                                                          start=(kk == 0), stop=(kk == 31)),
                          reads=["actT"] + wd_toks, writes=[btok])
                P.add("dve", lambda e, bk_=bk_, ds_=ds_: e.tensor_tensor(out=htile[:, ds_], in0=bk_[:, 0:512], in1=htile[:, ds_], op=ALU.add),
                      reads=[btok, htok], writes=[htok])
            return (htile, htok, row0, hi)

        def down_fin(ctx):
            htile, htok, row0, yi = ctx
            junk, jtok = xpool[1]
            ss = small2[:, 16 + yi * 4:16 + yi * 4 + 1]; ms = small2[:, 16 + yi * 4 + 1:16 + yi * 4 + 2]; rstd = small2[:, 16 + yi * 4 + 2:16 + yi * 4 + 3]
            P.add("act", lambda e: e.activation(out=junk, in_=htile, func=AF.Square, accum_out=ss),
                  reads=[htok], writes=[jtok, "f_ss%d" % yi])
            P.add("dve", lambda e: e.tensor_scalar(out=ms, in0=ss, scalar1=1.0 / D, scalar2=EPS, op0=ALU.mult, op1=ALU.add),
                  reads=["f_ss%d" % yi], writes=["f_ms%d" % yi])
            P.add("pool", lambda e: e.tensor_tensor(out=rstd, in0=ms, in1=mhalf[:, 0:1], op=ALU.pow),
                  reads=["f_ms%d" % yi, "mhalf"], writes=["f_rstd%d" % yi])
            P.add("dve", lambda e: e.scalar_tensor_tensor(out=htile, in0=htile, scalar=rstd, in1=gfin, op0=ALU.mult, op1=ALU.mult),
                  reads=[htok, "f_rstd%d" % yi, "gfin"], writes=[htok])
            P.add("sp", lambda e: e.dma_start(out=y_d[row0:row0 + 128, :], in_=htile),
                  reads=[htok], dma="yst%d" % yi)

        for st in range(8):
            trans_hn2(prep_hn2(0, st), 0, st)
        emit_router(0)
        load_wd()
        for T in range(4):
            emit_experts(T)
            for st in range(8):
                cx = prep_hn2(T + 1, st) if T < 3 else None
                dx = down_mm(T, st)
                if cx is not None:
                    trans_hn2(cx, T + 1, st)
                down_fin(dx)
            if T < 3:
                emit_router(T + 1)

    except StopBuild:
        pass

    sems = {}
    for en in Prog.ENGS:
        sems["eng:" + en] = es.enter_context(nc.semaphore("s_" + en))
    for sl in sorted(P.dma_slots):
        sems["dma:" + sl] = es.enter_context(nc.semaphore("d_" + sl))
    if os.environ.get('KMAXOPS'):
        P.ops = P.ops[:int(os.environ['KMAXOPS'])]
        for i_, o_ in enumerate(P.ops[-3:]):
            print('LASTOPS', o_.eng, o_.idx)
    body = P.emit(sems)
    if os.environ.get('KSTATS'):
        print('PROG stats', P.stats, 'ndma_slots', len(P.dma_slots))
    with nc.Block() as block:
        block.sync(body("sp"))
        block.tensor(body("pe"))
        block.scalar(body("act"))
        block.vector(body("dve"))
        block.gpsimd(body("pool"))
    es.close()
    return nc


def _dil_bias():
    out = np.full((128, 4 * 2 * 512 + 4 * 256), NEG, np.float32)
    k = np.arange(128)[:, None]; q = np.arange(128)[None, :]

    def f(delta, slope, d):
        return np.where(np.abs(delta) <= 64, -slope * d * np.abs(delta), NEG).astype(np.float32)
    for j in range(4):
        for hp in range(2):
            slope = 2.0 ** (-(2 * j + hp + 1))
            for pi, d in enumerate((1, 4)):
                c0 = (j * 2 + pi) * 512
                out[:, c0 + hp * 128:c0 + (hp + 1) * 128] = f(k - 64 - q, slope, d)
                out[:, c0 + 256 + hp * 128:c0 + 256 + (hp + 1) * 128] = f(k + 64 - q, slope, d)
            c0 = 4096 + j * 256
            out[:, c0 + hp * 128:c0 + (hp + 1) * 128] = f(k - q, slope, 16)
    return out


def _na_strips(rpb):
    kc = np.arange(64)[:, None]; qc = np.arange(64)[None, :]
    cs = np.clip(qc - 8, 0, 48)
    col_ok = (kc >= cs) & (kc < cs + 16)
    dcidx = np.clip(kc - qc + 15, 0, 30)
    out = np.full((4, 128, 2, 2, 896), NEG, np.float32)
    for h in range(8):
        j, hp = divmod(h, 2)
        for var in range(2):
            for i in range(2):
                for u in range(14):
                    dlt = 6 + i - u
                    ok = (-4 <= dlt <= 3) if var == 0 else (-7 <= dlt <= 7)
                    if not ok:
                        continue
                    blk = np.where(col_ok, rpb[h, dlt + 7][dcidx], NEG).astype(np.float32)
                    out[j, i * 64:(i + 1) * 64, hp, var, u * 64:(u + 1) * 64] = blk
    return out.reshape(4, 128, 4 * 896)


_NC_CACHE = {}


def kernel(x, norm_mix_g, w_in, rpb, g_out_dil, g_out_na, w_out, norm_ffn_g, w_group, b_group, w_router, b_router,
           w_gate, w_up, w_down, norm_final_g):
    f = lambda a: np.ascontiguousarray(np.asarray(a, dtype=np.float32))
    x = f(x).reshape(16 * S, D)
    col = lambda g: f(g).reshape(8, 128).T
    vecs = np.concatenate([col(norm_mix_g[0]), col(norm_ffn_g[0]),
                           col(np.concatenate([f(g_out_dil[0]), f(g_out_na[0])])),
                           np.broadcast_to(np.concatenate([f(b_group[0]), f(b_router[0])])[None, :], (128, 20))], axis=1)
    vecs = np.ascontiguousarray(vecs, dtype=np.float32)
    gfin = np.ascontiguousarray(np.broadcast_to(f(norm_final_g)[None, :], (128, D)))
    gmixr = np.ascontiguousarray(np.broadcast_to(f(norm_mix_g[0])[None, :], (128, D)))
    wr = np.ascontiguousarray(np.concatenate([f(w_group[0]), f(w_router[0])], axis=1))
    shared = {
        "w_in": f(w_in[0]), "w_out": f(w_out[0]), "w_gate": f(w_gate[0]), "w_up": f(w_up[0]), "w_down": f(w_down[0]),
        "wr": wr, "vecs": vecs, "gfin": gfin, "gmixr": gmixr, "dilb": _dil_bias(), "nas": _na_strips(f(rpb[0])),
    }
    if "nc" not in _NC_CACHE:
        _NC_CACHE["nc"] = build_program()
    nc = _NC_CACHE["nc"]
    in_maps = []
    for i in range(NCORES):
        m = dict(shared)
        m["x"] = np.ascontiguousarray(x[i * TOK:(i + 1) * TOK])
        in_maps.append(m)
    res = run_bass_kernel_spmd(nc, in_maps, core_ids=list(range(NCORES)))
    y = np.concatenate([r["y"] for r in res.results], axis=0)
    if DEBUG:
        kernel.dbg = [r.get("dbg") for r in res.results]
    return y.reshape(16, S, D).astype(np.float32)
```
